# Optimizing a Trainium2 kernel written in Bass

```python
import math
import jax, jax.numpy as jnp
from jax import lax
import numpy as np

D_MODEL = 1024
BATCH = 16
SEQ = 2048
DEPTH = 2

RWKV_HEADS = 8
RWKV_HEAD_DIM = 64
D_RWKV = RWKV_HEADS * RWKV_HEAD_DIM
W_LORA = 64
A_LORA = 64
RWKV_LN_EPS = 64e-5
RET_HEADS = 8
RET_HEAD_DIM = 64
D_RET = RET_HEADS * RET_HEAD_DIM
RET_CHUNK = 128
ROPE_BASE = 10000.0
S5_GROUP = 16
S5_STATE = 64
D_S5 = 512
S5_GROUPS = D_S5 // S5_GROUP
DT_MIN = 1e-3
DT_MAX = 1e-1
N_BRANCH = 3
EPS = 1e-6

IN_SPLITS = (
    D_RWKV, D_RWKV, D_RWKV, W_LORA, A_LORA, D_RWKV,
    D_RET, D_RET, D_RET, D_RET,
    D_S5, D_S5,
    D_MODEL, D_MODEL, D_MODEL,
)
D_IN = 4 * D_RWKV + W_LORA + A_LORA + 4 * D_RET + 2 * D_S5 + N_BRANCH * D_MODEL

kernel_name = "hybrid_rwkv7_retnet_s5_gated_block"


def _rmsnorm(x, g):
    x32 = x.astype(jnp.float32)
    y = x32 * lax.rsqrt(jnp.mean(x32 * x32, axis=-1, keepdims=True) + EPS)
    return (y * g.astype(jnp.float32)).astype(x.dtype)


def _head_norm(y, eps):
    mu = jnp.mean(y, axis=-1, keepdims=True)
    var = jnp.mean(jnp.square(y - mu), axis=-1, keepdims=True)
    return (y - mu) * lax.rsqrt(var + eps)


def _split_cols(z):
    out, start = [], 0
    for size in IN_SPLITS:
        out.append(z[..., start:start + size])
        start += size
    return out


def _token_shift(z, mu):
    prev = jnp.pad(z[:, :-1], ((0, 0), (1, 0), (0, 0)))
    return z + mu * (prev - z)


def _rwkv7_branch(r, k, v, xw, xa, mu_rkv, mu_wa, w0, w2, a0, a2, k_k, k_a, r_k, ln_w, ln_b):
    f32 = jnp.float32
    bsz, seq, _ = r.shape
    r = _token_shift(r.astype(f32), mu_rkv[0])
    k = _token_shift(k.astype(f32), mu_rkv[1])
    v = _token_shift(v.astype(f32), mu_rkv[2])
    xw = _token_shift(xw.astype(f32), mu_wa[0])
    xa = _token_shift(xa.astype(f32), mu_wa[1])
    w_log = -jax.nn.softplus(-(w0 + jnp.tanh(xw) @ w2)) - 0.5
    decay = jnp.exp(-jnp.exp(w_log))
    a = jax.nn.sigmoid(a0 + xa @ a2)
    kk = k * k_k
    k = k * (1.0 + (a - 1.0) * k_a)
    hs = lambda t: t.reshape(bsz, seq, RWKV_HEADS, RWKV_HEAD_DIM)
    r, k, v, decay, a, kk = map(hs, (r, k, v, decay, a, kk))
    kk = kk * lax.rsqrt(jnp.sum(kk * kk, axis=-1, keepdims=True) + 1e-12)
    a_neg, b_vec = -kk, kk * a

    def step(state, inp):
        r_t, w_t, k_t, v_t, an_t, b_t = inp
        sa = jnp.einsum('bhvk,bhk->bhv', state, an_t)
        state = (state * w_t[:, :, None, :] + sa[..., None] * b_t[:, :, None, :]
                 + v_t[..., None] * k_t[:, :, None, :])
        return state, jnp.einsum('bhvk,bhk->bhv', state, r_t)

    tm = lambda t: jnp.moveaxis(t, 1, 0)
    s0 = jnp.zeros((bsz, RWKV_HEADS, RWKV_HEAD_DIM, RWKV_HEAD_DIM), f32)
    _, y = lax.scan(step, s0, (tm(r), tm(decay), tm(k), tm(v), tm(a_neg), tm(b_vec)))
    y = jnp.moveaxis(y, 0, 1)
    y = _head_norm(y, RWKV_LN_EPS) * ln_w.reshape(RWKV_HEADS, RWKV_HEAD_DIM) \
        + ln_b.reshape(RWKV_HEADS, RWKV_HEAD_DIM)
    y = y + jnp.sum(r * k * r_k, axis=-1, keepdims=True) * v
    return y.reshape(bsz, seq, D_RWKV)


def _rope(x, pos):
    half = x.shape[-1] // 2
    inv = ROPE_BASE ** (-jnp.arange(half, dtype=jnp.float32) / half)
    ang = pos[:, None] * inv[None, :]
    cos, sin = jnp.cos(ang)[None, :, None, :], jnp.sin(ang)[None, :, None, :]
    x1, x2 = x[..., :half], x[..., half:]
    return jnp.concatenate([x1 * cos - x2 * sin, x1 * sin + x2 * cos], axis=-1)


def _retention_branch(q, k, v):
    f32 = jnp.float32
    bsz, seq, _ = q.shape
    nc, C, H, dh = seq // RET_CHUNK, RET_CHUNK, RET_HEADS, RET_HEAD_DIM
    hs = lambda t: t.astype(f32).reshape(bsz, seq, H, dh)
    q, k, v = hs(q), hs(k), hs(v)
    pos = jnp.arange(seq, dtype=f32)
    q = _rope(q, pos)
    k = _rope(k, pos) * (dh ** -0.5)
    ch = lambda t: t.reshape(bsz, nc, C, H, dh)
    q, k, v = ch(q), ch(k), ch(v)
    log_gamma = jnp.log(1.0 - 2.0 ** (-5.0 - jnp.arange(H, dtype=f32)))
    idx = jnp.arange(C, dtype=f32)
    diff = idx[:, None] - idx[None, :]
    dmask = jnp.where(diff >= 0, jnp.exp(log_gamma[:, None, None] * jnp.maximum(diff, 0.0)), 0.0)
    scores = jnp.einsum('bnihd,bnjhd->bnhij', q, k) * dmask
    intra = jnp.einsum('bnhij,bnjhd->bnihd', scores, v)
    k_w = jnp.exp(log_gamma[:, None] * (C - 1.0 - idx)[None, :])
    kv = jnp.einsum('bnjhd,hj,bnjhe->bnhde', k, k_w, v)
    chunk_decay = jnp.exp(log_gamma * C)[:, None, None]

    def step(R, kv_n):
        return R * chunk_decay + kv_n, R

    _, r_prev = lax.scan(step, jnp.zeros((bsz, H, dh, dh), f32), jnp.moveaxis(kv, 1, 0))
    r_prev = jnp.moveaxis(r_prev, 0, 1)
    q_w = jnp.exp(log_gamma[:, None] * (idx + 1.0)[None, :])
    cross = jnp.einsum('bnihd,hi,bnhde->bnihe', q, q_w, r_prev)
    y = (intra + cross).reshape(bsz, seq, H, dh)
    return _head_norm(y, EPS).reshape(bsz, seq, D_RET)


def _s5_branch(u, A_re, A_im, log_dt, B_re, B_im, C_re, C_im, D_skip, glu_w, glu_b):
    f32 = jnp.float32
    bsz, seq, _ = u.shape
    u32 = u.astype(f32)
    ug = u32.reshape(bsz, seq, S5_GROUPS, S5_GROUP)
    dt = jnp.exp(log_dt)[:, None]
    mag = jnp.exp(dt * A_re)
    ang = dt * A_im
    ab_re, ab_im = mag * jnp.cos(ang), mag * jnp.sin(ang)
    p, q = ab_re - 1.0, ab_im
    den = A_re * A_re + A_im * A_im
    c_re = (p * A_re + q * A_im) / den
    c_im = (q * A_re - p * A_im) / den
    bb_re = c_re[..., None] * B_re - c_im[..., None] * B_im
    bb_im = c_re[..., None] * B_im + c_im[..., None] * B_re
    bu_re = jnp.einsum('bsgp,gnp->bsgn', ug, bb_re)
    bu_im = jnp.einsum('bsgp,gnp->bsgn', ug, bb_im)
    a_re = jnp.broadcast_to(ab_re[None, None], (1, seq, S5_GROUPS, S5_STATE))
    a_im = jnp.broadcast_to(ab_im[None, None], (1, seq, S5_GROUPS, S5_STATE))

    def combine(e1, e2):
        a1r, a1i, b1r, b1i = e1
        a2r, a2i, b2r, b2i = e2
        return (a2r * a1r - a2i * a1i, a2r * a1i + a2i * a1r,
                a2r * b1r - a2i * b1i + b2r, a2r * b1i + a2i * b1r + b2i)

    _, _, xr, xi = lax.associative_scan(combine, (a_re, a_im, bu_re, bu_im), axis=1)
    y = jnp.einsum('bsgn,gpn->bsgp', xr, C_re) - jnp.einsum('bsgn,gpn->bsgp', xi, C_im)
    y = y.reshape(bsz, seq, D_S5) + D_skip * u32
    z = jax.nn.gelu(y)
    return z * jax.nn.sigmoid(z @ glu_w + glu_b)


def setup_inputs(seed: int = 0) -> dict:
    key = jax.random.key(seed)
    ks = jax.random.split(key, 32)
    nrm = lambda i, shape, s: s * jax.random.normal(ks[i], shape, jnp.float32)
    L, G, N, P = DEPTH, S5_GROUPS, S5_STATE, S5_GROUP
    inv2 = 2.0 ** -0.5
    return {
        "x": nrm(0, (BATCH, SEQ, D_MODEL), 1.0),
        "pre_norm": 1.0 + nrm(1, (L, D_MODEL), 0.02),
        "w_in": nrm(2, (L, D_MODEL, D_IN), D_MODEL ** -0.5),
        "rwkv_mu_rkv": jax.random.uniform(ks[3], (L, 3, D_RWKV), jnp.float32),
        "rwkv_mu_wa": jax.random.uniform(ks[4], (L, 2, W_LORA), jnp.float32),
        "rwkv_w0": jnp.linspace(-6.0, -1.0, D_RWKV, dtype=jnp.float32)[None, :] + nrm(5, (L, D_RWKV), 0.1),
        "rwkv_w2": nrm(6, (L, W_LORA, D_RWKV), 0.5 * W_LORA ** -0.5),
        "rwkv_a0": nrm(7, (L, D_RWKV), 0.1),
        "rwkv_a2": nrm(8, (L, A_LORA, D_RWKV), 0.5 * A_LORA ** -0.5),
        "rwkv_k_k": 0.85 + nrm(9, (L, D_RWKV), 0.05),
        "rwkv_k_a": 1.0 + nrm(10, (L, D_RWKV), 0.05),
        "rwkv_r_k": nrm(11, (L, RWKV_HEADS, RWKV_HEAD_DIM), 0.1),
        "rwkv_ln_w": 1.0 + nrm(12, (L, D_RWKV), 0.02),
        "rwkv_ln_b": nrm(13, (L, D_RWKV), 0.01),
        "s5_A_re": -0.5 + nrm(14, (L, G, N), 0.01),
        "s5_A_im": jnp.broadcast_to(math.pi * jnp.arange(N, dtype=jnp.float32), (L, G, N)) + 0.0 * nrm(15, (L, G, N), 1.0) if False else jnp.broadcast_to(math.pi * jnp.arange(N, dtype=jnp.float32), (L, G, N)) * jnp.ones((L, G, N), jnp.float32),
        "s5_log_dt": jax.random.uniform(ks[16], (L, G), jnp.float32, math.log(DT_MIN), math.log(DT_MAX)),
        "s5_B_re": nrm(17, (L, G, N, P), inv2 * P ** -0.5),
        "s5_B_im": nrm(18, (L, G, N, P), inv2 * P ** -0.5),
        "s5_C_re": nrm(19, (L, G, P, N), inv2 * N ** -0.5),
        "s5_C_im": nrm(20, (L, G, P, N), inv2 * N ** -0.5),
        "s5_D": nrm(21, (L, D_S5), 1.0),
        "s5_glu_w": nrm(22, (L, D_S5, D_S5), D_S5 ** -0.5),
        "s5_glu_b": nrm(23, (L, D_S5), 0.01),
        "w_proj_rwkv": nrm(24, (L, D_RWKV, D_MODEL), D_RWKV ** -0.5),
        "w_proj_ret": nrm(25, (L, D_RET, D_MODEL), D_RET ** -0.5),
        "w_proj_s5": nrm(26, (L, D_S5, D_MODEL), D_S5 ** -0.5),
        "b_merge": nrm(27, (L, N_BRANCH, D_MODEL), 0.01),
        "w_out": nrm(28, (L, D_MODEL, D_MODEL), D_MODEL ** -0.5),
        "post_norm": 1.0 + nrm(29, (L, D_MODEL), 0.02),
    }


def reference(x, pre_norm, w_in, rwkv_mu_rkv, rwkv_mu_wa, rwkv_w0, rwkv_w2, rwkv_a0, rwkv_a2,
              rwkv_k_k, rwkv_k_a, rwkv_r_k, rwkv_ln_w, rwkv_ln_b, s5_A_re, s5_A_im, s5_log_dt,
              s5_B_re, s5_B_im, s5_C_re, s5_C_im, s5_D, s5_glu_w, s5_glu_b, w_proj_rwkv,
              w_proj_ret, w_proj_s5, b_merge, w_out, post_norm):
    f32 = jnp.float32
    for l in range(DEPTH):
        h = _rmsnorm(x, pre_norm[l])
        proj = h @ w_in[l]
        (a_r, a_k, a_v, a_xw, a_xa, a_gate,
         b_q, b_k, b_v, b_gate,
         c_u, c_gate,
         g_a, g_b, g_c) = _split_cols(proj)
        y_a = _rwkv7_branch(a_r, a_k, a_v, a_xw, a_xa, rwkv_mu_rkv[l], rwkv_mu_wa[l],
                            rwkv_w0[l], rwkv_w2[l], rwkv_a0[l], rwkv_a2[l], rwkv_k_k[l],
                            rwkv_k_a[l], rwkv_r_k[l], rwkv_ln_w[l], rwkv_ln_b[l])
        y_a = y_a * jax.nn.silu(a_gate.astype(f32))
        y_b = _retention_branch(b_q, b_k, b_v) * jax.nn.silu(b_gate.astype(f32))
        y_c = _s5_branch(c_u, s5_A_re[l], s5_A_im[l], s5_log_dt[l], s5_B_re[l], s5_B_im[l],
                         s5_C_re[l], s5_C_im[l], s5_D[l], s5_glu_w[l], s5_glu_b[l])
        y_c = y_c * jax.nn.silu(c_gate.astype(f32))
        merged = (jax.nn.sigmoid(g_a.astype(f32) + b_merge[l, 0]) * (y_a @ w_proj_rwkv[l])
                  + jax.nn.sigmoid(g_b.astype(f32) + b_merge[l, 1]) * (y_b @ w_proj_ret[l])
                  + jax.nn.sigmoid(g_c.astype(f32) + b_merge[l, 2]) * (y_c @ w_proj_s5[l]))
        out = (merged @ w_out[l]).astype(x.dtype)
        x = x + _rmsnorm(out, post_norm[l])
    return x
```

```python
import contextlib
import numpy as np
import ml_dtypes
import concourse.bass as bass
import concourse.mybir as mybir
from concourse.bass_utils import run_bass_kernel_spmd

F32 = mybir.dt.float32
BF16 = mybir.dt.bfloat16
AF = mybir.ActivationFunctionType
ALU = mybir.AluOpType
AX = mybir.AxisListType

D = 1024
S = 2048
DEPTH = 2
NSEQ = 2
D_IN = 8320
EPS = 1e-6
NT = S // 128

O_AR, O_AK, O_AV, O_XW, O_XA, O_AG = 0, 512, 1024, 1536, 1600, 1664
O_BQ, O_BK, O_BV, O_BG = 2176, 2688, 3200, 3712
O_CU, O_CG = 4224, 4736
O_GA, O_GB, O_GC = 5248, 6272, 7296


class Buf:
    def __init__(self, name):
        self.name = name
        self.last_write = None
        self.reads = []


class Engine:
    EPOCH = 30000

    def __init__(self, fw, name):
        self.fw = fw
        self.name = name
        self.sems = []
        self.count = 0
        self.waited = {}
        self.ops = []
        self.n = 0
        self._new_sem()

    def _new_sem(self):
        s = self.fw.stack.enter_context(self.fw.nc.semaphore(f"s_{self.name}_{len(self.sems)}"))
        self.sems.append(s)
        self.count = 0

    def need(self, ev):
        sem, val = ev
        key = id(sem)
        if self.waited.get(key, 0) >= val:
            return None
        self.waited[key] = val
        return ev


class Fw:
    def __init__(self, nc, stack):
        self.nc = nc
        self.stack = stack
        self.eng = {n: Engine(self, n) for n in ("pe", "act", "dve", "pool", "sp")}
        self.dsem = {}
        for q, k in (("sp", 12), ("act", 6), ("pool", 6)):
            self.dsem[q] = [[stack.enter_context(nc.semaphore(f"d_{q}_{i}")), 0] for i in range(k)]
        self.dnext = {q: 0 for q in self.dsem}

    def _deps(self, e, reads, writes):
        evs = []
        for b in reads:
            if b.last_write is not None:
                evs.append(b.last_write)
        for b in writes:
            if b.last_write is not None:
                evs.append(b.last_write)
            evs.extend(b.reads)
        out = []
        for ev in evs:
            if e.name == "pe" and any(ev[0] is s_ for s_ in e.sems):
                continue
            ev2 = e.need(ev)
            if ev2 is not None:
                out.append(ev2)
        return out

    def op(self, engname, fn, reads=(), writes=(), inc=True):
        e = self.eng[engname]
        waits = self._deps(e, reads, writes)
        if e.count >= Engine.EPOCH and inc:
            e._new_sem()
        sem = e.sems[-1]
        if inc:
            e.count += 1
        ev = (sem, e.count if inc else e.count + 1)
        for b in reads:
            b.reads.append(ev)
        for b in writes:
            b.last_write = ev
            b.reads = []
        e.n += 1

        def run(h, waits=waits, fn=fn, sem=sem, inc=inc):
            for (s, v) in waits:
                h.wait_ge(s, v)
            ins = fn(h)
            if inc:
                ins.then_inc(sem, 1)

        e.ops.append(run)
        return ev

    def dma(self, q, out, in_, reads=(), writes=()):
        e = self.eng[q]
        waits = self._deps(e, reads, writes)
        slots = self.dsem[q]
        i = self.dnext[q]
        self.dnext[q] = (i + 1) % len(slots)
        slot = slots[i]
        sem = slot[0]
        prev = slot[1]
        if prev > 0:
            w = e.need((sem, prev))
            if w is not None:
                waits.append(w)
        slot[1] = prev + 16
        ev = (sem, slot[1])
        for b in reads:
            b.reads.append(ev)
        for b in writes:
            b.last_write = ev
            b.reads = []

        def run(h, waits=waits, sem=sem, out=out, in_=in_):
            for (s, v) in waits:
                h.wait_ge(s, v)
            h.dma_start(out=out, in_=in_).then_inc(sem, 16)

        e.ops.append(run)
        return ev

    def wait_all(self, engname, bufs):
        e = self.eng[engname]
        waits = []
        for b in bufs:
            for ev in ([b.last_write] if b.last_write else []) + list(b.reads):
                w = e.need(ev)
                if w is not None:
                    waits.append(w)

        def run(h, waits=waits):
            for (s, v) in waits:
                h.wait_ge(s, v)

        e.ops.append(run)

    def pe(self, fn, reads=(), writes=(), inc=True):
        return self.op("pe", fn, reads, writes, inc)

    def act(self, fn, reads=(), writes=()):
        return self.op("act", fn, reads, writes)

    def dve(self, fn, reads=(), writes=()):
        return self.op("dve", fn, reads, writes)

    def pool(self, fn, reads=(), writes=()):
        return self.op("pool", fn, reads, writes)

    def emit(self):
        nc = self.nc
        with nc.Block() as block:
            @block.tensor
            def _(h):
                for f in self.eng["pe"].ops:
                    f(h)

            @block.scalar
            def _(h):
                for f in self.eng["act"].ops:
                    f(h)

            @block.vector
            def _(h):
                for f in self.eng["dve"].ops:
                    f(h)

            @block.gpsimd
            def _(h):
                for f in self.eng["pool"].ops:
                    f(h)

            @block.sync
            def _(h):
                for f in self.eng["sp"].ops:
                    f(h)


class T:
    def __init__(self, fw, shape, dtype, name, psum=False, nsub=1):
        nc = fw.nc
        if psum:
            self.t = fw.stack.enter_context(nc.psum_tensor("ps_" + name, shape, dtype))
        else:
            self.t = fw.stack.enter_context(nc.sbuf_tensor("sb_" + name, shape, dtype))
        self.b = [Buf(f"{name}.{i}") for i in range(nsub)]
        self.name = name

    def __getitem__(self, idx):
        return self.t[idx]

    @property
    def B(self):
        return self.b[0]


def make_consts():
    c = {}
    c["ident"] = np.eye(128, dtype=np.float32).astype(ml_dtypes.bfloat16)
    bd = np.zeros((128, 128), np.float32)
    bd[:64, :64] = 1.0
    bd[64:, 64:] = 1.0
    c["bd32"] = bd
    c["bdo64"] = (bd / 64.0).astype(ml_dtypes.bfloat16)
    half = 32
    inv = (np.float32(10000.0) ** (-np.arange(half, dtype=np.float32) / np.float32(half))).astype(np.float32)
    pos = np.arange(S, dtype=np.float32)
    ang = (pos[None, :] * inv[:, None]).astype(np.float32).astype(np.float64)
    cos32, sin32 = np.cos(ang), np.sin(ang)
    cosT = np.zeros((128, S), np.float32)
    sinS = np.zeros((128, S), np.float32)
    for p in range(128):
        d = p % 64
        i = d % 32
        cosT[p] = cos32[i]
        sinS[p] = -sin32[i] if d < 32 else sin32[i]
    c["rope_cos"] = cosT
    c["rope_sin"] = sinS
    lg = np.log(1.0 - 2.0 ** (-5.0 - np.arange(8, dtype=np.float64)))
    idx = np.arange(128, dtype=np.float64)
    dmT = np.zeros((4, 128, 256), np.float32)
    kwt = np.zeros((4, 128, 128), np.float32)
    qw = np.zeros((4, 128, 128), np.float32)
    gc = np.zeros((128, 4), np.float32)
    for hp in range(4):
        for hh in range(2):
            g = lg[hp * 2 + hh]
            diff = idx[None, :] - idx[:, None]
            m = np.where(diff >= 0, np.exp(g * np.maximum(diff, 0.0)), 0.0) / 8.0
            dmT[hp, :, hh * 128:(hh + 1) * 128] = m
            kwt[hp, :, hh * 64:(hh + 1) * 64] = (np.exp(g * (127.0 - idx)) / 8.0)[:, None]
            qw[hp, hh * 64:(hh + 1) * 64, :] = np.exp(g * (idx + 1.0))[None, :]
            gc[hh * 64:(hh + 1) * 64, hp] = np.exp(g * 128.0)
    c["ret_dmT"] = dmT
    c["ret_kwt"] = kwt
    c["ret_qw"] = qw
    c["ret_gc"] = gc
    sw = np.zeros((128, 128), np.float32)
    for k in range(128):
        sw[k, (k + 64) % 128] = 1.0
    c["swapb"] = sw.astype(ml_dtypes.bfloat16)
    sg = np.zeros((128, 2), np.float32)
    sg[:64, 0], sg[64:, 0] = -1.0, 1.0
    sg[:64, 1], sg[64:, 1] = 1.0, -1.0
    c["sgn"] = sg
    rm = np.zeros((128, 8), np.float32)
    for p in range(128):
        rm[p, p // 16] = 1.0
    c["rowmask"] = rm
    ii = np.arange(128)
    su = (ii[:, None] < ii[None, :]).astype(np.float32)
    iu = (ii[:, None] <= ii[None, :]).astype(np.float32)
    sl_ = (ii[None, :] < ii[:, None]).astype(np.float32)
    c["rw_masks"] = np.concatenate([su, iu, su, iu, sl_, sl_], axis=1).astype(ml_dtypes.bfloat16)
    return c


CONST_SPECS = {
    "ident": ([128, 128], BF16), "bd32": ([128, 128], F32), "bdo64": ([128, 128], BF16),
    "rope_cos": ([128, S], F32), "rope_sin": ([128, S], F32),
    "ret_dmT": ([4, 128, 256], F32), "ret_kwt": ([4, 128, 128], F32), "ret_qw": ([4, 128, 128], F32),
    "ret_gc": ([128, 4], F32),
    "rw_masks": ([128, 768], BF16),
    "swapb": ([128, 128], BF16), "sgn": ([128, 2], F32), "rowmask": ([128, 8], F32),
}


LIM = {}
N_LAUNCH = 2


def build_program(nlayers=DEPTH, nseq=NSEQ, debug=None, phases="0CABM"):
    debug = debug or {}
    nc = bass.Bass("TRN2", target_bir_lowering=False)
    dr = {}

    def din(name, shape, dt=F32):
        dr[name] = nc.dram_tensor(name, list(shape), dt, kind="ExternalInput").ap()
        return dr[name]

    x_d = din("x", [nseq, S, D])
    pre_norm_d = din("pre_norm", [DEPTH, 128, 8])
    w_in_d = din("w_in", [DEPTH, D, D_IN])
    cd = {k: din(k, shp, dt) for k, (shp, dt) in CONST_SPECS.items()}
    wp_d = [din(n, [DEPTH, 512, D]) for n in ("w_proj_rwkv", "w_proj_ret", "w_proj_s5")]
    wout_d = din("w_out", [DEPTH, D, D])
    bmerge_d = din("b_merge", [DEPTH, 128, 3, 8])
    postn_d = din("post_norm", [DEPTH, D])
    s5A_d = din("s5_Aab", [DEPTH, 3, 128, 32])
    s5B_d = din("s5_Bst", [DEPTH, 2, 128, 512])
    s5C_d = din("s5_Cst", [DEPTH, 2, 128, 512])
    s5v_d = din("s5_vec", [DEPTH, 128, 8])
    gluw_d = din("s5_glu_w", [DEPTH, 512, 512])
    mu_rkv_d = din("rwkv_mu_rkv", [DEPTH, 3, 512])
    mu_wa_d = din("rwkv_mu_wa", [DEPTH, 1, 128])
    w2a2_d = din("rwkv_w2a2", [DEPTH, 2, 64, 512])
    rvec_d = din("rwkv_vec", [DEPTH, 128, 4, 8])
    out_d = nc.dram_tensor("out", [nseq, S, D], F32, kind="ExternalOutput").ap()
    dbg_d = {}
    for k, shp in debug.items():
        dbg_d[k] = nc.dram_tensor("dbg_" + k, list(shp[0]), shp[1], kind="ExternalOutput").ap()

    with contextlib.ExitStack() as stack:
        fw = Fw(nc, stack)
        x_sb = T(fw, [128, NT, D], F32, "x_sb", nsub=NT)
        hT = T(fw, [128, 8, S + 1], BF16, "hT", nsub=NT + 1)
        ya = T(fw, [128, 4, S], BF16, "ya", nsub=16)
        yb = T(fw, [128, 4, S], BF16, "yb", nsub=16)
        yc = T(fw, [128, 4, S], BF16, "yc", nsub=16)
        ident = T(fw, [128, 128], BF16, "ident")
        bd32 = T(fw, [128, 128], F32, "bd32")
        bdo64 = T(fw, [128, 128], BF16, "bdo64")
        gpre = T(fw, [128, DEPTH, 8], F32, "gpre")
        gexp = T(fw, [128, 8, 128], F32, "gexp")
        NF, NH = 8, 12
        wf = [T(fw, [128, 512], F32, f"wf{i}") for i in range(NF)]
        wh = [T(fw, [128, 512], BF16, f"wh{i}") for i in range(NH)]
        st6 = T(fw, [128, 2, 6], F32, "st6")
        mv = T(fw, [128, 2], F32, "mv")
        e2 = T(fw, [128, 1], F32, "e2")
        rstd = T(fw, [128, 1], F32, "rstd")
        R32t = T(fw, [128, 128], F32, "R32t")
        Rbt = T(fw, [128, 128], BF16, "Rbt")
        gct = T(fw, [128, 4], F32, "gct")
        bmt = T(fw, [128, DEPTH, 3, 8], F32, "bmt")
        swapb = T(fw, [128, 128], BF16, "swapb")
        chl = T(fw, [128, 32], BF16, "chl")
        cbk = T(fw, [128, 8], F32, "cbk")
        sgn = T(fw, [128, 2], F32, "sgn")
        rowmask = T(fw, [128, 8], F32, "rowmask")
        s5v = T(fw, [128, 8], F32, "s5v")
        carry = T(fw, [128, 8], F32, "carry")
        cst = T(fw, [128, 16], F32, "cst")
        onec = T(fw, [128, 1], F32, "onec")
        rvec = T(fw, [128, 4, 8], F32, "rvec")
        omka = T(fw, [128, 4], F32, "omka")
        lw2 = T(fw, [128, 128], BF16, "lw2")
        la2 = T(fw, [128, 128], BF16, "la2")
        St32, Stb = R32t, Rbt
        gpost = T(fw, [128, D], F32, "gpost")
        NSLOT = 7
        wslot = [T(fw, [128, 8, 128], BF16, f"wslot{i}") for i in range(NSLOT)]
        wst = [T(fw, [128, 8, 128], F32, f"wst{i}") for i in range(2)]
        wctr = [0, 0]
        tp_ps = [T(fw, [128, 8, 128], BF16, f"tp{i}", psum=True) for i in range(2)]
        pg = [T(fw, [128, 512], F32, f"pg{i}", psum=True) for i in range(6)]
        pctr = [0]

        def ps_next():
            t = pg[pctr[0] % 4]
            pctr[0] += 1
            return t

        yctr = [0]

        def ps_y():
            t = pg[4 + yctr[0] % 2]
            yctr[0] += 1
            return t

        fw.dma("sp", ident[:], cd["ident"], writes=[ident.B])
        fw.dma("sp", bd32[:], cd["bd32"], writes=[bd32.B])
        fw.dma("sp", bdo64[:], cd["bdo64"], writes=[bdo64.B])
        fw.dma("sp", gpre[:], pre_norm_d.rearrange("l p c -> p l c"), writes=[gpre.B])
        fw.dma("sp", bmt[:], bmerge_d.rearrange("l p b c -> p l b c"), writes=[bmt.B])
        fw.dma("sp", swapb[:], cd["swapb"], writes=[swapb.B])
        fw.dma("sp", sgn[:], cd["sgn"], writes=[sgn.B])
        fw.dma("sp", rowmask[:], cd["rowmask"], writes=[rowmask.B])
        fw.pool(lambda h: h.memset(hT[:, :, 0:1], 0.0), writes=[hT.b[NT]])
        fw.dve(lambda h: h.memset(onec[:], 1.0), writes=[onec.B])

        def hT_bufs(tok0, ntok, shift=0):
            a = tok0 - shift
            bl = []
            if a < 0:
                bl.append(hT.b[NT])
                a = 0
            for tt in range(a // 128, (tok0 - shift + ntok - 1) // 128 + 1):
                bl.append(hT.b[tt])
            return bl

        def layer_setup(l):
            for c in range(8):
                fw.dve(lambda h, c=c: h.tensor_copy(out=gexp[:, c, :], in_=gpre[:, l, c:c + 1].to_broadcast([128, 128])),
                       reads=[gpre.B], writes=[gexp.B])

        def wload(l, segs):
            slot = wslot[wctr[0] % NSLOT]
            wctr[0] += 1
            stg = wst[wctr[1] % 2]
            wctr[1] += 1
            o = 0
            for (c0, n) in segs:
                fw.dma("sp", stg[:, :, o:o + n],
                       w_in_d[l, :, c0:c0 + n].rearrange("(c p) n -> p c n", p=128),
                       writes=[stg.B])
                o += n
            assert o == 128
            fw.dve(lambda h, slot=slot, stg=stg: h.tensor_tensor(out=slot[:], in0=stg[:], in1=gexp[:], op=ALU.mult),
                    reads=[stg.B, gexp.B], writes=[slot.B])
            return slot

        def proj(ps, slot, tok0, ntok=512, shift=0, start=True, stop=True):
            hb = hT_bufs(tok0, ntok, shift)
            for c in range(8):
                fw.pe(lambda h, c=c: h.matmul(out=ps[:, 0:ntok], lhsT=slot[:, c, :],
                                              rhs=hT[:, c, 1 + tok0 - shift:1 + tok0 - shift + ntok],
                                              start=(start and c == 0), stop=(stop and c == 7)),
                      reads=[slot.B] + hb, writes=[ps.B], inc=(c == 7))

        def silu_from_psum(dst, ps):
            fw.act(lambda h: h.activation(out=dst[:], in_=ps[:], func=AF.Sigmoid), reads=[ps.B], writes=[dst.B])
            fw.dve(lambda h: h.tensor_tensor(out=dst[:], in0=ps[:], in1=dst[:], op=ALU.mult),
                   reads=[ps.B, dst.B], writes=[dst.B])

        def phase0(si, l):
            for tt in range(NT):
                xb = x_sb.b[tt]
                xt = x_sb[:, tt, :]
                for j in range(2):
                    fw.dve(lambda h, j=j, xt=xt: h.bn_stats(out=st6[:, j, :], in_=xt[:, j * 512:(j + 1) * 512]),
                           reads=[xb], writes=[st6.B])
                fw.dve(lambda h: h.bn_aggr(out=mv[:], in_=st6[:].rearrange("p a b -> p (a b)")),
                       reads=[st6.B], writes=[mv.B])
                fw.dve(lambda h: h.scalar_tensor_tensor(out=e2[:], in0=mv[:, 0:1], scalar=mv[:, 0:1],
                                                        in1=mv[:, 1:2], op0=ALU.mult, op1=ALU.add),
                       reads=[mv.B], writes=[e2.B])
                fw.act(lambda h: h.activation(out=e2[:], in_=e2[:], func=AF.Sqrt, bias=EPS, scale=1.0),
                       reads=[e2.B], writes=[e2.B])
                fw.dve(lambda h: h.reciprocal(out=rstd[:], in_=e2[:]), reads=[e2.B], writes=[rstd.B])
                for hf in range(2):
                    fw.dve(lambda h, hf=hf, xt=xt: h.tensor_scalar(out=wh[hf][:], in0=xt[:, hf * 512:(hf + 1) * 512],
                                                                   scalar1=rstd[:, 0:1], scalar2=None, op0=ALU.mult),
                           reads=[xb, rstd.B], writes=[wh[hf].B])
                ps = tp_ps[tt % 2]
                for c in range(8):
                    fw.pe(lambda h, ps=ps, c=c: h.transpose(out=ps[:, c, :],
                                                            in_=wh[c // 4][:, (c % 4) * 128:(c % 4 + 1) * 128],
                                                            identity=ident[:]),
                          reads=[wh[c // 4].B, ident.B], writes=[ps.B], inc=(c == 7))
                fw.act(lambda h, ps=ps, tt=tt: h.activation(out=hT[:, :, 1 + tt * 128:1 + (tt + 1) * 128],
                                                            in_=ps[:], func=AF.Copy),
                       reads=[ps.B], writes=[hT.b[tt]])

        def headnorm_gate(y_ps, sg, dst, dstb, eps, affine=None):
            y32, ybf, ycen, sq, rs = wf[0], wh[0], wf[1], wh[1], wf[2]
            fw.act(lambda h: h.activation(out=y32[:], in_=y_ps[:], func=AF.Copy), reads=[y_ps.B], writes=[y32.B])
            fw.dve(lambda h: h.tensor_copy(out=ybf[:], in_=y32[:]), reads=[y32.B], writes=[ybf.B])
            mean_ps = ps_next()
            fw.pe(lambda h: h.matmul(out=mean_ps[:], lhsT=bdo64[:], rhs=ybf[:], start=True, stop=True),
                  reads=[bdo64.B, ybf.B], writes=[mean_ps.B])
            fw.dve(lambda h: h.tensor_tensor(out=ycen[:], in0=y32[:], in1=mean_ps[:], op=ALU.subtract),
                   reads=[y32.B, mean_ps.B], writes=[ycen.B])
            fw.act(lambda h: h.activation(out=sq[:], in_=ycen[:], func=AF.Square), reads=[ycen.B], writes=[sq.B])
            var_ps = ps_next()
            fw.pe(lambda h: h.matmul(out=var_ps[:], lhsT=bdo64[:], rhs=sq[:], start=True, stop=True),
                  reads=[bdo64.B, sq.B], writes=[var_ps.B])
            fw.act(lambda h: h.activation(out=rs[:], in_=var_ps[:], func=AF.Sqrt, bias=eps, scale=1.0),
                   reads=[var_ps.B], writes=[rs.B])
            fw.dve(lambda h: h.reciprocal(out=rs[:], in_=rs[:]), reads=[rs.B], writes=[rs.B])
            fw.dve(lambda h: h.tensor_tensor(out=ycen[:], in0=ycen[:], in1=rs[:], op=ALU.mult),
                    reads=[ycen.B, rs.B], writes=[ycen.B])
            if affine is not None:
                affine(ycen)
            fw.dve(lambda h: h.tensor_tensor(out=dst, in0=ycen[:], in1=sg[:], op=ALU.mult),
                    reads=[ycen.B, sg.B], writes=[dstb])

        def phaseB(si, l):
            dmT = wf[4]
            kwt = wf[5]
            qwt = wf[6]
            fw.dma("sp", gct[:, 0:4], cd["ret_gc"], writes=[gct.B])
            R32 = R32t
            t1, t2 = wf[0], wf[1]
            cosb, sinb = wf[2], wf[3]
            qr, kr, qc, vsb, ktok, vp0, vp1 = wh[2], wh[3], wh[4], wh[5], wh[6], wh[7], wh[8]
            qpad = [wh[9], wh[10]]
            Ssb = wh[11]
            Rb = Rbt
            sgate = wf[7]
            for hp in range(LIM.get('hp', 4)):
                cb = O_BQ + hp * 128
                kb = O_BK + hp * 128
                sw = lambda b: [(b + 32, 32), (b, 32), (b + 96, 32), (b + 64, 32)]
                w_q = wload(l, [(cb, 128)])
                w_qs = wload(l, sw(cb))
                w_k = wload(l, [(kb, 128)])
                w_ks = wload(l, sw(kb))
                w_v = wload(l, [(O_BV + hp * 128, 128)])
                w_g = wload(l, [(O_BG + hp * 128, 128)])
                fw.dma("sp", dmT[:, 0:256], cd["ret_dmT"][hp], writes=[dmT.B])
                fw.dma("sp", kwt[:, 0:128], cd["ret_kwt"][hp], writes=[kwt.B])
                fw.dma("sp", qwt[:, 0:128], cd["ret_qw"][hp], writes=[qwt.B])
                fw.pool(lambda h: h.memset(R32[:], 0.0), writes=[R32.B])
                fw.pool(lambda h: h.memset(Rb[:], 0.0), writes=[Rb.B])
                for qp in qpad:
                    fw.pool(lambda h, qp=qp: h.memset(qp[:], 0.0), writes=[qp.B])
                fw.pool(lambda h: h.memset(vp0[:], 0.0), writes=[vp0.B])
                fw.pool(lambda h: h.memset(vp1[:], 0.0), writes=[vp1.B])
                def do_block(tb, hp=hp, w_q=w_q, w_qs=w_qs, w_k=w_k, w_ks=w_ks, w_v=w_v, w_g=w_g):
                    tok0 = tb * 512
                    fw.dma("sp", cosb[:], cd["rope_cos"][:, tok0:tok0 + 512], writes=[cosb.B])
                    fw.dma("sp", sinb[:], cd["rope_sin"][:, tok0:tok0 + 512], writes=[sinb.B])
                    if LIM.get('stage', 99) < 1:
                        return
                    pq, pqs = ps_next(), ps_next()
                    proj(pq, w_q, tok0)
                    proj(pqs, w_qs, tok0)
                    fw.dve(lambda h: h.tensor_tensor(out=t1[:], in0=pq[:], in1=cosb[:], op=ALU.mult),
                           reads=[pq.B, cosb.B], writes=[t1.B])
                    fw.dve(lambda h: h.tensor_tensor(out=t2[:], in0=pqs[:], in1=sinb[:], op=ALU.mult),
                           reads=[pqs.B, sinb.B], writes=[t2.B])
                    fw.dve(lambda h: h.tensor_tensor(out=qr[:], in0=t1[:], in1=t2[:], op=ALU.add),
                            reads=[t1.B, t2.B], writes=[qr.B])
                    if LIM.get('stage', 99) < 2:
                        return
                    for half in range(2):
                        qp = qpad[half]
                        for hh in range(2):
                            src = qr[hh * 64:(hh + 1) * 64, half * 256:(half + 1) * 256].rearrange("p (c i) -> p c i", c=2)
                            dstv = qp[hh * 64:(hh + 1) * 64, :].rearrange("p (c h i) -> p c h i", c=2, h=2)[:, :, hh, :]
                            fw.act(lambda h, src=src, dstv=dstv: h.activation(out=dstv, in_=src, func=AF.Copy),
                                   reads=[qr.B], writes=[qp.B])
                    for c4 in range(4):
                        fw.dve(lambda h, c4=c4: h.tensor_tensor(out=qc[:, c4 * 128:(c4 + 1) * 128],
                                                                 in0=qr[:, c4 * 128:(c4 + 1) * 128],
                                                                 in1=qwt[:, 0:128], op=ALU.mult),
                                reads=[qr.B, qwt.B], writes=[qc.B])
                    if LIM.get('stage', 99) < 3:
                        return
                    pk, pks = ps_next(), ps_next()
                    proj(pk, w_k, tok0)
                    proj(pks, w_ks, tok0)
                    fw.dve(lambda h: h.tensor_tensor(out=t1[:], in0=pk[:], in1=cosb[:], op=ALU.mult),
                           reads=[pk.B, cosb.B], writes=[t1.B])
                    fw.dve(lambda h: h.tensor_tensor(out=t2[:], in0=pks[:], in1=sinb[:], op=ALU.mult),
                           reads=[pks.B, sinb.B], writes=[t2.B])
                    fw.dve(lambda h: h.tensor_tensor(out=kr[:], in0=t1[:], in1=t2[:], op=ALU.add),
                            reads=[t1.B, t2.B], writes=[kr.B])
                    if LIM.get('stage', 99) < 4:
                        return
                    pv = ps_next()
                    proj(pv, w_v, tok0)
                    fw.act(lambda h: h.activation(out=vsb[:], in_=pv[:], func=AF.Copy), reads=[pv.B], writes=[vsb.B])
                    pgate = ps_next()
                    proj(pgate, w_g, tok0)
                    silu_from_psum(sgate, pgate)
                    if LIM.get('stage', 99) < 5:
                        return
                    tp = tp_ps[0]
                    for c4 in range(4):
                        fw.pe(lambda h, c4=c4: h.transpose(out=tp[:, c4, :], in_=kr[:, c4 * 128:(c4 + 1) * 128],
                                                           identity=ident[:]),
                              reads=[kr.B, ident.B], writes=[tp.B], inc=False)
                    for c4 in range(4):
                        fw.pe(lambda h, c4=c4: h.transpose(out=tp[:, 4 + c4, :], in_=vsb[:, c4 * 128:(c4 + 1) * 128],
                                                           identity=ident[:]),
                              reads=[vsb.B, ident.B], writes=[tp.B], inc=(c4 == 3))
                    for c4 in range(4):
                        fw.dve(lambda h, c4=c4: h.tensor_tensor(out=ktok[:, c4 * 128:(c4 + 1) * 128], in0=tp[:, c4, :],
                                                                in1=kwt[:, 0:128], op=ALU.mult),
                               reads=[tp.B, kwt.B], writes=[ktok.B])
                    fw.act(lambda h: h.activation(
                        out=vp0[:].rearrange("p (c f) -> p c f", c=4)[:, :, 0:64], in_=tp[:, 4:8, 0:64], func=AF.Copy),
                        reads=[tp.B], writes=[vp0.B])
                    fw.act(lambda h: h.activation(
                        out=vp1[:].rearrange("p (c f) -> p c f", c=4)[:, :, 64:128], in_=tp[:, 4:8, 64:128], func=AF.Copy),
                        reads=[tp.B], writes=[vp1.B])
                    if LIM.get('stage', 99) < 6:
                        return
                    y_ps = ps_y()

                    def do_chunk(c4):
                        cs = slice(c4 * 128, (c4 + 1) * 128)
                        sc = ps_next()
                        qp = qpad[c4 // 2]
                        fw.pe(lambda h, cs=cs, qp=qp, c4=c4, sc=sc: h.matmul(
                            out=sc[:, 0:256], lhsT=kr[:, cs], rhs=qp[:, (c4 % 2) * 256:(c4 % 2) * 256 + 256],
                            start=True, stop=True), reads=[kr.B, qp.B], writes=[sc.B])
                        sv = Ssb[:, (c4 % 2) * 256:(c4 % 2) * 256 + 256]
                        fw.dve(lambda h, sc=sc, sv=sv: h.tensor_tensor(out=sv, in0=sc[:, 0:256], in1=dmT[:, 0:256],
                                                                       op=ALU.mult),
                               reads=[sc.B, dmT.B], writes=[Ssb.B])
                        fw.pe(lambda h, cs=cs, sv=sv: h.matmul(out=y_ps[:, cs], lhsT=vp0[:, cs], rhs=sv[:, 0:128],
                                                               start=True, stop=False),
                              reads=[vp0.B, Ssb.B], writes=[y_ps.B], inc=False)
                        fw.pe(lambda h, cs=cs, sv=sv: h.matmul(out=y_ps[:, cs], lhsT=vp1[:, cs], rhs=sv[:, 128:256],
                                                               start=False, stop=False),
                              reads=[vp1.B, Ssb.B], writes=[y_ps.B], inc=False)
                        fw.pe(lambda h, cs=cs: h.matmul(out=y_ps[:, cs], lhsT=Rb[:], rhs=qc[:, cs],
                                                        start=False, stop=True),
                              reads=[Rb.B, qc.B], writes=[y_ps.B])
                        kv = ps_next()
                        fw.pe(lambda h, cs=cs, kv=kv: h.matmul(out=kv[:, 0:128], lhsT=ktok[:, cs], rhs=vp0[:, cs],
                                                               start=True, stop=False),
                              reads=[ktok.B, vp0.B], writes=[kv.B], inc=False)
                        fw.pe(lambda h, cs=cs, kv=kv: h.matmul(out=kv[:, 0:128], lhsT=ktok[:, cs], rhs=vp1[:, cs],
                                                               start=False, stop=True),
                              reads=[ktok.B, vp1.B], writes=[kv.B])
                        fw.dve(lambda h, kv=kv, hp=hp: h.scalar_tensor_tensor(
                            out=R32[:], in0=R32[:], scalar=gct[:, hp:hp + 1], in1=kv[:, 0:128],
                            op0=ALU.mult, op1=ALU.add), reads=[R32.B, gct.B, kv.B], writes=[R32.B])
                        fw.dve(lambda h: h.tensor_tensor(out=Rb[:], in0=R32[:], in1=bd32[:],
                                                          op=ALU.mult),
                                reads=[R32.B, bd32.B], writes=[Rb.B])
                    for c4 in range(LIM.get('c4', 4)):
                        do_chunk(c4)
                    if LIM.get('stage', 99) < 7:
                        return
                    headnorm_gate(y_ps, sgate, yb[:, hp, tok0:tok0 + 512], yb.b[hp * 4 + tb], EPS)

                for tb in range(LIM.get('tb', 4)):
                    do_block(tb)

        def phaseC(si, l):
            PA, PB = wf[0], wf[1]
            sl = lambda t, i: t[:, i * 32:(i + 1) * 32]
            A_RE, A_IM, DT, MAG, ANG, CC, SS, T1, T2, T3, PM, RDEN, CRE, CIM, QQ, SLS = range(16)
            tabs = ya.b + yb.b
            cosT = ya[:].rearrange("p a s -> p (a s)").bitcast(F32).rearrange("p (g j) -> p g j", g=32)
            sinT = yb[:].rearrange("p a s -> p (a s)").bitcast(F32).rearrange("p (g j) -> p g j", g=32)
            ycf = yc[:].rearrange("p a s -> p (a s)").bitcast(F32)
            tmp1 = ycf[:, 0:2048].rearrange("p (g j) -> p g j", g=32)
            tmp2 = ycf[:, 2048:4096].rearrange("p (g j) -> p g j", g=32)

            def pa(fn_, eng="dve", extra=()):
                fw.op(eng, fn_, reads=[PA.B, PB.B] + list(extra), writes=[PA.B, PB.B])

            def tt(o, a, b, op):
                pa(lambda h: h.tensor_tensor(out=o, in0=a, in1=b, op=op))

            P = lambda i: sl(PA, i)
            for i in range(3):
                fw.dma("sp", P(i), s5A_d[l, i], writes=[PA.B])
            fw.dma("sp", s5v[:], s5v_d[l], writes=[s5v.B])
            pa(lambda h: h.activation(out=P(DT), in_=P(DT), func=AF.Exp), "act")
            tt(P(T1), P(DT), P(A_RE), ALU.mult)
            pa(lambda h: h.activation(out=P(MAG), in_=P(T1), func=AF.Exp), "act")
            tt(P(ANG), P(DT), P(A_IM), ALU.mult)
            pa(lambda h: h.activation(out=P(SS), in_=P(ANG), func=AF.Sin, scale=1.0 / 16.0), "act")
            pa(lambda h: h.activation(out=P(CC), in_=P(ANG), func=AF.Sin, scale=1.0 / 16.0, bias=float(np.pi / 2)), "act")

            def dbl(co, so, ci, si_):
                tt(P(T1), ci, ci, ALU.mult)
                tt(P(T2), si_, si_, ALU.mult)
                tt(P(T3), si_, ci, ALU.mult)
                tt(co, P(T1), P(T2), ALU.subtract)
                pa(lambda h: h.tensor_scalar(out=so, in0=P(T3), scalar1=2.0, scalar2=None, op0=ALU.mult))

            for _ in range(3):
                dbl(P(CC), P(SS), P(CC), P(SS))
            dbl(sl(PB, 0), sl(PB, 8), P(CC), P(SS))
            for k in range(1, 8):
                dbl(sl(PB, k), sl(PB, 8 + k), sl(PB, k - 1), sl(PB, 8 + k - 1))
            pa(lambda h: h.tensor_scalar(out=P(SLS), in0=sl(PB, 15), scalar1=sgn[:, 0:1], scalar2=None, op0=ALU.mult),
               extra=[sgn.B])
            tt(P(PM), P(MAG), sl(PB, 0), ALU.mult)
            pa(lambda h: h.tensor_scalar(out=P(PM), in0=P(PM), scalar1=-1.0, scalar2=None, op0=ALU.add))
            tt(P(QQ), P(MAG), sl(PB, 8), ALU.mult)
            tt(P(T1), P(A_RE), P(A_RE), ALU.mult)
            tt(P(T2), P(A_IM), P(A_IM), ALU.mult)
            tt(P(T1), P(T1), P(T2), ALU.add)
            pa(lambda h: h.reciprocal(out=P(RDEN), in_=P(T1)))
            tt(P(T1), P(PM), P(A_RE), ALU.mult)
            tt(P(T2), P(QQ), P(A_IM), ALU.mult)
            tt(P(T1), P(T1), P(T2), ALU.add)
            tt(P(CRE), P(T1), P(RDEN), ALU.mult)
            tt(P(T1), P(QQ), P(A_RE), ALU.mult)
            tt(P(T2), P(PM), P(A_IM), ALU.mult)
            tt(P(T1), P(T1), P(T2), ALU.subtract)
            tt(P(CIM), P(T1), P(RDEN), ALU.mult)

            fw.dve(lambda h: h.memset(cosT[:, :, 0:1], 1.0), writes=tabs)
            fw.dve(lambda h: h.memset(sinT[:, :, 0:1], 0.0), writes=tabs)
            for k in range(7):
                m = 1 << k
                cmb = sl(PB, k).unsqueeze(2).to_broadcast([128, 32, m])
                smb = sl(PB, 8 + k).unsqueeze(2).to_broadcast([128, 32, m])

                def lvl(m=m, cmb=cmb, smb=smb):
                    rw = dict(reads=tabs + yc.b + [PB.B], writes=tabs + yc.b)
                    fw.dve(lambda h: h.tensor_tensor(out=tmp1[:, :, 0:m], in0=cosT[:, :, 0:m], in1=cmb, op=ALU.mult), **rw)
                    fw.dve(lambda h: h.tensor_tensor(out=tmp2[:, :, 0:m], in0=sinT[:, :, 0:m], in1=smb, op=ALU.mult), **rw)
                    fw.dve(lambda h: h.tensor_tensor(out=cosT[:, :, m:2 * m], in0=tmp1[:, :, 0:m], in1=tmp2[:, :, 0:m],
                                                     op=ALU.subtract), **rw)
                    fw.dve(lambda h: h.tensor_tensor(out=tmp1[:, :, 0:m], in0=sinT[:, :, 0:m], in1=cmb, op=ALU.mult), **rw)
                    fw.dve(lambda h: h.tensor_tensor(out=tmp2[:, :, 0:m], in0=cosT[:, :, 0:m], in1=smb, op=ALU.mult), **rw)
                    fw.dve(lambda h: h.tensor_tensor(out=sinT[:, :, m:2 * m], in0=tmp1[:, :, 0:m], in1=tmp2[:, :, 0:m],
                                                     op=ALU.add), **rw)
                lvl()

            xh = [wf[2], wf[3]]
            stg = wf[4]
            stg2 = wf[5]
            tri = wf[6]
            BmT = wh[0:4]
            CmT = wh[4:8]
            u_bf, g12 = wh[8], wh[9]
            for t_ in CmT:
                fw.dve(lambda h, t_=t_: h.memset(t_[:], 0.0), writes=[t_.B])

            def xh_ap(g8):
                return xh[g8 // 4][:, (g8 % 4) * 128:(g8 % 4 + 1) * 128]

            def do_gc(gc):
                gs = slice(gc * 128, (gc + 1) * 128)
                fw.dma("sp", stg[:, 0:128], s5B_d[l, 0, :, gs], writes=[stg.B])
                fw.dma("sp", stg[:, 128:256], s5B_d[l, 1, :, gs], writes=[stg.B])
                v3 = lambda ap: ap.rearrange("p (g q) -> p g q", g=8)
                creb = P(CRE)[:, gc * 8:(gc + 1) * 8].unsqueeze(2).to_broadcast([128, 8, 16])
                cimb = P(CIM)[:, gc * 8:(gc + 1) * 8].unsqueeze(2).to_broadcast([128, 8, 16])
                rw = dict(reads=[stg.B, stg2.B, PA.B], writes=[stg2.B])
                bre, bim = v3(stg[:, 0:128]), v3(stg[:, 128:256])
                ta, tb_ = v3(stg2[:, 0:128]), v3(stg2[:, 128:256])
                bbre, bbim = v3(g12[:, 0:128]), v3(g12[:, 128:256])
                rwb = dict(reads=[stg.B, stg2.B, PA.B], writes=[g12.B])
                fw.dve(lambda h: h.tensor_tensor(out=ta, in0=bre, in1=creb, op=ALU.mult), **rw)
                fw.dve(lambda h: h.tensor_tensor(out=tb_, in0=bim, in1=cimb, op=ALU.mult), **rw)
                fw.dve(lambda h: h.tensor_tensor(out=bbre, in0=ta, in1=tb_, op=ALU.subtract), **rwb)
                fw.dve(lambda h: h.tensor_tensor(out=ta, in0=bim, in1=creb, op=ALU.mult), **rw)
                fw.dve(lambda h: h.tensor_tensor(out=tb_, in0=bre, in1=cimb, op=ALU.mult), **rw)
                fw.dve(lambda h: h.tensor_tensor(out=bbim, in0=ta, in1=tb_, op=ALU.add), **rwb)
                tpx = tp_ps[0]
                ptr, pti = tpx[:, 0, :], tpx[:, 1, :]
                fw.pe(lambda h: h.transpose(out=ptr, in_=g12[:, 0:128], identity=ident[:]),
                      reads=[g12.B, ident.B], writes=[tpx.B], inc=False)
                fw.pe(lambda h: h.transpose(out=pti, in_=g12[:, 128:256], identity=ident[:]),
                      reads=[g12.B, ident.B], writes=[tpx.B])
                fw.act(lambda h: h.activation(out=tri[:, 0:64], in_=ptr[:, 0:64], func=AF.Copy), reads=[tpx.B], writes=[tri.B])
                fw.act(lambda h: h.activation(out=tri[:, 64:128], in_=pti[:, 0:64], func=AF.Copy), reads=[tpx.B], writes=[tri.B])
                fw.act(lambda h: h.activation(out=tri[:, 128:192], in_=pti[:, 0:64], func=AF.Copy), reads=[tpx.B], writes=[tri.B])
                fw.act(lambda h: h.activation(out=tri[:, 192:256], in_=ptr[:, 0:64], func=AF.Copy, scale=-1.0),
                       reads=[tpx.B], writes=[tri.B])
                for g8 in range(8):
                    bt = BmT[g8 // 2]
                    o = (g8 % 2) * 256
                    fw.dve(lambda h, bt=bt, o=o, g8=g8: h.tensor_scalar(
                        out=bt[:, o:o + 256], in0=tri[:, 0:256], scalar1=rowmask[:, g8:g8 + 1], scalar2=None, op0=ALU.mult),
                        reads=[tri.B, rowmask.B], writes=[bt.B])
                fw.dma("sp", stg[:, 0:128], s5C_d[l, 0, :, gs], writes=[stg.B])
                fw.dma("sp", stg[:, 128:256], s5C_d[l, 1, :, gs], writes=[stg.B])
                fw.dve(lambda h: h.tensor_scalar(out=stg[:, 0:128], in0=stg[:, 0:128], scalar1=sgn[:, 1:2], scalar2=None,
                                                 op0=ALU.mult), reads=[stg.B, sgn.B], writes=[stg.B])
                fw.dve(lambda h: h.tensor_scalar(out=stg[:, 128:256], in0=stg[:, 128:256], scalar1=-1.0, scalar2=None,
                                                 op0=ALU.mult), reads=[stg.B], writes=[stg.B])
                for g8 in range(8):
                    ct = CmT[g8 // 2]
                    o = (g8 % 2) * 256
                    for ver in range(2):
                        fw.act(lambda h, ct=ct, o=o, g8=g8, ver=ver: h.activation(
                            out=ct[:, o + ver * 128 + g8 * 16:o + ver * 128 + (g8 + 1) * 16],
                            in_=stg[:, ver * 128 + g8 * 16:ver * 128 + (g8 + 1) * 16], func=AF.Copy),
                            reads=[stg.B], writes=[ct.B])
                w_u = wload(l, [(O_CU + gc * 128, 128)])

                def do_block(tb):
                    tok0 = tb * 512
                    pu = ps_next()
                    proj(pu, w_u, tok0)
                    u32 = wf[7]
                    fw.act(lambda h: h.activation(out=u_bf[:], in_=pu[:], func=AF.Copy), reads=[pu.B], writes=[u_bf.B])
                    fw.act(lambda h: h.activation(out=u32[:], in_=pu[:], func=AF.Copy), reads=[pu.B], writes=[u32.B])
                    y_ps = ps_y()

                    def do_sb(sb):
                        ts = slice(sb * 128, (sb + 1) * 128)
                        first = (tb == 0 and sb == 0)

                        def do_g(g8):
                            g = gc * 8 + g8
                            bt, ct = BmT[g8 // 2], CmT[g8 // 2]
                            o = (g8 % 2) * 256
                            bu = ps_next()
                            fw.pe(lambda h: h.matmul(out=bu[:, 0:128], lhsT=bt[:, o:o + 128], rhs=u_bf[:, ts],
                                                     start=True, stop=True), reads=[bt.B, u_bf.B], writes=[bu.B], inc=False)
                            fw.pe(lambda h: h.matmul(out=bu[:, 128:256], lhsT=bt[:, o + 128:o + 256], rhs=u_bf[:, ts],
                                                     start=True, stop=True), reads=[bt.B, u_bf.B], writes=[bu.B])
                            wt = stg
                            fw.dve(lambda h: h.tensor_tensor(out=wt[:, 0:128], in0=bu[:, 0:128], in1=cosT[:, g, :],
                                                             op=ALU.mult), reads=[bu.B] + tabs, writes=[wt.B])
                            fw.dve(lambda h: h.tensor_tensor(out=wt[:, 128:256], in0=bu[:, 128:256], in1=sinT[:, g, :],
                                                             op=ALU.mult), reads=[bu.B] + tabs, writes=[wt.B])
                            fw.dve(lambda h: h.tensor_tensor(out=wt[:, 256:384], in0=wt[:, 0:128], in1=wt[:, 128:256],
                                                             op=ALU.add), reads=[wt.B], writes=[wt.B])
                            xg = xh_ap(g8)
                            xb = xh[g8 // 4].B
                            init = 0.0 if first else carry[:, g8:g8 + 1]
                            fw.dve(lambda h: h.tensor_tensor_scan(
                                out=xg, data0=P(MAG)[:, g:g + 1].to_broadcast([128, 128]), data1=wt[:, 256:384],
                                initial=init, op0=ALU.mult, op1=ALU.add),
                                reads=[wt.B, PA.B, carry.B], writes=[xb])
                            fw.dve(lambda h: h.tensor_tensor(out=g12[:, 0:128], in0=xg, in1=cosT[:, g, :], op=ALU.mult),
                                   reads=[xb] + tabs, writes=[g12.B])
                            fw.dve(lambda h: h.tensor_tensor(out=g12[:, 128:256], in0=xg, in1=sinT[:, g, :], op=ALU.mult),
                                   reads=[xb] + tabs, writes=[g12.B])
                            fw.pe(lambda h: h.matmul(out=y_ps[:, ts], lhsT=ct[:, o:o + 128], rhs=g12[:, 0:128],
                                                     start=(g8 == 0), stop=False), reads=[ct.B, g12.B], writes=[y_ps.B], inc=False)
                            fw.pe(lambda h: h.matmul(out=y_ps[:, ts], lhsT=ct[:, o + 128:o + 256], rhs=g12[:, 128:256],
                                                     start=False, stop=(g8 == 7)), reads=[ct.B, g12.B], writes=[y_ps.B])

                        for g8 in range(8):
                            do_g(g8)
                        csw = ps_next()
                        for hx in range(2):
                            xl = xh[hx][:].rearrange("p (g j) -> p g j", g=4)[:, :, 127]
                            fw.dve(lambda h, hx=hx, xl=xl: h.tensor_copy(out=chl[:, hx * 4:(hx + 1) * 4], in_=xl),
                                   reads=[xh[hx].B], writes=[chl.B])
                        fw.dve(lambda h: h.tensor_copy(out=cbk[:], in_=chl[:, 0:8]), reads=[chl.B], writes=[cbk.B])
                        for hx in range(2):
                            xl = xh[hx][:].rearrange("p (g j) -> p g j", g=4)[:, :, 127]
                            fw.dve(lambda h, hx=hx, xl=xl: h.tensor_tensor(out=chl[:, 8 + hx * 4:8 + (hx + 1) * 4], in0=xl,
                                                                          in1=cbk[:, hx * 4:(hx + 1) * 4], op=ALU.subtract),
                                   reads=[xh[hx].B, cbk.B], writes=[chl.B])
                        fw.pe(lambda h: h.matmul(out=csw[:, 0:8], lhsT=swapb[:], rhs=chl[:, 0:8], start=True, stop=False),
                              reads=[swapb.B, chl.B], writes=[csw.B], inc=False)
                        fw.pe(lambda h: h.matmul(out=csw[:, 0:8], lhsT=swapb[:], rhs=chl[:, 8:16], start=False, stop=True),
                              reads=[swapb.B, chl.B], writes=[csw.B])
                        fw.dve(lambda h: h.tensor_tensor(out=cst[:, 0:8], in0=csw[:, 0:8], in1=P(SLS)[:, gc * 8:(gc + 1) * 8],
                                                         op=ALU.mult), reads=[csw.B, PA.B], writes=[cst.B])
                        for hx in range(2):
                            xl = xh[hx][:].rearrange("p (g j) -> p g j", g=4)[:, :, 127]
                            fw.dve(lambda h, hx=hx, xl=xl: h.tensor_tensor(
                                out=cst[:, 8 + hx * 4:8 + (hx + 1) * 4], in0=xl,
                                in1=sl(PB, 7)[:, gc * 8 + hx * 4:gc * 8 + (hx + 1) * 4], op=ALU.mult),
                                reads=[xh[hx].B, PB.B], writes=[cst.B])
                        fw.dve(lambda h: h.tensor_tensor(out=carry[:], in0=cst[:, 0:8], in1=cst[:, 8:16], op=ALU.add),
                               reads=[cst.B], writes=[carry.B])

                    for sb in range(4):
                        do_sb(sb)
                    y32, gt = wf[6], wf[7]
                    fw.dve(lambda h: h.scalar_tensor_tensor(out=y32[:], in0=u32[:], scalar=s5v[:, gc:gc + 1], in1=y_ps[:],
                                                            op0=ALU.mult, op1=ALU.add),
                           reads=[u32.B, s5v.B, y_ps.B], writes=[y32.B])
                    fw.act(lambda h: h.activation(out=gt[:], in_=y32[:], func=AF.Square), reads=[y32.B], writes=[gt.B])
                    fw.dve(lambda h: h.tensor_scalar(out=gt[:], in0=gt[:], scalar1=0.044715, scalar2=1.0, op0=ALU.mult,
                                                     op1=ALU.add), reads=[gt.B], writes=[gt.B])
                    fw.dve(lambda h: h.tensor_tensor(out=gt[:], in0=gt[:], in1=y32[:], op=ALU.mult),
                           reads=[gt.B, y32.B], writes=[gt.B])
                    fw.act(lambda h: h.activation(out=gt[:], in_=gt[:], func=AF.Tanh, scale=0.7978845608028654),
                           reads=[gt.B], writes=[gt.B])
                    fw.dve(lambda h: h.tensor_scalar(out=gt[:], in0=gt[:], scalar1=1.0, scalar2=0.5, op0=ALU.add,
                                                     op1=ALU.mult), reads=[gt.B], writes=[gt.B])
                    fw.dve(lambda h: h.tensor_tensor(out=yc[:, gc, tok0:tok0 + 512], in0=gt[:], in1=y32[:], op=ALU.mult),
                           reads=[gt.B, y32.B], writes=[yc.b[gc * 4 + tb]])

                for tb in range(LIM.get('tb', 4)):
                    do_block(tb)

            for gc in range(4):
                do_gc(gc)

            gw = wh[0:4]
            for c in range(4):
                fw.dma("sp", stg[:], gluw_d[l, c * 128:(c + 1) * 128, :], writes=[stg.B])
                fw.dve(lambda h, c=c: h.tensor_copy(out=gw[c][:], in_=stg[:]), reads=[stg.B], writes=[gw[c].B])
            wgs = [wload(l, [(O_CG + oc * 128, 128)]) for oc in range(4)]

            def glu_block(tb):
                tok0 = tb * 512
                sgl = [wf[0], wf[1], wf[2], wf[3]]
                for oc in range(4):
                    gp = pg[oc]
                    for c in range(4):
                        fw.pe(lambda h, oc=oc, c=c, gp=gp: h.matmul(out=gp[:], lhsT=gw[c][:, oc * 128:(oc + 1) * 128],
                                                                    rhs=yc[:, c, tok0:tok0 + 512], start=(c == 0), stop=(c == 3)),
                              reads=[gw[c].B, yc.b[c * 4 + tb]], writes=[gp.B], inc=(c == 3))
                    fw.act(lambda h, oc=oc, gp=gp: h.activation(out=sgl[oc][:], in_=gp[:], func=AF.Sigmoid,
                                                                bias=s5v[:, 4 + oc:5 + oc], scale=1.0),
                           reads=[gp.B, s5v.B], writes=[sgl[oc].B])
                for oc in range(4):
                    pgt = ps_y()
                    proj(pgt, wgs[oc], tok0)
                    sgt = wf[4]
                    silu_from_psum(sgt, pgt)
                    fw.dve(lambda h, oc=oc, sgt=sgt: h.tensor_tensor(out=sgt[:], in0=sgt[:], in1=sgl[oc][:], op=ALU.mult),
                           reads=[sgt.B, sgl[oc].B], writes=[sgt.B])
                    fw.dve(lambda h, oc=oc, sgt=sgt: h.tensor_tensor(out=yc[:, oc, tok0:tok0 + 512],
                                                                     in0=yc[:, oc, tok0:tok0 + 512], in1=sgt[:], op=ALU.mult),
                           reads=[sgt.B, yc.b[oc * 4 + tb]], writes=[yc.b[oc * 4 + tb]])

            for tb in range(LIM.get('tb', 4)):
                glu_block(tb)

        def wload_shift(l, c0, mu_src):
            s1 = wslot[wctr[0] % NSLOT]
            wctr[0] += 1
            s2 = wslot[wctr[0] % NSLOT]
            wctr[0] += 1
            stg = wst[wctr[1] % 2]
            wctr[1] += 1
            mub = wf[2]
            fw.dma("sp", mub[:, 0:128], mu_src.broadcast_to([128, 128]), writes=[mub.B])
            fw.dve(lambda h: h.tensor_scalar(out=mub[:, 128:256], in0=mub[:, 0:128], scalar1=-1.0, scalar2=1.0,
                                             op0=ALU.mult, op1=ALU.add), reads=[mub.B], writes=[mub.B])
            fw.dma("sp", stg[:], w_in_d[l, :, c0:c0 + 128].rearrange("(c p) n -> p c n", p=128), writes=[stg.B])
            fw.dve(lambda h: h.tensor_tensor(out=stg[:], in0=stg[:], in1=gexp[:], op=ALU.mult),
                   reads=[stg.B, gexp.B], writes=[stg.B])
            fw.dve(lambda h: h.tensor_tensor(out=s2[:], in0=stg[:], in1=mub[:, 0:128].unsqueeze(1).to_broadcast([128, 8, 128]),
                                             op=ALU.mult), reads=[stg.B, mub.B], writes=[s2.B])
            fw.dve(lambda h: h.tensor_tensor(out=s1[:], in0=stg[:], in1=mub[:, 128:256].unsqueeze(1).to_broadcast([128, 8, 128]),
                                             op=ALU.mult), reads=[stg.B, mub.B], writes=[s1.B])
            return s1, s2

        def proj_shift(ps, s12, tok0):
            proj(ps, s12[0], tok0, shift=0, start=True, stop=False)
            proj(ps, s12[1], tok0, shift=1, start=False, stop=True)

        def phaseA(si, l):
            CDEC = 0.6065306597126334
            ybf = yb[:].rearrange("p a s -> p (a s)")
            SB = [Buf(f"scrA{i}") for i in range(16)]
            SV = [ybf[:, i * 512:(i + 1) * 512] for i in range(16)]
            fw.dve(lambda h: h.memset(cst[:, 0:1], 0.0), reads=yb.b, writes=SB + [cst.B])
            QP = [SV[i] for i in range(4)]
            QPB = SB[0:4]
            MK1, MK1B = SV[4], SB[4]
            MK2, MK2B = SV[5][:, 0:256], SB[5]
            E1, E2, E3 = SV[6], SV[7], SV[8]
            XB = [SV[9], SV[10]]
            PP = SV[11]
            MISC = SV[12]
            kt_tok, bt_tok, vp0 = SV[13], SV[14], SV[15]
            vp1 = wh[9]
            v_bf, sqk, rk_bf, rt, at, kt, bt = wh[2], wh[3], wh[4], wh[5], wh[6], wh[7], wh[8]
            sgate, Pq, r32, wf6, wf7, wf0, wf1, wf2 = wf[3], wf[4], wf[5], wf[6], wf[7], wf[0], wf[1], wf[2]
            fw.dma("sp", MK1, cd["rw_masks"][:, 0:512], writes=[MK1B])
            fw.dma("sp", MK2, cd["rw_masks"][:, 512:768], writes=[MK2B])
            fw.dma("sp", rvec[:], rvec_d[l], writes=[rvec.B])
            fw.dve(lambda h: h.tensor_scalar(out=omka[:], in0=rvec[:, :, 3], scalar1=-1.0, scalar2=1.0, op0=ALU.mult,
                                             op1=ALU.add), reads=[rvec.B], writes=[omka.B])
            for i in range(4):
                fw.dve(lambda h, i=i: h.memset(QP[i], 0.0), writes=[QPB[i]])
            fw.dve(lambda h: h.memset(vp0, 0.0), writes=[SB[15]])
            fw.dve(lambda h: h.memset(vp1[:], 0.0), writes=[vp1.B])
            fw.dve(lambda h: h.memset(MISC, 0.0), writes=[SB[12]])
            fw.dve(lambda h: h.memset(lw2[:], 0.0), writes=[lw2.B])
            fw.dve(lambda h: h.memset(la2[:], 0.0), writes=[la2.B])
            w_wa = wload_shift(l, O_XW, mu_wa_d[l])
            for tb in range(4):
                pwa = ps_next()
                proj_shift(pwa, w_wa, tb * 512)
                dst = ya[:, 3, tb * 512:(tb + 1) * 512]
                fw.act(lambda h, pwa=pwa, dst=dst: h.activation(out=dst[0:64, :], in_=pwa[0:64, :], func=AF.Tanh),
                       reads=[pwa.B], writes=[ya.b[12 + tb]])
                fw.act(lambda h, pwa=pwa, dst=dst: h.activation(out=dst[64:128, :], in_=pwa[64:128, :], func=AF.Copy),
                       reads=[pwa.B], writes=[ya.b[12 + tb]])

            def do_hp(hp):
                V = lambda j: rvec[:, hp, j:j + 1]
                W0, A0, KK, KA, LNW, LNB, RK = (V(j) for j in range(7))
                w_r = wload_shift(l, O_AR + hp * 128, mu_rkv_d[l, 0:1, hp * 128:(hp + 1) * 128])
                w_k = wload_shift(l, O_AK + hp * 128, mu_rkv_d[l, 1:2, hp * 128:(hp + 1) * 128])
                w_v = wload_shift(l, O_AV + hp * 128, mu_rkv_d[l, 2:3, hp * 128:(hp + 1) * 128])
                w_g = wload(l, [(O_AG + hp * 128, 128)])
                stg = wst[wctr[1] % 2]
                wctr[1] += 1
                stv = stg[:].rearrange("p c n -> p (c n)")
                fw.dma("sp", stv[0:64, 0:128], w2a2_d[l, 0, :, hp * 128:(hp + 1) * 128], writes=[stg.B])
                fw.dma("sp", stv[64:128, 0:128], w2a2_d[l, 1, :, hp * 128:(hp + 1) * 128], writes=[stg.B])
                fw.dve(lambda h: h.tensor_copy(out=lw2[0:64, :], in_=stv[0:64, 0:128]), reads=[stg.B], writes=[lw2.B])
                fw.dve(lambda h: h.tensor_copy(out=la2[64:128, :], in_=stv[64:128, 0:128]), reads=[stg.B], writes=[la2.B])
                fw.dve(lambda h: h.memset(St32[:], 0.0), writes=[St32.B])
                fw.dve(lambda h: h.memset(Stb[:], 0.0), writes=[Stb.B])

                def do_block(tb):
                    tok0 = tb * 512
                    twa = ya[:, 3, tok0:tok0 + 512]
                    twab = ya.b[12 + tb]
                    pr, pk = ps_next(), ps_next()
                    proj_shift(pr, w_r, tok0)
                    proj_shift(pk, w_k, tok0)
                    fw.act(lambda h: h.activation(out=r32[:], in_=pr[:], func=AF.Copy), reads=[pr.B], writes=[r32.B])
                    pw_, pa_ = ps_next(), ps_next()
                    fw.pe(lambda h: h.matmul(out=pw_[:], lhsT=lw2[:], rhs=twa, start=True, stop=True),
                          reads=[lw2.B, twab], writes=[pw_.B])
                    fw.pe(lambda h: h.matmul(out=pa_[:], lhsT=la2[:], rhs=twa, start=True, stop=True),
                          reads=[la2.B, twab], writes=[pa_.B])
                    sg, aa = wf0, wf6
                    fw.act(lambda h: h.activation(out=sg[:], in_=pw_[:], func=AF.Sigmoid, bias=W0, scale=1.0),
                           reads=[pw_.B, rvec.B], writes=[sg.B])
                    fw.act(lambda h: h.activation(out=aa[:], in_=pa_[:], func=AF.Sigmoid, bias=A0, scale=1.0),
                           reads=[pa_.B, rvec.B], writes=[aa.B])
                    kkn = wf7
                    fw.dve(lambda h: h.tensor_scalar(out=kkn[:], in0=pk[:], scalar1=KK, scalar2=None, op0=ALU.mult),
                           reads=[pk.B, rvec.B], writes=[kkn.B])
                    fw.act(lambda h: h.activation(out=sqk[:], in_=kkn[:], func=AF.Square), reads=[kkn.B], writes=[sqk.B])
                    ss = ps_next()
                    fw.pe(lambda h: h.matmul(out=ss[:], lhsT=bdo64[:], rhs=sqk[:], start=True, stop=True),
                          reads=[bdo64.B, sqk.B], writes=[ss.B])
                    rn = wf1
                    fw.act(lambda h: h.activation(out=rn[:], in_=ss[:], func=AF.Sqrt, bias=1e-12, scale=64.0),
                           reads=[ss.B], writes=[rn.B])
                    fw.dve(lambda h: h.reciprocal(out=rn[:], in_=rn[:]), reads=[rn.B], writes=[rn.B])
                    fw.dve(lambda h: h.tensor_tensor(out=kkn[:], in0=kkn[:], in1=rn[:], op=ALU.mult),
                           reads=[kkn.B, rn.B], writes=[kkn.B])
                    k2 = wf2
                    fw.dve(lambda h: h.tensor_scalar(out=wf1[:], in0=aa[:], scalar1=KA, scalar2=omka[:, hp:hp + 1],
                                                     op0=ALU.mult, op1=ALU.add), reads=[aa.B, rvec.B, omka.B], writes=[wf1.B])
                    fw.dve(lambda h: h.tensor_tensor(out=k2[:], in0=pk[:], in1=wf1[:], op=ALU.mult),
                           reads=[pk.B, wf1.B], writes=[k2.B])
                    fw.dve(lambda h: h.scalar_tensor_tensor(out=rk_bf[:], in0=r32[:], scalar=RK, in1=k2[:], op0=ALU.mult,
                                                            op1=ALU.mult), reads=[r32.B, rvec.B, k2.B], writes=[rk_bf.B])
                    bb_ = wf1
                    fw.dve(lambda h: h.tensor_tensor(out=bb_[:], in0=kkn[:], in1=aa[:], op=ALU.mult),
                           reads=[kkn.B, aa.B], writes=[bb_.B])
                    pv = ps_next()
                    proj_shift(pv, w_v, tok0)
                    fw.act(lambda h: h.activation(out=v_bf[:], in_=pv[:], func=AF.Copy), reads=[pv.B], writes=[v_bf.B])
                    pgt = ps_next()
                    proj(pgt, w_g, tok0)
                    silu_from_psum(sgate, pgt)
                    cs = wf6
                    for c4 in range(4):
                        fw.dve(lambda h, c4=c4: h.tensor_tensor_scan(
                            out=cs[:, c4 * 128:(c4 + 1) * 128], data0=onec[:, 0:1].to_broadcast([128, 128]),
                            data1=sg[:, c4 * 128:(c4 + 1) * 128], initial=0.0, op0=ALU.mult, op1=ALU.add),
                            reads=[sg.B, onec.B], writes=[cs.B])
                    fw.dve(lambda h: h.tensor_tensor(out=sg[:], in0=cs[:], in1=sg[:], op=ALU.subtract),
                           reads=[cs.B, sg.B], writes=[sg.B])
                    fw.act(lambda h: h.activation(out=Pq[:], in_=cs[:], func=AF.Exp, scale=-CDEC), reads=[cs.B], writes=[Pq.B])
                    fw.act(lambda h: h.activation(out=sg[:], in_=sg[:], func=AF.Exp, scale=-CDEC), reads=[sg.B], writes=[sg.B])
                    fw.act(lambda h: h.activation(out=cs[:], in_=cs[:], func=AF.Exp, scale=CDEC), reads=[cs.B], writes=[cs.B])
                    PqA, Pk = sg, cs
                    fw.dve(lambda h: h.tensor_tensor(out=rt[:], in0=r32[:], in1=Pq[:], op=ALU.mult),
                           reads=[r32.B, Pq.B], writes=[rt.B])
                    fw.dve(lambda h: h.scalar_tensor_tensor(out=at[:], in0=kkn[:], scalar=-1.0, in1=PqA[:], op0=ALU.mult,
                                                            op1=ALU.mult), reads=[kkn.B, PqA.B], writes=[at.B])
                    fw.dve(lambda h: h.tensor_tensor(out=kt[:], in0=k2[:], in1=Pk[:], op=ALU.mult),
                           reads=[k2.B, Pk.B], writes=[kt.B])
                    fw.dve(lambda h: h.tensor_tensor(out=bt[:], in0=bb_[:], in1=Pk[:], op=ALU.mult),
                           reads=[bb_.B, Pk.B], writes=[bt.B])
                    for c4 in range(4):
                        for hh in range(2):
                            rows = slice(hh * 64, (hh + 1) * 64)
                            fw.act(lambda h, c4=c4, hh=hh, rows=rows: h.activation(
                                out=QP[c4][rows, (2 * hh) * 128:(2 * hh + 1) * 128], in_=at[rows, c4 * 128:(c4 + 1) * 128],
                                func=AF.Copy), reads=[at.B], writes=[QPB[c4]])
                            fw.act(lambda h, c4=c4, hh=hh, rows=rows: h.activation(
                                out=QP[c4][rows, (2 * hh + 1) * 128:(2 * hh + 2) * 128], in_=rt[rows, c4 * 128:(c4 + 1) * 128],
                                func=AF.Copy), reads=[rt.B], writes=[QPB[c4]])
                    tpa, tpb = tp_ps[0], tp_ps[1]
                    for c4 in range(4):
                        fw.pe(lambda h, c4=c4: h.transpose(out=tpa[:, c4, :], in_=kt[:, c4 * 128:(c4 + 1) * 128],
                                                           identity=ident[:]), reads=[kt.B, ident.B], writes=[tpa.B], inc=False)
                    for c4 in range(4):
                        fw.pe(lambda h, c4=c4: h.transpose(out=tpa[:, 4 + c4, :], in_=bt[:, c4 * 128:(c4 + 1) * 128],
                                                           identity=ident[:]), reads=[bt.B, ident.B], writes=[tpa.B],
                              inc=(c4 == 3))
                    for c4 in range(4):
                        fw.pe(lambda h, c4=c4: h.transpose(out=tpb[:, c4, :], in_=v_bf[:, c4 * 128:(c4 + 1) * 128],
                                                           identity=ident[:]), reads=[v_bf.B, ident.B], writes=[tpb.B],
                              inc=(c4 == 3))
                    v4 = lambda ap: ap.rearrange("p (c f) -> p c f", c=4)
                    fw.act(lambda h: h.activation(out=v4(kt_tok), in_=tpa[:, 0:4, :], func=AF.Copy),
                           reads=[tpa.B], writes=[SB[13]])
                    fw.dve(lambda h: h.tensor_copy(out=v4(bt_tok), in_=tpa[:, 4:8, :]), reads=[tpa.B], writes=[SB[14]])
                    fw.act(lambda h: h.activation(out=v4(vp0)[:, :, 0:64], in_=tpb[:, 0:4, 0:64], func=AF.Copy),
                           reads=[tpb.B], writes=[SB[15]])
                    fw.dve(lambda h: h.tensor_copy(out=v4(vp1[:])[:, :, 64:128], in_=tpb[:, 0:4, 64:128]),
                           reads=[tpb.B], writes=[vp1.B])
                    y_ps = ps_y()

                    def do_chunk(c4):
                        cs_ = slice(c4 * 128, (c4 + 1) * 128)
                        pa1, pa2, pa3 = ps_next(), ps_next(), ps_next()
                        fw.pe(lambda h: h.matmul(out=pa1[:], lhsT=kt[:, cs_], rhs=QP[c4], start=True, stop=True),
                              reads=[kt.B, QPB[c4]], writes=[pa1.B])
                        fw.pe(lambda h: h.matmul(out=pa2[:], lhsT=bt[:, cs_], rhs=QP[c4], start=True, stop=True),
                              reads=[bt.B, QPB[c4]], writes=[pa2.B])
                        for hh in range(2):
                            fw.pe(lambda h, hh=hh: h.matmul(out=pa3[:, hh * 128:(hh + 1) * 128],
                                                            lhsT=QP[c4][:, (2 * hh) * 128:(2 * hh + 1) * 128], rhs=bt[:, cs_],
                                                            start=True, stop=True),
                                  reads=[bt.B, QPB[c4]], writes=[pa3.B], inc=(hh == 1))
                        fw.dve(lambda h: h.tensor_tensor(out=E1, in0=pa1[:], in1=MK1, op=ALU.mult),
                               reads=[pa1.B, MK1B], writes=[SB[6]])
                        fw.dve(lambda h: h.tensor_tensor(out=E2, in0=pa2[:], in1=MK1, op=ALU.mult),
                               reads=[pa2.B, MK1B], writes=[SB[7]])
                        fw.dve(lambda h: h.tensor_tensor(out=E3[:, 0:256], in0=pa3[:, 0:256], in1=MK2, op=ALU.mult),
                               reads=[pa3.B, MK2B], writes=[SB[8]])

                        def Xj(j, hh):
                            if j == 0:
                                return E3[:, hh * 128:(hh + 1) * 128], SB[8]
                            return XB[j % 2][:, (2 * hh) * 128:(2 * hh + 1) * 128], SB[9 + j % 2]

                        def Bj(j, hh):
                            if j == 0:
                                return E2[:, (2 * hh) * 128:(2 * hh + 1) * 128], SB[7]
                            return XB[j % 2][:, (2 * hh + 1) * 128:(2 * hh + 2) * 128], SB[9 + j % 2]

                        def Pj(j, hh):
                            o = (j % 2) * 256 + hh * 128
                            return PP[:, o:o + 128]

                        e2v = E2.rearrange("p (a b) -> p a b", a=2)[:, :, 0:128]
                        fw.dve(lambda h: h.tensor_tensor(out=PP[:, 0:256].rearrange("p (a b) -> p a b", a=2), in0=e2v,
                                                         in1=ident[:].unsqueeze(1).to_broadcast([128, 2, 128]), op=ALU.add),
                               reads=[SB[7], ident.B], writes=[SB[11]])
                        for j in range(6):
                            last = j == 5
                            pxb = ps_next()
                            for hh in range(2):
                                xa_, xb_ = Xj(j, hh)
                                ba_, bb2 = Bj(j, hh)
                                fw.pe(lambda h, hh=hh, xa_=xa_, ba_=ba_, pxb=pxb: h.matmul(
                                    out=pxb[:, (2 * hh) * 128:(2 * hh + 1) * 128], lhsT=ba_, rhs=xa_, start=True, stop=True),
                                    reads=[xb_, bb2], writes=[pxb.B], inc=(last and hh == 1))
                                if not last:
                                    fw.pe(lambda h, hh=hh, xa_=xa_, ba_=ba_, pxb=pxb: h.matmul(
                                        out=pxb[:, (2 * hh + 1) * 128:(2 * hh + 2) * 128], lhsT=xa_, rhs=ba_, start=True,
                                        stop=True), reads=[xb_, bb2], writes=[pxb.B], inc=(hh == 1))
                            nxt = XB[(j + 1) % 2]
                            if last:
                                fw.act(lambda h, pxb=pxb, nxt=nxt: h.activation(
                                    out=nxt.rearrange("p (a b) -> p a b", a=2)[:, :, 0:128],
                                    in_=pxb[:].rearrange("p (a b) -> p a b", a=2)[:, :, 0:128], func=AF.Copy),
                                    reads=[pxb.B], writes=[SB[9 + (j + 1) % 2]])
                            else:
                                fw.act(lambda h, pxb=pxb, nxt=nxt: h.activation(out=nxt, in_=pxb[:], func=AF.Copy),
                                       reads=[pxb.B], writes=[SB[9 + (j + 1) % 2]])
                            pp = ps_next()
                            for hh in range(2):
                                xn_, xnb_ = Xj(j + 1, hh)
                                fw.pe(lambda h, hh=hh, pp=pp, j=j: h.matmul(out=pp[:, hh * 128:(hh + 1) * 128], lhsT=ident[:],
                                                                             rhs=Pj(j, hh), start=True, stop=False),
                                      reads=[ident.B, SB[11]], writes=[pp.B], inc=False)
                                fw.pe(lambda h, hh=hh, pp=pp, j=j, xn_=xn_: h.matmul(
                                    out=pp[:, hh * 128:(hh + 1) * 128], lhsT=xn_, rhs=Pj(j, hh), start=False, stop=True),
                                    reads=[xnb_, SB[11]], writes=[pp.B], inc=(hh == 1))
                            o = ((j + 1) % 2) * 256
                            fw.dve(lambda h, pp=pp, o=o: h.tensor_copy(out=PP[:, o:o + 256], in_=pp[:, 0:256]),
                                   reads=[pp.B], writes=[SB[11]])
                        r0 = ps_next()
                        fw.pe(lambda h: h.matmul(out=r0[:, 0:128], lhsT=at[:, cs_], rhs=Stb[:], start=True, stop=False),
                              reads=[at.B, Stb.B], writes=[r0.B], inc=False)
                        vps = [vp0, vp1[:]]
                        vpb = [SB[15], vp1.B]
                        for hh in range(2):
                            fw.pe(lambda h, hh=hh: h.matmul(out=r0[:, hh * 64:(hh + 1) * 64],
                                                            lhsT=E1[:, (2 * hh) * 128:(2 * hh + 1) * 128],
                                                            rhs=vps[hh][:, c4 * 128 + hh * 64:c4 * 128 + (hh + 1) * 64],
                                                            start=False, stop=(hh == 1)),
                                  reads=[SB[6], vpb[hh]], writes=[r0.B], inc=(hh == 1))
                        r0b = MISC[:, 0:128]
                        fw.act(lambda h: h.activation(out=r0b, in_=r0[:, 0:128], func=AF.Copy), reads=[r0.B], writes=[SB[12]])
                        up = ps_next()
                        for hh in range(2):
                            fw.pe(lambda h, hh=hh: h.matmul(out=up[:, hh * 64:(hh + 1) * 64], lhsT=Pj(6, hh),
                                                            rhs=r0b[:, hh * 64:(hh + 1) * 64], start=True, stop=True),
                                  reads=[SB[11], SB[12]], writes=[up.B], inc=(hh == 1))
                        upad = [MISC[:, 128:256], MISC[:, 256:384]]
                        fw.act(lambda h: h.activation(out=upad[0][:, 0:64], in_=up[:, 0:64], func=AF.Copy),
                               reads=[up.B], writes=[SB[12]])
                        fw.dve(lambda h: h.tensor_copy(out=upad[1][:, 64:128], in_=up[:, 64:128]),
                               reads=[up.B], writes=[SB[12]])
                        fw.pe(lambda h: h.matmul(out=y_ps[:, cs_], lhsT=Stb[:], rhs=rt[:, cs_], start=True, stop=False),
                              reads=[Stb.B, rt.B], writes=[y_ps.B], inc=False)
                        for hh in range(2):
                            fw.pe(lambda h, hh=hh: h.matmul(out=y_ps[:, cs_], lhsT=upad[hh],
                                                            rhs=E2[:, (2 * hh + 1) * 128:(2 * hh + 2) * 128],
                                                            start=False, stop=False),
                                  reads=[SB[12], SB[7]], writes=[y_ps.B], inc=False)
                            fw.pe(lambda h, hh=hh: h.matmul(out=y_ps[:, cs_], lhsT=vps[hh][:, c4 * 128:(c4 + 1) * 128],
                                                            rhs=E1[:, (2 * hh + 1) * 128:(2 * hh + 2) * 128],
                                                            start=False, stop=(hh == 1)),
                                  reads=[vpb[hh], SB[6]], writes=[y_ps.B], inc=(hh == 1))
                        su = ps_next()
                        for hh in range(2):
                            fw.pe(lambda h, hh=hh: h.matmul(out=su[:, 0:128], lhsT=bt_tok[:, cs_], rhs=upad[hh],
                                                            start=(hh == 0), stop=False),
                                  reads=[SB[14], SB[12]], writes=[su.B], inc=False)
                        for hh in range(2):
                            fw.pe(lambda h, hh=hh: h.matmul(out=su[:, 0:128], lhsT=kt_tok[:, cs_],
                                                            rhs=vps[hh][:, c4 * 128:(c4 + 1) * 128],
                                                            start=False, stop=(hh == 1)),
                                  reads=[SB[13], vpb[hh]], writes=[su.B], inc=(hh == 1))
                        fw.dve(lambda h: h.tensor_tensor(out=St32[:], in0=su[:, 0:128], in1=St32[:], op=ALU.add),
                               reads=[su.B, St32.B], writes=[St32.B])
                        pend = Pq[:, c4 * 128 + 127:c4 * 128 + 128]
                        fw.dve(lambda h: h.tensor_scalar(out=St32[:], in0=St32[:], scalar1=pend, scalar2=None, op0=ALU.mult),
                               reads=[St32.B, Pq.B], writes=[St32.B])
                        fw.dve(lambda h: h.tensor_tensor(out=Stb[:], in0=St32[:], in1=bd32[:], op=ALU.mult),
                               reads=[St32.B, bd32.B], writes=[Stb.B])

                    for c4 in range(LIM.get('c4', 4)):
                        do_chunk(c4)
                    bs = ps_next()
                    fw.pe(lambda h: h.matmul(out=bs[:], lhsT=bdo64[:], rhs=rk_bf[:], start=True, stop=True),
                          reads=[bdo64.B, rk_bf.B], writes=[bs.B])
                    bon = wf7
                    fw.dve(lambda h: h.scalar_tensor_tensor(out=bon[:], in0=bs[:], scalar=64.0, in1=v_bf[:], op0=ALU.mult,
                                                            op1=ALU.mult), reads=[bs.B, v_bf.B], writes=[bon.B])

                    def affine(ycen):
                        fw.dve(lambda h: h.tensor_scalar(out=ycen[:], in0=ycen[:], scalar1=LNW, scalar2=LNB, op0=ALU.mult,
                                                         op1=ALU.add), reads=[ycen.B, rvec.B], writes=[ycen.B])
                        fw.dve(lambda h: h.tensor_tensor(out=ycen[:], in0=ycen[:], in1=bon[:], op=ALU.add),
                               reads=[ycen.B, bon.B], writes=[ycen.B])

                    headnorm_gate(y_ps, sgate, ya[:, hp, tok0:tok0 + 512], ya.b[hp * 4 + tb], 64e-5, affine=affine)

                for tb in range(LIM.get('tb', 4)):
                    do_block(tb)

            for hp in range(LIM.get('hp', 4)):
                do_hp(hp)
            fw.dve(lambda h: h.memset(cst[:, 0:1], 0.0), reads=SB, writes=yb.b + [cst.B])

        def wload_plain(src_ap, nchunk):
            slot = wslot[wctr[0] % NSLOT]
            wctr[0] += 1
            stg = wst[wctr[1] % 2]
            wctr[1] += 1
            fw.dma("sp", stg[:, 0:nchunk, :], src_ap, writes=[stg.B])
            fw.dve(lambda h: h.tensor_copy(out=slot[:, 0:nchunk, :], in_=stg[:, 0:nchunk, :]),
                   reads=[stg.B], writes=[slot.B])
            return slot

        def phaseM(si, l):
            fw.dma("sp", gpost[:], postn_d[l:l + 1, :].broadcast_to([128, D]), writes=[gpost.B])
            ybr = [ya, yb, yc]
            gofs = [O_GA, O_GB, O_GC]
            mT = wh[0:8]
            sig, tt_, macc = wf[0], wf[1], wf[2]

            def do_block(tb):
                tok0 = tb * 512

                def do_oc(oc):
                    for br in range(3):
                        if br not in LIM.get("branches", (0, 1, 2)):
                            continue
                        first = br == min(LIM.get("branches", (0, 1, 2)))
                        last = br == max(LIM.get("branches", (0, 1, 2)))
                        wg = wload(l, [(gofs[br] + oc * 128, 128)])
                        gl = ps_next()
                        proj(gl, wg, tok0)
                        fw.act(lambda h, gl=gl, br=br: h.activation(out=sig[:], in_=gl[:], func=AF.Sigmoid,
                                                                    bias=bmt[:, l, br, oc:oc + 1], scale=1.0),
                               reads=[gl.B, bmt.B], writes=[sig.B])
                        wp = wload_plain(wp_d[br][l, :, oc * 128:(oc + 1) * 128].rearrange("(c p) n -> p c n", p=128), 4)
                        pb = ps_next()
                        for c in range(4):
                            fw.pe(lambda h, c=c, pb=pb, wp=wp, br=br: h.matmul(
                                out=pb[:], lhsT=wp[:, c, :], rhs=ybr[br][:, c, tok0:tok0 + 512],
                                start=(c == 0), stop=(c == 3)),
                                reads=[wp.B, ybr[br].b[c * 4 + tb]], writes=[pb.B], inc=(c == 3))
                        if first and last:
                            fw.dve(lambda h, pb=pb: h.tensor_tensor(out=mT[oc][:], in0=pb[:], in1=sig[:], op=ALU.mult),
                                   reads=[pb.B, sig.B], writes=[mT[oc].B])
                        elif first:
                            fw.dve(lambda h, pb=pb: h.tensor_tensor(out=macc[:], in0=pb[:], in1=sig[:], op=ALU.mult),
                                   reads=[pb.B, sig.B], writes=[macc.B])
                        else:
                            fw.dve(lambda h, pb=pb: h.tensor_tensor(out=tt_[:], in0=pb[:], in1=sig[:], op=ALU.mult),
                                   reads=[pb.B, sig.B], writes=[tt_.B])
                            dst = mT[oc] if last else macc
                            fw.dve(lambda h, dst=dst: h.tensor_tensor(out=dst[:], in0=macc[:], in1=tt_[:], op=ALU.add),
                                   reads=[macc.B, tt_.B], writes=[dst.B])

                for oc in range(8):
                    do_oc(oc)

                def do_pair(k):
                    banks = [pg[0], pg[1], pg[2], pg[3]]
                    for oc in range(8):
                        wo = wload_plain(wout_d[l, oc * 128:(oc + 1) * 128, :].rearrange("p (c n) -> p c n", c=8), 8)
                        wov = wo[:].rearrange("p c n -> p (c n)")
                        for t in range(2):
                            for half in range(2):
                                bk = banks[t * 2 + half]
                                fw.pe(lambda h, oc=oc, t=t, half=half, bk=bk, wov=wov: h.matmul(
                                    out=bk[:], lhsT=mT[oc][:, (2 * k + t) * 128:(2 * k + t + 1) * 128],
                                    rhs=wov[:, half * 512:(half + 1) * 512], start=(oc == 0), stop=(oc == 7)),
                                    reads=[mT[oc].B, wo.B], writes=[bk.B], inc=(oc == 7 or (t == 1 and half == 1)))
                    for t in range(2):
                        tile_i = tb * 4 + 2 * k + t
                        for half in range(2):
                            bk = banks[t * 2 + half]
                            fw.dve(lambda h, half=half, bk=bk: h.bn_stats(out=st6[:, half, :], in_=bk[:]),
                                   reads=[bk.B], writes=[st6.B])
                        fw.dve(lambda h: h.bn_aggr(out=mv[:], in_=st6[:].rearrange("p a b -> p (a b)")),
                               reads=[st6.B], writes=[mv.B])
                        fw.dve(lambda h: h.scalar_tensor_tensor(out=e2[:], in0=mv[:, 0:1], scalar=mv[:, 0:1],
                                                                in1=mv[:, 1:2], op0=ALU.mult, op1=ALU.add),
                               reads=[mv.B], writes=[e2.B])
                        fw.act(lambda h: h.activation(out=e2[:], in_=e2[:], func=AF.Sqrt, bias=EPS, scale=1.0),
                               reads=[e2.B], writes=[e2.B])
                        fw.dve(lambda h: h.reciprocal(out=rstd[:], in_=e2[:]), reads=[e2.B], writes=[rstd.B])
                        for half in range(2):
                            bk = banks[t * 2 + half]
                            hs = slice(half * 512, (half + 1) * 512)
                            fw.dve(lambda h, bk=bk, hs=hs: h.scalar_tensor_tensor(
                                out=wf[3][:], in0=bk[:], scalar=rstd[:, 0:1], in1=gpost[:, hs],
                                op0=ALU.mult, op1=ALU.mult), reads=[bk.B, rstd.B, gpost.B], writes=[wf[3].B])
                            fw.dve(lambda h, hs=hs, tile_i=tile_i: h.tensor_tensor(
                                out=x_sb[:, tile_i, hs], in0=x_sb[:, tile_i, hs], in1=wf[3][:], op=ALU.add),
                                reads=[x_sb.b[tile_i], wf[3].B], writes=[x_sb.b[tile_i]])

                for k in range(2):
                    do_pair(k)

            for tb in range(LIM.get('tb', 4)):
                do_block(tb)

        for si in range(nseq):
            for tq in range(NT // 4):
                fw.dma("sp", x_sb[:, tq * 4:(tq + 1) * 4, :],
                       x_d[si, tq * 512:(tq + 1) * 512, :].rearrange("(t p) d -> p t d", p=128),
                       writes=[x_sb.b[tq * 4 + i] for i in range(4)])
            for l in range(nlayers):
                if l == 1 and "l2phases" in LIM:
                    phases = LIM["l2phases"]
                if not LIM.get('nosetup'):
                    layer_setup(l)
                phase0(si, l)
                if "hT" in debug and si == 0 and l == 0:
                    fw.dma("sp", dbg_d["hT"], hT[:], reads=hT.b)
                if "C" in phases:
                    phaseC(si, l)
                    if "yc" in debug and si == 0 and l == 0:
                        fw.dma("sp", dbg_d["yc"], yc[:], reads=yc.b)
                if "A" in phases:
                    phaseA(si, l)
                    if "ya" in debug and si == 0 and l == 0:
                        fw.dma("sp", dbg_d["ya"], ya[:], reads=ya.b)
                if "B" in phases:
                    phaseB(si, l)
                    if "yb" in debug and si == 0 and l == 0:
                        fw.dma("sp", dbg_d["yb"], yb[:], reads=yb.b)
                if "M" in phases:
                    phaseM(si, l)
            for tq in range(NT // 4):
                fw.dma("sp", out_d[si, tq * 512:(tq + 1) * 512, :].rearrange("(t p) d -> p t d", p=128),
                       x_sb[:, tq * 4:(tq + 1) * 4, :],
                       reads=[x_sb.b[tq * 4 + i] for i in range(4)])
        allb = x_sb.b + hT.b + ya.b + yb.b + yc.b
        fw.wait_all("sp", allb)
        fw.emit()
        print("instr counts:", {k: v.n for k, v in fw.eng.items()})
    return nc


def make_shared(inputs):
    f = lambda k: np.ascontiguousarray(np.asarray(inputs[k], dtype=np.float32))
    shared = dict(make_consts())
    shared["w_in"] = f("w_in")
    shared["pre_norm"] = np.ascontiguousarray(f("pre_norm").reshape(DEPTH, 8, 128).transpose(0, 2, 1))
    for n in ("w_proj_rwkv", "w_proj_ret", "w_proj_s5", "w_out", "post_norm"):
        shared[n] = f(n)
    shared["b_merge"] = np.ascontiguousarray(f("b_merge").reshape(DEPTH, 3, 8, 128).transpose(0, 3, 1, 2))
    shared["rwkv_mu_rkv"] = f("rwkv_mu_rkv")
    shared["rwkv_mu_wa"] = np.ascontiguousarray(f("rwkv_mu_wa").reshape(DEPTH, 1, 128))
    shared["rwkv_w2a2"] = np.ascontiguousarray(np.stack([f("rwkv_w2"), f("rwkv_a2")], axis=1))
    vec = np.zeros((DEPTH, 8, 512), np.float32)
    for j, n in enumerate(("rwkv_w0", "rwkv_a0", "rwkv_k_k", "rwkv_k_a", "rwkv_ln_w", "rwkv_ln_b")):
        vec[:, j] = f(n)
    vec[:, 6] = f("rwkv_r_k").reshape(DEPTH, 512)
    shared["rwkv_vec"] = np.ascontiguousarray(vec.reshape(DEPTH, 8, 4, 128).transpose(0, 3, 2, 1))
    dup = lambda a: np.concatenate([a, a], axis=1)
    a_re = dup(f("s5_A_re").transpose(0, 2, 1))
    a_im = dup(f("s5_A_im").transpose(0, 2, 1))
    ldt = np.broadcast_to(f("s5_log_dt")[:, None, :], (DEPTH, 128, 32))
    shared["s5_Aab"] = np.ascontiguousarray(np.stack([a_re, a_im, ldt], axis=1))
    b_re = dup(f("s5_B_re").transpose(0, 2, 1, 3).reshape(DEPTH, 64, 512))
    b_im = dup(f("s5_B_im").transpose(0, 2, 1, 3).reshape(DEPTH, 64, 512))
    shared["s5_Bst"] = np.ascontiguousarray(np.stack([b_re, b_im], axis=1))
    c_re = f("s5_C_re").transpose(0, 3, 1, 2).reshape(DEPTH, 64, 512)
    c_im = f("s5_C_im").transpose(0, 3, 1, 2).reshape(DEPTH, 64, 512)
    ca = np.concatenate([c_re, c_im], axis=1)
    cb = np.concatenate([c_im, c_re], axis=1)
    shared["s5_Cst"] = np.ascontiguousarray(np.stack([ca, cb], axis=1))
    dv = f("s5_D").reshape(DEPTH, 4, 128).transpose(0, 2, 1)
    gb = f("s5_glu_b").reshape(DEPTH, 4, 128).transpose(0, 2, 1)
    shared["s5_vec"] = np.ascontiguousarray(np.concatenate([dv, gb], axis=2))
    shared["s5_glu_w"] = f("s5_glu_w")
    return shared


def make_inputs(inputs, s0, n):
    m = make_shared(inputs)
    m["x"] = np.ascontiguousarray(np.asarray(inputs["x"], dtype=np.float32)[s0:s0 + n])
    return m


def kernel(**inputs):
    ncores = 8
    x = np.ascontiguousarray(np.asarray(inputs["x"], dtype=np.float32))
    shared = make_shared(inputs)
    nlaunch = N_LAUNCH
    per = NSEQ // nlaunch
    nc = build_program(nseq=per)
    out = np.zeros_like(x)
    for j in range(nlaunch):
        in_maps = []
        for c in range(ncores):
            m = dict(shared)
            s0 = c * NSEQ + j * per
            m["x"] = x[s0:s0 + per]
            in_maps.append(m)
        res = run_bass_kernel_spmd(nc, in_maps, core_ids=list(range(ncores)))
        for c in range(ncores):
            s0 = c * NSEQ + j * per
            out[s0:s0 + per] = np.asarray(res.results[c]["out"])
    return out.astype(np.float32)
```

```python
import contextlib
import numpy as np
import ml_dtypes
import concourse.bass as bass
import concourse.mybir as mybir
from concourse.bass_utils import run_bass_kernel_spmd

F32 = mybir.dt.float32
BF16 = mybir.dt.bfloat16
AF = mybir.ActivationFunctionType
ALU = mybir.AluOpType
AX = mybir.AxisListType

D = 1024
S = 2048
DEPTH = 2
NSEQ = 2
D_IN = 8320
EPS = 1e-6
NT = S // 128

O_AR, O_AK, O_AV, O_XW, O_XA, O_AG = 0, 512, 1024, 1536, 1600, 1664
O_BQ, O_BK, O_BV, O_BG = 2176, 2688, 3200, 3712
O_CU, O_CG = 4224, 4736
O_GA, O_GB, O_GC = 5248, 6272, 7296


class Buf:
    def __init__(self, name):
        self.name = name
        self.last_write = None
        self.reads = []


class Engine:
    EPOCH = 30000

    def __init__(self, fw, name):
        self.fw = fw
        self.name = name
        self.sems = []
        self.count = 0
        self.waited = {}
        self.ops = []
        self.n = 0
        self._new_sem()

    def _new_sem(self):
        s = self.fw.stack.enter_context(self.fw.nc.semaphore(f"s_{self.name}_{len(self.sems)}"))
        self.sems.append(s)
        self.count = 0

    def need(self, ev):
        sem, val = ev
        key = id(sem)
        if self.waited.get(key, 0) >= val:
            return None
        self.waited[key] = val
        return ev


class Fw:
    def __init__(self, nc, stack):
        self.nc = nc
        self.stack = stack
        self.eng = {n: Engine(self, n) for n in ("pe", "act", "dve", "pool", "sp")}
        self.dsem = {}
        for q, k in (("sp", 12), ("act", 6), ("pool", 6)):
            self.dsem[q] = [[stack.enter_context(nc.semaphore(f"d_{q}_{i}")), 0] for i in range(k)]
        self.dnext = {q: 0 for q in self.dsem}

    def _deps(self, e, reads, writes):
        evs = []
        for b in reads:
            if b.last_write is not None:
                evs.append(b.last_write)
        for b in writes:
            if b.last_write is not None:
                evs.append(b.last_write)
            evs.extend(b.reads)
        out = []
        for ev in evs:
            if e.name == "pe" and any(ev[0] is s_ for s_ in e.sems):
                continue
            ev2 = e.need(ev)
            if ev2 is not None:
                out.append(ev2)
        return out

    def op(self, engname, fn, reads=(), writes=(), inc=True):
        e = self.eng[engname]
        waits = self._deps(e, reads, writes)
        if e.count >= Engine.EPOCH and inc:
            e._new_sem()
        sem = e.sems[-1]
        if inc:
            e.count += 1
        ev = (sem, e.count if inc else e.count + 1)
        for b in reads:
            b.reads.append(ev)
        for b in writes:
            b.last_write = ev
            b.reads = []
        e.n += 1

        def run(h, waits=waits, fn=fn, sem=sem, inc=inc):
            for (s, v) in waits:
                h.wait_ge(s, v)
            ins = fn(h)
            if inc:
                ins.then_inc(sem, 1)

        e.ops.append(run)
        return ev

    def dma(self, q, out, in_, reads=(), writes=()):
        e = self.eng[q]
        waits = self._deps(e, reads, writes)
        slots = self.dsem[q]
        i = self.dnext[q]
        self.dnext[q] = (i + 1) % len(slots)
        slot = slots[i]
        sem = slot[0]
        prev = slot[1]
        if prev > 0:
            w = e.need((sem, prev))
            if w is not None:
                waits.append(w)
        slot[1] = prev + 16
        ev = (sem, slot[1])
        for b in reads:
            b.reads.append(ev)
        for b in writes:
            b.last_write = ev
            b.reads = []

        def run(h, waits=waits, sem=sem, out=out, in_=in_):
            for (s, v) in waits:
                h.wait_ge(s, v)
            h.dma_start(out=out, in_=in_).then_inc(sem, 16)

        e.ops.append(run)
        return ev

    def wait_all(self, engname, bufs):
        e = self.eng[engname]
        waits = []
        for b in bufs:
            for ev in ([b.last_write] if b.last_write else []) + list(b.reads):
                w = e.need(ev)
                if w is not None:
                    waits.append(w)

        def run(h, waits=waits):
            for (s, v) in waits:
                h.wait_ge(s, v)

        e.ops.append(run)

    def pe(self, fn, reads=(), writes=(), inc=True):
        return self.op("pe", fn, reads, writes, inc)

    def act(self, fn, reads=(), writes=()):
        return self.op("act", fn, reads, writes)

    def dve(self, fn, reads=(), writes=()):
        return self.op("dve", fn, reads, writes)

    def pool(self, fn, reads=(), writes=()):
        return self.op("pool", fn, reads, writes)

    def emit(self):
        nc = self.nc
        with nc.Block() as block:
            @block.tensor
            def _(h):
                for f in self.eng["pe"].ops:
                    f(h)

            @block.scalar
            def _(h):
                for f in self.eng["act"].ops:
                    f(h)

            @block.vector
            def _(h):
                for f in self.eng["dve"].ops:
                    f(h)

            @block.gpsimd
            def _(h):
                for f in self.eng["pool"].ops:
                    f(h)

            @block.sync
            def _(h):
                for f in self.eng["sp"].ops:
                    f(h)


class T:
    def __init__(self, fw, shape, dtype, name, psum=False, nsub=1):
        nc = fw.nc
        if psum:
            self.t = fw.stack.enter_context(nc.psum_tensor("ps_" + name, shape, dtype))
        else:
            self.t = fw.stack.enter_context(nc.sbuf_tensor("sb_" + name, shape, dtype))
        self.b = [Buf(f"{name}.{i}") for i in range(nsub)]
        self.name = name

    def __getitem__(self, idx):
        return self.t[idx]

    @property
    def B(self):
        return self.b[0]


def make_consts():
    c = {}
    c["ident"] = np.eye(128, dtype=np.float32).astype(ml_dtypes.bfloat16)
    bd = np.zeros((128, 128), np.float32)
    bd[:64, :64] = 1.0
    bd[64:, 64:] = 1.0
    c["bd32"] = bd
    c["bdo64"] = (bd / 64.0).astype(ml_dtypes.bfloat16)
    half = 32
    inv = (np.float32(10000.0) ** (-np.arange(half, dtype=np.float32) / np.float32(half))).astype(np.float32)
    pos = np.arange(S, dtype=np.float32)
    ang = (pos[None, :] * inv[:, None]).astype(np.float32).astype(np.float64)
    cos32, sin32 = np.cos(ang), np.sin(ang)
    cosT = np.zeros((128, S), np.float32)
    sinS = np.zeros((128, S), np.float32)
    for p in range(128):
        d = p % 64
        i = d % 32
        cosT[p] = cos32[i]
        sinS[p] = -sin32[i] if d < 32 else sin32[i]
    c["rope_cos"] = cosT
    c["rope_sin"] = sinS
    lg = np.log(1.0 - 2.0 ** (-5.0 - np.arange(8, dtype=np.float64)))
    idx = np.arange(128, dtype=np.float64)
    dmT = np.zeros((4, 128, 256), np.float32)
    kwt = np.zeros((4, 128, 128), np.float32)
    qw = np.zeros((4, 128, 128), np.float32)
    gc = np.zeros((128, 4), np.float32)
    for hp in range(4):
        for hh in range(2):
            g = lg[hp * 2 + hh]
            diff = idx[None, :] - idx[:, None]
            m = np.where(diff >= 0, np.exp(g * np.maximum(diff, 0.0)), 0.0) / 8.0
            dmT[hp, :, hh * 128:(hh + 1) * 128] = m
            kwt[hp, :, hh * 64:(hh + 1) * 64] = (np.exp(g * (127.0 - idx)) / 8.0)[:, None]
            qw[hp, hh * 64:(hh + 1) * 64, :] = np.exp(g * (idx + 1.0))[None, :]
            gc[hh * 64:(hh + 1) * 64, hp] = np.exp(g * 128.0)
    c["ret_dmT"] = dmT
    c["ret_kwt"] = kwt
    c["ret_qw"] = qw
    c["ret_gc"] = gc
    sw = np.zeros((128, 128), np.float32)
    for k in range(128):
        sw[k, (k + 64) % 128] = 1.0
    c["swapb"] = sw.astype(ml_dtypes.bfloat16)
    sg = np.zeros((128, 2), np.float32)
    sg[:64, 0], sg[64:, 0] = -1.0, 1.0
    sg[:64, 1], sg[64:, 1] = 1.0, -1.0
    c["sgn"] = sg
    rm = np.zeros((128, 8), np.float32)
    for p in range(128):
        rm[p, p // 16] = 1.0
    c["rowmask"] = rm
    ii = np.arange(128)
    su = (ii[:, None] < ii[None, :]).astype(np.float32)
    iu = (ii[:, None] <= ii[None, :]).astype(np.float32)
    sl_ = (ii[None, :] < ii[:, None]).astype(np.float32)
    c["rw_masks"] = np.concatenate([su, iu, su, iu, sl_, sl_], axis=1).astype(ml_dtypes.bfloat16)
    return c


CONST_SPECS = {
    "ident": ([128, 128], BF16), "bd32": ([128, 128], F32), "bdo64": ([128, 128], BF16),
    "rope_cos": ([128, S], F32), "rope_sin": ([128, S], F32),
    "ret_dmT": ([4, 128, 256], F32), "ret_kwt": ([4, 128, 128], F32), "ret_qw": ([4, 128, 128], F32),
    "ret_gc": ([128, 4], F32),
    "rw_masks": ([128, 768], BF16),
    "swapb": ([128, 128], BF16), "sgn": ([128, 2], F32), "rowmask": ([128, 8], F32),
}


LIM = {}
N_LAUNCH = 2


def build_program(nlayers=DEPTH, nseq=NSEQ, debug=None, phases="0CABM"):
    debug = debug or {}
    nc = bass.Bass("TRN2", target_bir_lowering=False)
    dr = {}

    def din(name, shape, dt=F32):
        dr[name] = nc.dram_tensor(name, list(shape), dt, kind="ExternalInput").ap()
        return dr[name]

    x_d = din("x", [nseq, S, D])
    pre_norm_d = din("pre_norm", [DEPTH, 128, 8])
    w_in_d = din("w_in", [DEPTH, D, D_IN])
    cd = {k: din(k, shp, dt) for k, (shp, dt) in CONST_SPECS.items()}
    wp_d = [din(n, [DEPTH, 512, D]) for n in ("w_proj_rwkv", "w_proj_ret", "w_proj_s5")]
    wout_d = din("w_out", [DEPTH, D, D])
    bmerge_d = din("b_merge", [DEPTH, 128, 3, 8])
    postn_d = din("post_norm", [DEPTH, D])
    s5A_d = din("s5_Aab", [DEPTH, 3, 128, 32])
    s5B_d = din("s5_Bst", [DEPTH, 2, 128, 512])
    s5C_d = din("s5_Cst", [DEPTH, 2, 128, 512])
    s5v_d = din("s5_vec", [DEPTH, 128, 8])
    gluw_d = din("s5_glu_w", [DEPTH, 512, 512])
    mu_rkv_d = din("rwkv_mu_rkv", [DEPTH, 3, 512])
    mu_wa_d = din("rwkv_mu_wa", [DEPTH, 1, 128])
    w2a2_d = din("rwkv_w2a2", [DEPTH, 2, 64, 512])
    rvec_d = din("rwkv_vec", [DEPTH, 128, 4, 8])
    out_d = nc.dram_tensor("out", [nseq, S, D], F32, kind="ExternalOutput").ap()
    dbg_d = {}
    for k, shp in debug.items():
        dbg_d[k] = nc.dram_tensor("dbg_" + k, list(shp[0]), shp[1], kind="ExternalOutput").ap()

    with contextlib.ExitStack() as stack:
        fw = Fw(nc, stack)
        x_sb = T(fw, [128, NT, D], F32, "x_sb", nsub=NT)
        hT = T(fw, [128, 8, S + 1], BF16, "hT", nsub=NT + 1)
        ya = T(fw, [128, 4, S], BF16, "ya", nsub=16)
        yb = T(fw, [128, 4, S], BF16, "yb", nsub=16)
        yc = T(fw, [128, 4, S], BF16, "yc", nsub=16)
        ident = T(fw, [128, 128], BF16, "ident")
        bd32 = T(fw, [128, 128], F32, "bd32")
        bdo64 = T(fw, [128, 128], BF16, "bdo64")
        gpre = T(fw, [128, DEPTH, 8], F32, "gpre")
        gexp = T(fw, [128, 8, 128], F32, "gexp")
        NF, NH = 8, 12
        wf = [T(fw, [128, 512], F32, f"wf{i}") for i in range(NF)]
        wh = [T(fw, [128, 512], BF16, f"wh{i}") for i in range(NH)]
        st6 = T(fw, [128, 2, 6], F32, "st6")
        mv = T(fw, [128, 2], F32, "mv")
        e2 = T(fw, [128, 1], F32, "e2")
        rstd = T(fw, [128, 1], F32, "rstd")
        R32t = T(fw, [128, 128], F32, "R32t")
        Rbt = T(fw, [128, 128], BF16, "Rbt")
        gct = T(fw, [128, 4], F32, "gct")
        bmt = T(fw, [128, DEPTH, 3, 8], F32, "bmt")
        swapb = T(fw, [128, 128], BF16, "swapb")
        chl = T(fw, [128, 32], BF16, "chl")
        cbk = T(fw, [128, 8], F32, "cbk")
        sgn = T(fw, [128, 2], F32, "sgn")
        rowmask = T(fw, [128, 8], F32, "rowmask")
        s5v = T(fw, [128, 8], F32, "s5v")
        carry = T(fw, [128, 8], F32, "carry")
        cst = T(fw, [128, 16], F32, "cst")
        onec = T(fw, [128, 1], F32, "onec")
        rvec = T(fw, [128, 4, 8], F32, "rvec")
        omka = T(fw, [128, 4], F32, "omka")
        lw2 = T(fw, [128, 128], BF16, "lw2")
        la2 = T(fw, [128, 128], BF16, "la2")
        St32, Stb = R32t, Rbt
        gpost = T(fw, [128, D], F32, "gpost")
        NSLOT = 7
        wslot = [T(fw, [128, 8, 128], BF16, f"wslot{i}") for i in range(NSLOT)]
        wst = [T(fw, [128, 8, 128], F32, f"wst{i}") for i in range(2)]
        wctr = [0, 0]
        tp_ps = [T(fw, [128, 8, 128], BF16, f"tp{i}", psum=True) for i in range(2)]
        pg = [T(fw, [128, 512], F32, f"pg{i}", psum=True) for i in range(6)]
        pctr = [0]

        def ps_next():
            t = pg[pctr[0] % 4]
            pctr[0] += 1
            return t

        yctr = [0]

        def ps_y():
            t = pg[4 + yctr[0] % 2]
            yctr[0] += 1
            return t

        fw.dma("sp", ident[:], cd["ident"], writes=[ident.B])
        fw.dma("sp", bd32[:], cd["bd32"], writes=[bd32.B])
        fw.dma("sp", bdo64[:], cd["bdo64"], writes=[bdo64.B])
        fw.dma("sp", gpre[:], pre_norm_d.rearrange("l p c -> p l c"), writes=[gpre.B])
        fw.dma("sp", bmt[:], bmerge_d.rearrange("l p b c -> p l b c"), writes=[bmt.B])
        fw.dma("sp", swapb[:], cd["swapb"], writes=[swapb.B])
        fw.dma("sp", sgn[:], cd["sgn"], writes=[sgn.B])
        fw.dma("sp", rowmask[:], cd["rowmask"], writes=[rowmask.B])
        fw.pool(lambda h: h.memset(hT[:, :, 0:1], 0.0), writes=[hT.b[NT]])
        fw.dve(lambda h: h.memset(onec[:], 1.0), writes=[onec.B])

        def hT_bufs(tok0, ntok, shift=0):
            a = tok0 - shift
            bl = []
            if a < 0:
                bl.append(hT.b[NT])
                a = 0
            for tt in range(a // 128, (tok0 - shift + ntok - 1) // 128 + 1):
                bl.append(hT.b[tt])
            return bl

        def layer_setup(l):
            for c in range(8):
                fw.dve(lambda h, c=c: h.tensor_copy(out=gexp[:, c, :], in_=gpre[:, l, c:c + 1].to_broadcast([128, 128])),
                       reads=[gpre.B], writes=[gexp.B])

        def wload(l, segs):
            slot = wslot[wctr[0] % NSLOT]
            wctr[0] += 1
            stg = wst[wctr[1] % 2]
            wctr[1] += 1
            o = 0
            for (c0, n) in segs:
                fw.dma("sp", stg[:, :, o:o + n],
                       w_in_d[l, :, c0:c0 + n].rearrange("(c p) n -> p c n", p=128),
                       writes=[stg.B])
                o += n
            assert o == 128
            fw.dve(lambda h, slot=slot, stg=stg: h.tensor_tensor(out=slot[:], in0=stg[:], in1=gexp[:], op=ALU.mult),
                    reads=[stg.B, gexp.B], writes=[slot.B])
            return slot

        def proj(ps, slot, tok0, ntok=512, shift=0, start=True, stop=True):
            hb = hT_bufs(tok0, ntok, shift)
            for c in range(8):
                fw.pe(lambda h, c=c: h.matmul(out=ps[:, 0:ntok], lhsT=slot[:, c, :],
                                              rhs=hT[:, c, 1 + tok0 - shift:1 + tok0 - shift + ntok],
                                              start=(start and c == 0), stop=(stop and c == 7)),
                      reads=[slot.B] + hb, writes=[ps.B], inc=(c == 7))

        def silu_from_psum(dst, ps):
            fw.act(lambda h: h.activation(out=dst[:], in_=ps[:], func=AF.Sigmoid), reads=[ps.B], writes=[dst.B])
            fw.dve(lambda h: h.tensor_tensor(out=dst[:], in0=ps[:], in1=dst[:], op=ALU.mult),
                   reads=[ps.B, dst.B], writes=[dst.B])

        def phase0(si, l):
            for tt in range(NT):
                xb = x_sb.b[tt]
                xt = x_sb[:, tt, :]
                for j in range(2):
                    fw.dve(lambda h, j=j, xt=xt: h.bn_stats(out=st6[:, j, :], in_=xt[:, j * 512:(j + 1) * 512]),
                           reads=[xb], writes=[st6.B])
                fw.dve(lambda h: h.bn_aggr(out=mv[:], in_=st6[:].rearrange("p a b -> p (a b)")),
                       reads=[st6.B], writes=[mv.B])
                fw.dve(lambda h: h.scalar_tensor_tensor(out=e2[:], in0=mv[:, 0:1], scalar=mv[:, 0:1],
                                                        in1=mv[:, 1:2], op0=ALU.mult, op1=ALU.add),
                       reads=[mv.B], writes=[e2.B])
                fw.act(lambda h: h.activation(out=e2[:], in_=e2[:], func=AF.Sqrt, bias=EPS, scale=1.0),
                       reads=[e2.B], writes=[e2.B])
                fw.dve(lambda h: h.reciprocal(out=rstd[:], in_=e2[:]), reads=[e2.B], writes=[rstd.B])
                for hf in range(2):
                    fw.dve(lambda h, hf=hf, xt=xt: h.tensor_scalar(out=wh[hf][:], in0=xt[:, hf * 512:(hf + 1) * 512],
                                                                   scalar1=rstd[:, 0:1], scalar2=None, op0=ALU.mult),
                           reads=[xb, rstd.B], writes=[wh[hf].B])
                ps = tp_ps[tt % 2]
                for c in range(8):
                    fw.pe(lambda h, ps=ps, c=c: h.transpose(out=ps[:, c, :],
                                                            in_=wh[c // 4][:, (c % 4) * 128:(c % 4 + 1) * 128],
                                                            identity=ident[:]),
                          reads=[wh[c // 4].B, ident.B], writes=[ps.B], inc=(c == 7))
                fw.act(lambda h, ps=ps, tt=tt: h.activation(out=hT[:, :, 1 + tt * 128:1 + (tt + 1) * 128],
                                                            in_=ps[:], func=AF.Copy),
                       reads=[ps.B], writes=[hT.b[tt]])

        def headnorm_gate(y_ps, sg, dst, dstb, eps, affine=None):
            y32, ybf, ycen, sq, rs = wf[0], wh[0], wf[1], wh[1], wf[2]
            fw.act(lambda h: h.activation(out=y32[:], in_=y_ps[:], func=AF.Copy), reads=[y_ps.B], writes=[y32.B])
            fw.dve(lambda h: h.tensor_copy(out=ybf[:], in_=y32[:]), reads=[y32.B], writes=[ybf.B])
            mean_ps = ps_next()
            fw.pe(lambda h: h.matmul(out=mean_ps[:], lhsT=bdo64[:], rhs=ybf[:], start=True, stop=True),
                  reads=[bdo64.B, ybf.B], writes=[mean_ps.B])
            fw.dve(lambda h: h.tensor_tensor(out=ycen[:], in0=y32[:], in1=mean_ps[:], op=ALU.subtract),
                   reads=[y32.B, mean_ps.B], writes=[ycen.B])
            fw.act(lambda h: h.activation(out=sq[:], in_=ycen[:], func=AF.Square), reads=[ycen.B], writes=[sq.B])
            var_ps = ps_next()
            fw.pe(lambda h: h.matmul(out=var_ps[:], lhsT=bdo64[:], rhs=sq[:], start=True, stop=True),
                  reads=[bdo64.B, sq.B], writes=[var_ps.B])
            fw.act(lambda h: h.activation(out=rs[:], in_=var_ps[:], func=AF.Sqrt, bias=eps, scale=1.0),
                   reads=[var_ps.B], writes=[rs.B])
            fw.dve(lambda h: h.reciprocal(out=rs[:], in_=rs[:]), reads=[rs.B], writes=[rs.B])
            fw.dve(lambda h: h.tensor_tensor(out=ycen[:], in0=ycen[:], in1=rs[:], op=ALU.mult),
                    reads=[ycen.B, rs.B], writes=[ycen.B])
            if affine is not None:
                affine(ycen)
            fw.dve(lambda h: h.tensor_tensor(out=dst, in0=ycen[:], in1=sg[:], op=ALU.mult),
                    reads=[ycen.B, sg.B], writes=[dstb])

        def phaseB(si, l):
            dmT = wf[4]
            kwt = wf[5]
            qwt = wf[6]
            fw.dma("sp", gct[:, 0:4], cd["ret_gc"], writes=[gct.B])
            R32 = R32t
            t1, t2 = wf[0], wf[1]
            cosb, sinb = wf[2], wf[3]
            qr, kr, qc, vsb, ktok, vp0, vp1 = wh[2], wh[3], wh[4], wh[5], wh[6], wh[7], wh[8]
            qpad = [wh[9], wh[10]]
            Ssb = wh[11]
            Rb = Rbt
            sgate = wf[7]
            for hp in range(LIM.get('hp', 4)):
                cb = O_BQ + hp * 128
                kb = O_BK + hp * 128
                sw = lambda b: [(b + 32, 32), (b, 32), (b + 96, 32), (b + 64, 32)]
                w_q = wload(l, [(cb, 128)])
                w_qs = wload(l, sw(cb))
                w_k = wload(l, [(kb, 128)])
                w_ks = wload(l, sw(kb))
                w_v = wload(l, [(O_BV + hp * 128, 128)])
                w_g = wload(l, [(O_BG + hp * 128, 128)])
                fw.dma("sp", dmT[:, 0:256], cd["ret_dmT"][hp], writes=[dmT.B])
                fw.dma("sp", kwt[:, 0:128], cd["ret_kwt"][hp], writes=[kwt.B])
                fw.dma("sp", qwt[:, 0:128], cd["ret_qw"][hp], writes=[qwt.B])
                fw.pool(lambda h: h.memset(R32[:], 0.0), writes=[R32.B])
                fw.pool(lambda h: h.memset(Rb[:], 0.0), writes=[Rb.B])
                for qp in qpad:
                    fw.pool(lambda h, qp=qp: h.memset(qp[:], 0.0), writes=[qp.B])
                fw.pool(lambda h: h.memset(vp0[:], 0.0), writes=[vp0.B])
                fw.pool(lambda h: h.memset(vp1[:], 0.0), writes=[vp1.B])
                def do_block(tb, hp=hp, w_q=w_q, w_qs=w_qs, w_k=w_k, w_ks=w_ks, w_v=w_v, w_g=w_g):
                    tok0 = tb * 512
                    fw.dma("sp", cosb[:], cd["rope_cos"][:, tok0:tok0 + 512], writes=[cosb.B])
                    fw.dma("sp", sinb[:], cd["rope_sin"][:, tok0:tok0 + 512], writes=[sinb.B])
                    if LIM.get('stage', 99) < 1:
                        return
                    pq, pqs = ps_next(), ps_next()
                    proj(pq, w_q, tok0)
                    proj(pqs, w_qs, tok0)
                    fw.dve(lambda h: h.tensor_tensor(out=t1[:], in0=pq[:], in1=cosb[:], op=ALU.mult),
                           reads=[pq.B, cosb.B], writes=[t1.B])
                    fw.dve(lambda h: h.tensor_tensor(out=t2[:], in0=pqs[:], in1=sinb[:], op=ALU.mult),
                           reads=[pqs.B, sinb.B], writes=[t2.B])
                    fw.dve(lambda h: h.tensor_tensor(out=qr[:], in0=t1[:], in1=t2[:], op=ALU.add),
                            reads=[t1.B, t2.B], writes=[qr.B])
                    if LIM.get('stage', 99) < 2:
                        return
                    for half in range(2):
                        qp = qpad[half]
                        for hh in range(2):
                            src = qr[hh * 64:(hh + 1) * 64, half * 256:(half + 1) * 256].rearrange("p (c i) -> p c i", c=2)
                            dstv = qp[hh * 64:(hh + 1) * 64, :].rearrange("p (c h i) -> p c h i", c=2, h=2)[:, :, hh, :]
                            fw.act(lambda h, src=src, dstv=dstv: h.activation(out=dstv, in_=src, func=AF.Copy),
                                   reads=[qr.B], writes=[qp.B])
                    for c4 in range(4):
                        fw.dve(lambda h, c4=c4: h.tensor_tensor(out=qc[:, c4 * 128:(c4 + 1) * 128],
                                                                 in0=qr[:, c4 * 128:(c4 + 1) * 128],
                                                                 in1=qwt[:, 0:128], op=ALU.mult),
                                reads=[qr.B, qwt.B], writes=[qc.B])
                    if LIM.get('stage', 99) < 3:
                        return
                    pk, pks = ps_next(), ps_next()
                    proj(pk, w_k, tok0)
                    proj(pks, w_ks, tok0)
                    fw.dve(lambda h: h.tensor_tensor(out=t1[:], in0=pk[:], in1=cosb[:], op=ALU.mult),
                           reads=[pk.B, cosb.B], writes=[t1.B])
                    fw.dve(lambda h: h.tensor_tensor(out=t2[:], in0=pks[:], in1=sinb[:], op=ALU.mult),
                           reads=[pks.B, sinb.B], writes=[t2.B])
                    fw.dve(lambda h: h.tensor_tensor(out=kr[:], in0=t1[:], in1=t2[:], op=ALU.add),
                            reads=[t1.B, t2.B], writes=[kr.B])
                    if LIM.get('stage', 99) < 4:
                        return
                    pv = ps_next()
                    proj(pv, w_v, tok0)
                    fw.act(lambda h: h.activation(out=vsb[:], in_=pv[:], func=AF.Copy), reads=[pv.B], writes=[vsb.B])
                    pgate = ps_next()
                    proj(pgate, w_g, tok0)
                    silu_from_psum(sgate, pgate)
                    if LIM.get('stage', 99) < 5:
                        return
                    tp = tp_ps[0]
                    for c4 in range(4):
                        fw.pe(lambda h, c4=c4: h.transpose(out=tp[:, c4, :], in_=kr[:, c4 * 128:(c4 + 1) * 128],
                                                           identity=ident[:]),
                              reads=[kr.B, ident.B], writes=[tp.B], inc=False)
                    for c4 in range(4):
                        fw.pe(lambda h, c4=c4: h.transpose(out=tp[:, 4 + c4, :], in_=vsb[:, c4 * 128:(c4 + 1) * 128],
                                                           identity=ident[:]),
                              reads=[vsb.B, ident.B], writes=[tp.B], inc=(c4 == 3))
                    for c4 in range(4):
                        fw.dve(lambda h, c4=c4: h.tensor_tensor(out=ktok[:, c4 * 128:(c4 + 1) * 128], in0=tp[:, c4, :],
                                                                in1=kwt[:, 0:128], op=ALU.mult),
                               reads=[tp.B, kwt.B], writes=[ktok.B])
                    fw.act(lambda h: h.activation(
                        out=vp0[:].rearrange("p (c f) -> p c f", c=4)[:, :, 0:64], in_=tp[:, 4:8, 0:64], func=AF.Copy),
                        reads=[tp.B], writes=[vp0.B])
                    fw.act(lambda h: h.activation(
                        out=vp1[:].rearrange("p (c f) -> p c f", c=4)[:, :, 64:128], in_=tp[:, 4:8, 64:128], func=AF.Copy),
                        reads=[tp.B], writes=[vp1.B])
                    if LIM.get('stage', 99) < 6:
                        return
                    y_ps = ps_y()

                    def do_chunk(c4):
                        cs = slice(c4 * 128, (c4 + 1) * 128)
                        sc = ps_next()
                        qp = qpad[c4 // 2]
                        fw.pe(lambda h, cs=cs, qp=qp, c4=c4, sc=sc: h.matmul(
                            out=sc[:, 0:256], lhsT=kr[:, cs], rhs=qp[:, (c4 % 2) * 256:(c4 % 2) * 256 + 256],
                            start=True, stop=True), reads=[kr.B, qp.B], writes=[sc.B])
                        sv = Ssb[:, (c4 % 2) * 256:(c4 % 2) * 256 + 256]
                        fw.dve(lambda h, sc=sc, sv=sv: h.tensor_tensor(out=sv, in0=sc[:, 0:256], in1=dmT[:, 0:256],
                                                                       op=ALU.mult),
                               reads=[sc.B, dmT.B], writes=[Ssb.B])
                        fw.pe(lambda h, cs=cs, sv=sv: h.matmul(out=y_ps[:, cs], lhsT=vp0[:, cs], rhs=sv[:, 0:128],
                                                               start=True, stop=False),
                              reads=[vp0.B, Ssb.B], writes=[y_ps.B], inc=False)
                        fw.pe(lambda h, cs=cs, sv=sv: h.matmul(out=y_ps[:, cs], lhsT=vp1[:, cs], rhs=sv[:, 128:256],
                                                               start=False, stop=False),
                              reads=[vp1.B, Ssb.B], writes=[y_ps.B], inc=False)
                        fw.pe(lambda h, cs=cs: h.matmul(out=y_ps[:, cs], lhsT=Rb[:], rhs=qc[:, cs],
                                                        start=False, stop=True),
                              reads=[Rb.B, qc.B], writes=[y_ps.B])
                        kv = ps_next()
                        fw.pe(lambda h, cs=cs, kv=kv: h.matmul(out=kv[:, 0:128], lhsT=ktok[:, cs], rhs=vp0[:, cs],
                                                               start=True, stop=False),
                              reads=[ktok.B, vp0.B], writes=[kv.B], inc=False)
                        fw.pe(lambda h, cs=cs, kv=kv: h.matmul(out=kv[:, 0:128], lhsT=ktok[:, cs], rhs=vp1[:, cs],
                                                               start=False, stop=True),
                              reads=[ktok.B, vp1.B], writes=[kv.B])
                        fw.dve(lambda h, kv=kv, hp=hp: h.scalar_tensor_tensor(
                            out=R32[:], in0=R32[:], scalar=gct[:, hp:hp + 1], in1=kv[:, 0:128],
                            op0=ALU.mult, op1=ALU.add), reads=[R32.B, gct.B, kv.B], writes=[R32.B])
                        fw.dve(lambda h: h.tensor_tensor(out=Rb[:], in0=R32[:], in1=bd32[:],
                                                          op=ALU.mult),
                                reads=[R32.B, bd32.B], writes=[Rb.B])
                    for c4 in range(LIM.get('c4', 4)):
                        do_chunk(c4)
                    if LIM.get('stage', 99) < 7:
                        return
                    headnorm_gate(y_ps, sgate, yb[:, hp, tok0:tok0 + 512], yb.b[hp * 4 + tb], EPS)

                for tb in range(LIM.get('tb', 4)):
                    do_block(tb)

        def phaseC(si, l):
            PA, PB = wf[0], wf[1]
            sl = lambda t, i: t[:, i * 32:(i + 1) * 32]
            A_RE, A_IM, DT, MAG, ANG, CC, SS, T1, T2, T3, PM, RDEN, CRE, CIM, QQ, SLS = range(16)
            tabs = ya.b + yb.b
            cosT = ya[:].rearrange("p a s -> p (a s)").bitcast(F32).rearrange("p (g j) -> p g j", g=32)
            sinT = yb[:].rearrange("p a s -> p (a s)").bitcast(F32).rearrange("p (g j) -> p g j", g=32)
            ycf = yc[:].rearrange("p a s -> p (a s)").bitcast(F32)
            tmp1 = ycf[:, 0:2048].rearrange("p (g j) -> p g j", g=32)
            tmp2 = ycf[:, 2048:4096].rearrange("p (g j) -> p g j", g=32)

            def pa(fn_, eng="dve", extra=()):
                fw.op(eng, fn_, reads=[PA.B, PB.B] + list(extra), writes=[PA.B, PB.B])

            def tt(o, a, b, op):
                pa(lambda h: h.tensor_tensor(out=o, in0=a, in1=b, op=op))

            P = lambda i: sl(PA, i)
            for i in range(3):
                fw.dma("sp", P(i), s5A_d[l, i], writes=[PA.B])
            fw.dma("sp", s5v[:], s5v_d[l], writes=[s5v.B])
            pa(lambda h: h.activation(out=P(DT), in_=P(DT), func=AF.Exp), "act")
            tt(P(T1), P(DT), P(A_RE), ALU.mult)
            pa(lambda h: h.activation(out=P(MAG), in_=P(T1), func=AF.Exp), "act")
            tt(P(ANG), P(DT), P(A_IM), ALU.mult)
            pa(lambda h: h.activation(out=P(SS), in_=P(ANG), func=AF.Sin, scale=1.0 / 16.0), "act")
            pa(lambda h: h.activation(out=P(CC), in_=P(ANG), func=AF.Sin, scale=1.0 / 16.0, bias=float(np.pi / 2)), "act")

            def dbl(co, so, ci, si_):
                tt(P(T1), ci, ci, ALU.mult)
                tt(P(T2), si_, si_, ALU.mult)
                tt(P(T3), si_, ci, ALU.mult)
                tt(co, P(T1), P(T2), ALU.subtract)
                pa(lambda h: h.tensor_scalar(out=so, in0=P(T3), scalar1=2.0, scalar2=None, op0=ALU.mult))

            for _ in range(3):
                dbl(P(CC), P(SS), P(CC), P(SS))
            dbl(sl(PB, 0), sl(PB, 8), P(CC), P(SS))
            for k in range(1, 8):
                dbl(sl(PB, k), sl(PB, 8 + k), sl(PB, k - 1), sl(PB, 8 + k - 1))
            pa(lambda h: h.tensor_scalar(out=P(SLS), in0=sl(PB, 15), scalar1=sgn[:, 0:1], scalar2=None, op0=ALU.mult),
               extra=[sgn.B])
            tt(P(PM), P(MAG), sl(PB, 0), ALU.mult)
            pa(lambda h: h.tensor_scalar(out=P(PM), in0=P(PM), scalar1=-1.0, scalar2=None, op0=ALU.add))
            tt(P(QQ), P(MAG), sl(PB, 8), ALU.mult)
            tt(P(T1), P(A_RE), P(A_RE), ALU.mult)
            tt(P(T2), P(A_IM), P(A_IM), ALU.mult)
            tt(P(T1), P(T1), P(T2), ALU.add)
            pa(lambda h: h.reciprocal(out=P(RDEN), in_=P(T1)))
            tt(P(T1), P(PM), P(A_RE), ALU.mult)
            tt(P(T2), P(QQ), P(A_IM), ALU.mult)
            tt(P(T1), P(T1), P(T2), ALU.add)
            tt(P(CRE), P(T1), P(RDEN), ALU.mult)
            tt(P(T1), P(QQ), P(A_RE), ALU.mult)
            tt(P(T2), P(PM), P(A_IM), ALU.mult)
            tt(P(T1), P(T1), P(T2), ALU.subtract)
            tt(P(CIM), P(T1), P(RDEN), ALU.mult)

            fw.dve(lambda h: h.memset(cosT[:, :, 0:1], 1.0), writes=tabs)
            fw.dve(lambda h: h.memset(sinT[:, :, 0:1], 0.0), writes=tabs)
            for k in range(7):
                m = 1 << k
                cmb = sl(PB, k).unsqueeze(2).to_broadcast([128, 32, m])
                smb = sl(PB, 8 + k).unsqueeze(2).to_broadcast([128, 32, m])

                def lvl(m=m, cmb=cmb, smb=smb):
                    rw = dict(reads=tabs + yc.b + [PB.B], writes=tabs + yc.b)
                    fw.dve(lambda h: h.tensor_tensor(out=tmp1[:, :, 0:m], in0=cosT[:, :, 0:m], in1=cmb, op=ALU.mult), **rw)
                    fw.dve(lambda h: h.tensor_tensor(out=tmp2[:, :, 0:m], in0=sinT[:, :, 0:m], in1=smb, op=ALU.mult), **rw)
                    fw.dve(lambda h: h.tensor_tensor(out=cosT[:, :, m:2 * m], in0=tmp1[:, :, 0:m], in1=tmp2[:, :, 0:m],
                                                     op=ALU.subtract), **rw)
                    fw.dve(lambda h: h.tensor_tensor(out=tmp1[:, :, 0:m], in0=sinT[:, :, 0:m], in1=cmb, op=ALU.mult), **rw)
                    fw.dve(lambda h: h.tensor_tensor(out=tmp2[:, :, 0:m], in0=cosT[:, :, 0:m], in1=smb, op=ALU.mult), **rw)
                    fw.dve(lambda h: h.tensor_tensor(out=sinT[:, :, m:2 * m], in0=tmp1[:, :, 0:m], in1=tmp2[:, :, 0:m],
                                                     op=ALU.add), **rw)
                lvl()

            xh = [wf[2], wf[3]]
            stg = wf[4]
            stg2 = wf[5]
            tri = wf[6]
            BmT = wh[0:4]
            CmT = wh[4:8]
            u_bf, g12 = wh[8], wh[9]
            for t_ in CmT:
                fw.dve(lambda h, t_=t_: h.memset(t_[:], 0.0), writes=[t_.B])

            def xh_ap(g8):
                return xh[g8 // 4][:, (g8 % 4) * 128:(g8 % 4 + 1) * 128]

            def do_gc(gc):
                gs = slice(gc * 128, (gc + 1) * 128)
                fw.dma("sp", stg[:, 0:128], s5B_d[l, 0, :, gs], writes=[stg.B])
                fw.dma("sp", stg[:, 128:256], s5B_d[l, 1, :, gs], writes=[stg.B])
                v3 = lambda ap: ap.rearrange("p (g q) -> p g q", g=8)
                creb = P(CRE)[:, gc * 8:(gc + 1) * 8].unsqueeze(2).to_broadcast([128, 8, 16])
                cimb = P(CIM)[:, gc * 8:(gc + 1) * 8].unsqueeze(2).to_broadcast([128, 8, 16])
                rw = dict(reads=[stg.B, stg2.B, PA.B], writes=[stg2.B])
                bre, bim = v3(stg[:, 0:128]), v3(stg[:, 128:256])
                ta, tb_ = v3(stg2[:, 0:128]), v3(stg2[:, 128:256])
                bbre, bbim = v3(g12[:, 0:128]), v3(g12[:, 128:256])
                rwb = dict(reads=[stg.B, stg2.B, PA.B], writes=[g12.B])
                fw.dve(lambda h: h.tensor_tensor(out=ta, in0=bre, in1=creb, op=ALU.mult), **rw)
                fw.dve(lambda h: h.tensor_tensor(out=tb_, in0=bim, in1=cimb, op=ALU.mult), **rw)
                fw.dve(lambda h: h.tensor_tensor(out=bbre, in0=ta, in1=tb_, op=ALU.subtract), **rwb)
                fw.dve(lambda h: h.tensor_tensor(out=ta, in0=bim, in1=creb, op=ALU.mult), **rw)
                fw.dve(lambda h: h.tensor_tensor(out=tb_, in0=bre, in1=cimb, op=ALU.mult), **rw)
                fw.dve(lambda h: h.tensor_tensor(out=bbim, in0=ta, in1=tb_, op=ALU.add), **rwb)
                tpx = tp_ps[0]
                ptr, pti = tpx[:, 0, :], tpx[:, 1, :]
                fw.pe(lambda h: h.transpose(out=ptr, in_=g12[:, 0:128], identity=ident[:]),
                      reads=[g12.B, ident.B], writes=[tpx.B], inc=False)
                fw.pe(lambda h: h.transpose(out=pti, in_=g12[:, 128:256], identity=ident[:]),
                      reads=[g12.B, ident.B], writes=[tpx.B])
                fw.act(lambda h: h.activation(out=tri[:, 0:64], in_=ptr[:, 0:64], func=AF.Copy), reads=[tpx.B], writes=[tri.B])
                fw.act(lambda h: h.activation(out=tri[:, 64:128], in_=pti[:, 0:64], func=AF.Copy), reads=[tpx.B], writes=[tri.B])
                fw.act(lambda h: h.activation(out=tri[:, 128:192], in_=pti[:, 0:64], func=AF.Copy), reads=[tpx.B], writes=[tri.B])
                fw.act(lambda h: h.activation(out=tri[:, 192:256], in_=ptr[:, 0:64], func=AF.Copy, scale=-1.0),
                       reads=[tpx.B], writes=[tri.B])
                for g8 in range(8):
                    bt = BmT[g8 // 2]
                    o = (g8 % 2) * 256
                    fw.dve(lambda h, bt=bt, o=o, g8=g8: h.tensor_scalar(
                        out=bt[:, o:o + 256], in0=tri[:, 0:256], scalar1=rowmask[:, g8:g8 + 1], scalar2=None, op0=ALU.mult),
                        reads=[tri.B, rowmask.B], writes=[bt.B])
                fw.dma("sp", stg[:, 0:128], s5C_d[l, 0, :, gs], writes=[stg.B])
                fw.dma("sp", stg[:, 128:256], s5C_d[l, 1, :, gs], writes=[stg.B])
                fw.dve(lambda h: h.tensor_scalar(out=stg[:, 0:128], in0=stg[:, 0:128], scalar1=sgn[:, 1:2], scalar2=None,
                                                 op0=ALU.mult), reads=[stg.B, sgn.B], writes=[stg.B])
                fw.dve(lambda h: h.tensor_scalar(out=stg[:, 128:256], in0=stg[:, 128:256], scalar1=-1.0, scalar2=None,
                                                 op0=ALU.mult), reads=[stg.B], writes=[stg.B])
                for g8 in range(8):
                    ct = CmT[g8 // 2]
                    o = (g8 % 2) * 256
                    for ver in range(2):
                        fw.act(lambda h, ct=ct, o=o, g8=g8, ver=ver: h.activation(
                            out=ct[:, o + ver * 128 + g8 * 16:o + ver * 128 + (g8 + 1) * 16],
                            in_=stg[:, ver * 128 + g8 * 16:ver * 128 + (g8 + 1) * 16], func=AF.Copy),
                            reads=[stg.B], writes=[ct.B])
                w_u = wload(l, [(O_CU + gc * 128, 128)])

                def do_block(tb):
                    tok0 = tb * 512
                    pu = ps_next()
                    proj(pu, w_u, tok0)
                    u32 = wf[7]
                    fw.act(lambda h: h.activation(out=u_bf[:], in_=pu[:], func=AF.Copy), reads=[pu.B], writes=[u_bf.B])
                    fw.act(lambda h: h.activation(out=u32[:], in_=pu[:], func=AF.Copy), reads=[pu.B], writes=[u32.B])
                    y_ps = ps_y()

                    def do_sb(sb):
                        ts = slice(sb * 128, (sb + 1) * 128)
                        first = (tb == 0 and sb == 0)

                        def do_batch(bi):
                            g0 = gc * 8 + bi * 4
                            bun, bus = ps_next(), ps_next()
                            for q in range(4):
                                g8 = bi * 4 + q
                                bt = BmT[g8 // 2]
                                o = (g8 % 2) * 256
                                fw.pe(lambda h, q=q, bt=bt, o=o: h.matmul(out=bun[:, q * 128:(q + 1) * 128], lhsT=bt[:, o:o + 128],
                                                                          rhs=u_bf[:, ts], start=True, stop=True),
                                      reads=[bt.B, u_bf.B], writes=[bun.B], inc=(q == 3))
                            for q in range(4):
                                g8 = bi * 4 + q
                                bt = BmT[g8 // 2]
                                o = (g8 % 2) * 256
                                fw.pe(lambda h, q=q, bt=bt, o=o: h.matmul(out=bus[:, q * 128:(q + 1) * 128],
                                                                          lhsT=bt[:, o + 128:o + 256], rhs=u_bf[:, ts],
                                                                          start=True, stop=True),
                                      reads=[bt.B, u_bf.B], writes=[bus.B], inc=(q == 3))
                            w1, w2 = stg, stg2
                            cs4 = cosT[:, g0:g0 + 4, :]
                            sn4 = sinT[:, g0:g0 + 4, :]
                            v4 = lambda ap: ap.rearrange("p (g j) -> p g j", g=4)
                            fw.dve(lambda h: h.tensor_tensor(out=v4(w1[:]), in0=v4(bun[:]), in1=cs4, op=ALU.mult),
                                   reads=[bun.B] + tabs, writes=[w1.B])
                            fw.dve(lambda h: h.tensor_tensor(out=v4(w2[:]), in0=v4(bus[:]), in1=sn4, op=ALU.mult),
                                   reads=[bus.B] + tabs, writes=[w2.B])
                            fw.dve(lambda h: h.tensor_tensor(out=w1[:], in0=w1[:], in1=w2[:], op=ALU.add),
                                   reads=[w1.B, w2.B], writes=[w1.B])
                            xt_ = xh[bi]
                            for q in range(4):
                                g8 = bi * 4 + q
                                g = gc * 8 + g8
                                init = 0.0 if first else carry[:, g8:g8 + 1]
                                fw.dve(lambda h, q=q, g=g, init=init: h.tensor_tensor_scan(
                                    out=xt_[:, q * 128:(q + 1) * 128], data0=P(MAG)[:, g:g + 1].to_broadcast([128, 128]),
                                    data1=w1[:, q * 128:(q + 1) * 128], initial=init, op0=ALU.mult, op1=ALU.add),
                                    reads=[w1.B, PA.B, carry.B], writes=[xt_.B])
                            G1, G2 = g12, wh[10]
                            fw.dve(lambda h: h.tensor_tensor(out=v4(G1[:]), in0=v4(xt_[:]), in1=cs4, op=ALU.mult),
                                   reads=[xt_.B] + tabs, writes=[G1.B])
                            fw.dve(lambda h: h.tensor_tensor(out=v4(G2[:]), in0=v4(xt_[:]), in1=sn4, op=ALU.mult),
                                   reads=[xt_.B] + tabs, writes=[G2.B])
                            for q in range(4):
                                g8 = bi * 4 + q
                                ct = CmT[g8 // 2]
                                o = (g8 % 2) * 256
                                fw.pe(lambda h, q=q, ct=ct, o=o, g8=g8: h.matmul(
                                    out=y_ps[:, ts], lhsT=ct[:, o:o + 128], rhs=G1[:, q * 128:(q + 1) * 128],
                                    start=(g8 == 0), stop=False), reads=[ct.B, G1.B], writes=[y_ps.B], inc=(q == 3))
                            for q in range(4):
                                g8 = bi * 4 + q
                                ct = CmT[g8 // 2]
                                o = (g8 % 2) * 256
                                fw.pe(lambda h, q=q, ct=ct, o=o, g8=g8: h.matmul(
                                    out=y_ps[:, ts], lhsT=ct[:, o + 128:o + 256], rhs=G2[:, q * 128:(q + 1) * 128],
                                    start=False, stop=(g8 == 7)), reads=[ct.B, G2.B], writes=[y_ps.B], inc=(q == 3))

                        for bi in range(2):
                            do_batch(bi)
                        csw = ps_next()
                        for hx in range(2):
                            xl = xh[hx][:].rearrange("p (g j) -> p g j", g=4)[:, :, 127]
                            fw.dve(lambda h, hx=hx, xl=xl: h.tensor_copy(out=chl[:, hx * 4:(hx + 1) * 4], in_=xl),
                                   reads=[xh[hx].B], writes=[chl.B])
                        fw.dve(lambda h: h.tensor_copy(out=cbk[:], in_=chl[:, 0:8]), reads=[chl.B], writes=[cbk.B])
                        for hx in range(2):
                            xl = xh[hx][:].rearrange("p (g j) -> p g j", g=4)[:, :, 127]
                            fw.dve(lambda h, hx=hx, xl=xl: h.tensor_tensor(out=chl[:, 8 + hx * 4:8 + (hx + 1) * 4], in0=xl,
                                                                          in1=cbk[:, hx * 4:(hx + 1) * 4], op=ALU.subtract),
                                   reads=[xh[hx].B, cbk.B], writes=[chl.B])
                        fw.pe(lambda h: h.matmul(out=csw[:, 0:8], lhsT=swapb[:], rhs=chl[:, 0:8], start=True, stop=False),
                              reads=[swapb.B, chl.B], writes=[csw.B], inc=False)
                        fw.pe(lambda h: h.matmul(out=csw[:, 0:8], lhsT=swapb[:], rhs=chl[:, 8:16], start=False, stop=True),
                              reads=[swapb.B, chl.B], writes=[csw.B])
                        fw.dve(lambda h: h.tensor_tensor(out=cst[:, 0:8], in0=csw[:, 0:8], in1=P(SLS)[:, gc * 8:(gc + 1) * 8],
                                                         op=ALU.mult), reads=[csw.B, PA.B], writes=[cst.B])
                        for hx in range(2):
                            xl = xh[hx][:].rearrange("p (g j) -> p g j", g=4)[:, :, 127]
                            fw.dve(lambda h, hx=hx, xl=xl: h.tensor_tensor(
                                out=cst[:, 8 + hx * 4:8 + (hx + 1) * 4], in0=xl,
                                in1=sl(PB, 7)[:, gc * 8 + hx * 4:gc * 8 + (hx + 1) * 4], op=ALU.mult),
                                reads=[xh[hx].B, PB.B], writes=[cst.B])
                        fw.dve(lambda h: h.tensor_tensor(out=carry[:], in0=cst[:, 0:8], in1=cst[:, 8:16], op=ALU.add),
                               reads=[cst.B], writes=[carry.B])

                    for sb in range(4):
                        do_sb(sb)
                    y32, gt = wf[6], wf[7]
                    fw.dve(lambda h: h.scalar_tensor_tensor(out=y32[:], in0=u32[:], scalar=s5v[:, gc:gc + 1], in1=y_ps[:],
                                                            op0=ALU.mult, op1=ALU.add),
                           reads=[u32.B, s5v.B, y_ps.B], writes=[y32.B])
                    fw.act(lambda h: h.activation(out=gt[:], in_=y32[:], func=AF.Square), reads=[y32.B], writes=[gt.B])
                    fw.dve(lambda h: h.tensor_scalar(out=gt[:], in0=gt[:], scalar1=0.044715, scalar2=1.0, op0=ALU.mult,
                                                     op1=ALU.add), reads=[gt.B], writes=[gt.B])
                    fw.dve(lambda h: h.tensor_tensor(out=gt[:], in0=gt[:], in1=y32[:], op=ALU.mult),
                           reads=[gt.B, y32.B], writes=[gt.B])
                    fw.act(lambda h: h.activation(out=gt[:], in_=gt[:], func=AF.Tanh, scale=0.7978845608028654),
                           reads=[gt.B], writes=[gt.B])
                    fw.dve(lambda h: h.tensor_scalar(out=gt[:], in0=gt[:], scalar1=1.0, scalar2=0.5, op0=ALU.add,
                                                     op1=ALU.mult), reads=[gt.B], writes=[gt.B])
                    fw.dve(lambda h: h.tensor_tensor(out=yc[:, gc, tok0:tok0 + 512], in0=gt[:], in1=y32[:], op=ALU.mult),
                           reads=[gt.B, y32.B], writes=[yc.b[gc * 4 + tb]])

                for tb in range(LIM.get('tb', 4)):
                    do_block(tb)

            for gc in range(4):
                do_gc(gc)

            gw = wh[0:4]
            for c in range(4):
                fw.dma("sp", stg[:], gluw_d[l, c * 128:(c + 1) * 128, :], writes=[stg.B])
                fw.dve(lambda h, c=c: h.tensor_copy(out=gw[c][:], in_=stg[:]), reads=[stg.B], writes=[gw[c].B])
            wgs = [wload(l, [(O_CG + oc * 128, 128)]) for oc in range(4)]

            def glu_block(tb):
                tok0 = tb * 512
                sgl = [wf[0], wf[1], wf[2], wf[3]]
                for oc in range(4):
                    gp = pg[oc]
                    for c in range(4):
                        fw.pe(lambda h, oc=oc, c=c, gp=gp: h.matmul(out=gp[:], lhsT=gw[c][:, oc * 128:(oc + 1) * 128],
                                                                    rhs=yc[:, c, tok0:tok0 + 512], start=(c == 0), stop=(c == 3)),
                              reads=[gw[c].B, yc.b[c * 4 + tb]], writes=[gp.B], inc=(c == 3))
                    fw.act(lambda h, oc=oc, gp=gp: h.activation(out=sgl[oc][:], in_=gp[:], func=AF.Sigmoid,
                                                                bias=s5v[:, 4 + oc:5 + oc], scale=1.0),
                           reads=[gp.B, s5v.B], writes=[sgl[oc].B])
                for oc in range(4):
                    pgt = ps_y()
                    proj(pgt, wgs[oc], tok0)
                    sgt = wf[4]
                    silu_from_psum(sgt, pgt)
                    fw.dve(lambda h, oc=oc, sgt=sgt: h.tensor_tensor(out=sgt[:], in0=sgt[:], in1=sgl[oc][:], op=ALU.mult),
                           reads=[sgt.B, sgl[oc].B], writes=[sgt.B])
                    fw.dve(lambda h, oc=oc, sgt=sgt: h.tensor_tensor(out=yc[:, oc, tok0:tok0 + 512],
                                                                     in0=yc[:, oc, tok0:tok0 + 512], in1=sgt[:], op=ALU.mult),
                           reads=[sgt.B, yc.b[oc * 4 + tb]], writes=[yc.b[oc * 4 + tb]])

            for tb in range(LIM.get('tb', 4)):
                glu_block(tb)

        def wload_shift(l, c0, mu_src):
            s1 = wslot[wctr[0] % NSLOT]
            wctr[0] += 1
            s2 = wslot[wctr[0] % NSLOT]
            wctr[0] += 1
            stg = wst[wctr[1] % 2]
            wctr[1] += 1
            mub = wf[2]
            fw.dma("sp", mub[:, 0:128], mu_src.broadcast_to([128, 128]), writes=[mub.B])
            fw.dve(lambda h: h.tensor_scalar(out=mub[:, 128:256], in0=mub[:, 0:128], scalar1=-1.0, scalar2=1.0,
                                             op0=ALU.mult, op1=ALU.add), reads=[mub.B], writes=[mub.B])
            fw.dma("sp", stg[:], w_in_d[l, :, c0:c0 + 128].rearrange("(c p) n -> p c n", p=128), writes=[stg.B])
            fw.dve(lambda h: h.tensor_tensor(out=stg[:], in0=stg[:], in1=gexp[:], op=ALU.mult),
                   reads=[stg.B, gexp.B], writes=[stg.B])
            fw.dve(lambda h: h.tensor_tensor(out=s2[:], in0=stg[:], in1=mub[:, 0:128].unsqueeze(1).to_broadcast([128, 8, 128]),
                                             op=ALU.mult), reads=[stg.B, mub.B], writes=[s2.B])
            fw.dve(lambda h: h.tensor_tensor(out=s1[:], in0=stg[:], in1=mub[:, 128:256].unsqueeze(1).to_broadcast([128, 8, 128]),
                                             op=ALU.mult), reads=[stg.B, mub.B], writes=[s1.B])
            return s1, s2

        def proj_shift(ps, s12, tok0):
            proj(ps, s12[0], tok0, shift=0, start=True, stop=False)
            proj(ps, s12[1], tok0, shift=1, start=False, stop=True)

        def phaseA(si, l):
            CDEC = 0.6065306597126334
            ybf = yb[:].rearrange("p a s -> p (a s)")
            SB = [Buf(f"scrA{i}") for i in range(16)]
            SV = [ybf[:, i * 512:(i + 1) * 512] for i in range(16)]
            fw.dve(lambda h: h.memset(cst[:, 0:1], 0.0), reads=yb.b, writes=SB + [cst.B])
            QP = [SV[i] for i in range(4)]
            QPB = SB[0:4]
            MK1, MK1B = SV[4], SB[4]
            MK2, MK2B = SV[5][:, 0:256], SB[5]
            E1, E2, E3 = SV[6], SV[7], SV[8]
            XB = [SV[9], SV[10]]
            PP = SV[11]
            MISC = SV[12]
            kt_tok, bt_tok, vp0 = SV[13], SV[14], SV[15]
            vp1 = wh[9]
            v_bf, sqk, rk_bf, rt, at, kt, bt = wh[2], wh[3], wh[4], wh[5], wh[6], wh[7], wh[8]
            sgate, Pq, r32, wf6, wf7, wf0, wf1, wf2 = wf[3], wf[4], wf[5], wf[6], wf[7], wf[0], wf[1], wf[2]
            fw.dma("sp", MK1, cd["rw_masks"][:, 0:512], writes=[MK1B])
            fw.dma("sp", MK2, cd["rw_masks"][:, 512:768], writes=[MK2B])
            fw.dma("sp", rvec[:], rvec_d[l], writes=[rvec.B])
            fw.dve(lambda h: h.tensor_scalar(out=omka[:], in0=rvec[:, :, 3], scalar1=-1.0, scalar2=1.0, op0=ALU.mult,
                                             op1=ALU.add), reads=[rvec.B], writes=[omka.B])
            for i in range(4):
                fw.dve(lambda h, i=i: h.memset(QP[i], 0.0), writes=[QPB[i]])
            fw.dve(lambda h: h.memset(vp0, 0.0), writes=[SB[15]])
            fw.dve(lambda h: h.memset(vp1[:], 0.0), writes=[vp1.B])
            fw.dve(lambda h: h.memset(MISC, 0.0), writes=[SB[12]])
            fw.dve(lambda h: h.memset(lw2[:], 0.0), writes=[lw2.B])
            fw.dve(lambda h: h.memset(la2[:], 0.0), writes=[la2.B])
            w_wa = wload_shift(l, O_XW, mu_wa_d[l])
            for tb in range(4):
                pwa = ps_next()
                proj_shift(pwa, w_wa, tb * 512)
                dst = ya[:, 3, tb * 512:(tb + 1) * 512]
                fw.act(lambda h, pwa=pwa, dst=dst: h.activation(out=dst[0:64, :], in_=pwa[0:64, :], func=AF.Tanh),
                       reads=[pwa.B], writes=[ya.b[12 + tb]])
                fw.act(lambda h, pwa=pwa, dst=dst: h.activation(out=dst[64:128, :], in_=pwa[64:128, :], func=AF.Copy),
                       reads=[pwa.B], writes=[ya.b[12 + tb]])

            def do_hp(hp):
                V = lambda j: rvec[:, hp, j:j + 1]
                W0, A0, KK, KA, LNW, LNB, RK = (V(j) for j in range(7))
                w_r = wload_shift(l, O_AR + hp * 128, mu_rkv_d[l, 0:1, hp * 128:(hp + 1) * 128])
                w_k = wload_shift(l, O_AK + hp * 128, mu_rkv_d[l, 1:2, hp * 128:(hp + 1) * 128])
                w_v = wload_shift(l, O_AV + hp * 128, mu_rkv_d[l, 2:3, hp * 128:(hp + 1) * 128])
                w_g = wload(l, [(O_AG + hp * 128, 128)])
                stg = wst[wctr[1] % 2]
                wctr[1] += 1
                stv = stg[:].rearrange("p c n -> p (c n)")
                fw.dma("sp", stv[0:64, 0:128], w2a2_d[l, 0, :, hp * 128:(hp + 1) * 128], writes=[stg.B])
                fw.dma("sp", stv[64:128, 0:128], w2a2_d[l, 1, :, hp * 128:(hp + 1) * 128], writes=[stg.B])
                fw.dve(lambda h: h.tensor_copy(out=lw2[0:64, :], in_=stv[0:64, 0:128]), reads=[stg.B], writes=[lw2.B])
                fw.dve(lambda h: h.tensor_copy(out=la2[64:128, :], in_=stv[64:128, 0:128]), reads=[stg.B], writes=[la2.B])
                fw.dve(lambda h: h.memset(St32[:], 0.0), writes=[St32.B])
                fw.dve(lambda h: h.memset(Stb[:], 0.0), writes=[Stb.B])

                def do_block(tb):
                    tok0 = tb * 512
                    twa = ya[:, 3, tok0:tok0 + 512]
                    twab = ya.b[12 + tb]
                    pr, pk = ps_next(), ps_next()
                    proj_shift(pr, w_r, tok0)
                    proj_shift(pk, w_k, tok0)
                    fw.act(lambda h: h.activation(out=r32[:], in_=pr[:], func=AF.Copy), reads=[pr.B], writes=[r32.B])
                    pw_, pa_ = ps_next(), ps_next()
                    fw.pe(lambda h: h.matmul(out=pw_[:], lhsT=lw2[:], rhs=twa, start=True, stop=True),
                          reads=[lw2.B, twab], writes=[pw_.B])
                    fw.pe(lambda h: h.matmul(out=pa_[:], lhsT=la2[:], rhs=twa, start=True, stop=True),
                          reads=[la2.B, twab], writes=[pa_.B])
                    sg, aa = wf0, wf6
                    fw.act(lambda h: h.activation(out=sg[:], in_=pw_[:], func=AF.Sigmoid, bias=W0, scale=1.0),
                           reads=[pw_.B, rvec.B], writes=[sg.B])
                    fw.act(lambda h: h.activation(out=aa[:], in_=pa_[:], func=AF.Sigmoid, bias=A0, scale=1.0),
                           reads=[pa_.B, rvec.B], writes=[aa.B])
                    kkn = wf7
                    fw.dve(lambda h: h.tensor_scalar(out=kkn[:], in0=pk[:], scalar1=KK, scalar2=None, op0=ALU.mult),
                           reads=[pk.B, rvec.B], writes=[kkn.B])
                    fw.act(lambda h: h.activation(out=sqk[:], in_=kkn[:], func=AF.Square), reads=[kkn.B], writes=[sqk.B])
                    ss = ps_next()
                    fw.pe(lambda h: h.matmul(out=ss[:], lhsT=bdo64[:], rhs=sqk[:], start=True, stop=True),
                          reads=[bdo64.B, sqk.B], writes=[ss.B])
                    rn = wf1
                    fw.act(lambda h: h.activation(out=rn[:], in_=ss[:], func=AF.Sqrt, bias=1e-12, scale=64.0),
                           reads=[ss.B], writes=[rn.B])
                    fw.dve(lambda h: h.reciprocal(out=rn[:], in_=rn[:]), reads=[rn.B], writes=[rn.B])
                    fw.dve(lambda h: h.tensor_tensor(out=kkn[:], in0=kkn[:], in1=rn[:], op=ALU.mult),
                           reads=[kkn.B, rn.B], writes=[kkn.B])
                    k2 = wf2
                    fw.dve(lambda h: h.tensor_scalar(out=wf1[:], in0=aa[:], scalar1=KA, scalar2=omka[:, hp:hp + 1],
                                                     op0=ALU.mult, op1=ALU.add), reads=[aa.B, rvec.B, omka.B], writes=[wf1.B])
                    fw.dve(lambda h: h.tensor_tensor(out=k2[:], in0=pk[:], in1=wf1[:], op=ALU.mult),
                           reads=[pk.B, wf1.B], writes=[k2.B])
                    fw.dve(lambda h: h.scalar_tensor_tensor(out=rk_bf[:], in0=r32[:], scalar=RK, in1=k2[:], op0=ALU.mult,
                                                            op1=ALU.mult), reads=[r32.B, rvec.B, k2.B], writes=[rk_bf.B])
                    bb_ = wf1
                    fw.dve(lambda h: h.tensor_tensor(out=bb_[:], in0=kkn[:], in1=aa[:], op=ALU.mult),
                           reads=[kkn.B, aa.B], writes=[bb_.B])
                    pv = ps_next()
                    proj_shift(pv, w_v, tok0)
                    fw.act(lambda h: h.activation(out=v_bf[:], in_=pv[:], func=AF.Copy), reads=[pv.B], writes=[v_bf.B])
                    pgt = ps_next()
                    proj(pgt, w_g, tok0)
                    silu_from_psum(sgate, pgt)
                    cs = wf6
                    for c4 in range(4):
                        fw.dve(lambda h, c4=c4: h.tensor_tensor_scan(
                            out=cs[:, c4 * 128:(c4 + 1) * 128], data0=onec[:, 0:1].to_broadcast([128, 128]),
                            data1=sg[:, c4 * 128:(c4 + 1) * 128], initial=0.0, op0=ALU.mult, op1=ALU.add),
                            reads=[sg.B, onec.B], writes=[cs.B])
                    fw.dve(lambda h: h.tensor_tensor(out=sg[:], in0=cs[:], in1=sg[:], op=ALU.subtract),
                           reads=[cs.B, sg.B], writes=[sg.B])
                    fw.act(lambda h: h.activation(out=Pq[:], in_=cs[:], func=AF.Exp, scale=-CDEC), reads=[cs.B], writes=[Pq.B])
                    fw.act(lambda h: h.activation(out=sg[:], in_=sg[:], func=AF.Exp, scale=-CDEC), reads=[sg.B], writes=[sg.B])
                    fw.act(lambda h: h.activation(out=cs[:], in_=cs[:], func=AF.Exp, scale=CDEC), reads=[cs.B], writes=[cs.B])
                    PqA, Pk = sg, cs
                    fw.dve(lambda h: h.tensor_tensor(out=rt[:], in0=r32[:], in1=Pq[:], op=ALU.mult),
                           reads=[r32.B, Pq.B], writes=[rt.B])
                    fw.dve(lambda h: h.scalar_tensor_tensor(out=at[:], in0=kkn[:], scalar=-1.0, in1=PqA[:], op0=ALU.mult,
                                                            op1=ALU.mult), reads=[kkn.B, PqA.B], writes=[at.B])
                    fw.dve(lambda h: h.tensor_tensor(out=kt[:], in0=k2[:], in1=Pk[:], op=ALU.mult),
                           reads=[k2.B, Pk.B], writes=[kt.B])
                    fw.dve(lambda h: h.tensor_tensor(out=bt[:], in0=bb_[:], in1=Pk[:], op=ALU.mult),
                           reads=[bb_.B, Pk.B], writes=[bt.B])
                    for c4 in range(4):
                        for hh in range(2):
                            rows = slice(hh * 64, (hh + 1) * 64)
                            fw.act(lambda h, c4=c4, hh=hh, rows=rows: h.activation(
                                out=QP[c4][rows, (2 * hh) * 128:(2 * hh + 1) * 128], in_=at[rows, c4 * 128:(c4 + 1) * 128],
                                func=AF.Copy), reads=[at.B], writes=[QPB[c4]])
                            fw.act(lambda h, c4=c4, hh=hh, rows=rows: h.activation(
                                out=QP[c4][rows, (2 * hh + 1) * 128:(2 * hh + 2) * 128], in_=rt[rows, c4 * 128:(c4 + 1) * 128],
                                func=AF.Copy), reads=[rt.B], writes=[QPB[c4]])
                    tpa, tpb = tp_ps[0], tp_ps[1]
                    for c4 in range(4):
                        fw.pe(lambda h, c4=c4: h.transpose(out=tpa[:, c4, :], in_=kt[:, c4 * 128:(c4 + 1) * 128],
                                                           identity=ident[:]), reads=[kt.B, ident.B], writes=[tpa.B], inc=False)
                    for c4 in range(4):
                        fw.pe(lambda h, c4=c4: h.transpose(out=tpa[:, 4 + c4, :], in_=bt[:, c4 * 128:(c4 + 1) * 128],
                                                           identity=ident[:]), reads=[bt.B, ident.B], writes=[tpa.B],
                              inc=(c4 == 3))
                    for c4 in range(4):
                        fw.pe(lambda h, c4=c4: h.transpose(out=tpb[:, c4, :], in_=v_bf[:, c4 * 128:(c4 + 1) * 128],
                                                           identity=ident[:]), reads=[v_bf.B, ident.B], writes=[tpb.B],
                              inc=(c4 == 3))
                    v4 = lambda ap: ap.rearrange("p (c f) -> p c f", c=4)
                    fw.act(lambda h: h.activation(out=v4(kt_tok), in_=tpa[:, 0:4, :], func=AF.Copy),
                           reads=[tpa.B], writes=[SB[13]])
                    fw.dve(lambda h: h.tensor_copy(out=v4(bt_tok), in_=tpa[:, 4:8, :]), reads=[tpa.B], writes=[SB[14]])
                    fw.act(lambda h: h.activation(out=v4(vp0)[:, :, 0:64], in_=tpb[:, 0:4, 0:64], func=AF.Copy),
                           reads=[tpb.B], writes=[SB[15]])
                    fw.dve(lambda h: h.tensor_copy(out=v4(vp1[:])[:, :, 64:128], in_=tpb[:, 0:4, 64:128]),
                           reads=[tpb.B], writes=[vp1.B])
                    y_ps = ps_y()

                    def do_chunk(c4):
                        cs_ = slice(c4 * 128, (c4 + 1) * 128)
                        pa1, pa2, pa3 = ps_next(), ps_next(), ps_next()
                        fw.pe(lambda h: h.matmul(out=pa1[:], lhsT=kt[:, cs_], rhs=QP[c4], start=True, stop=True),
                              reads=[kt.B, QPB[c4]], writes=[pa1.B])
                        fw.pe(lambda h: h.matmul(out=pa2[:], lhsT=bt[:, cs_], rhs=QP[c4], start=True, stop=True),
                              reads=[bt.B, QPB[c4]], writes=[pa2.B])
                        for hh in range(2):
                            fw.pe(lambda h, hh=hh: h.matmul(out=pa3[:, hh * 128:(hh + 1) * 128],
                                                            lhsT=QP[c4][:, (2 * hh) * 128:(2 * hh + 1) * 128], rhs=bt[:, cs_],
                                                            start=True, stop=True),
                                  reads=[bt.B, QPB[c4]], writes=[pa3.B], inc=(hh == 1))
                        fw.dve(lambda h: h.tensor_tensor(out=E1, in0=pa1[:], in1=MK1, op=ALU.mult),
                               reads=[pa1.B, MK1B], writes=[SB[6]])
                        fw.dve(lambda h: h.tensor_tensor(out=E2, in0=pa2[:], in1=MK1, op=ALU.mult),
                               reads=[pa2.B, MK1B], writes=[SB[7]])
                        fw.dve(lambda h: h.tensor_tensor(out=E3[:, 0:256], in0=pa3[:, 0:256], in1=MK2, op=ALU.mult),
                               reads=[pa3.B, MK2B], writes=[SB[8]])

                        def Xj(j, hh):
                            if j == 0:
                                return E3[:, hh * 128:(hh + 1) * 128], SB[8]
                            return XB[j % 2][:, (2 * hh) * 128:(2 * hh + 1) * 128], SB[9 + j % 2]

                        def Bj(j, hh):
                            if j == 0:
                                return E2[:, (2 * hh) * 128:(2 * hh + 1) * 128], SB[7]
                            return XB[j % 2][:, (2 * hh + 1) * 128:(2 * hh + 2) * 128], SB[9 + j % 2]

                        def Pj(j, hh):
                            o = (j % 2) * 256 + hh * 128
                            return PP[:, o:o + 128]

                        e2v = E2.rearrange("p (a b) -> p a b", a=2)[:, :, 0:128]
                        fw.dve(lambda h: h.tensor_tensor(out=PP[:, 0:256].rearrange("p (a b) -> p a b", a=2), in0=e2v,
                                                         in1=ident[:].unsqueeze(1).to_broadcast([128, 2, 128]), op=ALU.add),
                               reads=[SB[7], ident.B], writes=[SB[11]])
                        for j in range(6):
                            last = j == 5
                            pxb = ps_next()
                            for hh in range(2):
                                xa_, xb_ = Xj(j, hh)
                                ba_, bb2 = Bj(j, hh)
                                fw.pe(lambda h, hh=hh, xa_=xa_, ba_=ba_, pxb=pxb: h.matmul(
                                    out=pxb[:, (2 * hh) * 128:(2 * hh + 1) * 128], lhsT=ba_, rhs=xa_, start=True, stop=True),
                                    reads=[xb_, bb2], writes=[pxb.B], inc=(last and hh == 1))
                                if not last:
                                    fw.pe(lambda h, hh=hh, xa_=xa_, ba_=ba_, pxb=pxb: h.matmul(
                                        out=pxb[:, (2 * hh + 1) * 128:(2 * hh + 2) * 128], lhsT=xa_, rhs=ba_, start=True,
                                        stop=True), reads=[xb_, bb2], writes=[pxb.B], inc=(hh == 1))
                            nxt = XB[(j + 1) % 2]
                            if last:
                                fw.act(lambda h, pxb=pxb, nxt=nxt: h.activation(
                                    out=nxt.rearrange("p (a b) -> p a b", a=2)[:, :, 0:128],
                                    in_=pxb[:].rearrange("p (a b) -> p a b", a=2)[:, :, 0:128], func=AF.Copy),
                                    reads=[pxb.B], writes=[SB[9 + (j + 1) % 2]])
                            else:
                                fw.act(lambda h, pxb=pxb, nxt=nxt: h.activation(out=nxt, in_=pxb[:], func=AF.Copy),
                                       reads=[pxb.B], writes=[SB[9 + (j + 1) % 2]])
                            pp = ps_next()
                            for hh in range(2):
                                xn_, xnb_ = Xj(j + 1, hh)
                                fw.pe(lambda h, hh=hh, pp=pp, j=j: h.matmul(out=pp[:, hh * 128:(hh + 1) * 128], lhsT=ident[:],
                                                                             rhs=Pj(j, hh), start=True, stop=False),
                                      reads=[ident.B, SB[11]], writes=[pp.B], inc=False)
                                fw.pe(lambda h, hh=hh, pp=pp, j=j, xn_=xn_: h.matmul(
                                    out=pp[:, hh * 128:(hh + 1) * 128], lhsT=xn_, rhs=Pj(j, hh), start=False, stop=True),
                                    reads=[xnb_, SB[11]], writes=[pp.B], inc=(hh == 1))
                            o = ((j + 1) % 2) * 256
                            fw.dve(lambda h, pp=pp, o=o: h.tensor_copy(out=PP[:, o:o + 256], in_=pp[:, 0:256]),
                                   reads=[pp.B], writes=[SB[11]])
                        r0 = ps_next()
                        fw.pe(lambda h: h.matmul(out=r0[:, 0:128], lhsT=at[:, cs_], rhs=Stb[:], start=True, stop=False),
                              reads=[at.B, Stb.B], writes=[r0.B], inc=False)
                        vps = [vp0, vp1[:]]
                        vpb = [SB[15], vp1.B]
                        for hh in range(2):
                            fw.pe(lambda h, hh=hh: h.matmul(out=r0[:, hh * 64:(hh + 1) * 64],
                                                            lhsT=E1[:, (2 * hh) * 128:(2 * hh + 1) * 128],
                                                            rhs=vps[hh][:, c4 * 128 + hh * 64:c4 * 128 + (hh + 1) * 64],
                                                            start=False, stop=(hh == 1)),
                                  reads=[SB[6], vpb[hh]], writes=[r0.B], inc=(hh == 1))
                        r0b = MISC[:, 0:128]
                        fw.act(lambda h: h.activation(out=r0b, in_=r0[:, 0:128], func=AF.Copy), reads=[r0.B], writes=[SB[12]])
                        up = ps_next()
                        for hh in range(2):
                            fw.pe(lambda h, hh=hh: h.matmul(out=up[:, hh * 64:(hh + 1) * 64], lhsT=Pj(6, hh),
                                                            rhs=r0b[:, hh * 64:(hh + 1) * 64], start=True, stop=True),
                                  reads=[SB[11], SB[12]], writes=[up.B], inc=(hh == 1))
                        upad = [MISC[:, 128:256], MISC[:, 256:384]]
                        fw.act(lambda h: h.activation(out=upad[0][:, 0:64], in_=up[:, 0:64], func=AF.Copy),
                               reads=[up.B], writes=[SB[12]])
                        fw.dve(lambda h: h.tensor_copy(out=upad[1][:, 64:128], in_=up[:, 64:128]),
                               reads=[up.B], writes=[SB[12]])
                        fw.pe(lambda h: h.matmul(out=y_ps[:, cs_], lhsT=Stb[:], rhs=rt[:, cs_], start=True, stop=False),
                              reads=[Stb.B, rt.B], writes=[y_ps.B], inc=False)
                        for hh in range(2):
                            fw.pe(lambda h, hh=hh: h.matmul(out=y_ps[:, cs_], lhsT=upad[hh],
                                                            rhs=E2[:, (2 * hh + 1) * 128:(2 * hh + 2) * 128],
                                                            start=False, stop=False),
                                  reads=[SB[12], SB[7]], writes=[y_ps.B], inc=False)
                            fw.pe(lambda h, hh=hh: h.matmul(out=y_ps[:, cs_], lhsT=vps[hh][:, c4 * 128:(c4 + 1) * 128],
                                                            rhs=E1[:, (2 * hh + 1) * 128:(2 * hh + 2) * 128],
                                                            start=False, stop=(hh == 1)),
                                  reads=[vpb[hh], SB[6]], writes=[y_ps.B], inc=(hh == 1))
                        su = ps_next()
                        for hh in range(2):
                            fw.pe(lambda h, hh=hh: h.matmul(out=su[:, 0:128], lhsT=bt_tok[:, cs_], rhs=upad[hh],
                                                            start=(hh == 0), stop=False),
                                  reads=[SB[14], SB[12]], writes=[su.B], inc=False)
                        for hh in range(2):
                            fw.pe(lambda h, hh=hh: h.matmul(out=su[:, 0:128], lhsT=kt_tok[:, cs_],
                                                            rhs=vps[hh][:, c4 * 128:(c4 + 1) * 128],
                                                            start=False, stop=(hh == 1)),
                                  reads=[SB[13], vpb[hh]], writes=[su.B], inc=(hh == 1))
                        fw.dve(lambda h: h.tensor_tensor(out=St32[:], in0=su[:, 0:128], in1=St32[:], op=ALU.add),
                               reads=[su.B, St32.B], writes=[St32.B])
                        pend = Pq[:, c4 * 128 + 127:c4 * 128 + 128]
                        fw.dve(lambda h: h.tensor_scalar(out=St32[:], in0=St32[:], scalar1=pend, scalar2=None, op0=ALU.mult),
                               reads=[St32.B, Pq.B], writes=[St32.B])
                        fw.dve(lambda h: h.tensor_tensor(out=Stb[:], in0=St32[:], in1=bd32[:], op=ALU.mult),
                               reads=[St32.B, bd32.B], writes=[Stb.B])

                    for c4 in range(LIM.get('c4', 4)):
                        do_chunk(c4)
                    bs = ps_next()
                    fw.pe(lambda h: h.matmul(out=bs[:], lhsT=bdo64[:], rhs=rk_bf[:], start=True, stop=True),
                          reads=[bdo64.B, rk_bf.B], writes=[bs.B])
                    bon = wf7
                    fw.dve(lambda h: h.scalar_tensor_tensor(out=bon[:], in0=bs[:], scalar=64.0, in1=v_bf[:], op0=ALU.mult,
                                                            op1=ALU.mult), reads=[bs.B, v_bf.B], writes=[bon.B])

                    def affine(ycen):
                        fw.dve(lambda h: h.tensor_scalar(out=ycen[:], in0=ycen[:], scalar1=LNW, scalar2=LNB, op0=ALU.mult,
                                                         op1=ALU.add), reads=[ycen.B, rvec.B], writes=[ycen.B])
                        fw.dve(lambda h: h.tensor_tensor(out=ycen[:], in0=ycen[:], in1=bon[:], op=ALU.add),
                               reads=[ycen.B, bon.B], writes=[ycen.B])

                    headnorm_gate(y_ps, sgate, ya[:, hp, tok0:tok0 + 512], ya.b[hp * 4 + tb], 64e-5, affine=affine)

                for tb in range(LIM.get('tb', 4)):
                    do_block(tb)

            for hp in range(LIM.get('hp', 4)):
                do_hp(hp)
            fw.dve(lambda h: h.memset(cst[:, 0:1], 0.0), reads=SB, writes=yb.b + [cst.B])

        def wload_plain(src_ap, nchunk):
            slot = wslot[wctr[0] % NSLOT]
            wctr[0] += 1
            stg = wst[wctr[1] % 2]
            wctr[1] += 1
            fw.dma("sp", stg[:, 0:nchunk, :], src_ap, writes=[stg.B])
            fw.dve(lambda h: h.tensor_copy(out=slot[:, 0:nchunk, :], in_=stg[:, 0:nchunk, :]),
                   reads=[stg.B], writes=[slot.B])
            return slot

        def phaseM(si, l):
            fw.dma("sp", gpost[:], postn_d[l:l + 1, :].broadcast_to([128, D]), writes=[gpost.B])
            ybr = [ya, yb, yc]
            gofs = [O_GA, O_GB, O_GC]
            mT = wh[0:8]
            sig, tt_, macc = wf[0], wf[1], wf[2]

            def do_block(tb):
                tok0 = tb * 512

                def do_oc(oc):
                    for br in range(3):
                        if br not in LIM.get("branches", (0, 1, 2)):
                            continue
                        first = br == min(LIM.get("branches", (0, 1, 2)))
                        last = br == max(LIM.get("branches", (0, 1, 2)))
                        wg = wload(l, [(gofs[br] + oc * 128, 128)])
                        gl = ps_next()
                        proj(gl, wg, tok0)
                        fw.act(lambda h, gl=gl, br=br: h.activation(out=sig[:], in_=gl[:], func=AF.Sigmoid,
                                                                    bias=bmt[:, l, br, oc:oc + 1], scale=1.0),
                               reads=[gl.B, bmt.B], writes=[sig.B])
                        wp = wload_plain(wp_d[br][l, :, oc * 128:(oc + 1) * 128].rearrange("(c p) n -> p c n", p=128), 4)
                        pb = ps_next()
                        for c in range(4):
                            fw.pe(lambda h, c=c, pb=pb, wp=wp, br=br: h.matmul(
                                out=pb[:], lhsT=wp[:, c, :], rhs=ybr[br][:, c, tok0:tok0 + 512],
                                start=(c == 0), stop=(c == 3)),
                                reads=[wp.B, ybr[br].b[c * 4 + tb]], writes=[pb.B], inc=(c == 3))
                        if first and last:
                            fw.dve(lambda h, pb=pb: h.tensor_tensor(out=mT[oc][:], in0=pb[:], in1=sig[:], op=ALU.mult),
                                   reads=[pb.B, sig.B], writes=[mT[oc].B])
                        elif first:
                            fw.dve(lambda h, pb=pb: h.tensor_tensor(out=macc[:], in0=pb[:], in1=sig[:], op=ALU.mult),
                                   reads=[pb.B, sig.B], writes=[macc.B])
                        else:
                            fw.dve(lambda h, pb=pb: h.tensor_tensor(out=tt_[:], in0=pb[:], in1=sig[:], op=ALU.mult),
                                   reads=[pb.B, sig.B], writes=[tt_.B])
                            dst = mT[oc] if last else macc
                            fw.dve(lambda h, dst=dst: h.tensor_tensor(out=dst[:], in0=macc[:], in1=tt_[:], op=ALU.add),
                                   reads=[macc.B, tt_.B], writes=[dst.B])

                for oc in range(8):
                    do_oc(oc)

                def do_pair(k):
                    banks = [pg[0], pg[1], pg[2], pg[3]]
                    for oc in range(8):
                        wo = wload_plain(wout_d[l, oc * 128:(oc + 1) * 128, :].rearrange("p (c n) -> p c n", c=8), 8)
                        wov = wo[:].rearrange("p c n -> p (c n)")
                        for t in range(2):
                            for half in range(2):
                                bk = banks[t * 2 + half]
                                fw.pe(lambda h, oc=oc, t=t, half=half, bk=bk, wov=wov: h.matmul(
                                    out=bk[:], lhsT=mT[oc][:, (2 * k + t) * 128:(2 * k + t + 1) * 128],
                                    rhs=wov[:, half * 512:(half + 1) * 512], start=(oc == 0), stop=(oc == 7)),
                                    reads=[mT[oc].B, wo.B], writes=[bk.B], inc=(oc == 7 or (t == 1 and half == 1)))
                    for t in range(2):
                        tile_i = tb * 4 + 2 * k + t
                        for half in range(2):
                            bk = banks[t * 2 + half]
                            fw.dve(lambda h, half=half, bk=bk: h.bn_stats(out=st6[:, half, :], in_=bk[:]),
                                   reads=[bk.B], writes=[st6.B])
                        fw.dve(lambda h: h.bn_aggr(out=mv[:], in_=st6[:].rearrange("p a b -> p (a b)")),
                               reads=[st6.B], writes=[mv.B])
                        fw.dve(lambda h: h.scalar_tensor_tensor(out=e2[:], in0=mv[:, 0:1], scalar=mv[:, 0:1],
                                                                in1=mv[:, 1:2], op0=ALU.mult, op1=ALU.add),
                               reads=[mv.B], writes=[e2.B])
                        fw.act(lambda h: h.activation(out=e2[:], in_=e2[:], func=AF.Sqrt, bias=EPS, scale=1.0),
                               reads=[e2.B], writes=[e2.B])
                        fw.dve(lambda h: h.reciprocal(out=rstd[:], in_=e2[:]), reads=[e2.B], writes=[rstd.B])
                        for half in range(2):
                            bk = banks[t * 2 + half]
                            hs = slice(half * 512, (half + 1) * 512)
                            fw.dve(lambda h, bk=bk, hs=hs: h.scalar_tensor_tensor(
                                out=wf[3][:], in0=bk[:], scalar=rstd[:, 0:1], in1=gpost[:, hs],
                                op0=ALU.mult, op1=ALU.mult), reads=[bk.B, rstd.B, gpost.B], writes=[wf[3].B])
                            fw.dve(lambda h, hs=hs, tile_i=tile_i: h.tensor_tensor(
                                out=x_sb[:, tile_i, hs], in0=x_sb[:, tile_i, hs], in1=wf[3][:], op=ALU.add),
                                reads=[x_sb.b[tile_i], wf[3].B], writes=[x_sb.b[tile_i]])

                for k in range(2):
                    do_pair(k)

            for tb in range(LIM.get('tb', 4)):
                do_block(tb)

        for si in range(nseq):
            for tq in range(NT // 4):
                fw.dma("sp", x_sb[:, tq * 4:(tq + 1) * 4, :],
                       x_d[si, tq * 512:(tq + 1) * 512, :].rearrange("(t p) d -> p t d", p=128),
                       writes=[x_sb.b[tq * 4 + i] for i in range(4)])
            for l in range(nlayers):
                if l == 1 and "l2phases" in LIM:
                    phases = LIM["l2phases"]
                if not LIM.get('nosetup'):
                    layer_setup(l)
                phase0(si, l)
                if "hT" in debug and si == 0 and l == 0:
                    fw.dma("sp", dbg_d["hT"], hT[:], reads=hT.b)
                if "C" in phases:
                    phaseC(si, l)
                    if "yc" in debug and si == 0 and l == 0:
                        fw.dma("sp", dbg_d["yc"], yc[:], reads=yc.b)
                if "A" in phases:
                    phaseA(si, l)
                    if "ya" in debug and si == 0 and l == 0:
                        fw.dma("sp", dbg_d["ya"], ya[:], reads=ya.b)
                if "B" in phases:
                    phaseB(si, l)
                    if "yb" in debug and si == 0 and l == 0:
                        fw.dma("sp", dbg_d["yb"], yb[:], reads=yb.b)
                if "M" in phases:
                    phaseM(si, l)
            for tq in range(NT // 4):
                fw.dma("sp", out_d[si, tq * 512:(tq + 1) * 512, :].rearrange("(t p) d -> p t d", p=128),
                       x_sb[:, tq * 4:(tq + 1) * 4, :],
                       reads=[x_sb.b[tq * 4 + i] for i in range(4)])
        allb = x_sb.b + hT.b + ya.b + yb.b + yc.b
        fw.wait_all("sp", allb)
        fw.emit()
        print("instr counts:", {k: v.n for k, v in fw.eng.items()})
    return nc


def make_shared(inputs):
    f = lambda k: np.ascontiguousarray(np.asarray(inputs[k], dtype=np.float32))
    shared = dict(make_consts())
    shared["w_in"] = f("w_in")
    shared["pre_norm"] = np.ascontiguousarray(f("pre_norm").reshape(DEPTH, 8, 128).transpose(0, 2, 1))
    for n in ("w_proj_rwkv", "w_proj_ret", "w_proj_s5", "w_out", "post_norm"):
        shared[n] = f(n)
    shared["b_merge"] = np.ascontiguousarray(f("b_merge").reshape(DEPTH, 3, 8, 128).transpose(0, 3, 1, 2))
    shared["rwkv_mu_rkv"] = f("rwkv_mu_rkv")
    shared["rwkv_mu_wa"] = np.ascontiguousarray(f("rwkv_mu_wa").reshape(DEPTH, 1, 128))
    shared["rwkv_w2a2"] = np.ascontiguousarray(np.stack([f("rwkv_w2"), f("rwkv_a2")], axis=1))
    vec = np.zeros((DEPTH, 8, 512), np.float32)
    for j, n in enumerate(("rwkv_w0", "rwkv_a0", "rwkv_k_k", "rwkv_k_a", "rwkv_ln_w", "rwkv_ln_b")):
        vec[:, j] = f(n)
    vec[:, 6] = f("rwkv_r_k").reshape(DEPTH, 512)
    shared["rwkv_vec"] = np.ascontiguousarray(vec.reshape(DEPTH, 8, 4, 128).transpose(0, 3, 2, 1))
    dup = lambda a: np.concatenate([a, a], axis=1)
    a_re = dup(f("s5_A_re").transpose(0, 2, 1))
    a_im = dup(f("s5_A_im").transpose(0, 2, 1))
    ldt = np.broadcast_to(f("s5_log_dt")[:, None, :], (DEPTH, 128, 32))
    shared["s5_Aab"] = np.ascontiguousarray(np.stack([a_re, a_im, ldt], axis=1))
    b_re = dup(f("s5_B_re").transpose(0, 2, 1, 3).reshape(DEPTH, 64, 512))
    b_im = dup(f("s5_B_im").transpose(0, 2, 1, 3).reshape(DEPTH, 64, 512))
    shared["s5_Bst"] = np.ascontiguousarray(np.stack([b_re, b_im], axis=1))
    c_re = f("s5_C_re").transpose(0, 3, 1, 2).reshape(DEPTH, 64, 512)
    c_im = f("s5_C_im").transpose(0, 3, 1, 2).reshape(DEPTH, 64, 512)
    ca = np.concatenate([c_re, c_im], axis=1)
    cb = np.concatenate([c_im, c_re], axis=1)
    shared["s5_Cst"] = np.ascontiguousarray(np.stack([ca, cb], axis=1))
    dv = f("s5_D").reshape(DEPTH, 4, 128).transpose(0, 2, 1)
    gb = f("s5_glu_b").reshape(DEPTH, 4, 128).transpose(0, 2, 1)
    shared["s5_vec"] = np.ascontiguousarray(np.concatenate([dv, gb], axis=2))
    shared["s5_glu_w"] = f("s5_glu_w")
    return shared


def make_inputs(inputs, s0, n):
    m = make_shared(inputs)
    m["x"] = np.ascontiguousarray(np.asarray(inputs["x"], dtype=np.float32)[s0:s0 + n])
    return m


def kernel(**inputs):
    ncores = 8
    x = np.ascontiguousarray(np.asarray(inputs["x"], dtype=np.float32))
    shared = make_shared(inputs)
    nlaunch = N_LAUNCH
    per = NSEQ // nlaunch
    nc = build_program(nseq=per)
    out = np.zeros_like(x)
    for j in range(nlaunch):
        in_maps = []
        for c in range(ncores):
            m = dict(shared)
            s0 = c * NSEQ + j * per
            m["x"] = x[s0:s0 + per]
            in_maps.append(m)
        res = run_bass_kernel_spmd(nc, in_maps, core_ids=list(range(ncores)))
        for c in range(ncores):
            s0 = c * NSEQ + j * per
            out[s0:s0 + per] = np.asarray(res.results[c]["out"])
    return out.astype(np.float32)
```

```python
import contextlib
import numpy as np
import ml_dtypes
import concourse.bass as bass
import concourse.mybir as mybir
from concourse.bass_utils import run_bass_kernel_spmd

F32 = mybir.dt.float32
BF16 = mybir.dt.bfloat16
AF = mybir.ActivationFunctionType
ALU = mybir.AluOpType
AX = mybir.AxisListType

D = 1024
S = 2048
DEPTH = 2
NSEQ = 2
D_IN = 8320
EPS = 1e-6
NT = S // 128

O_AR, O_AK, O_AV, O_XW, O_XA, O_AG = 0, 512, 1024, 1536, 1600, 1664
O_BQ, O_BK, O_BV, O_BG = 2176, 2688, 3200, 3712
O_CU, O_CG = 4224, 4736
O_GA, O_GB, O_GC = 5248, 6272, 7296


class Buf:
    def __init__(self, name):
        self.name = name
        self.last_write = None
        self.reads = []


class Engine:
    EPOCH = 30000

    def __init__(self, fw, name):
        self.fw = fw
        self.name = name
        self.sems = []
        self.count = 0
        self.waited = {}
        self.ops = []
        self.n = 0
        self._new_sem()

    def _new_sem(self):
        s = self.fw.stack.enter_context(self.fw.nc.semaphore(f"s_{self.name}_{len(self.sems)}"))
        self.sems.append(s)
        self.count = 0

    def need(self, ev):
        sem, val = ev
        key = id(sem)
        if self.waited.get(key, 0) >= val:
            return None
        self.waited[key] = val
        return ev


class Fw:
    def __init__(self, nc, stack):
        self.nc = nc
        self.stack = stack
        self.eng = {n: Engine(self, n) for n in ("pe", "act", "dve", "pool", "sp")}
        self.dsem = {}
        for q, k in (("sp", 12), ("act", 6), ("pool", 6)):
            self.dsem[q] = [[stack.enter_context(nc.semaphore(f"d_{q}_{i}")), 0] for i in range(k)]
        self.dnext = {q: 0 for q in self.dsem}

    def _deps(self, e, reads, writes):
        evs = []
        for b in reads:
            if b.last_write is not None:
                evs.append(b.last_write)
        for b in writes:
            if b.last_write is not None:
                evs.append(b.last_write)
            evs.extend(b.reads)
        out = []
        for ev in evs:
            if e.name == "pe" and any(ev[0] is s_ for s_ in e.sems):
                continue
            ev2 = e.need(ev)
            if ev2 is not None:
                out.append(ev2)
        return out

    def op(self, engname, fn, reads=(), writes=(), inc=True):
        e = self.eng[engname]
        waits = self._deps(e, reads, writes)
        if e.count >= Engine.EPOCH and inc:
            e._new_sem()
        sem = e.sems[-1]
        if inc:
            e.count += 1
        ev = (sem, e.count if inc else e.count + 1)
        for b in reads:
            b.reads.append(ev)
        for b in writes:
            b.last_write = ev
            b.reads = []
        e.n += 1

        def run(h, waits=waits, fn=fn, sem=sem, inc=inc):
            for (s, v) in waits[1:]:
                h.wait_ge(s, v)
            ins = fn(h)
            if waits:
                ins._wait_ge(waits[0][0], waits[0][1])
            if inc:
                ins.then_inc(sem, 1)

        e.ops.append(run)
        return ev

    def dma(self, q, out, in_, reads=(), writes=()):
        e = self.eng[q]
        waits = self._deps(e, reads, writes)
        slots = self.dsem[q]
        i = self.dnext[q]
        self.dnext[q] = (i + 1) % len(slots)
        slot = slots[i]
        sem = slot[0]
        prev = slot[1]
        if prev > 0:
            w = e.need((sem, prev))
            if w is not None:
                waits.append(w)
        slot[1] = prev + 16
        ev = (sem, slot[1])
        for b in reads:
            b.reads.append(ev)
        for b in writes:
            b.last_write = ev
            b.reads = []

        def run(h, waits=waits, sem=sem, out=out, in_=in_):
            for (s, v) in waits:
                h.wait_ge(s, v)
            h.dma_start(out=out, in_=in_).then_inc(sem, 16)

        e.ops.append(run)
        return ev

    def wait_all(self, engname, bufs):
        e = self.eng[engname]
        waits = []
        for b in bufs:
            for ev in ([b.last_write] if b.last_write else []) + list(b.reads):
                w = e.need(ev)
                if w is not None:
                    waits.append(w)

        def run(h, waits=waits):
            for (s, v) in waits:
                h.wait_ge(s, v)

        e.ops.append(run)

    def pe(self, fn, reads=(), writes=(), inc=True):
        return self.op("pe", fn, reads, writes, inc)

    def act(self, fn, reads=(), writes=()):
        return self.op("act", fn, reads, writes)

    def dve(self, fn, reads=(), writes=()):
        return self.op("dve", fn, reads, writes)

    def pool(self, fn, reads=(), writes=()):
        return self.op("pool", fn, reads, writes)

    def emit(self):
        nc = self.nc
        with nc.Block() as block:
            @block.tensor
            def _(h):
                for f in self.eng["pe"].ops:
                    f(h)

            @block.scalar
            def _(h):
                for f in self.eng["act"].ops:
                    f(h)

            @block.vector
            def _(h):
                for f in self.eng["dve"].ops:
                    f(h)

            @block.gpsimd
            def _(h):
                for f in self.eng["pool"].ops:
                    f(h)

            @block.sync
            def _(h):
                for f in self.eng["sp"].ops:
                    f(h)


class T:
    def __init__(self, fw, shape, dtype, name, psum=False, nsub=1):
        nc = fw.nc
        if psum:
            self.t = fw.stack.enter_context(nc.psum_tensor("ps_" + name, shape, dtype))
        else:
            self.t = fw.stack.enter_context(nc.sbuf_tensor("sb_" + name, shape, dtype))
        self.b = [Buf(f"{name}.{i}") for i in range(nsub)]
        self.name = name

    def __getitem__(self, idx):
        return self.t[idx]

    @property
    def B(self):
        return self.b[0]


def make_consts():
    c = {}
    c["ident"] = np.eye(128, dtype=np.float32).astype(ml_dtypes.bfloat16)
    bd = np.zeros((128, 128), np.float32)
    bd[:64, :64] = 1.0
    bd[64:, 64:] = 1.0
    c["bd32"] = bd
    c["bdo64"] = (bd / 64.0).astype(ml_dtypes.bfloat16)
    half = 32
    inv = (np.float32(10000.0) ** (-np.arange(half, dtype=np.float32) / np.float32(half))).astype(np.float32)
    pos = np.arange(S, dtype=np.float32)
    ang = (pos[None, :] * inv[:, None]).astype(np.float32).astype(np.float64)
    cos32, sin32 = np.cos(ang), np.sin(ang)
    cosT = np.zeros((128, S), np.float32)
    sinS = np.zeros((128, S), np.float32)
    for p in range(128):
        d = p % 64
        i = d % 32
        cosT[p] = cos32[i]
        sinS[p] = -sin32[i] if d < 32 else sin32[i]
    c["rope_cos"] = cosT
    c["rope_sin"] = sinS
    lg = np.log(1.0 - 2.0 ** (-5.0 - np.arange(8, dtype=np.float64)))
    idx = np.arange(128, dtype=np.float64)
    dmT = np.zeros((4, 128, 256), np.float32)
    kwt = np.zeros((4, 128, 128), np.float32)
    qw = np.zeros((4, 128, 128), np.float32)
    gc = np.zeros((128, 4), np.float32)
    for hp in range(4):
        for hh in range(2):
            g = lg[hp * 2 + hh]
            diff = idx[None, :] - idx[:, None]
            m = np.where(diff >= 0, np.exp(g * np.maximum(diff, 0.0)), 0.0) / 8.0
            dmT[hp, :, hh * 128:(hh + 1) * 128] = m
            kwt[hp, :, hh * 64:(hh + 1) * 64] = (np.exp(g * (127.0 - idx)) / 8.0)[:, None]
            qw[hp, hh * 64:(hh + 1) * 64, :] = np.exp(g * (idx + 1.0))[None, :]
            gc[hh * 64:(hh + 1) * 64, hp] = np.exp(g * 128.0)
    c["ret_dmT"] = dmT
    c["ret_kwt"] = kwt
    c["ret_qw"] = qw
    c["ret_gc"] = gc
    sw = np.zeros((128, 128), np.float32)
    for k in range(128):
        sw[k, (k + 64) % 128] = 1.0
    c["swapb"] = sw.astype(ml_dtypes.bfloat16)
    sg = np.zeros((128, 2), np.float32)
    sg[:64, 0], sg[64:, 0] = -1.0, 1.0
    sg[:64, 1], sg[64:, 1] = 1.0, -1.0
    c["sgn"] = sg
    rm = np.zeros((128, 8), np.float32)
    for p in range(128):
        rm[p, p // 16] = 1.0
    c["rowmask"] = rm
    ii = np.arange(128)
    su = (ii[:, None] < ii[None, :]).astype(np.float32)
    iu = (ii[:, None] <= ii[None, :]).astype(np.float32)
    sl_ = (ii[None, :] < ii[:, None]).astype(np.float32)
    c["rw_masks"] = np.concatenate([su, iu, su, iu, sl_, sl_], axis=1).astype(ml_dtypes.bfloat16)
    return c


CONST_SPECS = {
    "ident": ([128, 128], BF16), "bd32": ([128, 128], F32), "bdo64": ([128, 128], BF16),
    "rope_cos": ([128, S], F32), "rope_sin": ([128, S], F32),
    "ret_dmT": ([4, 128, 256], F32), "ret_kwt": ([4, 128, 128], F32), "ret_qw": ([4, 128, 128], F32),
    "ret_gc": ([128, 4], F32),
    "rw_masks": ([128, 768], BF16),
    "swapb": ([128, 128], BF16), "sgn": ([128, 2], F32), "rowmask": ([128, 8], F32),
}


LIM = {}
N_LAUNCH = 1


def build_program(nlayers=DEPTH, nseq=NSEQ, debug=None, phases="0CABM"):
    debug = debug or {}
    nc = bass.Bass("TRN2", target_bir_lowering=False)
    dr = {}

    def din(name, shape, dt=F32):
        dr[name] = nc.dram_tensor(name, list(shape), dt, kind="ExternalInput").ap()
        return dr[name]

    x_d = din("x", [nseq, S, D])
    pre_norm_d = din("pre_norm", [DEPTH, 128, 8])
    w_in_d = din("w_in", [DEPTH, D, D_IN])
    cd = {k: din(k, shp, dt) for k, (shp, dt) in CONST_SPECS.items()}
    wp_d = [din(n, [DEPTH, 512, D]) for n in ("w_proj_rwkv", "w_proj_ret", "w_proj_s5")]
    wout_d = din("w_out", [DEPTH, D, D])
    bmerge_d = din("b_merge", [DEPTH, 128, 3, 8])
    postn_d = din("post_norm", [DEPTH, D])
    s5A_d = din("s5_Aab", [DEPTH, 3, 128, 32])
    s5B_d = din("s5_Bst", [DEPTH, 2, 128, 512])
    s5C_d = din("s5_Cst", [DEPTH, 2, 128, 512])
    s5v_d = din("s5_vec", [DEPTH, 128, 8])
    gluw_d = din("s5_glu_w", [DEPTH, 512, 512])
    mu_rkv_d = din("rwkv_mu_rkv", [DEPTH, 3, 512])
    mu_wa_d = din("rwkv_mu_wa", [DEPTH, 1, 128])
    w2a2_d = din("rwkv_w2a2", [DEPTH, 2, 64, 512])
    rvec_d = din("rwkv_vec", [DEPTH, 128, 4, 8])
    out_d = nc.dram_tensor("out", [nseq, S, D], F32, kind="ExternalOutput").ap()
    dbg_d = {}
    for k, shp in debug.items():
        dbg_d[k] = nc.dram_tensor("dbg_" + k, list(shp[0]), shp[1], kind="ExternalOutput").ap()

    with contextlib.ExitStack() as stack:
        fw = Fw(nc, stack)
        x_sb = T(fw, [128, NT, D], F32, "x_sb", nsub=NT)
        hT = T(fw, [128, 8, S + 1], BF16, "hT", nsub=NT + 1)
        ya = T(fw, [128, 4, S], BF16, "ya", nsub=16)
        yb = T(fw, [128, 4, S], BF16, "yb", nsub=16)
        yc = T(fw, [128, 4, S], BF16, "yc", nsub=16)
        ident = T(fw, [128, 128], BF16, "ident")
        bd32 = T(fw, [128, 128], F32, "bd32")
        bdo64 = T(fw, [128, 128], BF16, "bdo64")
        gpre = T(fw, [128, DEPTH, 8], F32, "gpre")
        gexp = T(fw, [128, 8, 128], F32, "gexp")
        NF, NH = 8, 12
        wf = [T(fw, [128, 512], F32, f"wf{i}") for i in range(NF)]
        wh = [T(fw, [128, 512], BF16, f"wh{i}") for i in range(NH)]
        st6 = T(fw, [128, 2, 6], F32, "st6")
        mv = T(fw, [128, 2], F32, "mv")
        e2 = T(fw, [128, 1], F32, "e2")
        rstd = T(fw, [128, 1], F32, "rstd")
        R32t = T(fw, [128, 128], F32, "R32t")
        Rbt = T(fw, [128, 128], BF16, "Rbt")
        gct = T(fw, [128, 4], F32, "gct")
        bmt = T(fw, [128, DEPTH, 3, 8], F32, "bmt")
        swapb = T(fw, [128, 128], BF16, "swapb")
        chl = T(fw, [128, 32], BF16, "chl")
        cbk = T(fw, [128, 8], F32, "cbk")
        sgn = T(fw, [128, 2], F32, "sgn")
        rowmask = T(fw, [128, 8], F32, "rowmask")
        s5v = T(fw, [128, 8], F32, "s5v")
        carry = T(fw, [128, 8], F32, "carry")
        cst = T(fw, [128, 16], F32, "cst")
        onec = T(fw, [128, 1], F32, "onec")
        rvec = T(fw, [128, 4, 8], F32, "rvec")
        omka = T(fw, [128, 4], F32, "omka")
        lw2 = T(fw, [128, 128], BF16, "lw2")
        la2 = T(fw, [128, 128], BF16, "la2")
        St32, Stb = R32t, Rbt
        gpost = T(fw, [128, D], F32, "gpost")
        NSLOT = 7
        wslot = [T(fw, [128, 8, 128], BF16, f"wslot{i}") for i in range(NSLOT)]
        wst = [T(fw, [128, 8, 128], F32, f"wst{i}") for i in range(2)]
        wctr = [0, 0]
        tp_ps = [T(fw, [128, 8, 128], BF16, f"tp{i}", psum=True) for i in range(2)]
        pg = [T(fw, [128, 512], F32, f"pg{i}", psum=True) for i in range(6)]
        pctr = [0]

        def ps_next():
            t = pg[pctr[0] % 4]
            pctr[0] += 1
            return t

        yctr = [0]

        def ps_y():
            t = pg[4 + yctr[0] % 2]
            yctr[0] += 1
            return t

        fw.dma("sp", ident[:], cd["ident"], writes=[ident.B])
        fw.dma("sp", bd32[:], cd["bd32"], writes=[bd32.B])
        fw.dma("sp", bdo64[:], cd["bdo64"], writes=[bdo64.B])
        fw.dma("sp", gpre[:], pre_norm_d.rearrange("l p c -> p l c"), writes=[gpre.B])
        fw.dma("sp", bmt[:], bmerge_d.rearrange("l p b c -> p l b c"), writes=[bmt.B])
        fw.dma("sp", swapb[:], cd["swapb"], writes=[swapb.B])
        fw.dma("sp", sgn[:], cd["sgn"], writes=[sgn.B])
        fw.dma("sp", rowmask[:], cd["rowmask"], writes=[rowmask.B])
        fw.pool(lambda h: h.memset(hT[:, :, 0:1], 0.0), writes=[hT.b[NT]])
        fw.dve(lambda h: h.memset(onec[:], 1.0), writes=[onec.B])

        def hT_bufs(tok0, ntok, shift=0):
            a = tok0 - shift
            bl = []
            if a < 0:
                bl.append(hT.b[NT])
                a = 0
            for tt in range(a // 128, (tok0 - shift + ntok - 1) // 128 + 1):
                bl.append(hT.b[tt])
            return bl

        def layer_setup(l):
            for c in range(8):
                fw.dve(lambda h, c=c: h.tensor_copy(out=gexp[:, c, :], in_=gpre[:, l, c:c + 1].to_broadcast([128, 128])),
                       reads=[gpre.B], writes=[gexp.B])

        def wload(l, segs):
            slot = wslot[wctr[0] % NSLOT]
            wctr[0] += 1
            stg = wst[wctr[1] % 2]
            wctr[1] += 1
            o = 0
            for (c0, n) in segs:
                fw.dma("sp", stg[:, :, o:o + n],
                       w_in_d[l, :, c0:c0 + n].rearrange("(c p) n -> p c n", p=128),
                       writes=[stg.B])
                o += n
            assert o == 128
            fw.dve(lambda h, slot=slot, stg=stg: h.tensor_tensor(out=slot[:], in0=stg[:], in1=gexp[:], op=ALU.mult),
                    reads=[stg.B, gexp.B], writes=[slot.B])
            return slot

        def proj(ps, slot, tok0, ntok=512, shift=0, start=True, stop=True):
            hb = hT_bufs(tok0, ntok, shift)
            for c in range(8):
                fw.pe(lambda h, c=c: h.matmul(out=ps[:, 0:ntok], lhsT=slot[:, c, :],
                                              rhs=hT[:, c, 1 + tok0 - shift:1 + tok0 - shift + ntok],
                                              start=(start and c == 0), stop=(stop and c == 7)),
                      reads=[slot.B] + hb, writes=[ps.B], inc=(c == 7))

        def silu_from_psum(dst, ps):
            fw.act(lambda h: h.activation(out=dst[:], in_=ps[:], func=AF.Sigmoid), reads=[ps.B], writes=[dst.B])
            fw.dve(lambda h: h.tensor_tensor(out=dst[:], in0=ps[:], in1=dst[:], op=ALU.mult),
                   reads=[ps.B, dst.B], writes=[dst.B])

        def phase0(si, l):
            for tt in range(NT):
                xb = x_sb.b[tt]
                xt = x_sb[:, tt, :]
                for j in range(2):
                    fw.dve(lambda h, j=j, xt=xt: h.bn_stats(out=st6[:, j, :], in_=xt[:, j * 512:(j + 1) * 512]),
                           reads=[xb], writes=[st6.B])
                fw.dve(lambda h: h.bn_aggr(out=mv[:], in_=st6[:].rearrange("p a b -> p (a b)")),
                       reads=[st6.B], writes=[mv.B])
                fw.dve(lambda h: h.scalar_tensor_tensor(out=e2[:], in0=mv[:, 0:1], scalar=mv[:, 0:1],
                                                        in1=mv[:, 1:2], op0=ALU.mult, op1=ALU.add),
                       reads=[mv.B], writes=[e2.B])
                fw.act(lambda h: h.activation(out=e2[:], in_=e2[:], func=AF.Sqrt, bias=EPS, scale=1.0),
                       reads=[e2.B], writes=[e2.B])
                fw.dve(lambda h: h.reciprocal(out=rstd[:], in_=e2[:]), reads=[e2.B], writes=[rstd.B])
                for hf in range(2):
                    fw.dve(lambda h, hf=hf, xt=xt: h.tensor_scalar(out=wh[hf][:], in0=xt[:, hf * 512:(hf + 1) * 512],
                                                                   scalar1=rstd[:, 0:1], scalar2=None, op0=ALU.mult),
                           reads=[xb, rstd.B], writes=[wh[hf].B])
                ps = tp_ps[tt % 2]
                for c in range(8):
                    fw.pe(lambda h, ps=ps, c=c: h.transpose(out=ps[:, c, :],
                                                            in_=wh[c // 4][:, (c % 4) * 128:(c % 4 + 1) * 128],
                                                            identity=ident[:]),
                          reads=[wh[c // 4].B, ident.B], writes=[ps.B], inc=(c == 7))
                fw.act(lambda h, ps=ps, tt=tt: h.activation(out=hT[:, :, 1 + tt * 128:1 + (tt + 1) * 128],
                                                            in_=ps[:], func=AF.Copy),
                       reads=[ps.B], writes=[hT.b[tt]])

        def headnorm_gate(y_ps, sg, dst, dstb, eps, affine=None):
            y32, ybf, ycen, sq, rs = wf[0], wh[0], wf[1], wh[1], wf[2]
            fw.act(lambda h: h.activation(out=y32[:], in_=y_ps[:], func=AF.Copy), reads=[y_ps.B], writes=[y32.B])
            fw.dve(lambda h: h.tensor_copy(out=ybf[:], in_=y32[:]), reads=[y32.B], writes=[ybf.B])
            mean_ps = ps_next()
            fw.pe(lambda h: h.matmul(out=mean_ps[:], lhsT=bdo64[:], rhs=ybf[:], start=True, stop=True),
                  reads=[bdo64.B, ybf.B], writes=[mean_ps.B])
            fw.dve(lambda h: h.tensor_tensor(out=ycen[:], in0=y32[:], in1=mean_ps[:], op=ALU.subtract),
                   reads=[y32.B, mean_ps.B], writes=[ycen.B])
            fw.act(lambda h: h.activation(out=sq[:], in_=ycen[:], func=AF.Square), reads=[ycen.B], writes=[sq.B])
            var_ps = ps_next()
            fw.pe(lambda h: h.matmul(out=var_ps[:], lhsT=bdo64[:], rhs=sq[:], start=True, stop=True),
                  reads=[bdo64.B, sq.B], writes=[var_ps.B])
            fw.act(lambda h: h.activation(out=rs[:], in_=var_ps[:], func=AF.Sqrt, bias=eps, scale=1.0),
                   reads=[var_ps.B], writes=[rs.B])
            fw.dve(lambda h: h.reciprocal(out=rs[:], in_=rs[:]), reads=[rs.B], writes=[rs.B])
            fw.dve(lambda h: h.tensor_tensor(out=ycen[:], in0=ycen[:], in1=rs[:], op=ALU.mult),
                    reads=[ycen.B, rs.B], writes=[ycen.B])
            if affine is not None:
                affine(ycen)
            fw.dve(lambda h: h.tensor_tensor(out=dst, in0=ycen[:], in1=sg[:], op=ALU.mult),
                    reads=[ycen.B, sg.B], writes=[dstb])

        def phaseB(si, l):
            dmT = wf[4]
            kwt = wf[5]
            qwt = wf[6]
            fw.dma("sp", gct[:, 0:4], cd["ret_gc"], writes=[gct.B])
            R32 = R32t
            t1, t2 = wf[0], wf[1]
            cosb, sinb = wf[2], wf[3]
            qr, kr, qc, vsb, ktok, vp0, vp1 = wh[2], wh[3], wh[4], wh[5], wh[6], wh[7], wh[8]
            qpad = [wh[9], wh[10]]
            Ssb = wh[11]
            Rb = Rbt
            sgate = wf[7]
            for hp in range(LIM.get('hp', 4)):
                cb = O_BQ + hp * 128
                kb = O_BK + hp * 128
                sw = lambda b: [(b + 32, 32), (b, 32), (b + 96, 32), (b + 64, 32)]
                w_q = wload(l, [(cb, 128)])
                w_qs = wload(l, sw(cb))
                w_k = wload(l, [(kb, 128)])
                w_ks = wload(l, sw(kb))
                w_v = wload(l, [(O_BV + hp * 128, 128)])
                w_g = wload(l, [(O_BG + hp * 128, 128)])
                fw.dma("sp", dmT[:, 0:256], cd["ret_dmT"][hp], writes=[dmT.B])
                fw.dma("sp", kwt[:, 0:128], cd["ret_kwt"][hp], writes=[kwt.B])
                fw.dma("sp", qwt[:, 0:128], cd["ret_qw"][hp], writes=[qwt.B])
                fw.pool(lambda h: h.memset(R32[:], 0.0), writes=[R32.B])
                fw.pool(lambda h: h.memset(Rb[:], 0.0), writes=[Rb.B])
                for qp in qpad:
                    fw.pool(lambda h, qp=qp: h.memset(qp[:], 0.0), writes=[qp.B])
                fw.pool(lambda h: h.memset(vp0[:], 0.0), writes=[vp0.B])
                fw.pool(lambda h: h.memset(vp1[:], 0.0), writes=[vp1.B])
                def do_block(tb, hp=hp, w_q=w_q, w_qs=w_qs, w_k=w_k, w_ks=w_ks, w_v=w_v, w_g=w_g):
                    tok0 = tb * 512
                    fw.dma("sp", cosb[:], cd["rope_cos"][:, tok0:tok0 + 512], writes=[cosb.B])
                    fw.dma("sp", sinb[:], cd["rope_sin"][:, tok0:tok0 + 512], writes=[sinb.B])
                    if LIM.get('stage', 99) < 1:
                        return
                    pq, pqs = ps_next(), ps_next()
                    proj(pq, w_q, tok0)
                    proj(pqs, w_qs, tok0)
                    fw.dve(lambda h: h.tensor_tensor(out=t1[:], in0=pq[:], in1=cosb[:], op=ALU.mult),
                           reads=[pq.B, cosb.B], writes=[t1.B])
                    fw.dve(lambda h: h.tensor_tensor(out=t2[:], in0=pqs[:], in1=sinb[:], op=ALU.mult),
                           reads=[pqs.B, sinb.B], writes=[t2.B])
                    fw.dve(lambda h: h.tensor_tensor(out=qr[:], in0=t1[:], in1=t2[:], op=ALU.add),
                            reads=[t1.B, t2.B], writes=[qr.B])
                    if LIM.get('stage', 99) < 2:
                        return
                    for half in range(2):
                        qp = qpad[half]
                        for hh in range(2):
                            src = qr[hh * 64:(hh + 1) * 64, half * 256:(half + 1) * 256].rearrange("p (c i) -> p c i", c=2)
                            dstv = qp[hh * 64:(hh + 1) * 64, :].rearrange("p (c h i) -> p c h i", c=2, h=2)[:, :, hh, :]
                            fw.act(lambda h, src=src, dstv=dstv: h.activation(out=dstv, in_=src, func=AF.Copy),
                                   reads=[qr.B], writes=[qp.B])
                    for c4 in range(4):
                        fw.dve(lambda h, c4=c4: h.tensor_tensor(out=qc[:, c4 * 128:(c4 + 1) * 128],
                                                                 in0=qr[:, c4 * 128:(c4 + 1) * 128],
                                                                 in1=qwt[:, 0:128], op=ALU.mult),
                                reads=[qr.B, qwt.B], writes=[qc.B])
                    if LIM.get('stage', 99) < 3:
                        return
                    pk, pks = ps_next(), ps_next()
                    proj(pk, w_k, tok0)
                    proj(pks, w_ks, tok0)
                    fw.dve(lambda h: h.tensor_tensor(out=t1[:], in0=pk[:], in1=cosb[:], op=ALU.mult),
                           reads=[pk.B, cosb.B], writes=[t1.B])
                    fw.dve(lambda h: h.tensor_tensor(out=t2[:], in0=pks[:], in1=sinb[:], op=ALU.mult),
                           reads=[pks.B, sinb.B], writes=[t2.B])
                    fw.dve(lambda h: h.tensor_tensor(out=kr[:], in0=t1[:], in1=t2[:], op=ALU.add),
                            reads=[t1.B, t2.B], writes=[kr.B])
                    if LIM.get('stage', 99) < 4:
                        return
                    pv = ps_next()
                    proj(pv, w_v, tok0)
                    fw.act(lambda h: h.activation(out=vsb[:], in_=pv[:], func=AF.Copy), reads=[pv.B], writes=[vsb.B])
                    pgate = ps_next()
                    proj(pgate, w_g, tok0)
                    silu_from_psum(sgate, pgate)
                    if LIM.get('stage', 99) < 5:
                        return
                    tp = tp_ps[0]
                    for c4 in range(4):
                        fw.pe(lambda h, c4=c4: h.transpose(out=tp[:, c4, :], in_=kr[:, c4 * 128:(c4 + 1) * 128],
                                                           identity=ident[:]),
                              reads=[kr.B, ident.B], writes=[tp.B], inc=False)
                    for c4 in range(4):
                        fw.pe(lambda h, c4=c4: h.transpose(out=tp[:, 4 + c4, :], in_=vsb[:, c4 * 128:(c4 + 1) * 128],
                                                           identity=ident[:]),
                              reads=[vsb.B, ident.B], writes=[tp.B], inc=(c4 == 3))
                    for c4 in range(4):
                        fw.dve(lambda h, c4=c4: h.tensor_tensor(out=ktok[:, c4 * 128:(c4 + 1) * 128], in0=tp[:, c4, :],
                                                                in1=kwt[:, 0:128], op=ALU.mult),
                               reads=[tp.B, kwt.B], writes=[ktok.B])
                    fw.act(lambda h: h.activation(
                        out=vp0[:].rearrange("p (c f) -> p c f", c=4)[:, :, 0:64], in_=tp[:, 4:8, 0:64], func=AF.Copy),
                        reads=[tp.B], writes=[vp0.B])
                    fw.act(lambda h: h.activation(
                        out=vp1[:].rearrange("p (c f) -> p c f", c=4)[:, :, 64:128], in_=tp[:, 4:8, 64:128], func=AF.Copy),
                        reads=[tp.B], writes=[vp1.B])
                    if LIM.get('stage', 99) < 6:
                        return
                    y_ps = ps_y()

                    def do_chunk(c4):
                        cs = slice(c4 * 128, (c4 + 1) * 128)
                        sc = ps_next()
                        qp = qpad[c4 // 2]
                        fw.pe(lambda h, cs=cs, qp=qp, c4=c4, sc=sc: h.matmul(
                            out=sc[:, 0:256], lhsT=kr[:, cs], rhs=qp[:, (c4 % 2) * 256:(c4 % 2) * 256 + 256],
                            start=True, stop=True), reads=[kr.B, qp.B], writes=[sc.B])
                        sv = Ssb[:, (c4 % 2) * 256:(c4 % 2) * 256 + 256]
                        fw.dve(lambda h, sc=sc, sv=sv: h.tensor_tensor(out=sv, in0=sc[:, 0:256], in1=dmT[:, 0:256],
                                                                       op=ALU.mult),
                               reads=[sc.B, dmT.B], writes=[Ssb.B])
                        fw.pe(lambda h, cs=cs, sv=sv: h.matmul(out=y_ps[:, cs], lhsT=vp0[:, cs], rhs=sv[:, 0:128],
                                                               start=True, stop=False),
                              reads=[vp0.B, Ssb.B], writes=[y_ps.B], inc=False)
                        fw.pe(lambda h, cs=cs, sv=sv: h.matmul(out=y_ps[:, cs], lhsT=vp1[:, cs], rhs=sv[:, 128:256],
                                                               start=False, stop=False),
                              reads=[vp1.B, Ssb.B], writes=[y_ps.B], inc=False)
                        fw.pe(lambda h, cs=cs: h.matmul(out=y_ps[:, cs], lhsT=Rb[:], rhs=qc[:, cs],
                                                        start=False, stop=True),
                              reads=[Rb.B, qc.B], writes=[y_ps.B])
                        kv = ps_next()
                        fw.pe(lambda h, cs=cs, kv=kv: h.matmul(out=kv[:, 0:128], lhsT=ktok[:, cs], rhs=vp0[:, cs],
                                                               start=True, stop=False),
                              reads=[ktok.B, vp0.B], writes=[kv.B], inc=False)
                        fw.pe(lambda h, cs=cs, kv=kv: h.matmul(out=kv[:, 0:128], lhsT=ktok[:, cs], rhs=vp1[:, cs],
                                                               start=False, stop=True),
                              reads=[ktok.B, vp1.B], writes=[kv.B])
                        fw.dve(lambda h, kv=kv, hp=hp: h.scalar_tensor_tensor(
                            out=R32[:], in0=R32[:], scalar=gct[:, hp:hp + 1], in1=kv[:, 0:128],
                            op0=ALU.mult, op1=ALU.add), reads=[R32.B, gct.B, kv.B], writes=[R32.B])
                        fw.dve(lambda h: h.tensor_tensor(out=Rb[:], in0=R32[:], in1=bd32[:],
                                                          op=ALU.mult),
                                reads=[R32.B, bd32.B], writes=[Rb.B])
                    for c4 in range(LIM.get('c4', 4)):
                        do_chunk(c4)
                    if LIM.get('stage', 99) < 7:
                        return
                    headnorm_gate(y_ps, sgate, yb[:, hp, tok0:tok0 + 512], yb.b[hp * 4 + tb], EPS)

                for tb in range(LIM.get('tb', 4)):
                    do_block(tb)

        def phaseC(si, l):
            PA, PB = wf[0], wf[1]
            sl = lambda t, i: t[:, i * 32:(i + 1) * 32]
            A_RE, A_IM, DT, MAG, ANG, CC, SS, T1, T2, T3, PM, RDEN, CRE, CIM, QQ, SLS = range(16)
            tabs = ya.b + yb.b
            cosT = ya[:].rearrange("p a s -> p (a s)").bitcast(F32).rearrange("p (g j) -> p g j", g=32)
            sinT = yb[:].rearrange("p a s -> p (a s)").bitcast(F32).rearrange("p (g j) -> p g j", g=32)
            ycf = yc[:].rearrange("p a s -> p (a s)").bitcast(F32)
            tmp1 = ycf[:, 0:2048].rearrange("p (g j) -> p g j", g=32)
            tmp2 = ycf[:, 2048:4096].rearrange("p (g j) -> p g j", g=32)

            def pa(fn_, eng="dve", extra=()):
                fw.op(eng, fn_, reads=[PA.B, PB.B] + list(extra), writes=[PA.B, PB.B])

            def tt(o, a, b, op):
                pa(lambda h: h.tensor_tensor(out=o, in0=a, in1=b, op=op))

            P = lambda i: sl(PA, i)
            for i in range(3):
                fw.dma("sp", P(i), s5A_d[l, i], writes=[PA.B])
            fw.dma("sp", s5v[:], s5v_d[l], writes=[s5v.B])
            pa(lambda h: h.activation(out=P(DT), in_=P(DT), func=AF.Exp), "act")
            tt(P(T1), P(DT), P(A_RE), ALU.mult)
            pa(lambda h: h.activation(out=P(MAG), in_=P(T1), func=AF.Exp), "act")
            tt(P(ANG), P(DT), P(A_IM), ALU.mult)
            pa(lambda h: h.activation(out=P(SS), in_=P(ANG), func=AF.Sin, scale=1.0 / 16.0), "act")
            pa(lambda h: h.activation(out=P(CC), in_=P(ANG), func=AF.Sin, scale=1.0 / 16.0, bias=float(np.pi / 2)), "act")

            def dbl(co, so, ci, si_):
                tt(P(T1), ci, ci, ALU.mult)
                tt(P(T2), si_, si_, ALU.mult)
                tt(P(T3), si_, ci, ALU.mult)
                tt(co, P(T1), P(T2), ALU.subtract)
                pa(lambda h: h.tensor_scalar(out=so, in0=P(T3), scalar1=2.0, scalar2=None, op0=ALU.mult))

            for _ in range(3):
                dbl(P(CC), P(SS), P(CC), P(SS))
            dbl(sl(PB, 0), sl(PB, 8), P(CC), P(SS))
            for k in range(1, 8):
                dbl(sl(PB, k), sl(PB, 8 + k), sl(PB, k - 1), sl(PB, 8 + k - 1))
            pa(lambda h: h.tensor_scalar(out=P(SLS), in0=sl(PB, 15), scalar1=sgn[:, 0:1], scalar2=None, op0=ALU.mult),
               extra=[sgn.B])
            tt(P(PM), P(MAG), sl(PB, 0), ALU.mult)
            pa(lambda h: h.tensor_scalar(out=P(PM), in0=P(PM), scalar1=-1.0, scalar2=None, op0=ALU.add))
            tt(P(QQ), P(MAG), sl(PB, 8), ALU.mult)
            tt(P(T1), P(A_RE), P(A_RE), ALU.mult)
            tt(P(T2), P(A_IM), P(A_IM), ALU.mult)
            tt(P(T1), P(T1), P(T2), ALU.add)
            pa(lambda h: h.reciprocal(out=P(RDEN), in_=P(T1)))
            tt(P(T1), P(PM), P(A_RE), ALU.mult)
            tt(P(T2), P(QQ), P(A_IM), ALU.mult)
            tt(P(T1), P(T1), P(T2), ALU.add)
            tt(P(CRE), P(T1), P(RDEN), ALU.mult)
            tt(P(T1), P(QQ), P(A_RE), ALU.mult)
            tt(P(T2), P(PM), P(A_IM), ALU.mult)
            tt(P(T1), P(T1), P(T2), ALU.subtract)
            tt(P(CIM), P(T1), P(RDEN), ALU.mult)

            fw.dve(lambda h: h.memset(cosT[:, :, 0:1], 1.0), writes=tabs)
            fw.dve(lambda h: h.memset(sinT[:, :, 0:1], 0.0), writes=tabs)
            for k in range(7):
                m = 1 << k
                cmb = sl(PB, k).unsqueeze(2).to_broadcast([128, 32, m])
                smb = sl(PB, 8 + k).unsqueeze(2).to_broadcast([128, 32, m])

                def lvl(m=m, cmb=cmb, smb=smb):
                    rw = dict(reads=tabs + yc.b + [PB.B], writes=tabs + yc.b)
                    fw.dve(lambda h: h.tensor_tensor(out=tmp1[:, :, 0:m], in0=cosT[:, :, 0:m], in1=cmb, op=ALU.mult), **rw)
                    fw.dve(lambda h: h.tensor_tensor(out=tmp2[:, :, 0:m], in0=sinT[:, :, 0:m], in1=smb, op=ALU.mult), **rw)
                    fw.dve(lambda h: h.tensor_tensor(out=cosT[:, :, m:2 * m], in0=tmp1[:, :, 0:m], in1=tmp2[:, :, 0:m],
                                                     op=ALU.subtract), **rw)
                    fw.dve(lambda h: h.tensor_tensor(out=tmp1[:, :, 0:m], in0=sinT[:, :, 0:m], in1=cmb, op=ALU.mult), **rw)
                    fw.dve(lambda h: h.tensor_tensor(out=tmp2[:, :, 0:m], in0=cosT[:, :, 0:m], in1=smb, op=ALU.mult), **rw)
                    fw.dve(lambda h: h.tensor_tensor(out=sinT[:, :, m:2 * m], in0=tmp1[:, :, 0:m], in1=tmp2[:, :, 0:m],
                                                     op=ALU.add), **rw)
                lvl()

            xh = [wf[2], wf[3]]
            stg = wf[4]
            stg2 = wf[5]
            tri = wf[6]
            BmT = wh[0:4]
            CmT = wh[4:8]
            u_bf, g12 = wh[8], wh[9]
            for t_ in CmT:
                fw.dve(lambda h, t_=t_: h.memset(t_[:], 0.0), writes=[t_.B])

            def xh_ap(g8):
                return xh[g8 // 4][:, (g8 % 4) * 128:(g8 % 4 + 1) * 128]

            def do_gc(gc):
                gs = slice(gc * 128, (gc + 1) * 128)
                fw.dma("sp", stg[:, 0:128], s5B_d[l, 0, :, gs], writes=[stg.B])
                fw.dma("sp", stg[:, 128:256], s5B_d[l, 1, :, gs], writes=[stg.B])
                v3 = lambda ap: ap.rearrange("p (g q) -> p g q", g=8)
                creb = P(CRE)[:, gc * 8:(gc + 1) * 8].unsqueeze(2).to_broadcast([128, 8, 16])
                cimb = P(CIM)[:, gc * 8:(gc + 1) * 8].unsqueeze(2).to_broadcast([128, 8, 16])
                rw = dict(reads=[stg.B, stg2.B, PA.B], writes=[stg2.B])
                bre, bim = v3(stg[:, 0:128]), v3(stg[:, 128:256])
                ta, tb_ = v3(stg2[:, 0:128]), v3(stg2[:, 128:256])
                bbre, bbim = v3(g12[:, 0:128]), v3(g12[:, 128:256])
                rwb = dict(reads=[stg.B, stg2.B, PA.B], writes=[g12.B])
                fw.dve(lambda h: h.tensor_tensor(out=ta, in0=bre, in1=creb, op=ALU.mult), **rw)
                fw.dve(lambda h: h.tensor_tensor(out=tb_, in0=bim, in1=cimb, op=ALU.mult), **rw)
                fw.dve(lambda h: h.tensor_tensor(out=bbre, in0=ta, in1=tb_, op=ALU.subtract), **rwb)
                fw.dve(lambda h: h.tensor_tensor(out=ta, in0=bim, in1=creb, op=ALU.mult), **rw)
                fw.dve(lambda h: h.tensor_tensor(out=tb_, in0=bre, in1=cimb, op=ALU.mult), **rw)
                fw.dve(lambda h: h.tensor_tensor(out=bbim, in0=ta, in1=tb_, op=ALU.add), **rwb)
                tpx = tp_ps[0]
                ptr, pti = tpx[:, 0, :], tpx[:, 1, :]
                fw.pe(lambda h: h.transpose(out=ptr, in_=g12[:, 0:128], identity=ident[:]),
                      reads=[g12.B, ident.B], writes=[tpx.B], inc=False)
                fw.pe(lambda h: h.transpose(out=pti, in_=g12[:, 128:256], identity=ident[:]),
                      reads=[g12.B, ident.B], writes=[tpx.B])
                fw.act(lambda h: h.activation(out=tri[:, 0:64], in_=ptr[:, 0:64], func=AF.Copy), reads=[tpx.B], writes=[tri.B])
                fw.act(lambda h: h.activation(out=tri[:, 64:128], in_=pti[:, 0:64], func=AF.Copy), reads=[tpx.B], writes=[tri.B])
                fw.act(lambda h: h.activation(out=tri[:, 128:192], in_=pti[:, 0:64], func=AF.Copy), reads=[tpx.B], writes=[tri.B])
                fw.act(lambda h: h.activation(out=tri[:, 192:256], in_=ptr[:, 0:64], func=AF.Copy, scale=-1.0),
                       reads=[tpx.B], writes=[tri.B])
                for g8 in range(8):
                    bt = BmT[g8 // 2]
                    o = (g8 % 2) * 256
                    fw.dve(lambda h, bt=bt, o=o, g8=g8: h.tensor_scalar(
                        out=bt[:, o:o + 256], in0=tri[:, 0:256], scalar1=rowmask[:, g8:g8 + 1], scalar2=None, op0=ALU.mult),
                        reads=[tri.B, rowmask.B], writes=[bt.B])
                fw.dma("sp", stg[:, 0:128], s5C_d[l, 0, :, gs], writes=[stg.B])
                fw.dma("sp", stg[:, 128:256], s5C_d[l, 1, :, gs], writes=[stg.B])
                fw.dve(lambda h: h.tensor_scalar(out=stg[:, 0:128], in0=stg[:, 0:128], scalar1=sgn[:, 1:2], scalar2=None,
                                                 op0=ALU.mult), reads=[stg.B, sgn.B], writes=[stg.B])
                fw.dve(lambda h: h.tensor_scalar(out=stg[:, 128:256], in0=stg[:, 128:256], scalar1=-1.0, scalar2=None,
                                                 op0=ALU.mult), reads=[stg.B], writes=[stg.B])
                for g8 in range(8):
                    ct = CmT[g8 // 2]
                    o = (g8 % 2) * 256
                    for ver in range(2):
                        fw.act(lambda h, ct=ct, o=o, g8=g8, ver=ver: h.activation(
                            out=ct[:, o + ver * 128 + g8 * 16:o + ver * 128 + (g8 + 1) * 16],
                            in_=stg[:, ver * 128 + g8 * 16:ver * 128 + (g8 + 1) * 16], func=AF.Copy),
                            reads=[stg.B], writes=[ct.B])
                w_u = wload(l, [(O_CU + gc * 128, 128)])

                def do_block(tb):
                    tok0 = tb * 512
                    pu = ps_next()
                    proj(pu, w_u, tok0)
                    u32 = wf[7]
                    fw.act(lambda h: h.activation(out=u_bf[:], in_=pu[:], func=AF.Copy), reads=[pu.B], writes=[u_bf.B])
                    fw.act(lambda h: h.activation(out=u32[:], in_=pu[:], func=AF.Copy), reads=[pu.B], writes=[u32.B])
                    y_ps = ps_y()

                    def do_sb(sb):
                        ts = slice(sb * 128, (sb + 1) * 128)
                        first = (tb == 0 and sb == 0)

                        def do_batch(bi):
                            g0 = gc * 8 + bi * 4
                            bun, bus = ps_next(), ps_next()
                            for q in range(4):
                                g8 = bi * 4 + q
                                bt = BmT[g8 // 2]
                                o = (g8 % 2) * 256
                                fw.pe(lambda h, q=q, bt=bt, o=o: h.matmul(out=bun[:, q * 128:(q + 1) * 128], lhsT=bt[:, o:o + 128],
                                                                          rhs=u_bf[:, ts], start=True, stop=True),
                                      reads=[bt.B, u_bf.B], writes=[bun.B], inc=(q == 3))
                            for q in range(4):
                                g8 = bi * 4 + q
                                bt = BmT[g8 // 2]
                                o = (g8 % 2) * 256
                                fw.pe(lambda h, q=q, bt=bt, o=o: h.matmul(out=bus[:, q * 128:(q + 1) * 128],
                                                                          lhsT=bt[:, o + 128:o + 256], rhs=u_bf[:, ts],
                                                                          start=True, stop=True),
                                      reads=[bt.B, u_bf.B], writes=[bus.B], inc=(q == 3))
                            w1, w2 = stg, stg2
                            cs4 = cosT[:, g0:g0 + 4, :]
                            sn4 = sinT[:, g0:g0 + 4, :]
                            v4 = lambda ap: ap.rearrange("p (g j) -> p g j", g=4)
                            fw.dve(lambda h: h.tensor_tensor(out=v4(w1[:]), in0=v4(bun[:]), in1=cs4, op=ALU.mult),
                                   reads=[bun.B] + tabs, writes=[w1.B])
                            fw.dve(lambda h: h.tensor_tensor(out=v4(w2[:]), in0=v4(bus[:]), in1=sn4, op=ALU.mult),
                                   reads=[bus.B] + tabs, writes=[w2.B])
                            fw.dve(lambda h: h.tensor_tensor(out=w1[:], in0=w1[:], in1=w2[:], op=ALU.add),
                                   reads=[w1.B, w2.B], writes=[w1.B])
                            xt_ = xh[bi]
                            for q in range(4):
                                g8 = bi * 4 + q
                                g = gc * 8 + g8
                                init = 0.0 if first else carry[:, g8:g8 + 1]
                                fw.dve(lambda h, q=q, g=g, init=init: h.tensor_tensor_scan(
                                    out=xt_[:, q * 128:(q + 1) * 128], data0=P(MAG)[:, g:g + 1].to_broadcast([128, 128]),
                                    data1=w1[:, q * 128:(q + 1) * 128], initial=init, op0=ALU.mult, op1=ALU.add),
                                    reads=[w1.B, PA.B, carry.B], writes=[xt_.B])
                            G1, G2 = g12, wh[10]
                            fw.dve(lambda h: h.tensor_tensor(out=v4(G1[:]), in0=v4(xt_[:]), in1=cs4, op=ALU.mult),
                                   reads=[xt_.B] + tabs, writes=[G1.B])
                            fw.dve(lambda h: h.tensor_tensor(out=v4(G2[:]), in0=v4(xt_[:]), in1=sn4, op=ALU.mult),
                                   reads=[xt_.B] + tabs, writes=[G2.B])
                            for q in range(4):
                                g8 = bi * 4 + q
                                ct = CmT[g8 // 2]
                                o = (g8 % 2) * 256
                                fw.pe(lambda h, q=q, ct=ct, o=o, g8=g8: h.matmul(
                                    out=y_ps[:, ts], lhsT=ct[:, o:o + 128], rhs=G1[:, q * 128:(q + 1) * 128],
                                    start=(g8 == 0), stop=False), reads=[ct.B, G1.B], writes=[y_ps.B], inc=(q == 3))
                            for q in range(4):
                                g8 = bi * 4 + q
                                ct = CmT[g8 // 2]
                                o = (g8 % 2) * 256
                                fw.pe(lambda h, q=q, ct=ct, o=o, g8=g8: h.matmul(
                                    out=y_ps[:, ts], lhsT=ct[:, o + 128:o + 256], rhs=G2[:, q * 128:(q + 1) * 128],
                                    start=False, stop=(g8 == 7)), reads=[ct.B, G2.B], writes=[y_ps.B], inc=(q == 3))

                        for bi in range(2):
                            do_batch(bi)
                        csw = ps_next()
                        for hx in range(2):
                            xl = xh[hx][:].rearrange("p (g j) -> p g j", g=4)[:, :, 127]
                            fw.dve(lambda h, hx=hx, xl=xl: h.tensor_copy(out=chl[:, hx * 4:(hx + 1) * 4], in_=xl),
                                   reads=[xh[hx].B], writes=[chl.B])
                        fw.dve(lambda h: h.tensor_copy(out=cbk[:], in_=chl[:, 0:8]), reads=[chl.B], writes=[cbk.B])
                        for hx in range(2):
                            xl = xh[hx][:].rearrange("p (g j) -> p g j", g=4)[:, :, 127]
                            fw.dve(lambda h, hx=hx, xl=xl: h.tensor_tensor(out=chl[:, 8 + hx * 4:8 + (hx + 1) * 4], in0=xl,
                                                                          in1=cbk[:, hx * 4:(hx + 1) * 4], op=ALU.subtract),
                                   reads=[xh[hx].B, cbk.B], writes=[chl.B])
                        fw.pe(lambda h: h.matmul(out=csw[:, 0:8], lhsT=swapb[:], rhs=chl[:, 0:8], start=True, stop=False),
                              reads=[swapb.B, chl.B], writes=[csw.B], inc=False)
                        fw.pe(lambda h: h.matmul(out=csw[:, 0:8], lhsT=swapb[:], rhs=chl[:, 8:16], start=False, stop=True),
                              reads=[swapb.B, chl.B], writes=[csw.B])
                        fw.dve(lambda h: h.tensor_tensor(out=cst[:, 0:8], in0=csw[:, 0:8], in1=P(SLS)[:, gc * 8:(gc + 1) * 8],
                                                         op=ALU.mult), reads=[csw.B, PA.B], writes=[cst.B])
                        for hx in range(2):
                            xl = xh[hx][:].rearrange("p (g j) -> p g j", g=4)[:, :, 127]
                            fw.dve(lambda h, hx=hx, xl=xl: h.tensor_tensor(
                                out=cst[:, 8 + hx * 4:8 + (hx + 1) * 4], in0=xl,
                                in1=sl(PB, 7)[:, gc * 8 + hx * 4:gc * 8 + (hx + 1) * 4], op=ALU.mult),
                                reads=[xh[hx].B, PB.B], writes=[cst.B])
                        fw.dve(lambda h: h.tensor_tensor(out=carry[:], in0=cst[:, 0:8], in1=cst[:, 8:16], op=ALU.add),
                               reads=[cst.B], writes=[carry.B])

                    for sb in range(4):
                        do_sb(sb)
                    y32, gt = wf[6], wf[7]
                    fw.dve(lambda h: h.scalar_tensor_tensor(out=y32[:], in0=u32[:], scalar=s5v[:, gc:gc + 1], in1=y_ps[:],
                                                            op0=ALU.mult, op1=ALU.add),
                           reads=[u32.B, s5v.B, y_ps.B], writes=[y32.B])
                    fw.act(lambda h: h.activation(out=gt[:], in_=y32[:], func=AF.Square), reads=[y32.B], writes=[gt.B])
                    fw.dve(lambda h: h.tensor_scalar(out=gt[:], in0=gt[:], scalar1=0.044715, scalar2=1.0, op0=ALU.mult,
                                                     op1=ALU.add), reads=[gt.B], writes=[gt.B])
                    fw.dve(lambda h: h.tensor_tensor(out=gt[:], in0=gt[:], in1=y32[:], op=ALU.mult),
                           reads=[gt.B, y32.B], writes=[gt.B])
                    fw.act(lambda h: h.activation(out=gt[:], in_=gt[:], func=AF.Tanh, scale=0.7978845608028654),
                           reads=[gt.B], writes=[gt.B])
                    fw.dve(lambda h: h.tensor_scalar(out=gt[:], in0=gt[:], scalar1=1.0, scalar2=0.5, op0=ALU.add,
                                                     op1=ALU.mult), reads=[gt.B], writes=[gt.B])
                    fw.dve(lambda h: h.tensor_tensor(out=yc[:, gc, tok0:tok0 + 512], in0=gt[:], in1=y32[:], op=ALU.mult),
                           reads=[gt.B, y32.B], writes=[yc.b[gc * 4 + tb]])

                for tb in range(LIM.get('tb', 4)):
                    do_block(tb)

            for gc in range(4):
                do_gc(gc)

            gw = wh[0:4]
            for c in range(4):
                fw.dma("sp", stg[:], gluw_d[l, c * 128:(c + 1) * 128, :], writes=[stg.B])
                fw.dve(lambda h, c=c: h.tensor_copy(out=gw[c][:], in_=stg[:]), reads=[stg.B], writes=[gw[c].B])
            wgs = [wload(l, [(O_CG + oc * 128, 128)]) for oc in range(4)]

            def glu_block(tb):
                tok0 = tb * 512
                sgl = [wf[0], wf[1], wf[2], wf[3]]
                for oc in range(4):
                    gp = pg[oc]
                    for c in range(4):
                        fw.pe(lambda h, oc=oc, c=c, gp=gp: h.matmul(out=gp[:], lhsT=gw[c][:, oc * 128:(oc + 1) * 128],
                                                                    rhs=yc[:, c, tok0:tok0 + 512], start=(c == 0), stop=(c == 3)),
                              reads=[gw[c].B, yc.b[c * 4 + tb]], writes=[gp.B], inc=(c == 3))
                    fw.act(lambda h, oc=oc, gp=gp: h.activation(out=sgl[oc][:], in_=gp[:], func=AF.Sigmoid,
                                                                bias=s5v[:, 4 + oc:5 + oc], scale=1.0),
                           reads=[gp.B, s5v.B], writes=[sgl[oc].B])
                for oc in range(4):
                    pgt = ps_y()
                    proj(pgt, wgs[oc], tok0)
                    sgt = wf[4]
                    silu_from_psum(sgt, pgt)
                    fw.dve(lambda h, oc=oc, sgt=sgt: h.tensor_tensor(out=sgt[:], in0=sgt[:], in1=sgl[oc][:], op=ALU.mult),
                           reads=[sgt.B, sgl[oc].B], writes=[sgt.B])
                    fw.dve(lambda h, oc=oc, sgt=sgt: h.tensor_tensor(out=yc[:, oc, tok0:tok0 + 512],
                                                                     in0=yc[:, oc, tok0:tok0 + 512], in1=sgt[:], op=ALU.mult),
                           reads=[sgt.B, yc.b[oc * 4 + tb]], writes=[yc.b[oc * 4 + tb]])

            for tb in range(LIM.get('tb', 4)):
                glu_block(tb)

        def wload_shift(l, c0, mu_src):
            s1 = wslot[wctr[0] % NSLOT]
            wctr[0] += 1
            s2 = wslot[wctr[0] % NSLOT]
            wctr[0] += 1
            stg = wst[wctr[1] % 2]
            wctr[1] += 1
            mub = wf[2]
            fw.dma("sp", mub[:, 0:128], mu_src.broadcast_to([128, 128]), writes=[mub.B])
            fw.dve(lambda h: h.tensor_scalar(out=mub[:, 128:256], in0=mub[:, 0:128], scalar1=-1.0, scalar2=1.0,
                                             op0=ALU.mult, op1=ALU.add), reads=[mub.B], writes=[mub.B])
            fw.dma("sp", stg[:], w_in_d[l, :, c0:c0 + 128].rearrange("(c p) n -> p c n", p=128), writes=[stg.B])
            fw.dve(lambda h: h.tensor_tensor(out=stg[:], in0=stg[:], in1=gexp[:], op=ALU.mult),
                   reads=[stg.B, gexp.B], writes=[stg.B])
            fw.dve(lambda h: h.tensor_tensor(out=s2[:], in0=stg[:], in1=mub[:, 0:128].unsqueeze(1).to_broadcast([128, 8, 128]),
                                             op=ALU.mult), reads=[stg.B, mub.B], writes=[s2.B])
            fw.dve(lambda h: h.tensor_tensor(out=s1[:], in0=stg[:], in1=mub[:, 128:256].unsqueeze(1).to_broadcast([128, 8, 128]),
                                             op=ALU.mult), reads=[stg.B, mub.B], writes=[s1.B])
            return s1, s2

        def proj_shift(ps, s12, tok0):
            proj(ps, s12[0], tok0, shift=0, start=True, stop=False)
            proj(ps, s12[1], tok0, shift=1, start=False, stop=True)

        def phaseA(si, l):
            CDEC = 0.6065306597126334
            ybf = yb[:].rearrange("p a s -> p (a s)")
            SB = [Buf(f"scrA{i}") for i in range(16)]
            SV = [ybf[:, i * 512:(i + 1) * 512] for i in range(16)]
            fw.dve(lambda h: h.memset(cst[:, 0:1], 0.0), reads=yb.b, writes=SB + [cst.B])
            QP = [SV[i] for i in range(4)]
            QPB = SB[0:4]
            MK1, MK1B = SV[4], SB[4]
            MK2, MK2B = SV[5][:, 0:256], SB[5]
            E1, E2, E3 = SV[6], SV[7], SV[8]
            XB = [SV[9], SV[10]]
            PP = SV[11]
            MISC = SV[12]
            kt_tok, bt_tok, vp0 = SV[13], SV[14], SV[15]
            vp1 = wh[9]
            v_bf, sqk, rk_bf, rt, at, kt, bt = wh[2], wh[3], wh[4], wh[5], wh[6], wh[7], wh[8]
            sgate, Pq, r32, wf6, wf7, wf0, wf1, wf2 = wf[3], wf[4], wf[5], wf[6], wf[7], wf[0], wf[1], wf[2]
            fw.dma("sp", MK1, cd["rw_masks"][:, 0:512], writes=[MK1B])
            fw.dma("sp", MK2, cd["rw_masks"][:, 512:768], writes=[MK2B])
            fw.dma("sp", rvec[:], rvec_d[l], writes=[rvec.B])
            fw.dve(lambda h: h.tensor_scalar(out=omka[:], in0=rvec[:, :, 3], scalar1=-1.0, scalar2=1.0, op0=ALU.mult,
                                             op1=ALU.add), reads=[rvec.B], writes=[omka.B])
            for i in range(4):
                fw.dve(lambda h, i=i: h.memset(QP[i], 0.0), writes=[QPB[i]])
            fw.dve(lambda h: h.memset(vp0, 0.0), writes=[SB[15]])
            fw.dve(lambda h: h.memset(vp1[:], 0.0), writes=[vp1.B])
            fw.dve(lambda h: h.memset(MISC, 0.0), writes=[SB[12]])
            fw.dve(lambda h: h.memset(lw2[:], 0.0), writes=[lw2.B])
            fw.dve(lambda h: h.memset(la2[:], 0.0), writes=[la2.B])
            w_wa = wload_shift(l, O_XW, mu_wa_d[l])
            for tb in range(4):
                pwa = ps_next()
                proj_shift(pwa, w_wa, tb * 512)
                dst = ya[:, 3, tb * 512:(tb + 1) * 512]
                fw.act(lambda h, pwa=pwa, dst=dst: h.activation(out=dst[0:64, :], in_=pwa[0:64, :], func=AF.Tanh),
                       reads=[pwa.B], writes=[ya.b[12 + tb]])
                fw.act(lambda h, pwa=pwa, dst=dst: h.activation(out=dst[64:128, :], in_=pwa[64:128, :], func=AF.Copy),
                       reads=[pwa.B], writes=[ya.b[12 + tb]])

            def do_hp(hp):
                V = lambda j: rvec[:, hp, j:j + 1]
                W0, A0, KK, KA, LNW, LNB, RK = (V(j) for j in range(7))
                w_r = wload_shift(l, O_AR + hp * 128, mu_rkv_d[l, 0:1, hp * 128:(hp + 1) * 128])
                w_k = wload_shift(l, O_AK + hp * 128, mu_rkv_d[l, 1:2, hp * 128:(hp + 1) * 128])
                w_v = wload_shift(l, O_AV + hp * 128, mu_rkv_d[l, 2:3, hp * 128:(hp + 1) * 128])
                w_g = wload(l, [(O_AG + hp * 128, 128)])
                stg = wst[wctr[1] % 2]
                wctr[1] += 1
                stv = stg[:].rearrange("p c n -> p (c n)")
                fw.dma("sp", stv[0:64, 0:128], w2a2_d[l, 0, :, hp * 128:(hp + 1) * 128], writes=[stg.B])
                fw.dma("sp", stv[64:128, 0:128], w2a2_d[l, 1, :, hp * 128:(hp + 1) * 128], writes=[stg.B])
                fw.dve(lambda h: h.tensor_copy(out=lw2[0:64, :], in_=stv[0:64, 0:128]), reads=[stg.B], writes=[lw2.B])
                fw.dve(lambda h: h.tensor_copy(out=la2[64:128, :], in_=stv[64:128, 0:128]), reads=[stg.B], writes=[la2.B])
                fw.dve(lambda h: h.memset(St32[:], 0.0), writes=[St32.B])
                fw.dve(lambda h: h.memset(Stb[:], 0.0), writes=[Stb.B])

                def do_block(tb):
                    tok0 = tb * 512
                    twa = ya[:, 3, tok0:tok0 + 512]
                    twab = ya.b[12 + tb]
                    pr, pk = ps_next(), ps_next()
                    proj_shift(pr, w_r, tok0)
                    proj_shift(pk, w_k, tok0)
                    fw.act(lambda h: h.activation(out=r32[:], in_=pr[:], func=AF.Copy), reads=[pr.B], writes=[r32.B])
                    pw_, pa_ = ps_next(), ps_next()
                    fw.pe(lambda h: h.matmul(out=pw_[:], lhsT=lw2[:], rhs=twa, start=True, stop=True),
                          reads=[lw2.B, twab], writes=[pw_.B])
                    fw.pe(lambda h: h.matmul(out=pa_[:], lhsT=la2[:], rhs=twa, start=True, stop=True),
                          reads=[la2.B, twab], writes=[pa_.B])
                    sg, aa = wf0, wf6
                    fw.act(lambda h: h.activation(out=sg[:], in_=pw_[:], func=AF.Sigmoid, bias=W0, scale=1.0),
                           reads=[pw_.B, rvec.B], writes=[sg.B])
                    fw.act(lambda h: h.activation(out=aa[:], in_=pa_[:], func=AF.Sigmoid, bias=A0, scale=1.0),
                           reads=[pa_.B, rvec.B], writes=[aa.B])
                    kkn = wf7
                    fw.dve(lambda h: h.tensor_scalar(out=kkn[:], in0=pk[:], scalar1=KK, scalar2=None, op0=ALU.mult),
                           reads=[pk.B, rvec.B], writes=[kkn.B])
                    fw.act(lambda h: h.activation(out=sqk[:], in_=kkn[:], func=AF.Square), reads=[kkn.B], writes=[sqk.B])
                    ss = ps_next()
                    fw.pe(lambda h: h.matmul(out=ss[:], lhsT=bdo64[:], rhs=sqk[:], start=True, stop=True),
                          reads=[bdo64.B, sqk.B], writes=[ss.B])
                    rn = wf1
                    fw.act(lambda h: h.activation(out=rn[:], in_=ss[:], func=AF.Sqrt, bias=1e-12, scale=64.0),
                           reads=[ss.B], writes=[rn.B])
                    fw.dve(lambda h: h.reciprocal(out=rn[:], in_=rn[:]), reads=[rn.B], writes=[rn.B])
                    fw.dve(lambda h: h.tensor_tensor(out=kkn[:], in0=kkn[:], in1=rn[:], op=ALU.mult),
                           reads=[kkn.B, rn.B], writes=[kkn.B])
                    k2 = wf2
                    fw.dve(lambda h: h.tensor_scalar(out=wf1[:], in0=aa[:], scalar1=KA, scalar2=omka[:, hp:hp + 1],
                                                     op0=ALU.mult, op1=ALU.add), reads=[aa.B, rvec.B, omka.B], writes=[wf1.B])
                    fw.dve(lambda h: h.tensor_tensor(out=k2[:], in0=pk[:], in1=wf1[:], op=ALU.mult),
                           reads=[pk.B, wf1.B], writes=[k2.B])
                    fw.dve(lambda h: h.scalar_tensor_tensor(out=rk_bf[:], in0=r32[:], scalar=RK, in1=k2[:], op0=ALU.mult,
                                                            op1=ALU.mult), reads=[r32.B, rvec.B, k2.B], writes=[rk_bf.B])
                    bb_ = wf1
                    fw.dve(lambda h: h.tensor_tensor(out=bb_[:], in0=kkn[:], in1=aa[:], op=ALU.mult),
                           reads=[kkn.B, aa.B], writes=[bb_.B])
                    pv = ps_next()
                    proj_shift(pv, w_v, tok0)
                    fw.act(lambda h: h.activation(out=v_bf[:], in_=pv[:], func=AF.Copy), reads=[pv.B], writes=[v_bf.B])
                    pgt = ps_next()
                    proj(pgt, w_g, tok0)
                    silu_from_psum(sgate, pgt)
                    cs = wf6
                    for c4 in range(4):
                        fw.dve(lambda h, c4=c4: h.tensor_tensor_scan(
                            out=cs[:, c4 * 128:(c4 + 1) * 128], data0=onec[:, 0:1].to_broadcast([128, 128]),
                            data1=sg[:, c4 * 128:(c4 + 1) * 128], initial=0.0, op0=ALU.mult, op1=ALU.add),
                            reads=[sg.B, onec.B], writes=[cs.B])
                    fw.dve(lambda h: h.tensor_tensor(out=sg[:], in0=cs[:], in1=sg[:], op=ALU.subtract),
                           reads=[cs.B, sg.B], writes=[sg.B])
                    fw.act(lambda h: h.activation(out=Pq[:], in_=cs[:], func=AF.Exp, scale=-CDEC), reads=[cs.B], writes=[Pq.B])
                    fw.act(lambda h: h.activation(out=sg[:], in_=sg[:], func=AF.Exp, scale=-CDEC), reads=[sg.B], writes=[sg.B])
                    fw.act(lambda h: h.activation(out=cs[:], in_=cs[:], func=AF.Exp, scale=CDEC), reads=[cs.B], writes=[cs.B])
                    PqA, Pk = sg, cs
                    fw.dve(lambda h: h.tensor_tensor(out=rt[:], in0=r32[:], in1=Pq[:], op=ALU.mult),
                           reads=[r32.B, Pq.B], writes=[rt.B])
                    fw.dve(lambda h: h.scalar_tensor_tensor(out=at[:], in0=kkn[:], scalar=-1.0, in1=PqA[:], op0=ALU.mult,
                                                            op1=ALU.mult), reads=[kkn.B, PqA.B], writes=[at.B])
                    fw.dve(lambda h: h.tensor_tensor(out=kt[:], in0=k2[:], in1=Pk[:], op=ALU.mult),
                           reads=[k2.B, Pk.B], writes=[kt.B])
                    fw.dve(lambda h: h.tensor_tensor(out=bt[:], in0=bb_[:], in1=Pk[:], op=ALU.mult),
                           reads=[bb_.B, Pk.B], writes=[bt.B])
                    for c4 in range(4):
                        for hh in range(2):
                            rows = slice(hh * 64, (hh + 1) * 64)
                            fw.act(lambda h, c4=c4, hh=hh, rows=rows: h.activation(
                                out=QP[c4][rows, (2 * hh) * 128:(2 * hh + 1) * 128], in_=at[rows, c4 * 128:(c4 + 1) * 128],
                                func=AF.Copy), reads=[at.B], writes=[QPB[c4]])
                            fw.act(lambda h, c4=c4, hh=hh, rows=rows: h.activation(
                                out=QP[c4][rows, (2 * hh + 1) * 128:(2 * hh + 2) * 128], in_=rt[rows, c4 * 128:(c4 + 1) * 128],
                                func=AF.Copy), reads=[rt.B], writes=[QPB[c4]])
                    tpa, tpb = tp_ps[0], tp_ps[1]
                    for c4 in range(4):
                        fw.pe(lambda h, c4=c4: h.transpose(out=tpa[:, c4, :], in_=kt[:, c4 * 128:(c4 + 1) * 128],
                                                           identity=ident[:]), reads=[kt.B, ident.B], writes=[tpa.B], inc=False)
                    for c4 in range(4):
                        fw.pe(lambda h, c4=c4: h.transpose(out=tpa[:, 4 + c4, :], in_=bt[:, c4 * 128:(c4 + 1) * 128],
                                                           identity=ident[:]), reads=[bt.B, ident.B], writes=[tpa.B],
                              inc=(c4 == 3))
                    for c4 in range(4):
                        fw.pe(lambda h, c4=c4: h.transpose(out=tpb[:, c4, :], in_=v_bf[:, c4 * 128:(c4 + 1) * 128],
                                                           identity=ident[:]), reads=[v_bf.B, ident.B], writes=[tpb.B],
                              inc=(c4 == 3))
                    v4 = lambda ap: ap.rearrange("p (c f) -> p c f", c=4)
                    fw.act(lambda h: h.activation(out=v4(kt_tok), in_=tpa[:, 0:4, :], func=AF.Copy),
                           reads=[tpa.B], writes=[SB[13]])
                    fw.dve(lambda h: h.tensor_copy(out=v4(bt_tok), in_=tpa[:, 4:8, :]), reads=[tpa.B], writes=[SB[14]])
                    fw.act(lambda h: h.activation(out=v4(vp0)[:, :, 0:64], in_=tpb[:, 0:4, 0:64], func=AF.Copy),
                           reads=[tpb.B], writes=[SB[15]])
                    fw.dve(lambda h: h.tensor_copy(out=v4(vp1[:])[:, :, 64:128], in_=tpb[:, 0:4, 64:128]),
                           reads=[tpb.B], writes=[vp1.B])
                    y_ps = ps_y()

                    def do_chunk(c4):
                        cs_ = slice(c4 * 128, (c4 + 1) * 128)
                        pa1, pa2, pa3 = ps_next(), ps_next(), ps_next()
                        fw.pe(lambda h: h.matmul(out=pa1[:], lhsT=kt[:, cs_], rhs=QP[c4], start=True, stop=True),
                              reads=[kt.B, QPB[c4]], writes=[pa1.B])
                        fw.pe(lambda h: h.matmul(out=pa2[:], lhsT=bt[:, cs_], rhs=QP[c4], start=True, stop=True),
                              reads=[bt.B, QPB[c4]], writes=[pa2.B])
                        for hh in range(2):
                            fw.pe(lambda h, hh=hh: h.matmul(out=pa3[:, hh * 128:(hh + 1) * 128],
                                                            lhsT=QP[c4][:, (2 * hh) * 128:(2 * hh + 1) * 128], rhs=bt[:, cs_],
                                                            start=True, stop=True),
                                  reads=[bt.B, QPB[c4]], writes=[pa3.B], inc=(hh == 1))
                        fw.dve(lambda h: h.tensor_tensor(out=E1, in0=pa1[:], in1=MK1, op=ALU.mult),
                               reads=[pa1.B, MK1B], writes=[SB[6]])
                        fw.dve(lambda h: h.tensor_tensor(out=E2, in0=pa2[:], in1=MK1, op=ALU.mult),
                               reads=[pa2.B, MK1B], writes=[SB[7]])
                        fw.dve(lambda h: h.tensor_tensor(out=E3[:, 0:256], in0=pa3[:, 0:256], in1=MK2, op=ALU.mult),
                               reads=[pa3.B, MK2B], writes=[SB[8]])

                        def Xj(j, hh):
                            if j == 0:
                                return E3[:, hh * 128:(hh + 1) * 128], SB[8]
                            return XB[j % 2][:, (2 * hh) * 128:(2 * hh + 1) * 128], SB[9 + j % 2]

                        def Bj(j, hh):
                            if j == 0:
                                return E2[:, (2 * hh) * 128:(2 * hh + 1) * 128], SB[7]
                            return XB[j % 2][:, (2 * hh + 1) * 128:(2 * hh + 2) * 128], SB[9 + j % 2]

                        def Pj(j, hh):
                            o = (j % 2) * 256 + hh * 128
                            return PP[:, o:o + 128]

                        e2v = E2.rearrange("p (a b) -> p a b", a=2)[:, :, 0:128]
                        fw.dve(lambda h: h.tensor_tensor(out=PP[:, 0:256].rearrange("p (a b) -> p a b", a=2), in0=e2v,
                                                         in1=ident[:].unsqueeze(1).to_broadcast([128, 2, 128]), op=ALU.add),
                               reads=[SB[7], ident.B], writes=[SB[11]])
                        for j in range(6):
                            last = j == 5
                            pxb = ps_next()
                            for hh in range(2):
                                xa_, xb_ = Xj(j, hh)
                                ba_, bb2 = Bj(j, hh)
                                fw.pe(lambda h, hh=hh, xa_=xa_, ba_=ba_, pxb=pxb: h.matmul(
                                    out=pxb[:, (2 * hh) * 128:(2 * hh + 1) * 128], lhsT=ba_, rhs=xa_, start=True, stop=True),
                                    reads=[xb_, bb2], writes=[pxb.B], inc=(last and hh == 1))
                                if not last:
                                    fw.pe(lambda h, hh=hh, xa_=xa_, ba_=ba_, pxb=pxb: h.matmul(
                                        out=pxb[:, (2 * hh + 1) * 128:(2 * hh + 2) * 128], lhsT=xa_, rhs=ba_, start=True,
                                        stop=True), reads=[xb_, bb2], writes=[pxb.B], inc=(hh == 1))
                            nxt = XB[(j + 1) % 2]
                            if last:
                                fw.act(lambda h, pxb=pxb, nxt=nxt: h.activation(
                                    out=nxt.rearrange("p (a b) -> p a b", a=2)[:, :, 0:128],
                                    in_=pxb[:].rearrange("p (a b) -> p a b", a=2)[:, :, 0:128], func=AF.Copy),
                                    reads=[pxb.B], writes=[SB[9 + (j + 1) % 2]])
                            else:
                                fw.act(lambda h, pxb=pxb, nxt=nxt: h.activation(out=nxt, in_=pxb[:], func=AF.Copy),
                                       reads=[pxb.B], writes=[SB[9 + (j + 1) % 2]])
                            pp = ps_next()
                            for hh in range(2):
                                xn_, xnb_ = Xj(j + 1, hh)
                                fw.pe(lambda h, hh=hh, pp=pp, j=j: h.matmul(out=pp[:, hh * 128:(hh + 1) * 128], lhsT=ident[:],
                                                                             rhs=Pj(j, hh), start=True, stop=False),
                                      reads=[ident.B, SB[11]], writes=[pp.B], inc=False)
                                fw.pe(lambda h, hh=hh, pp=pp, j=j, xn_=xn_: h.matmul(
                                    out=pp[:, hh * 128:(hh + 1) * 128], lhsT=xn_, rhs=Pj(j, hh), start=False, stop=True),
                                    reads=[xnb_, SB[11]], writes=[pp.B], inc=(hh == 1))
                            o = ((j + 1) % 2) * 256
                            fw.dve(lambda h, pp=pp, o=o: h.tensor_copy(out=PP[:, o:o + 256], in_=pp[:, 0:256]),
                                   reads=[pp.B], writes=[SB[11]])
                        r0 = ps_next()
                        fw.pe(lambda h: h.matmul(out=r0[:, 0:128], lhsT=at[:, cs_], rhs=Stb[:], start=True, stop=False),
                              reads=[at.B, Stb.B], writes=[r0.B], inc=False)
                        vps = [vp0, vp1[:]]
                        vpb = [SB[15], vp1.B]
                        for hh in range(2):
                            fw.pe(lambda h, hh=hh: h.matmul(out=r0[:, hh * 64:(hh + 1) * 64],
                                                            lhsT=E1[:, (2 * hh) * 128:(2 * hh + 1) * 128],
                                                            rhs=vps[hh][:, c4 * 128 + hh * 64:c4 * 128 + (hh + 1) * 64],
                                                            start=False, stop=(hh == 1)),
                                  reads=[SB[6], vpb[hh]], writes=[r0.B], inc=(hh == 1))
                        r0b = MISC[:, 0:128]
                        fw.act(lambda h: h.activation(out=r0b, in_=r0[:, 0:128], func=AF.Copy), reads=[r0.B], writes=[SB[12]])
                        up = ps_next()
                        for hh in range(2):
                            fw.pe(lambda h, hh=hh: h.matmul(out=up[:, hh * 64:(hh + 1) * 64], lhsT=Pj(6, hh),
                                                            rhs=r0b[:, hh * 64:(hh + 1) * 64], start=True, stop=True),
                                  reads=[SB[11], SB[12]], writes=[up.B], inc=(hh == 1))
                        upad = [MISC[:, 128:256], MISC[:, 256:384]]
                        fw.act(lambda h: h.activation(out=upad[0][:, 0:64], in_=up[:, 0:64], func=AF.Copy),
                               reads=[up.B], writes=[SB[12]])
                        fw.dve(lambda h: h.tensor_copy(out=upad[1][:, 64:128], in_=up[:, 64:128]),
                               reads=[up.B], writes=[SB[12]])
                        fw.pe(lambda h: h.matmul(out=y_ps[:, cs_], lhsT=Stb[:], rhs=rt[:, cs_], start=True, stop=False),
                              reads=[Stb.B, rt.B], writes=[y_ps.B], inc=False)
                        for hh in range(2):
                            fw.pe(lambda h, hh=hh: h.matmul(out=y_ps[:, cs_], lhsT=upad[hh],
                                                            rhs=E2[:, (2 * hh + 1) * 128:(2 * hh + 2) * 128],
                                                            start=False, stop=False),
                                  reads=[SB[12], SB[7]], writes=[y_ps.B], inc=False)
                            fw.pe(lambda h, hh=hh: h.matmul(out=y_ps[:, cs_], lhsT=vps[hh][:, c4 * 128:(c4 + 1) * 128],
                                                            rhs=E1[:, (2 * hh + 1) * 128:(2 * hh + 2) * 128],
                                                            start=False, stop=(hh == 1)),
                                  reads=[vpb[hh], SB[6]], writes=[y_ps.B], inc=(hh == 1))
                        su = ps_next()
                        for hh in range(2):
                            fw.pe(lambda h, hh=hh: h.matmul(out=su[:, 0:128], lhsT=bt_tok[:, cs_], rhs=upad[hh],
                                                            start=(hh == 0), stop=False),
                                  reads=[SB[14], SB[12]], writes=[su.B], inc=False)
                        for hh in range(2):
                            fw.pe(lambda h, hh=hh: h.matmul(out=su[:, 0:128], lhsT=kt_tok[:, cs_],
                                                            rhs=vps[hh][:, c4 * 128:(c4 + 1) * 128],
                                                            start=False, stop=(hh == 1)),
                                  reads=[SB[13], vpb[hh]], writes=[su.B], inc=(hh == 1))
                        fw.dve(lambda h: h.tensor_tensor(out=St32[:], in0=su[:, 0:128], in1=St32[:], op=ALU.add),
                               reads=[su.B, St32.B], writes=[St32.B])
                        pend = Pq[:, c4 * 128 + 127:c4 * 128 + 128]
                        fw.dve(lambda h: h.tensor_scalar(out=St32[:], in0=St32[:], scalar1=pend, scalar2=None, op0=ALU.mult),
                               reads=[St32.B, Pq.B], writes=[St32.B])
                        fw.dve(lambda h: h.tensor_tensor(out=Stb[:], in0=St32[:], in1=bd32[:], op=ALU.mult),
                               reads=[St32.B, bd32.B], writes=[Stb.B])

                    for c4 in range(LIM.get('c4', 4)):
                        do_chunk(c4)
                    bs = ps_next()
                    fw.pe(lambda h: h.matmul(out=bs[:], lhsT=bdo64[:], rhs=rk_bf[:], start=True, stop=True),
                          reads=[bdo64.B, rk_bf.B], writes=[bs.B])
                    bon = wf7
                    fw.dve(lambda h: h.scalar_tensor_tensor(out=bon[:], in0=bs[:], scalar=64.0, in1=v_bf[:], op0=ALU.mult,
                                                            op1=ALU.mult), reads=[bs.B, v_bf.B], writes=[bon.B])

                    def affine(ycen):
                        fw.dve(lambda h: h.tensor_scalar(out=ycen[:], in0=ycen[:], scalar1=LNW, scalar2=LNB, op0=ALU.mult,
                                                         op1=ALU.add), reads=[ycen.B, rvec.B], writes=[ycen.B])
                        fw.dve(lambda h: h.tensor_tensor(out=ycen[:], in0=ycen[:], in1=bon[:], op=ALU.add),
                               reads=[ycen.B, bon.B], writes=[ycen.B])

                    headnorm_gate(y_ps, sgate, ya[:, hp, tok0:tok0 + 512], ya.b[hp * 4 + tb], 64e-5, affine=affine)

                for tb in range(LIM.get('tb', 4)):
                    do_block(tb)

            for hp in range(LIM.get('hp', 4)):
                do_hp(hp)
            fw.dve(lambda h: h.memset(cst[:, 0:1], 0.0), reads=SB, writes=yb.b + [cst.B])

        def wload_plain(src_ap, nchunk):
            slot = wslot[wctr[0] % NSLOT]
            wctr[0] += 1
            stg = wst[wctr[1] % 2]
            wctr[1] += 1
            fw.dma("sp", stg[:, 0:nchunk, :], src_ap, writes=[stg.B])
            fw.dve(lambda h: h.tensor_copy(out=slot[:, 0:nchunk, :], in_=stg[:, 0:nchunk, :]),
                   reads=[stg.B], writes=[slot.B])
            return slot

        def phaseM(si, l):
            fw.dma("sp", gpost[:], postn_d[l:l + 1, :].broadcast_to([128, D]), writes=[gpost.B])
            ybr = [ya, yb, yc]
            gofs = [O_GA, O_GB, O_GC]
            mT = wh[0:8]
            sig, tt_, macc = wf[0], wf[1], wf[2]

            def do_block(tb):
                tok0 = tb * 512

                def do_oc(oc):
                    for br in range(3):
                        if br not in LIM.get("branches", (0, 1, 2)):
                            continue
                        first = br == min(LIM.get("branches", (0, 1, 2)))
                        last = br == max(LIM.get("branches", (0, 1, 2)))
                        wg = wload(l, [(gofs[br] + oc * 128, 128)])
                        gl = ps_next()
                        proj(gl, wg, tok0)
                        fw.act(lambda h, gl=gl, br=br: h.activation(out=sig[:], in_=gl[:], func=AF.Sigmoid,
                                                                    bias=bmt[:, l, br, oc:oc + 1], scale=1.0),
                               reads=[gl.B, bmt.B], writes=[sig.B])
                        wp = wload_plain(wp_d[br][l, :, oc * 128:(oc + 1) * 128].rearrange("(c p) n -> p c n", p=128), 4)
                        pb = ps_next()
                        for c in range(4):
                            fw.pe(lambda h, c=c, pb=pb, wp=wp, br=br: h.matmul(
                                out=pb[:], lhsT=wp[:, c, :], rhs=ybr[br][:, c, tok0:tok0 + 512],
                                start=(c == 0), stop=(c == 3)),
                                reads=[wp.B, ybr[br].b[c * 4 + tb]], writes=[pb.B], inc=(c == 3))
                        if first and last:
                            fw.dve(lambda h, pb=pb: h.tensor_tensor(out=mT[oc][:], in0=pb[:], in1=sig[:], op=ALU.mult),
                                   reads=[pb.B, sig.B], writes=[mT[oc].B])
                        elif first:
                            fw.dve(lambda h, pb=pb: h.tensor_tensor(out=macc[:], in0=pb[:], in1=sig[:], op=ALU.mult),
                                   reads=[pb.B, sig.B], writes=[macc.B])
                        else:
                            fw.dve(lambda h, pb=pb: h.tensor_tensor(out=tt_[:], in0=pb[:], in1=sig[:], op=ALU.mult),
                                   reads=[pb.B, sig.B], writes=[tt_.B])
                            dst = mT[oc] if last else macc
                            fw.dve(lambda h, dst=dst: h.tensor_tensor(out=dst[:], in0=macc[:], in1=tt_[:], op=ALU.add),
                                   reads=[macc.B, tt_.B], writes=[dst.B])

                for oc in range(8):
                    do_oc(oc)

                def do_pair(k):
                    banks = [pg[0], pg[1], pg[2], pg[3]]
                    for oc in range(8):
                        wo = wload_plain(wout_d[l, oc * 128:(oc + 1) * 128, :].rearrange("p (c n) -> p c n", c=8), 8)
                        wov = wo[:].rearrange("p c n -> p (c n)")
                        for t in range(2):
                            for half in range(2):
                                bk = banks[t * 2 + half]
                                fw.pe(lambda h, oc=oc, t=t, half=half, bk=bk, wov=wov: h.matmul(
                                    out=bk[:], lhsT=mT[oc][:, (2 * k + t) * 128:(2 * k + t + 1) * 128],
                                    rhs=wov[:, half * 512:(half + 1) * 512], start=(oc == 0), stop=(oc == 7)),
                                    reads=[mT[oc].B, wo.B], writes=[bk.B], inc=(oc == 7 or (t == 1 and half == 1)))
                    for t in range(2):
                        tile_i = tb * 4 + 2 * k + t
                        for half in range(2):
                            bk = banks[t * 2 + half]
                            fw.dve(lambda h, half=half, bk=bk: h.bn_stats(out=st6[:, half, :], in_=bk[:]),
                                   reads=[bk.B], writes=[st6.B])
                        fw.dve(lambda h: h.bn_aggr(out=mv[:], in_=st6[:].rearrange("p a b -> p (a b)")),
                               reads=[st6.B], writes=[mv.B])
                        fw.dve(lambda h: h.scalar_tensor_tensor(out=e2[:], in0=mv[:, 0:1], scalar=mv[:, 0:1],
                                                                in1=mv[:, 1:2], op0=ALU.mult, op1=ALU.add),
                               reads=[mv.B], writes=[e2.B])
                        fw.act(lambda h: h.activation(out=e2[:], in_=e2[:], func=AF.Sqrt, bias=EPS, scale=1.0),
                               reads=[e2.B], writes=[e2.B])
                        fw.dve(lambda h: h.reciprocal(out=rstd[:], in_=e2[:]), reads=[e2.B], writes=[rstd.B])
                        for half in range(2):
                            bk = banks[t * 2 + half]
                            hs = slice(half * 512, (half + 1) * 512)
                            fw.dve(lambda h, bk=bk, hs=hs: h.scalar_tensor_tensor(
                                out=wf[3][:], in0=bk[:], scalar=rstd[:, 0:1], in1=gpost[:, hs],
                                op0=ALU.mult, op1=ALU.mult), reads=[bk.B, rstd.B, gpost.B], writes=[wf[3].B])
                            fw.dve(lambda h, hs=hs, tile_i=tile_i: h.tensor_tensor(
                                out=x_sb[:, tile_i, hs], in0=x_sb[:, tile_i, hs], in1=wf[3][:], op=ALU.add),
                                reads=[x_sb.b[tile_i], wf[3].B], writes=[x_sb.b[tile_i]])

                for k in range(2):
                    do_pair(k)

            for tb in range(LIM.get('tb', 4)):
                do_block(tb)

        for si in range(nseq):
            for tq in range(NT // 4):
                fw.dma("sp", x_sb[:, tq * 4:(tq + 1) * 4, :],
                       x_d[si, tq * 512:(tq + 1) * 512, :].rearrange("(t p) d -> p t d", p=128),
                       writes=[x_sb.b[tq * 4 + i] for i in range(4)])
            for l in range(nlayers):
                if l == 1 and "l2phases" in LIM:
                    phases = LIM["l2phases"]
                if not LIM.get('nosetup'):
                    layer_setup(l)
                phase0(si, l)
                if "hT" in debug and si == 0 and l == 0:
                    fw.dma("sp", dbg_d["hT"], hT[:], reads=hT.b)
                if "C" in phases:
                    phaseC(si, l)
                    if "yc" in debug and si == 0 and l == 0:
                        fw.dma("sp", dbg_d["yc"], yc[:], reads=yc.b)
                if "A" in phases:
                    phaseA(si, l)
                    if "ya" in debug and si == 0 and l == 0:
                        fw.dma("sp", dbg_d["ya"], ya[:], reads=ya.b)
                if "B" in phases:
                    phaseB(si, l)
                    if "yb" in debug and si == 0 and l == 0:
                        fw.dma("sp", dbg_d["yb"], yb[:], reads=yb.b)
                if "M" in phases:
                    phaseM(si, l)
            for tq in range(NT // 4):
                fw.dma("sp", out_d[si, tq * 512:(tq + 1) * 512, :].rearrange("(t p) d -> p t d", p=128),
                       x_sb[:, tq * 4:(tq + 1) * 4, :],
                       reads=[x_sb.b[tq * 4 + i] for i in range(4)])
        allb = x_sb.b + hT.b + ya.b + yb.b + yc.b
        fw.wait_all("sp", allb)
        fw.emit()
        print("instr counts:", {k: v.n for k, v in fw.eng.items()})
    return nc


def make_shared(inputs):
    f = lambda k: np.ascontiguousarray(np.asarray(inputs[k], dtype=np.float32))
    shared = dict(make_consts())
    shared["w_in"] = f("w_in")
    shared["pre_norm"] = np.ascontiguousarray(f("pre_norm").reshape(DEPTH, 8, 128).transpose(0, 2, 1))
    for n in ("w_proj_rwkv", "w_proj_ret", "w_proj_s5", "w_out", "post_norm"):
        shared[n] = f(n)
    shared["b_merge"] = np.ascontiguousarray(f("b_merge").reshape(DEPTH, 3, 8, 128).transpose(0, 3, 1, 2))
    shared["rwkv_mu_rkv"] = f("rwkv_mu_rkv")
    shared["rwkv_mu_wa"] = np.ascontiguousarray(f("rwkv_mu_wa").reshape(DEPTH, 1, 128))
    shared["rwkv_w2a2"] = np.ascontiguousarray(np.stack([f("rwkv_w2"), f("rwkv_a2")], axis=1))
    vec = np.zeros((DEPTH, 8, 512), np.float32)
    for j, n in enumerate(("rwkv_w0", "rwkv_a0", "rwkv_k_k", "rwkv_k_a", "rwkv_ln_w", "rwkv_ln_b")):
        vec[:, j] = f(n)
    vec[:, 6] = f("rwkv_r_k").reshape(DEPTH, 512)
    shared["rwkv_vec"] = np.ascontiguousarray(vec.reshape(DEPTH, 8, 4, 128).transpose(0, 3, 2, 1))
    dup = lambda a: np.concatenate([a, a], axis=1)
    a_re = dup(f("s5_A_re").transpose(0, 2, 1))
    a_im = dup(f("s5_A_im").transpose(0, 2, 1))
    ldt = np.broadcast_to(f("s5_log_dt")[:, None, :], (DEPTH, 128, 32))
    shared["s5_Aab"] = np.ascontiguousarray(np.stack([a_re, a_im, ldt], axis=1))
    b_re = dup(f("s5_B_re").transpose(0, 2, 1, 3).reshape(DEPTH, 64, 512))
    b_im = dup(f("s5_B_im").transpose(0, 2, 1, 3).reshape(DEPTH, 64, 512))
    shared["s5_Bst"] = np.ascontiguousarray(np.stack([b_re, b_im], axis=1))
    c_re = f("s5_C_re").transpose(0, 3, 1, 2).reshape(DEPTH, 64, 512)
    c_im = f("s5_C_im").transpose(0, 3, 1, 2).reshape(DEPTH, 64, 512)
    ca = np.concatenate([c_re, c_im], axis=1)
    cb = np.concatenate([c_im, c_re], axis=1)
    shared["s5_Cst"] = np.ascontiguousarray(np.stack([ca, cb], axis=1))
    dv = f("s5_D").reshape(DEPTH, 4, 128).transpose(0, 2, 1)
    gb = f("s5_glu_b").reshape(DEPTH, 4, 128).transpose(0, 2, 1)
    shared["s5_vec"] = np.ascontiguousarray(np.concatenate([dv, gb], axis=2))
    shared["s5_glu_w"] = f("s5_glu_w")
    return shared


def make_inputs(inputs, s0, n):
    m = make_shared(inputs)
    m["x"] = np.ascontiguousarray(np.asarray(inputs["x"], dtype=np.float32)[s0:s0 + n])
    return m


def kernel(**inputs):
    ncores = 8
    x = np.ascontiguousarray(np.asarray(inputs["x"], dtype=np.float32))
    shared = make_shared(inputs)
    nlaunch = N_LAUNCH
    per = NSEQ // nlaunch
    nc = build_program(nseq=per)
    out = np.zeros_like(x)
    for j in range(nlaunch):
        in_maps = []
        for c in range(ncores):
            m = dict(shared)
            s0 = c * NSEQ + j * per
            m["x"] = x[s0:s0 + per]
            in_maps.append(m)
        res = run_bass_kernel_spmd(nc, in_maps, core_ids=list(range(ncores)))
        for c in range(ncores):
            s0 = c * NSEQ + j * per
            out[s0:s0 + per] = np.asarray(res.results[c]["out"])
    return out.astype(np.float32)
```

```python
import contextlib
import numpy as np
import ml_dtypes
import concourse.bass as bass
import concourse.mybir as mybir
from concourse.bass_utils import run_bass_kernel_spmd

F32 = mybir.dt.float32
BF16 = mybir.dt.bfloat16
AF = mybir.ActivationFunctionType
ALU = mybir.AluOpType
AX = mybir.AxisListType

D = 1024
S = 2048
DEPTH = 2
NSEQ = 2
D_IN = 8320
EPS = 1e-6
NT = S // 128

O_AR, O_AK, O_AV, O_XW, O_XA, O_AG = 0, 512, 1024, 1536, 1600, 1664
O_BQ, O_BK, O_BV, O_BG = 2176, 2688, 3200, 3712
O_CU, O_CG = 4224, 4736
O_GA, O_GB, O_GC = 5248, 6272, 7296


class Buf:
    def __init__(self, name):
        self.name = name
        self.last_write = None
        self.reads = []


class Engine:
    EPOCH = 30000

    def __init__(self, fw, name):
        self.fw = fw
        self.name = name
        self.sems = []
        self.count = 0
        self.waited = {}
        self.ops = []
        self.n = 0
        self._new_sem()

    def _new_sem(self):
        s = self.fw.stack.enter_context(self.fw.nc.semaphore(f"s_{self.name}_{len(self.sems)}"))
        self.sems.append(s)
        self.count = 0

    def need(self, ev):
        sem, val = ev
        key = id(sem)
        if self.waited.get(key, 0) >= val:
            return None
        self.waited[key] = val
        return ev


class Fw:
    def __init__(self, nc, stack):
        self.nc = nc
        self.stack = stack
        self.eng = {n: Engine(self, n) for n in ("pe", "act", "dve", "pool", "sp")}
        self.dsem = {}
        for q, k in (("sp", 12), ("act", 6), ("pool", 6)):
            self.dsem[q] = [[stack.enter_context(nc.semaphore(f"d_{q}_{i}")), 0] for i in range(k)]
        self.dnext = {q: 0 for q in self.dsem}

    def _deps(self, e, reads, writes):
        evs = []
        for b in reads:
            if b.last_write is not None:
                evs.append(b.last_write)
        for b in writes:
            if b.last_write is not None:
                evs.append(b.last_write)
            evs.extend(b.reads)
        out = []
        for ev in evs:
            if e.name == "pe" and any(ev[0] is s_ for s_ in e.sems):
                continue
            ev2 = e.need(ev)
            if ev2 is not None:
                out.append(ev2)
        return out

    def op(self, engname, fn, reads=(), writes=(), inc=True):
        e = self.eng[engname]
        waits = self._deps(e, reads, writes)
        if e.count >= Engine.EPOCH and inc:
            e._new_sem()
        sem = e.sems[-1]
        if inc:
            e.count += 1
        ev = (sem, e.count if inc else e.count + 1)
        for b in reads:
            b.reads.append(ev)
        for b in writes:
            b.last_write = ev
            b.reads = []
        e.n += 1

        def run(h, waits=waits, fn=fn, sem=sem, inc=inc):
            for (s, v) in waits[1:]:
                h.wait_ge(s, v)
            ins = fn(h)
            if waits:
                ins._wait_ge(waits[0][0], waits[0][1])
            if inc:
                ins.then_inc(sem, 1)

        e.ops.append(run)
        return ev

    def dma(self, q, out, in_, reads=(), writes=()):
        e = self.eng[q]
        waits = self._deps(e, reads, writes)
        slots = self.dsem[q]
        i = self.dnext[q]
        self.dnext[q] = (i + 1) % len(slots)
        slot = slots[i]
        sem = slot[0]
        prev = slot[1]
        if prev > 0:
            w = e.need((sem, prev))
            if w is not None:
                waits.append(w)
        slot[1] = prev + 16
        ev = (sem, slot[1])
        for b in reads:
            b.reads.append(ev)
        for b in writes:
            b.last_write = ev
            b.reads = []

        def run(h, waits=waits, sem=sem, out=out, in_=in_):
            for (s, v) in waits:
                h.wait_ge(s, v)
            h.dma_start(out=out, in_=in_).then_inc(sem, 16)

        e.ops.append(run)
        return ev

    def wait_all(self, engname, bufs):
        e = self.eng[engname]
        waits = []
        for b in bufs:
            for ev in ([b.last_write] if b.last_write else []) + list(b.reads):
                w = e.need(ev)
                if w is not None:
                    waits.append(w)

        def run(h, waits=waits):
            for (s, v) in waits:
                h.wait_ge(s, v)

        e.ops.append(run)

    def pe(self, fn, reads=(), writes=(), inc=True):
        return self.op("pe", fn, reads, writes, inc)

    def act(self, fn, reads=(), writes=()):
        return self.op("act", fn, reads, writes)

    def dve(self, fn, reads=(), writes=()):
        return self.op("dve", fn, reads, writes)

    def pool(self, fn, reads=(), writes=()):
        return self.op("pool", fn, reads, writes)

    def emit(self):
        nc = self.nc
        with nc.Block() as block:
            @block.tensor
            def _(h):
                for f in self.eng["pe"].ops:
                    f(h)

            @block.scalar
            def _(h):
                for f in self.eng["act"].ops:
                    f(h)

            @block.vector
            def _(h):
                for f in self.eng["dve"].ops:
                    f(h)

            @block.gpsimd
            def _(h):
                for f in self.eng["pool"].ops:
                    f(h)

            @block.sync
            def _(h):
                for f in self.eng["sp"].ops:
                    f(h)


class T:
    def __init__(self, fw, shape, dtype, name, psum=False, nsub=1):
        nc = fw.nc
        if psum:
            self.t = fw.stack.enter_context(nc.psum_tensor("ps_" + name, shape, dtype))
        else:
            self.t = fw.stack.enter_context(nc.sbuf_tensor("sb_" + name, shape, dtype))
        self.b = [Buf(f"{name}.{i}") for i in range(nsub)]
        self.name = name

    def __getitem__(self, idx):
        return self.t[idx]

    @property
    def B(self):
        return self.b[0]


def make_consts():
    c = {}
    c["ident"] = np.eye(128, dtype=np.float32).astype(ml_dtypes.bfloat16)
    bd = np.zeros((128, 128), np.float32)
    bd[:64, :64] = 1.0
    bd[64:, 64:] = 1.0
    c["bd32"] = bd
    c["bdo64"] = (bd / 64.0).astype(ml_dtypes.bfloat16)
    half = 32
    inv = (np.float32(10000.0) ** (-np.arange(half, dtype=np.float32) / np.float32(half))).astype(np.float32)
    pos = np.arange(S, dtype=np.float32)
    ang = (pos[None, :] * inv[:, None]).astype(np.float32).astype(np.float64)
    cos32, sin32 = np.cos(ang), np.sin(ang)
    cosT = np.zeros((128, S), np.float32)
    sinS = np.zeros((128, S), np.float32)
    for p in range(128):
        d = p % 64
        i = d % 32
        cosT[p] = cos32[i]
        sinS[p] = -sin32[i] if d < 32 else sin32[i]
    c["rope_cos"] = cosT
    c["rope_sin"] = sinS
    lg = np.log(1.0 - 2.0 ** (-5.0 - np.arange(8, dtype=np.float64)))
    idx = np.arange(128, dtype=np.float64)
    dmT = np.zeros((4, 128, 256), np.float32)
    kwt = np.zeros((4, 128, 128), np.float32)
    qw = np.zeros((4, 128, 128), np.float32)
    gc = np.zeros((128, 4), np.float32)
    for hp in range(4):
        for hh in range(2):
            g = lg[hp * 2 + hh]
            diff = idx[None, :] - idx[:, None]
            m = np.where(diff >= 0, np.exp(g * np.maximum(diff, 0.0)), 0.0) / 8.0
            dmT[hp, :, hh * 128:(hh + 1) * 128] = m
            kwt[hp, :, hh * 64:(hh + 1) * 64] = (np.exp(g * (127.0 - idx)) / 8.0)[:, None]
            qw[hp, hh * 64:(hh + 1) * 64, :] = np.exp(g * (idx + 1.0))[None, :]
            gc[hh * 64:(hh + 1) * 64, hp] = np.exp(g * 128.0)
    c["ret_dmT"] = dmT
    c["ret_kwt"] = kwt
    c["ret_qw"] = qw
    c["ret_gc"] = gc
    sw = np.zeros((128, 128), np.float32)
    for k in range(128):
        sw[k, (k + 64) % 128] = 1.0
    c["swapb"] = sw.astype(ml_dtypes.bfloat16)
    sg = np.zeros((128, 2), np.float32)
    sg[:64, 0], sg[64:, 0] = -1.0, 1.0
    sg[:64, 1], sg[64:, 1] = 1.0, -1.0
    c["sgn"] = sg
    rm = np.zeros((128, 8), np.float32)
    for p in range(128):
        rm[p, p // 16] = 1.0
    c["rowmask"] = rm
    ii = np.arange(128)
    su = (ii[:, None] < ii[None, :]).astype(np.float32)
    iu = (ii[:, None] <= ii[None, :]).astype(np.float32)
    sl_ = (ii[None, :] < ii[:, None]).astype(np.float32)
    c["rw_masks"] = np.concatenate([su, iu, su, iu, sl_, sl_], axis=1).astype(ml_dtypes.bfloat16)
    return c


CONST_SPECS = {
    "ident": ([128, 128], BF16), "bd32": ([128, 128], F32), "bdo64": ([128, 128], BF16),
    "rope_cos": ([128, S], F32), "rope_sin": ([128, S], F32),
    "ret_dmT": ([4, 128, 256], F32), "ret_kwt": ([4, 128, 128], F32), "ret_qw": ([4, 128, 128], F32),
    "ret_gc": ([128, 4], F32),
    "rw_masks": ([128, 768], BF16),
    "swapb": ([128, 128], BF16), "sgn": ([128, 2], F32), "rowmask": ([128, 8], F32),
}


LIM = {}
WENG = "dve"
N_LAUNCH = 1


def build_program(nlayers=DEPTH, nseq=NSEQ, debug=None, phases="0CABM"):
    debug = debug or {}
    nc = bass.Bass("TRN2", target_bir_lowering=False)
    dr = {}

    def din(name, shape, dt=F32):
        dr[name] = nc.dram_tensor(name, list(shape), dt, kind="ExternalInput").ap()
        return dr[name]

    x_d = din("x", [nseq, S, D])
    pre_norm_d = din("pre_norm", [DEPTH, 128, 8])
    w_in_d = din("w_in", [DEPTH, D, D_IN])
    cd = {k: din(k, shp, dt) for k, (shp, dt) in CONST_SPECS.items()}
    wp_d = [din(n, [DEPTH, 512, D]) for n in ("w_proj_rwkv", "w_proj_ret", "w_proj_s5")]
    wout_d = din("w_out", [DEPTH, D, D])
    bmerge_d = din("b_merge", [DEPTH, 128, 3, 8])
    postn_d = din("post_norm", [DEPTH, D])
    s5A_d = din("s5_Aab", [DEPTH, 3, 128, 32])
    s5B_d = din("s5_Bst", [DEPTH, 2, 128, 512])
    s5C_d = din("s5_Cst", [DEPTH, 2, 128, 512])
    s5v_d = din("s5_vec", [DEPTH, 128, 8])
    gluw_d = din("s5_glu_w", [DEPTH, 512, 512])
    mu_rkv_d = din("rwkv_mu_rkv", [DEPTH, 3, 512])
    mu_wa_d = din("rwkv_mu_wa", [DEPTH, 1, 128])
    w2a2_d = din("rwkv_w2a2", [DEPTH, 2, 64, 512])
    rvec_d = din("rwkv_vec", [DEPTH, 128, 4, 8])
    out_d = nc.dram_tensor("out", [nseq, S, D], F32, kind="ExternalOutput").ap()
    wcache_d = nc.dram_tensor("wcache", [DEPTH, 56, 128, 1024], BF16).ap()
    dbg_d = {}
    for k, shp in debug.items():
        dbg_d[k] = nc.dram_tensor("dbg_" + k, list(shp[0]), shp[1], kind="ExternalOutput").ap()

    with contextlib.ExitStack() as stack:
        fw = Fw(nc, stack)
        x_sb = T(fw, [128, NT, D], F32, "x_sb", nsub=NT)
        hT = T(fw, [128, 8, S + 1], BF16, "hT", nsub=NT + 1)
        ya = T(fw, [128, 4, S], BF16, "ya", nsub=16)
        yb = T(fw, [128, 4, S], BF16, "yb", nsub=16)
        yc = T(fw, [128, 4, S], BF16, "yc", nsub=16)
        ident = T(fw, [128, 128], BF16, "ident")
        bd32 = T(fw, [128, 128], F32, "bd32")
        bdo64 = T(fw, [128, 128], BF16, "bdo64")
        gpre = T(fw, [128, DEPTH, 8], F32, "gpre")
        gexp = T(fw, [128, 8, 128], F32, "gexp")
        NF, NH = 8, 12
        wf = [T(fw, [128, 512], F32, f"wf{i}") for i in range(NF)]
        wh = [T(fw, [128, 512], BF16, f"wh{i}") for i in range(NH)]
        st6 = T(fw, [128, 2, 6], F32, "st6")
        mv = T(fw, [128, 2], F32, "mv")
        e2 = T(fw, [128, 1], F32, "e2")
        rstd = T(fw, [128, 1], F32, "rstd")
        R32t = T(fw, [128, 128], F32, "R32t")
        Rbt = T(fw, [128, 128], BF16, "Rbt")
        gct = T(fw, [128, 4], F32, "gct")
        bmt = T(fw, [128, DEPTH, 3, 8], F32, "bmt")
        swapb = T(fw, [128, 128], BF16, "swapb")
        chl = T(fw, [128, 32], BF16, "chl")
        cbk = T(fw, [128, 8], F32, "cbk")
        sgn = T(fw, [128, 2], F32, "sgn")
        rowmask = T(fw, [128, 8], F32, "rowmask")
        s5v = T(fw, [128, 8], F32, "s5v")
        carry = T(fw, [128, 8], F32, "carry")
        cst = T(fw, [128, 16], F32, "cst")
        onec = T(fw, [128, 1], F32, "onec")
        rvec = T(fw, [128, 4, 8], F32, "rvec")
        omka = T(fw, [128, 4], F32, "omka")
        lw2 = T(fw, [128, 128], BF16, "lw2")
        la2 = T(fw, [128, 128], BF16, "la2")
        St32, Stb = R32t, Rbt
        gpost = T(fw, [128, D], F32, "gpost")
        NSLOT = 7
        wslot = [T(fw, [128, 8, 128], BF16, f"wslot{i}") for i in range(NSLOT)]
        wst = [T(fw, [128, 8, 128], F32, f"wst{i}") for i in range(2)]
        wctr = [0, 0]
        tp_ps = [T(fw, [128, 8, 128], BF16, f"tp{i}", psum=True) for i in range(2)]
        pg = [T(fw, [128, 512], F32, f"pg{i}", psum=True) for i in range(6)]
        pctr = [0]

        def ps_next():
            t = pg[pctr[0] % 4]
            pctr[0] += 1
            return t

        yctr = [0]

        def ps_y():
            t = pg[4 + yctr[0] % 2]
            yctr[0] += 1
            return t

        fw.dma("sp", ident[:], cd["ident"], writes=[ident.B])
        fw.dma("sp", bd32[:], cd["bd32"], writes=[bd32.B])
        fw.dma("sp", bdo64[:], cd["bdo64"], writes=[bdo64.B])
        fw.dma("sp", gpre[:], pre_norm_d.rearrange("l p c -> p l c"), writes=[gpre.B])
        fw.dma("sp", bmt[:], bmerge_d.rearrange("l p b c -> p l b c"), writes=[bmt.B])
        fw.dma("sp", swapb[:], cd["swapb"], writes=[swapb.B])
        fw.dma("sp", sgn[:], cd["sgn"], writes=[sgn.B])
        fw.dma("sp", rowmask[:], cd["rowmask"], writes=[rowmask.B])
        fw.pool(lambda h: h.memset(hT[:, :, 0:1], 0.0), writes=[hT.b[NT]])
        fw.dve(lambda h: h.memset(onec[:], 1.0), writes=[onec.B])

        def hT_bufs(tok0, ntok, shift=0):
            a = tok0 - shift
            bl = []
            if a < 0:
                bl.append(hT.b[NT])
                a = 0
            for tt in range(a // 128, (tok0 - shift + ntok - 1) // 128 + 1):
                bl.append(hT.b[tt])
            return bl

        def layer_setup(l):
            for c in range(8):
                fw.dve(lambda h, c=c: h.tensor_copy(out=gexp[:, c, :], in_=gpre[:, l, c:c + 1].to_broadcast([128, 128])),
                       reads=[gpre.B], writes=[gexp.B])

        def wload(l, segs):
            slot = wslot[wctr[0] % NSLOT]
            wctr[0] += 1
            stg = wst[wctr[1] % 2]
            wctr[1] += 1
            o = 0
            for (c0, n) in segs:
                fw.dma("sp", stg[:, :, o:o + n],
                       w_in_d[l, :, c0:c0 + n].rearrange("(c p) n -> p c n", p=128),
                       writes=[stg.B])
                o += n
            assert o == 128
            fw.op(WENG, lambda h, slot=slot, stg=stg: h.tensor_tensor(out=slot[:], in0=stg[:], in1=gexp[:], op=ALU.mult),
                  reads=[stg.B, gexp.B], writes=[slot.B])
            return slot

        def proj(ps, slot, tok0, ntok=512, shift=0, start=True, stop=True):
            hb = hT_bufs(tok0, ntok, shift)
            for c in range(8):
                fw.pe(lambda h, c=c: h.matmul(out=ps[:, 0:ntok], lhsT=slot[:, c, :],
                                              rhs=hT[:, c, 1 + tok0 - shift:1 + tok0 - shift + ntok],
                                              start=(start and c == 0), stop=(stop and c == 7)),
                      reads=[slot.B] + hb, writes=[ps.B], inc=(c == 7))

        def silu_from_psum(dst, ps):
            fw.act(lambda h: h.activation(out=dst[:], in_=ps[:], func=AF.Sigmoid), reads=[ps.B], writes=[dst.B])
            fw.dve(lambda h: h.tensor_tensor(out=dst[:], in0=ps[:], in1=dst[:], op=ALU.mult),
                   reads=[ps.B, dst.B], writes=[dst.B])

        def phase0(si, l):
            for tt in range(NT):
                xb = x_sb.b[tt]
                xt = x_sb[:, tt, :]
                for j in range(2):
                    fw.dve(lambda h, j=j, xt=xt: h.bn_stats(out=st6[:, j, :], in_=xt[:, j * 512:(j + 1) * 512]),
                           reads=[xb], writes=[st6.B])
                fw.dve(lambda h: h.bn_aggr(out=mv[:], in_=st6[:].rearrange("p a b -> p (a b)")),
                       reads=[st6.B], writes=[mv.B])
                fw.dve(lambda h: h.scalar_tensor_tensor(out=e2[:], in0=mv[:, 0:1], scalar=mv[:, 0:1],
                                                        in1=mv[:, 1:2], op0=ALU.mult, op1=ALU.add),
                       reads=[mv.B], writes=[e2.B])
                fw.act(lambda h: h.activation(out=e2[:], in_=e2[:], func=AF.Sqrt, bias=EPS, scale=1.0),
                       reads=[e2.B], writes=[e2.B])
                fw.dve(lambda h: h.reciprocal(out=rstd[:], in_=e2[:]), reads=[e2.B], writes=[rstd.B])
                for hf in range(2):
                    fw.dve(lambda h, hf=hf, xt=xt: h.tensor_scalar(out=wh[hf][:], in0=xt[:, hf * 512:(hf + 1) * 512],
                                                                   scalar1=rstd[:, 0:1], scalar2=None, op0=ALU.mult),
                           reads=[xb, rstd.B], writes=[wh[hf].B])
                ps = tp_ps[tt % 2]
                for c in range(8):
                    fw.pe(lambda h, ps=ps, c=c: h.transpose(out=ps[:, c, :],
                                                            in_=wh[c // 4][:, (c % 4) * 128:(c % 4 + 1) * 128],
                                                            identity=ident[:]),
                          reads=[wh[c // 4].B, ident.B], writes=[ps.B], inc=(c == 7))
                fw.act(lambda h, ps=ps, tt=tt: h.activation(out=hT[:, :, 1 + tt * 128:1 + (tt + 1) * 128],
                                                            in_=ps[:], func=AF.Copy),
                       reads=[ps.B], writes=[hT.b[tt]])

        def headnorm_gate(y_ps, sg, dst, dstb, eps, affine=None):
            y32, ybf, ycen, sq, rs = wf[0], wh[0], wf[1], wh[1], wf[2]
            fw.act(lambda h: h.activation(out=y32[:], in_=y_ps[:], func=AF.Copy), reads=[y_ps.B], writes=[y32.B])
            fw.dve(lambda h: h.tensor_copy(out=ybf[:], in_=y32[:]), reads=[y32.B], writes=[ybf.B])
            mean_ps = ps_next()
            fw.pe(lambda h: h.matmul(out=mean_ps[:], lhsT=bdo64[:], rhs=ybf[:], start=True, stop=True),
                  reads=[bdo64.B, ybf.B], writes=[mean_ps.B])
            fw.dve(lambda h: h.tensor_tensor(out=ycen[:], in0=y32[:], in1=mean_ps[:], op=ALU.subtract),
                   reads=[y32.B, mean_ps.B], writes=[ycen.B])
            fw.act(lambda h: h.activation(out=sq[:], in_=ycen[:], func=AF.Square), reads=[ycen.B], writes=[sq.B])
            var_ps = ps_next()
            fw.pe(lambda h: h.matmul(out=var_ps[:], lhsT=bdo64[:], rhs=sq[:], start=True, stop=True),
                  reads=[bdo64.B, sq.B], writes=[var_ps.B])
            fw.act(lambda h: h.activation(out=rs[:], in_=var_ps[:], func=AF.Sqrt, bias=eps, scale=1.0),
                   reads=[var_ps.B], writes=[rs.B])
            fw.dve(lambda h: h.reciprocal(out=rs[:], in_=rs[:]), reads=[rs.B], writes=[rs.B])
            fw.dve(lambda h: h.tensor_tensor(out=ycen[:], in0=ycen[:], in1=rs[:], op=ALU.mult),
                    reads=[ycen.B, rs.B], writes=[ycen.B])
            if affine is not None:
                affine(ycen)
            fw.dve(lambda h: h.tensor_tensor(out=dst, in0=ycen[:], in1=sg[:], op=ALU.mult),
                    reads=[ycen.B, sg.B], writes=[dstb])

        def phaseB(si, l):
            dmT = wf[4]
            kwt = wf[5]
            qwt = wf[6]
            fw.dma("sp", gct[:, 0:4], cd["ret_gc"], writes=[gct.B])
            R32 = R32t
            t1, t2 = wf[0], wf[1]
            cosb, sinb = wf[2], wf[3]
            qr, kr, qc, vsb, ktok, vp0, vp1 = wh[2], wh[3], wh[4], wh[5], wh[6], wh[7], wh[8]
            qpad = [wh[9], wh[10]]
            Ssb = wh[11]
            Rb = Rbt
            sgate = wf[7]
            for hp in range(LIM.get('hp', 4)):
                cb = O_BQ + hp * 128
                kb = O_BK + hp * 128
                sw = lambda b: [(b + 32, 32), (b, 32), (b + 96, 32), (b + 64, 32)]
                w_q = wload(l, [(cb, 128)])
                w_qs = wload(l, sw(cb))
                w_k = wload(l, [(kb, 128)])
                w_ks = wload(l, sw(kb))
                w_v = wload(l, [(O_BV + hp * 128, 128)])
                w_g = wload(l, [(O_BG + hp * 128, 128)])
                fw.dma("sp", dmT[:, 0:256], cd["ret_dmT"][hp], writes=[dmT.B])
                fw.dma("sp", kwt[:, 0:128], cd["ret_kwt"][hp], writes=[kwt.B])
                fw.dma("sp", qwt[:, 0:128], cd["ret_qw"][hp], writes=[qwt.B])
                fw.pool(lambda h: h.memset(R32[:], 0.0), writes=[R32.B])
                fw.pool(lambda h: h.memset(Rb[:], 0.0), writes=[Rb.B])
                for qp in qpad:
                    fw.pool(lambda h, qp=qp: h.memset(qp[:], 0.0), writes=[qp.B])
                fw.pool(lambda h: h.memset(vp0[:], 0.0), writes=[vp0.B])
                fw.pool(lambda h: h.memset(vp1[:], 0.0), writes=[vp1.B])
                def do_block(tb, hp=hp, w_q=w_q, w_qs=w_qs, w_k=w_k, w_ks=w_ks, w_v=w_v, w_g=w_g):
                    tok0 = tb * 512
                    fw.dma("sp", cosb[:], cd["rope_cos"][:, tok0:tok0 + 512], writes=[cosb.B])
                    fw.dma("sp", sinb[:], cd["rope_sin"][:, tok0:tok0 + 512], writes=[sinb.B])
                    if LIM.get('stage', 99) < 1:
                        return
                    pq, pqs = ps_next(), ps_next()
                    proj(pq, w_q, tok0)
                    proj(pqs, w_qs, tok0)
                    fw.dve(lambda h: h.tensor_tensor(out=t1[:], in0=pq[:], in1=cosb[:], op=ALU.mult),
                           reads=[pq.B, cosb.B], writes=[t1.B])
                    fw.dve(lambda h: h.tensor_tensor(out=t2[:], in0=pqs[:], in1=sinb[:], op=ALU.mult),
                           reads=[pqs.B, sinb.B], writes=[t2.B])
                    fw.dve(lambda h: h.tensor_tensor(out=qr[:], in0=t1[:], in1=t2[:], op=ALU.add),
                            reads=[t1.B, t2.B], writes=[qr.B])
                    if LIM.get('stage', 99) < 2:
                        return
                    for half in range(2):
                        qp = qpad[half]
                        for hh in range(2):
                            src = qr[hh * 64:(hh + 1) * 64, half * 256:(half + 1) * 256].rearrange("p (c i) -> p c i", c=2)
                            dstv = qp[hh * 64:(hh + 1) * 64, :].rearrange("p (c h i) -> p c h i", c=2, h=2)[:, :, hh, :]
                            fw.act(lambda h, src=src, dstv=dstv: h.activation(out=dstv, in_=src, func=AF.Copy),
                                   reads=[qr.B], writes=[qp.B])
                    for c4 in range(4):
                        fw.dve(lambda h, c4=c4: h.tensor_tensor(out=qc[:, c4 * 128:(c4 + 1) * 128],
                                                                 in0=qr[:, c4 * 128:(c4 + 1) * 128],
                                                                 in1=qwt[:, 0:128], op=ALU.mult),
                                reads=[qr.B, qwt.B], writes=[qc.B])
                    if LIM.get('stage', 99) < 3:
                        return
                    pk, pks = ps_next(), ps_next()
                    proj(pk, w_k, tok0)
                    proj(pks, w_ks, tok0)
                    fw.dve(lambda h: h.tensor_tensor(out=t1[:], in0=pk[:], in1=cosb[:], op=ALU.mult),
                           reads=[pk.B, cosb.B], writes=[t1.B])
                    fw.dve(lambda h: h.tensor_tensor(out=t2[:], in0=pks[:], in1=sinb[:], op=ALU.mult),
                           reads=[pks.B, sinb.B], writes=[t2.B])
                    fw.dve(lambda h: h.tensor_tensor(out=kr[:], in0=t1[:], in1=t2[:], op=ALU.add),
                            reads=[t1.B, t2.B], writes=[kr.B])
                    if LIM.get('stage', 99) < 4:
                        return
                    pv = ps_next()
                    proj(pv, w_v, tok0)
                    fw.act(lambda h: h.activation(out=vsb[:], in_=pv[:], func=AF.Copy), reads=[pv.B], writes=[vsb.B])
                    pgate = ps_next()
                    proj(pgate, w_g, tok0)
                    silu_from_psum(sgate, pgate)
                    if LIM.get('stage', 99) < 5:
                        return
                    tp = tp_ps[0]
                    for c4 in range(4):
                        fw.pe(lambda h, c4=c4: h.transpose(out=tp[:, c4, :], in_=kr[:, c4 * 128:(c4 + 1) * 128],
                                                           identity=ident[:]),
                              reads=[kr.B, ident.B], writes=[tp.B], inc=False)
                    for c4 in range(4):
                        fw.pe(lambda h, c4=c4: h.transpose(out=tp[:, 4 + c4, :], in_=vsb[:, c4 * 128:(c4 + 1) * 128],
                                                           identity=ident[:]),
                              reads=[vsb.B, ident.B], writes=[tp.B], inc=(c4 == 3))
                    for c4 in range(4):
                        fw.dve(lambda h, c4=c4: h.tensor_tensor(out=ktok[:, c4 * 128:(c4 + 1) * 128], in0=tp[:, c4, :],
                                                                in1=kwt[:, 0:128], op=ALU.mult),
                               reads=[tp.B, kwt.B], writes=[ktok.B])
                    fw.act(lambda h: h.activation(
                        out=vp0[:].rearrange("p (c f) -> p c f", c=4)[:, :, 0:64], in_=tp[:, 4:8, 0:64], func=AF.Copy),
                        reads=[tp.B], writes=[vp0.B])
                    fw.act(lambda h: h.activation(
                        out=vp1[:].rearrange("p (c f) -> p c f", c=4)[:, :, 64:128], in_=tp[:, 4:8, 64:128], func=AF.Copy),
                        reads=[tp.B], writes=[vp1.B])
                    if LIM.get('stage', 99) < 6:
                        return
                    y_ps = ps_y()

                    def do_chunk(c4):
                        cs = slice(c4 * 128, (c4 + 1) * 128)
                        sc = ps_next()
                        qp = qpad[c4 // 2]
                        fw.pe(lambda h, cs=cs, qp=qp, c4=c4, sc=sc: h.matmul(
                            out=sc[:, 0:256], lhsT=kr[:, cs], rhs=qp[:, (c4 % 2) * 256:(c4 % 2) * 256 + 256],
                            start=True, stop=True), reads=[kr.B, qp.B], writes=[sc.B])
                        sv = Ssb[:, (c4 % 2) * 256:(c4 % 2) * 256 + 256]
                        fw.dve(lambda h, sc=sc, sv=sv: h.tensor_tensor(out=sv, in0=sc[:, 0:256], in1=dmT[:, 0:256],
                                                                       op=ALU.mult),
                               reads=[sc.B, dmT.B], writes=[Ssb.B])
                        fw.pe(lambda h, cs=cs, sv=sv: h.matmul(out=y_ps[:, cs], lhsT=vp0[:, cs], rhs=sv[:, 0:128],
                                                               start=True, stop=False),
                              reads=[vp0.B, Ssb.B], writes=[y_ps.B], inc=False)
                        fw.pe(lambda h, cs=cs, sv=sv: h.matmul(out=y_ps[:, cs], lhsT=vp1[:, cs], rhs=sv[:, 128:256],
                                                               start=False, stop=False),
                              reads=[vp1.B, Ssb.B], writes=[y_ps.B], inc=False)
                        fw.pe(lambda h, cs=cs: h.matmul(out=y_ps[:, cs], lhsT=Rb[:], rhs=qc[:, cs],
                                                        start=False, stop=True),
                              reads=[Rb.B, qc.B], writes=[y_ps.B])
                        kv = ps_next()
                        fw.pe(lambda h, cs=cs, kv=kv: h.matmul(out=kv[:, 0:128], lhsT=ktok[:, cs], rhs=vp0[:, cs],
                                                               start=True, stop=False),
                              reads=[ktok.B, vp0.B], writes=[kv.B], inc=False)
                        fw.pe(lambda h, cs=cs, kv=kv: h.matmul(out=kv[:, 0:128], lhsT=ktok[:, cs], rhs=vp1[:, cs],
                                                               start=False, stop=True),
                              reads=[ktok.B, vp1.B], writes=[kv.B])
                        fw.dve(lambda h, kv=kv, hp=hp: h.scalar_tensor_tensor(
                            out=R32[:], in0=R32[:], scalar=gct[:, hp:hp + 1], in1=kv[:, 0:128],
                            op0=ALU.mult, op1=ALU.add), reads=[R32.B, gct.B, kv.B], writes=[R32.B])
                        fw.dve(lambda h: h.tensor_tensor(out=Rb[:], in0=R32[:], in1=bd32[:],
                                                          op=ALU.mult),
                                reads=[R32.B, bd32.B], writes=[Rb.B])
                    for c4 in range(LIM.get('c4', 4)):
                        do_chunk(c4)
                    if LIM.get('stage', 99) < 7:
                        return
                    headnorm_gate(y_ps, sgate, yb[:, hp, tok0:tok0 + 512], yb.b[hp * 4 + tb], EPS)

                for tb in range(LIM.get('tb', 4)):
                    do_block(tb)

        def phaseC(si, l):
            PA, PB = wf[0], wf[1]
            sl = lambda t, i: t[:, i * 32:(i + 1) * 32]
            A_RE, A_IM, DT, MAG, ANG, CC, SS, T1, T2, T3, PM, RDEN, CRE, CIM, QQ, SLS = range(16)
            tabs = ya.b + yb.b
            cosT = ya[:].rearrange("p a s -> p (a s)").bitcast(F32).rearrange("p (g j) -> p g j", g=32)
            sinT = yb[:].rearrange("p a s -> p (a s)").bitcast(F32).rearrange("p (g j) -> p g j", g=32)
            ycf = yc[:].rearrange("p a s -> p (a s)").bitcast(F32)
            tmp1 = ycf[:, 0:2048].rearrange("p (g j) -> p g j", g=32)
            tmp2 = ycf[:, 2048:4096].rearrange("p (g j) -> p g j", g=32)

            def pa(fn_, eng="dve", extra=()):
                fw.op(eng, fn_, reads=[PA.B, PB.B] + list(extra), writes=[PA.B, PB.B])

            def tt(o, a, b, op):
                pa(lambda h: h.tensor_tensor(out=o, in0=a, in1=b, op=op))

            P = lambda i: sl(PA, i)
            for i in range(3):
                fw.dma("sp", P(i), s5A_d[l, i], writes=[PA.B])
            fw.dma("sp", s5v[:], s5v_d[l], writes=[s5v.B])
            pa(lambda h: h.activation(out=P(DT), in_=P(DT), func=AF.Exp), "act")
            tt(P(T1), P(DT), P(A_RE), ALU.mult)
            pa(lambda h: h.activation(out=P(MAG), in_=P(T1), func=AF.Exp), "act")
            tt(P(ANG), P(DT), P(A_IM), ALU.mult)
            pa(lambda h: h.activation(out=P(SS), in_=P(ANG), func=AF.Sin, scale=1.0 / 16.0), "act")
            pa(lambda h: h.activation(out=P(CC), in_=P(ANG), func=AF.Sin, scale=1.0 / 16.0, bias=float(np.pi / 2)), "act")

            def dbl(co, so, ci, si_):
                tt(P(T1), ci, ci, ALU.mult)
                tt(P(T2), si_, si_, ALU.mult)
                tt(P(T3), si_, ci, ALU.mult)
                tt(co, P(T1), P(T2), ALU.subtract)
                pa(lambda h: h.tensor_scalar(out=so, in0=P(T3), scalar1=2.0, scalar2=None, op0=ALU.mult))

            for _ in range(3):
                dbl(P(CC), P(SS), P(CC), P(SS))
            dbl(sl(PB, 0), sl(PB, 8), P(CC), P(SS))
            for k in range(1, 8):
                dbl(sl(PB, k), sl(PB, 8 + k), sl(PB, k - 1), sl(PB, 8 + k - 1))
            pa(lambda h: h.tensor_scalar(out=P(SLS), in0=sl(PB, 15), scalar1=sgn[:, 0:1], scalar2=None, op0=ALU.mult),
               extra=[sgn.B])
            tt(P(PM), P(MAG), sl(PB, 0), ALU.mult)
            pa(lambda h: h.tensor_scalar(out=P(PM), in0=P(PM), scalar1=-1.0, scalar2=None, op0=ALU.add))
            tt(P(QQ), P(MAG), sl(PB, 8), ALU.mult)
            tt(P(T1), P(A_RE), P(A_RE), ALU.mult)
            tt(P(T2), P(A_IM), P(A_IM), ALU.mult)
            tt(P(T1), P(T1), P(T2), ALU.add)
            pa(lambda h: h.reciprocal(out=P(RDEN), in_=P(T1)))
            tt(P(T1), P(PM), P(A_RE), ALU.mult)
            tt(P(T2), P(QQ), P(A_IM), ALU.mult)
            tt(P(T1), P(T1), P(T2), ALU.add)
            tt(P(CRE), P(T1), P(RDEN), ALU.mult)
            tt(P(T1), P(QQ), P(A_RE), ALU.mult)
            tt(P(T2), P(PM), P(A_IM), ALU.mult)
            tt(P(T1), P(T1), P(T2), ALU.subtract)
            tt(P(CIM), P(T1), P(RDEN), ALU.mult)

            fw.dve(lambda h: h.memset(cosT[:, :, 0:1], 1.0), writes=tabs)
            fw.dve(lambda h: h.memset(sinT[:, :, 0:1], 0.0), writes=tabs)
            for k in range(7):
                m = 1 << k
                cmb = sl(PB, k).unsqueeze(2).to_broadcast([128, 32, m])
                smb = sl(PB, 8 + k).unsqueeze(2).to_broadcast([128, 32, m])

                def lvl(m=m, cmb=cmb, smb=smb):
                    rw = dict(reads=tabs + yc.b + [PB.B], writes=tabs + yc.b)
                    fw.dve(lambda h: h.tensor_tensor(out=tmp1[:, :, 0:m], in0=cosT[:, :, 0:m], in1=cmb, op=ALU.mult), **rw)
                    fw.dve(lambda h: h.tensor_tensor(out=tmp2[:, :, 0:m], in0=sinT[:, :, 0:m], in1=smb, op=ALU.mult), **rw)
                    fw.dve(lambda h: h.tensor_tensor(out=cosT[:, :, m:2 * m], in0=tmp1[:, :, 0:m], in1=tmp2[:, :, 0:m],
                                                     op=ALU.subtract), **rw)
                    fw.dve(lambda h: h.tensor_tensor(out=tmp1[:, :, 0:m], in0=sinT[:, :, 0:m], in1=cmb, op=ALU.mult), **rw)
                    fw.dve(lambda h: h.tensor_tensor(out=tmp2[:, :, 0:m], in0=cosT[:, :, 0:m], in1=smb, op=ALU.mult), **rw)
                    fw.dve(lambda h: h.tensor_tensor(out=sinT[:, :, m:2 * m], in0=tmp1[:, :, 0:m], in1=tmp2[:, :, 0:m],
                                                     op=ALU.add), **rw)
                lvl()

            xh = [wf[2], wf[3]]
            stg = wf[4]
            stg2 = wf[5]
            tri = wf[6]
            BmT = wh[0:4]
            CmT = wh[4:8]
            u_bf, g12 = wh[8], wh[9]
            for t_ in CmT:
                fw.dve(lambda h, t_=t_: h.memset(t_[:], 0.0), writes=[t_.B])

            def xh_ap(g8):
                return xh[g8 // 4][:, (g8 % 4) * 128:(g8 % 4 + 1) * 128]

            def do_gc(gc):
                gs = slice(gc * 128, (gc + 1) * 128)
                fw.dma("sp", stg[:, 0:128], s5B_d[l, 0, :, gs], writes=[stg.B])
                fw.dma("sp", stg[:, 128:256], s5B_d[l, 1, :, gs], writes=[stg.B])
                v3 = lambda ap: ap.rearrange("p (g q) -> p g q", g=8)
                creb = P(CRE)[:, gc * 8:(gc + 1) * 8].unsqueeze(2).to_broadcast([128, 8, 16])
                cimb = P(CIM)[:, gc * 8:(gc + 1) * 8].unsqueeze(2).to_broadcast([128, 8, 16])
                rw = dict(reads=[stg.B, stg2.B, PA.B], writes=[stg2.B])
                bre, bim = v3(stg[:, 0:128]), v3(stg[:, 128:256])
                ta, tb_ = v3(stg2[:, 0:128]), v3(stg2[:, 128:256])
                bbre, bbim = v3(g12[:, 0:128]), v3(g12[:, 128:256])
                rwb = dict(reads=[stg.B, stg2.B, PA.B], writes=[g12.B])
                fw.dve(lambda h: h.tensor_tensor(out=ta, in0=bre, in1=creb, op=ALU.mult), **rw)
                fw.dve(lambda h: h.tensor_tensor(out=tb_, in0=bim, in1=cimb, op=ALU.mult), **rw)
                fw.dve(lambda h: h.tensor_tensor(out=bbre, in0=ta, in1=tb_, op=ALU.subtract), **rwb)
                fw.dve(lambda h: h.tensor_tensor(out=ta, in0=bim, in1=creb, op=ALU.mult), **rw)
                fw.dve(lambda h: h.tensor_tensor(out=tb_, in0=bre, in1=cimb, op=ALU.mult), **rw)
                fw.dve(lambda h: h.tensor_tensor(out=bbim, in0=ta, in1=tb_, op=ALU.add), **rwb)
                tpx = tp_ps[0]
                ptr, pti = tpx[:, 0, :], tpx[:, 1, :]
                fw.pe(lambda h: h.transpose(out=ptr, in_=g12[:, 0:128], identity=ident[:]),
                      reads=[g12.B, ident.B], writes=[tpx.B], inc=False)
                fw.pe(lambda h: h.transpose(out=pti, in_=g12[:, 128:256], identity=ident[:]),
                      reads=[g12.B, ident.B], writes=[tpx.B])
                fw.act(lambda h: h.activation(out=tri[:, 0:64], in_=ptr[:, 0:64], func=AF.Copy), reads=[tpx.B], writes=[tri.B])
                fw.act(lambda h: h.activation(out=tri[:, 64:128], in_=pti[:, 0:64], func=AF.Copy), reads=[tpx.B], writes=[tri.B])
                fw.act(lambda h: h.activation(out=tri[:, 128:192], in_=pti[:, 0:64], func=AF.Copy), reads=[tpx.B], writes=[tri.B])
                fw.act(lambda h: h.activation(out=tri[:, 192:256], in_=ptr[:, 0:64], func=AF.Copy, scale=-1.0),
                       reads=[tpx.B], writes=[tri.B])
                for g8 in range(8):
                    bt = BmT[g8 // 2]
                    o = (g8 % 2) * 256
                    fw.dve(lambda h, bt=bt, o=o, g8=g8: h.tensor_scalar(
                        out=bt[:, o:o + 256], in0=tri[:, 0:256], scalar1=rowmask[:, g8:g8 + 1], scalar2=None, op0=ALU.mult),
                        reads=[tri.B, rowmask.B], writes=[bt.B])
                fw.dma("sp", stg[:, 0:128], s5C_d[l, 0, :, gs], writes=[stg.B])
                fw.dma("sp", stg[:, 128:256], s5C_d[l, 1, :, gs], writes=[stg.B])
                fw.dve(lambda h: h.tensor_scalar(out=stg[:, 0:128], in0=stg[:, 0:128], scalar1=sgn[:, 1:2], scalar2=None,
                                                 op0=ALU.mult), reads=[stg.B, sgn.B], writes=[stg.B])
                fw.dve(lambda h: h.tensor_scalar(out=stg[:, 128:256], in0=stg[:, 128:256], scalar1=-1.0, scalar2=None,
                                                 op0=ALU.mult), reads=[stg.B], writes=[stg.B])
                for g8 in range(8):
                    ct = CmT[g8 // 2]
                    o = (g8 % 2) * 256
                    for ver in range(2):
                        fw.act(lambda h, ct=ct, o=o, g8=g8, ver=ver: h.activation(
                            out=ct[:, o + ver * 128 + g8 * 16:o + ver * 128 + (g8 + 1) * 16],
                            in_=stg[:, ver * 128 + g8 * 16:ver * 128 + (g8 + 1) * 16], func=AF.Copy),
                            reads=[stg.B], writes=[ct.B])
                w_u = wload(l, [(O_CU + gc * 128, 128)])

                def do_block(tb):
                    tok0 = tb * 512
                    pu = ps_next()
                    proj(pu, w_u, tok0)
                    u32 = wf[7]
                    fw.act(lambda h: h.activation(out=u_bf[:], in_=pu[:], func=AF.Copy), reads=[pu.B], writes=[u_bf.B])
                    fw.act(lambda h: h.activation(out=u32[:], in_=pu[:], func=AF.Copy), reads=[pu.B], writes=[u32.B])
                    y_ps = ps_y()

                    def do_sb(sb):
                        ts = slice(sb * 128, (sb + 1) * 128)
                        first = (tb == 0 and sb == 0)

                        def do_batch(bi):
                            g0 = gc * 8 + bi * 4
                            bun, bus = ps_next(), ps_next()
                            for q in range(4):
                                g8 = bi * 4 + q
                                bt = BmT[g8 // 2]
                                o = (g8 % 2) * 256
                                fw.pe(lambda h, q=q, bt=bt, o=o: h.matmul(out=bun[:, q * 128:(q + 1) * 128], lhsT=bt[:, o:o + 128],
                                                                          rhs=u_bf[:, ts], start=True, stop=True),
                                      reads=[bt.B, u_bf.B], writes=[bun.B], inc=(q == 3))
                            for q in range(4):
                                g8 = bi * 4 + q
                                bt = BmT[g8 // 2]
                                o = (g8 % 2) * 256
                                fw.pe(lambda h, q=q, bt=bt, o=o: h.matmul(out=bus[:, q * 128:(q + 1) * 128],
                                                                          lhsT=bt[:, o + 128:o + 256], rhs=u_bf[:, ts],
                                                                          start=True, stop=True),
                                      reads=[bt.B, u_bf.B], writes=[bus.B], inc=(q == 3))
                            w1, w2 = stg, stg2
                            cs4 = cosT[:, g0:g0 + 4, :]
                            sn4 = sinT[:, g0:g0 + 4, :]
                            v4 = lambda ap: ap.rearrange("p (g j) -> p g j", g=4)
                            fw.dve(lambda h: h.tensor_tensor(out=v4(w1[:]), in0=v4(bun[:]), in1=cs4, op=ALU.mult),
                                   reads=[bun.B] + tabs, writes=[w1.B])
                            fw.dve(lambda h: h.tensor_tensor(out=v4(w2[:]), in0=v4(bus[:]), in1=sn4, op=ALU.mult),
                                   reads=[bus.B] + tabs, writes=[w2.B])
                            fw.dve(lambda h: h.tensor_tensor(out=w1[:], in0=w1[:], in1=w2[:], op=ALU.add),
                                   reads=[w1.B, w2.B], writes=[w1.B])
                            xt_ = xh[bi]
                            for q in range(4):
                                g8 = bi * 4 + q
                                g = gc * 8 + g8
                                init = 0.0 if first else carry[:, g8:g8 + 1]
                                fw.dve(lambda h, q=q, g=g, init=init: h.tensor_tensor_scan(
                                    out=xt_[:, q * 128:(q + 1) * 128], data0=P(MAG)[:, g:g + 1].to_broadcast([128, 128]),
                                    data1=w1[:, q * 128:(q + 1) * 128], initial=init, op0=ALU.mult, op1=ALU.add),
                                    reads=[w1.B, PA.B, carry.B], writes=[xt_.B])
                            G1, G2 = g12, wh[10]
                            fw.dve(lambda h: h.tensor_tensor(out=v4(G1[:]), in0=v4(xt_[:]), in1=cs4, op=ALU.mult),
                                   reads=[xt_.B] + tabs, writes=[G1.B])
                            fw.dve(lambda h: h.tensor_tensor(out=v4(G2[:]), in0=v4(xt_[:]), in1=sn4, op=ALU.mult),
                                   reads=[xt_.B] + tabs, writes=[G2.B])
                            for q in range(4):
                                g8 = bi * 4 + q
                                ct = CmT[g8 // 2]
                                o = (g8 % 2) * 256
                                fw.pe(lambda h, q=q, ct=ct, o=o, g8=g8: h.matmul(
                                    out=y_ps[:, ts], lhsT=ct[:, o:o + 128], rhs=G1[:, q * 128:(q + 1) * 128],
                                    start=(g8 == 0), stop=False), reads=[ct.B, G1.B], writes=[y_ps.B], inc=(q == 3))
                            for q in range(4):
                                g8 = bi * 4 + q
                                ct = CmT[g8 // 2]
                                o = (g8 % 2) * 256
                                fw.pe(lambda h, q=q, ct=ct, o=o, g8=g8: h.matmul(
                                    out=y_ps[:, ts], lhsT=ct[:, o + 128:o + 256], rhs=G2[:, q * 128:(q + 1) * 128],
                                    start=False, stop=(g8 == 7)), reads=[ct.B, G2.B], writes=[y_ps.B], inc=(q == 3))

                        for bi in range(2):
                            do_batch(bi)
                        csw = ps_next()
                        for hx in range(2):
                            xl = xh[hx][:].rearrange("p (g j) -> p g j", g=4)[:, :, 127]
                            fw.dve(lambda h, hx=hx, xl=xl: h.tensor_copy(out=chl[:, hx * 4:(hx + 1) * 4], in_=xl),
                                   reads=[xh[hx].B], writes=[chl.B])
                        fw.dve(lambda h: h.tensor_copy(out=cbk[:], in_=chl[:, 0:8]), reads=[chl.B], writes=[cbk.B])
                        for hx in range(2):
                            xl = xh[hx][:].rearrange("p (g j) -> p g j", g=4)[:, :, 127]
                            fw.dve(lambda h, hx=hx, xl=xl: h.tensor_tensor(out=chl[:, 8 + hx * 4:8 + (hx + 1) * 4], in0=xl,
                                                                          in1=cbk[:, hx * 4:(hx + 1) * 4], op=ALU.subtract),
                                   reads=[xh[hx].B, cbk.B], writes=[chl.B])
                        fw.pe(lambda h: h.matmul(out=csw[:, 0:8], lhsT=swapb[:], rhs=chl[:, 0:8], start=True, stop=False),
                              reads=[swapb.B, chl.B], writes=[csw.B], inc=False)
                        fw.pe(lambda h: h.matmul(out=csw[:, 0:8], lhsT=swapb[:], rhs=chl[:, 8:16], start=False, stop=True),
                              reads=[swapb.B, chl.B], writes=[csw.B])
                        fw.dve(lambda h: h.tensor_tensor(out=cst[:, 0:8], in0=csw[:, 0:8], in1=P(SLS)[:, gc * 8:(gc + 1) * 8],
                                                         op=ALU.mult), reads=[csw.B, PA.B], writes=[cst.B])
                        for hx in range(2):
                            xl = xh[hx][:].rearrange("p (g j) -> p g j", g=4)[:, :, 127]
                            fw.dve(lambda h, hx=hx, xl=xl: h.tensor_tensor(
                                out=cst[:, 8 + hx * 4:8 + (hx + 1) * 4], in0=xl,
                                in1=sl(PB, 7)[:, gc * 8 + hx * 4:gc * 8 + (hx + 1) * 4], op=ALU.mult),
                                reads=[xh[hx].B, PB.B], writes=[cst.B])
                        fw.dve(lambda h: h.tensor_tensor(out=carry[:], in0=cst[:, 0:8], in1=cst[:, 8:16], op=ALU.add),
                               reads=[cst.B], writes=[carry.B])

                    for sb in range(4):
                        do_sb(sb)
                    y32, gt = wf[6], wf[7]
                    fw.dve(lambda h: h.scalar_tensor_tensor(out=y32[:], in0=u32[:], scalar=s5v[:, gc:gc + 1], in1=y_ps[:],
                                                            op0=ALU.mult, op1=ALU.add),
                           reads=[u32.B, s5v.B, y_ps.B], writes=[y32.B])
                    fw.act(lambda h: h.activation(out=gt[:], in_=y32[:], func=AF.Square), reads=[y32.B], writes=[gt.B])
                    fw.dve(lambda h: h.tensor_scalar(out=gt[:], in0=gt[:], scalar1=0.044715, scalar2=1.0, op0=ALU.mult,
                                                     op1=ALU.add), reads=[gt.B], writes=[gt.B])
                    fw.dve(lambda h: h.tensor_tensor(out=gt[:], in0=gt[:], in1=y32[:], op=ALU.mult),
                           reads=[gt.B, y32.B], writes=[gt.B])
                    fw.act(lambda h: h.activation(out=gt[:], in_=gt[:], func=AF.Tanh, scale=0.7978845608028654),
                           reads=[gt.B], writes=[gt.B])
                    fw.dve(lambda h: h.tensor_scalar(out=gt[:], in0=gt[:], scalar1=1.0, scalar2=0.5, op0=ALU.add,
                                                     op1=ALU.mult), reads=[gt.B], writes=[gt.B])
                    fw.dve(lambda h: h.tensor_tensor(out=yc[:, gc, tok0:tok0 + 512], in0=gt[:], in1=y32[:], op=ALU.mult),
                           reads=[gt.B, y32.B], writes=[yc.b[gc * 4 + tb]])

                for tb in range(LIM.get('tb', 4)):
                    do_block(tb)

            for gc in range(4):
                do_gc(gc)

            gw = wh[0:4]
            for c in range(4):
                fw.dma("sp", stg[:], gluw_d[l, c * 128:(c + 1) * 128, :], writes=[stg.B])
                fw.dve(lambda h, c=c: h.tensor_copy(out=gw[c][:], in_=stg[:]), reads=[stg.B], writes=[gw[c].B])
            wgs = [wload(l, [(O_CG + oc * 128, 128)]) for oc in range(4)]

            def glu_block(tb):
                tok0 = tb * 512
                sgl = [wf[0], wf[1], wf[2], wf[3]]
                for oc in range(4):
                    gp = pg[oc]
                    for c in range(4):
                        fw.pe(lambda h, oc=oc, c=c, gp=gp: h.matmul(out=gp[:], lhsT=gw[c][:, oc * 128:(oc + 1) * 128],
                                                                    rhs=yc[:, c, tok0:tok0 + 512], start=(c == 0), stop=(c == 3)),
                              reads=[gw[c].B, yc.b[c * 4 + tb]], writes=[gp.B], inc=(c == 3))
                    fw.act(lambda h, oc=oc, gp=gp: h.activation(out=sgl[oc][:], in_=gp[:], func=AF.Sigmoid,
                                                                bias=s5v[:, 4 + oc:5 + oc], scale=1.0),
                           reads=[gp.B, s5v.B], writes=[sgl[oc].B])
                for oc in range(4):
                    pgt = ps_y()
                    proj(pgt, wgs[oc], tok0)
                    sgt = wf[4]
                    silu_from_psum(sgt, pgt)
                    fw.dve(lambda h, oc=oc, sgt=sgt: h.tensor_tensor(out=sgt[:], in0=sgt[:], in1=sgl[oc][:], op=ALU.mult),
                           reads=[sgt.B, sgl[oc].B], writes=[sgt.B])
                    fw.dve(lambda h, oc=oc, sgt=sgt: h.tensor_tensor(out=yc[:, oc, tok0:tok0 + 512],
                                                                     in0=yc[:, oc, tok0:tok0 + 512], in1=sgt[:], op=ALU.mult),
                           reads=[sgt.B, yc.b[oc * 4 + tb]], writes=[yc.b[oc * 4 + tb]])

            for tb in range(LIM.get('tb', 4)):
                glu_block(tb)

        def wload_shift(l, c0, mu_src):
            s1 = wslot[wctr[0] % NSLOT]
            wctr[0] += 1
            s2 = wslot[wctr[0] % NSLOT]
            wctr[0] += 1
            stg = wst[wctr[1] % 2]
            wctr[1] += 1
            mub = wf[2]
            fw.dma("sp", mub[:, 0:128], mu_src.broadcast_to([128, 128]), writes=[mub.B])
            fw.dve(lambda h: h.tensor_scalar(out=mub[:, 128:256], in0=mub[:, 0:128], scalar1=-1.0, scalar2=1.0,
                                             op0=ALU.mult, op1=ALU.add), reads=[mub.B], writes=[mub.B])
            fw.dma("sp", stg[:], w_in_d[l, :, c0:c0 + 128].rearrange("(c p) n -> p c n", p=128), writes=[stg.B])
            fw.dve(lambda h: h.tensor_tensor(out=stg[:], in0=stg[:], in1=gexp[:], op=ALU.mult),
                   reads=[stg.B, gexp.B], writes=[stg.B])
            fw.dve(lambda h: h.tensor_tensor(out=s2[:], in0=stg[:], in1=mub[:, 0:128].unsqueeze(1).to_broadcast([128, 8, 128]),
                                             op=ALU.mult), reads=[stg.B, mub.B], writes=[s2.B])
            fw.dve(lambda h: h.tensor_tensor(out=s1[:], in0=stg[:], in1=mub[:, 128:256].unsqueeze(1).to_broadcast([128, 8, 128]),
                                             op=ALU.mult), reads=[stg.B, mub.B], writes=[s1.B])
            return s1, s2

        def proj_shift(ps, s12, tok0):
            proj(ps, s12[0], tok0, shift=0, start=True, stop=False)
            proj(ps, s12[1], tok0, shift=1, start=False, stop=True)

        def phaseA(si, l):
            CDEC = 0.6065306597126334
            ybf = yb[:].rearrange("p a s -> p (a s)")
            SB = [Buf(f"scrA{i}") for i in range(16)]
            SV = [ybf[:, i * 512:(i + 1) * 512] for i in range(16)]
            fw.dve(lambda h: h.memset(cst[:, 0:1], 0.0), reads=yb.b, writes=SB + [cst.B])
            QP = [SV[i] for i in range(4)]
            QPB = SB[0:4]
            MK1, MK1B = SV[4], SB[4]
            MK2, MK2B = SV[5][:, 0:256], SB[5]
            E1, E2, E3 = SV[6], SV[7], SV[8]
            XB = [SV[9], SV[10]]
            PP = SV[11]
            MISC = SV[12]
            kt_tok, bt_tok, vp0 = SV[13], SV[14], SV[15]
            vp1 = wh[9]
            v_bf, sqk, rk_bf, rt, at, kt, bt = wh[2], wh[3], wh[4], wh[5], wh[6], wh[7], wh[8]
            sgate, Pq, r32, wf6, wf7, wf0, wf1, wf2 = wf[3], wf[4], wf[5], wf[6], wf[7], wf[0], wf[1], wf[2]
            fw.dma("sp", MK1, cd["rw_masks"][:, 0:512], writes=[MK1B])
            fw.dma("sp", MK2, cd["rw_masks"][:, 512:768], writes=[MK2B])
            fw.dma("sp", rvec[:], rvec_d[l], writes=[rvec.B])
            fw.dve(lambda h: h.tensor_scalar(out=omka[:], in0=rvec[:, :, 3], scalar1=-1.0, scalar2=1.0, op0=ALU.mult,
                                             op1=ALU.add), reads=[rvec.B], writes=[omka.B])
            for i in range(4):
                fw.dve(lambda h, i=i: h.memset(QP[i], 0.0), writes=[QPB[i]])
            fw.dve(lambda h: h.memset(vp0, 0.0), writes=[SB[15]])
            fw.dve(lambda h: h.memset(vp1[:], 0.0), writes=[vp1.B])
            fw.dve(lambda h: h.memset(MISC, 0.0), writes=[SB[12]])
            fw.dve(lambda h: h.memset(lw2[:], 0.0), writes=[lw2.B])
            fw.dve(lambda h: h.memset(la2[:], 0.0), writes=[la2.B])
            w_wa = wload_shift(l, O_XW, mu_wa_d[l])
            for tb in range(4):
                pwa = ps_next()
                proj_shift(pwa, w_wa, tb * 512)
                dst = ya[:, 3, tb * 512:(tb + 1) * 512]
                fw.act(lambda h, pwa=pwa, dst=dst: h.activation(out=dst[0:64, :], in_=pwa[0:64, :], func=AF.Tanh),
                       reads=[pwa.B], writes=[ya.b[12 + tb]])
                fw.act(lambda h, pwa=pwa, dst=dst: h.activation(out=dst[64:128, :], in_=pwa[64:128, :], func=AF.Copy),
                       reads=[pwa.B], writes=[ya.b[12 + tb]])

            def do_hp(hp):
                V = lambda j: rvec[:, hp, j:j + 1]
                W0, A0, KK, KA, LNW, LNB, RK = (V(j) for j in range(7))
                w_r = wload_shift(l, O_AR + hp * 128, mu_rkv_d[l, 0:1, hp * 128:(hp + 1) * 128])
                w_k = wload_shift(l, O_AK + hp * 128, mu_rkv_d[l, 1:2, hp * 128:(hp + 1) * 128])
                w_v = wload_shift(l, O_AV + hp * 128, mu_rkv_d[l, 2:3, hp * 128:(hp + 1) * 128])
                w_g = wload(l, [(O_AG + hp * 128, 128)])
                stg = wst[wctr[1] % 2]
                wctr[1] += 1
                stv = stg[:].rearrange("p c n -> p (c n)")
                fw.dma("sp", stv[0:64, 0:128], w2a2_d[l, 0, :, hp * 128:(hp + 1) * 128], writes=[stg.B])
                fw.dma("sp", stv[64:128, 0:128], w2a2_d[l, 1, :, hp * 128:(hp + 1) * 128], writes=[stg.B])
                fw.dve(lambda h: h.tensor_copy(out=lw2[0:64, :], in_=stv[0:64, 0:128]), reads=[stg.B], writes=[lw2.B])
                fw.dve(lambda h: h.tensor_copy(out=la2[64:128, :], in_=stv[64:128, 0:128]), reads=[stg.B], writes=[la2.B])
                fw.dve(lambda h: h.memset(St32[:], 0.0), writes=[St32.B])
                fw.dve(lambda h: h.memset(Stb[:], 0.0), writes=[Stb.B])

                def do_block(tb):
                    tok0 = tb * 512
                    twa = ya[:, 3, tok0:tok0 + 512]
                    twab = ya.b[12 + tb]
                    pr, pk = ps_next(), ps_next()
                    proj_shift(pr, w_r, tok0)
                    proj_shift(pk, w_k, tok0)
                    fw.act(lambda h: h.activation(out=r32[:], in_=pr[:], func=AF.Copy), reads=[pr.B], writes=[r32.B])
                    pw_, pa_ = ps_next(), ps_next()
                    fw.pe(lambda h: h.matmul(out=pw_[:], lhsT=lw2[:], rhs=twa, start=True, stop=True),
                          reads=[lw2.B, twab], writes=[pw_.B])
                    fw.pe(lambda h: h.matmul(out=pa_[:], lhsT=la2[:], rhs=twa, start=True, stop=True),
                          reads=[la2.B, twab], writes=[pa_.B])
                    sg, aa = wf0, wf6
                    fw.act(lambda h: h.activation(out=sg[:], in_=pw_[:], func=AF.Sigmoid, bias=W0, scale=1.0),
                           reads=[pw_.B, rvec.B], writes=[sg.B])
                    fw.act(lambda h: h.activation(out=aa[:], in_=pa_[:], func=AF.Sigmoid, bias=A0, scale=1.0),
                           reads=[pa_.B, rvec.B], writes=[aa.B])
                    kkn = wf7
                    fw.dve(lambda h: h.tensor_scalar(out=kkn[:], in0=pk[:], scalar1=KK, scalar2=None, op0=ALU.mult),
                           reads=[pk.B, rvec.B], writes=[kkn.B])
                    fw.act(lambda h: h.activation(out=sqk[:], in_=kkn[:], func=AF.Square), reads=[kkn.B], writes=[sqk.B])
                    ss = ps_next()
                    fw.pe(lambda h: h.matmul(out=ss[:], lhsT=bdo64[:], rhs=sqk[:], start=True, stop=True),
                          reads=[bdo64.B, sqk.B], writes=[ss.B])
                    rn = wf1
                    fw.act(lambda h: h.activation(out=rn[:], in_=ss[:], func=AF.Sqrt, bias=1e-12, scale=64.0),
                           reads=[ss.B], writes=[rn.B])
                    fw.dve(lambda h: h.reciprocal(out=rn[:], in_=rn[:]), reads=[rn.B], writes=[rn.B])
                    fw.dve(lambda h: h.tensor_tensor(out=kkn[:], in0=kkn[:], in1=rn[:], op=ALU.mult),
                           reads=[kkn.B, rn.B], writes=[kkn.B])
                    k2 = wf2
                    fw.dve(lambda h: h.tensor_scalar(out=wf1[:], in0=aa[:], scalar1=KA, scalar2=omka[:, hp:hp + 1],
                                                     op0=ALU.mult, op1=ALU.add), reads=[aa.B, rvec.B, omka.B], writes=[wf1.B])
                    fw.dve(lambda h: h.tensor_tensor(out=k2[:], in0=pk[:], in1=wf1[:], op=ALU.mult),
                           reads=[pk.B, wf1.B], writes=[k2.B])
                    fw.dve(lambda h: h.scalar_tensor_tensor(out=rk_bf[:], in0=r32[:], scalar=RK, in1=k2[:], op0=ALU.mult,
                                                            op1=ALU.mult), reads=[r32.B, rvec.B, k2.B], writes=[rk_bf.B])
                    bb_ = wf1
                    fw.dve(lambda h: h.tensor_tensor(out=bb_[:], in0=kkn[:], in1=aa[:], op=ALU.mult),
                           reads=[kkn.B, aa.B], writes=[bb_.B])
                    pv = ps_next()
                    proj_shift(pv, w_v, tok0)
                    fw.act(lambda h: h.activation(out=v_bf[:], in_=pv[:], func=AF.Copy), reads=[pv.B], writes=[v_bf.B])
                    pgt = ps_next()
                    proj(pgt, w_g, tok0)
                    silu_from_psum(sgate, pgt)
                    cs = wf6
                    for c4 in range(4):
                        fw.dve(lambda h, c4=c4: h.tensor_tensor_scan(
                            out=cs[:, c4 * 128:(c4 + 1) * 128], data0=onec[:, 0:1].to_broadcast([128, 128]),
                            data1=sg[:, c4 * 128:(c4 + 1) * 128], initial=0.0, op0=ALU.mult, op1=ALU.add),
                            reads=[sg.B, onec.B], writes=[cs.B])
                    fw.dve(lambda h: h.tensor_tensor(out=sg[:], in0=cs[:], in1=sg[:], op=ALU.subtract),
                           reads=[cs.B, sg.B], writes=[sg.B])
                    fw.act(lambda h: h.activation(out=Pq[:], in_=cs[:], func=AF.Exp, scale=-CDEC), reads=[cs.B], writes=[Pq.B])
                    fw.act(lambda h: h.activation(out=sg[:], in_=sg[:], func=AF.Exp, scale=-CDEC), reads=[sg.B], writes=[sg.B])
                    fw.act(lambda h: h.activation(out=cs[:], in_=cs[:], func=AF.Exp, scale=CDEC), reads=[cs.B], writes=[cs.B])
                    PqA, Pk = sg, cs
                    fw.dve(lambda h: h.tensor_tensor(out=rt[:], in0=r32[:], in1=Pq[:], op=ALU.mult),
                           reads=[r32.B, Pq.B], writes=[rt.B])
                    fw.dve(lambda h: h.scalar_tensor_tensor(out=at[:], in0=kkn[:], scalar=-1.0, in1=PqA[:], op0=ALU.mult,
                                                            op1=ALU.mult), reads=[kkn.B, PqA.B], writes=[at.B])
                    fw.dve(lambda h: h.tensor_tensor(out=kt[:], in0=k2[:], in1=Pk[:], op=ALU.mult),
                           reads=[k2.B, Pk.B], writes=[kt.B])
                    fw.dve(lambda h: h.tensor_tensor(out=bt[:], in0=bb_[:], in1=Pk[:], op=ALU.mult),
                           reads=[bb_.B, Pk.B], writes=[bt.B])
                    for c4 in range(4):
                        for hh in range(2):
                            rows = slice(hh * 64, (hh + 1) * 64)
                            fw.act(lambda h, c4=c4, hh=hh, rows=rows: h.activation(
                                out=QP[c4][rows, (2 * hh) * 128:(2 * hh + 1) * 128], in_=at[rows, c4 * 128:(c4 + 1) * 128],
                                func=AF.Copy), reads=[at.B], writes=[QPB[c4]])
                            fw.act(lambda h, c4=c4, hh=hh, rows=rows: h.activation(
                                out=QP[c4][rows, (2 * hh + 1) * 128:(2 * hh + 2) * 128], in_=rt[rows, c4 * 128:(c4 + 1) * 128],
                                func=AF.Copy), reads=[rt.B], writes=[QPB[c4]])
                    tpa, tpb = tp_ps[0], tp_ps[1]
                    for c4 in range(4):
                        fw.pe(lambda h, c4=c4: h.transpose(out=tpa[:, c4, :], in_=kt[:, c4 * 128:(c4 + 1) * 128],
                                                           identity=ident[:]), reads=[kt.B, ident.B], writes=[tpa.B], inc=False)
                    for c4 in range(4):
                        fw.pe(lambda h, c4=c4: h.transpose(out=tpa[:, 4 + c4, :], in_=bt[:, c4 * 128:(c4 + 1) * 128],
                                                           identity=ident[:]), reads=[bt.B, ident.B], writes=[tpa.B],
                              inc=(c4 == 3))
                    for c4 in range(4):
                        fw.pe(lambda h, c4=c4: h.transpose(out=tpb[:, c4, :], in_=v_bf[:, c4 * 128:(c4 + 1) * 128],
                                                           identity=ident[:]), reads=[v_bf.B, ident.B], writes=[tpb.B],
                              inc=(c4 == 3))
                    v4 = lambda ap: ap.rearrange("p (c f) -> p c f", c=4)
                    fw.act(lambda h: h.activation(out=v4(kt_tok), in_=tpa[:, 0:4, :], func=AF.Copy),
                           reads=[tpa.B], writes=[SB[13]])
                    fw.dve(lambda h: h.tensor_copy(out=v4(bt_tok), in_=tpa[:, 4:8, :]), reads=[tpa.B], writes=[SB[14]])
                    fw.act(lambda h: h.activation(out=v4(vp0)[:, :, 0:64], in_=tpb[:, 0:4, 0:64], func=AF.Copy),
                           reads=[tpb.B], writes=[SB[15]])
                    fw.dve(lambda h: h.tensor_copy(out=v4(vp1[:])[:, :, 64:128], in_=tpb[:, 0:4, 64:128]),
                           reads=[tpb.B], writes=[vp1.B])
                    y_ps = ps_y()

                    def do_chunk(c4):
                        cs_ = slice(c4 * 128, (c4 + 1) * 128)
                        pa1, pa2, pa3 = ps_next(), ps_next(), ps_next()
                        fw.pe(lambda h: h.matmul(out=pa1[:], lhsT=kt[:, cs_], rhs=QP[c4], start=True, stop=True),
                              reads=[kt.B, QPB[c4]], writes=[pa1.B])
                        fw.pe(lambda h: h.matmul(out=pa2[:], lhsT=bt[:, cs_], rhs=QP[c4], start=True, stop=True),
                              reads=[bt.B, QPB[c4]], writes=[pa2.B])
                        for hh in range(2):
                            fw.pe(lambda h, hh=hh: h.matmul(out=pa3[:, hh * 128:(hh + 1) * 128],
                                                            lhsT=QP[c4][:, (2 * hh) * 128:(2 * hh + 1) * 128], rhs=bt[:, cs_],
                                                            start=True, stop=True),
                                  reads=[bt.B, QPB[c4]], writes=[pa3.B], inc=(hh == 1))
                        fw.dve(lambda h: h.tensor_tensor(out=E1, in0=pa1[:], in1=MK1, op=ALU.mult),
                               reads=[pa1.B, MK1B], writes=[SB[6]])
                        fw.dve(lambda h: h.tensor_tensor(out=E2, in0=pa2[:], in1=MK1, op=ALU.mult),
                               reads=[pa2.B, MK1B], writes=[SB[7]])
                        fw.dve(lambda h: h.tensor_tensor(out=E3[:, 0:256], in0=pa3[:, 0:256], in1=MK2, op=ALU.mult),
                               reads=[pa3.B, MK2B], writes=[SB[8]])

                        def Xj(j, hh):
                            if j == 0:
                                return E3[:, hh * 128:(hh + 1) * 128], SB[8]
                            return XB[j % 2][:, (2 * hh) * 128:(2 * hh + 1) * 128], SB[9 + j % 2]

                        def Bj(j, hh):
                            if j == 0:
                                return E2[:, (2 * hh) * 128:(2 * hh + 1) * 128], SB[7]
                            return XB[j % 2][:, (2 * hh + 1) * 128:(2 * hh + 2) * 128], SB[9 + j % 2]

                        def Pj(j, hh):
                            o = (j % 2) * 256 + hh * 128
                            return PP[:, o:o + 128]

                        e2v = E2.rearrange("p (a b) -> p a b", a=2)[:, :, 0:128]
                        fw.dve(lambda h: h.tensor_tensor(out=PP[:, 0:256].rearrange("p (a b) -> p a b", a=2), in0=e2v,
                                                         in1=ident[:].unsqueeze(1).to_broadcast([128, 2, 128]), op=ALU.add),
                               reads=[SB[7], ident.B], writes=[SB[11]])
                        for j in range(6):
                            last = j == 5
                            pxb = ps_next()
                            for hh in range(2):
                                xa_, xb_ = Xj(j, hh)
                                ba_, bb2 = Bj(j, hh)
                                fw.pe(lambda h, hh=hh, xa_=xa_, ba_=ba_, pxb=pxb: h.matmul(
                                    out=pxb[:, (2 * hh) * 128:(2 * hh + 1) * 128], lhsT=ba_, rhs=xa_, start=True, stop=True),
                                    reads=[xb_, bb2], writes=[pxb.B], inc=(last and hh == 1))
                                if not last:
                                    fw.pe(lambda h, hh=hh, xa_=xa_, ba_=ba_, pxb=pxb: h.matmul(
                                        out=pxb[:, (2 * hh + 1) * 128:(2 * hh + 2) * 128], lhsT=xa_, rhs=ba_, start=True,
                                        stop=True), reads=[xb_, bb2], writes=[pxb.B], inc=(hh == 1))
                            nxt = XB[(j + 1) % 2]
                            if last:
                                fw.act(lambda h, pxb=pxb, nxt=nxt: h.activation(
                                    out=nxt.rearrange("p (a b) -> p a b", a=2)[:, :, 0:128],
                                    in_=pxb[:].rearrange("p (a b) -> p a b", a=2)[:, :, 0:128], func=AF.Copy),
                                    reads=[pxb.B], writes=[SB[9 + (j + 1) % 2]])
                            else:
                                fw.act(lambda h, pxb=pxb, nxt=nxt: h.activation(out=nxt, in_=pxb[:], func=AF.Copy),
                                       reads=[pxb.B], writes=[SB[9 + (j + 1) % 2]])
                            pp = ps_next()
                            for hh in range(2):
                                xn_, xnb_ = Xj(j + 1, hh)
                                fw.pe(lambda h, hh=hh, pp=pp, j=j: h.matmul(out=pp[:, hh * 128:(hh + 1) * 128], lhsT=ident[:],
                                                                             rhs=Pj(j, hh), start=True, stop=False),
                                      reads=[ident.B, SB[11]], writes=[pp.B], inc=False)
                                fw.pe(lambda h, hh=hh, pp=pp, j=j, xn_=xn_: h.matmul(
                                    out=pp[:, hh * 128:(hh + 1) * 128], lhsT=xn_, rhs=Pj(j, hh), start=False, stop=True),
                                    reads=[xnb_, SB[11]], writes=[pp.B], inc=(hh == 1))
                            o = ((j + 1) % 2) * 256
                            fw.dve(lambda h, pp=pp, o=o: h.tensor_copy(out=PP[:, o:o + 256], in_=pp[:, 0:256]),
                                   reads=[pp.B], writes=[SB[11]])
                        r0 = ps_next()
                        fw.pe(lambda h: h.matmul(out=r0[:, 0:128], lhsT=at[:, cs_], rhs=Stb[:], start=True, stop=False),
                              reads=[at.B, Stb.B], writes=[r0.B], inc=False)
                        vps = [vp0, vp1[:]]
                        vpb = [SB[15], vp1.B]
                        for hh in range(2):
                            fw.pe(lambda h, hh=hh: h.matmul(out=r0[:, hh * 64:(hh + 1) * 64],
                                                            lhsT=E1[:, (2 * hh) * 128:(2 * hh + 1) * 128],
                                                            rhs=vps[hh][:, c4 * 128 + hh * 64:c4 * 128 + (hh + 1) * 64],
                                                            start=False, stop=(hh == 1)),
                                  reads=[SB[6], vpb[hh]], writes=[r0.B], inc=(hh == 1))
                        r0b = MISC[:, 0:128]
                        fw.act(lambda h: h.activation(out=r0b, in_=r0[:, 0:128], func=AF.Copy), reads=[r0.B], writes=[SB[12]])
                        up = ps_next()
                        for hh in range(2):
                            fw.pe(lambda h, hh=hh: h.matmul(out=up[:, hh * 64:(hh + 1) * 64], lhsT=Pj(6, hh),
                                                            rhs=r0b[:, hh * 64:(hh + 1) * 64], start=True, stop=True),
                                  reads=[SB[11], SB[12]], writes=[up.B], inc=(hh == 1))
                        upad = [MISC[:, 128:256], MISC[:, 256:384]]
                        fw.act(lambda h: h.activation(out=upad[0][:, 0:64], in_=up[:, 0:64], func=AF.Copy),
                               reads=[up.B], writes=[SB[12]])
                        fw.dve(lambda h: h.tensor_copy(out=upad[1][:, 64:128], in_=up[:, 64:128]),
                               reads=[up.B], writes=[SB[12]])
                        fw.pe(lambda h: h.matmul(out=y_ps[:, cs_], lhsT=Stb[:], rhs=rt[:, cs_], start=True, stop=False),
                              reads=[Stb.B, rt.B], writes=[y_ps.B], inc=False)
                        for hh in range(2):
                            fw.pe(lambda h, hh=hh: h.matmul(out=y_ps[:, cs_], lhsT=upad[hh],
                                                            rhs=E2[:, (2 * hh + 1) * 128:(2 * hh + 2) * 128],
                                                            start=False, stop=False),
                                  reads=[SB[12], SB[7]], writes=[y_ps.B], inc=False)
                            fw.pe(lambda h, hh=hh: h.matmul(out=y_ps[:, cs_], lhsT=vps[hh][:, c4 * 128:(c4 + 1) * 128],
                                                            rhs=E1[:, (2 * hh + 1) * 128:(2 * hh + 2) * 128],
                                                            start=False, stop=(hh == 1)),
                                  reads=[vpb[hh], SB[6]], writes=[y_ps.B], inc=(hh == 1))
                        su = ps_next()
                        for hh in range(2):
                            fw.pe(lambda h, hh=hh: h.matmul(out=su[:, 0:128], lhsT=bt_tok[:, cs_], rhs=upad[hh],
                                                            start=(hh == 0), stop=False),
                                  reads=[SB[14], SB[12]], writes=[su.B], inc=False)
                        for hh in range(2):
                            fw.pe(lambda h, hh=hh: h.matmul(out=su[:, 0:128], lhsT=kt_tok[:, cs_],
                                                            rhs=vps[hh][:, c4 * 128:(c4 + 1) * 128],
                                                            start=False, stop=(hh == 1)),
                                  reads=[SB[13], vpb[hh]], writes=[su.B], inc=(hh == 1))
                        fw.dve(lambda h: h.tensor_tensor(out=St32[:], in0=su[:, 0:128], in1=St32[:], op=ALU.add),
                               reads=[su.B, St32.B], writes=[St32.B])
                        pend = Pq[:, c4 * 128 + 127:c4 * 128 + 128]
                        fw.dve(lambda h: h.tensor_scalar(out=St32[:], in0=St32[:], scalar1=pend, scalar2=None, op0=ALU.mult),
                               reads=[St32.B, Pq.B], writes=[St32.B])
                        fw.dve(lambda h: h.tensor_tensor(out=Stb[:], in0=St32[:], in1=bd32[:], op=ALU.mult),
                               reads=[St32.B, bd32.B], writes=[Stb.B])

                    for c4 in range(LIM.get('c4', 4)):
                        do_chunk(c4)
                    bs = ps_next()
                    fw.pe(lambda h: h.matmul(out=bs[:], lhsT=bdo64[:], rhs=rk_bf[:], start=True, stop=True),
                          reads=[bdo64.B, rk_bf.B], writes=[bs.B])
                    bon = wf7
                    fw.dve(lambda h: h.scalar_tensor_tensor(out=bon[:], in0=bs[:], scalar=64.0, in1=v_bf[:], op0=ALU.mult,
                                                            op1=ALU.mult), reads=[bs.B, v_bf.B], writes=[bon.B])

                    def affine(ycen):
                        fw.dve(lambda h: h.tensor_scalar(out=ycen[:], in0=ycen[:], scalar1=LNW, scalar2=LNB, op0=ALU.mult,
                                                         op1=ALU.add), reads=[ycen.B, rvec.B], writes=[ycen.B])
                        fw.dve(lambda h: h.tensor_tensor(out=ycen[:], in0=ycen[:], in1=bon[:], op=ALU.add),
                               reads=[ycen.B, bon.B], writes=[ycen.B])

                    headnorm_gate(y_ps, sgate, ya[:, hp, tok0:tok0 + 512], ya.b[hp * 4 + tb], 64e-5, affine=affine)

                for tb in range(LIM.get('tb', 4)):
                    do_block(tb)

            for hp in range(LIM.get('hp', 4)):
                do_hp(hp)
            fw.dve(lambda h: h.memset(cst[:, 0:1], 0.0), reads=SB, writes=yb.b + [cst.B])

        def wload_plain(src_ap, nchunk):
            slot = wslot[wctr[0] % NSLOT]
            wctr[0] += 1
            stg = wst[wctr[1] % 2]
            wctr[1] += 1
            fw.dma("sp", stg[:, 0:nchunk, :], src_ap, writes=[stg.B])
            fw.act(lambda h: h.activation(out=slot[:, 0:nchunk, :], in_=stg[:, 0:nchunk, :], func=AF.Copy),
                   reads=[stg.B], writes=[slot.B])
            return slot

        wc_bufs = {}

        def cached(l, idx, nchunk, loader):
            n = nchunk * 128
            key = (l, idx)
            if key not in wc_bufs:
                slot = loader()
                wc_bufs[key] = Buf(f"wc{l}_{idx}")
                fw.dma("sp", wcache_d[l, idx, :, 0:n], slot[:, 0:nchunk, :].rearrange("p c n -> p (c n)"),
                       reads=[slot.B], writes=[wc_bufs[key]])
                return slot
            slot = wslot[wctr[0] % NSLOT]
            wctr[0] += 1
            fw.dma("sp", slot[:, 0:nchunk, :].rearrange("p c n -> p (c n)"), wcache_d[l, idx, :, 0:n],
                   reads=[wc_bufs[key]], writes=[slot.B])
            return slot

        def phaseM(si, l):
            fw.dma("sp", gpost[:], postn_d[l:l + 1, :].broadcast_to([128, D]), writes=[gpost.B])
            ybr = [ya, yb, yc]
            gofs = [O_GA, O_GB, O_GC]
            mT = wh[0:8]
            sig, tt_, macc = wf[0], wf[1], wf[2]

            def do_block(tb):
                tok0 = tb * 512

                def do_oc(oc):
                    for br in range(3):
                        if br not in LIM.get("branches", (0, 1, 2)):
                            continue
                        first = br == min(LIM.get("branches", (0, 1, 2)))
                        last = br == max(LIM.get("branches", (0, 1, 2)))
                        wg = cached(l, br * 8 + oc, 8, lambda br=br: wload(l, [(gofs[br] + oc * 128, 128)]))
                        gl = ps_next()
                        proj(gl, wg, tok0)
                        fw.act(lambda h, gl=gl, br=br: h.activation(out=sig[:], in_=gl[:], func=AF.Sigmoid,
                                                                    bias=bmt[:, l, br, oc:oc + 1], scale=1.0),
                               reads=[gl.B, bmt.B], writes=[sig.B])
                        wp = cached(l, 24 + br * 8 + oc, 4, lambda br=br: wload_plain(
                            wp_d[br][l, :, oc * 128:(oc + 1) * 128].rearrange("(c p) n -> p c n", p=128), 4))
                        pb = ps_next()
                        for c in range(4):
                            fw.pe(lambda h, c=c, pb=pb, wp=wp, br=br: h.matmul(
                                out=pb[:], lhsT=wp[:, c, :], rhs=ybr[br][:, c, tok0:tok0 + 512],
                                start=(c == 0), stop=(c == 3)),
                                reads=[wp.B, ybr[br].b[c * 4 + tb]], writes=[pb.B], inc=(c == 3))
                        if first and last:
                            fw.dve(lambda h, pb=pb: h.tensor_tensor(out=mT[oc][:], in0=pb[:], in1=sig[:], op=ALU.mult),
                                   reads=[pb.B, sig.B], writes=[mT[oc].B])
                        elif first:
                            fw.dve(lambda h, pb=pb: h.tensor_tensor(out=macc[:], in0=pb[:], in1=sig[:], op=ALU.mult),
                                   reads=[pb.B, sig.B], writes=[macc.B])
                        else:
                            fw.dve(lambda h, pb=pb: h.tensor_tensor(out=tt_[:], in0=pb[:], in1=sig[:], op=ALU.mult),
                                   reads=[pb.B, sig.B], writes=[tt_.B])
                            dst = mT[oc] if last else macc
                            fw.dve(lambda h, dst=dst: h.tensor_tensor(out=dst[:], in0=macc[:], in1=tt_[:], op=ALU.add),
                                   reads=[macc.B, tt_.B], writes=[dst.B])

                for oc in range(8):
                    do_oc(oc)

                def do_pair(k):
                    banks = [pg[0], pg[1], pg[2], pg[3]]
                    for oc in range(8):
                        wo = cached(l, 48 + oc, 8, lambda oc=oc: wload_plain(
                            wout_d[l, oc * 128:(oc + 1) * 128, :].rearrange("p (c n) -> p c n", c=8), 8))
                        wov = wo[:].rearrange("p c n -> p (c n)")
                        for t in range(2):
                            for half in range(2):
                                bk = banks[t * 2 + half]
                                fw.pe(lambda h, oc=oc, t=t, half=half, bk=bk, wov=wov: h.matmul(
                                    out=bk[:], lhsT=mT[oc][:, (2 * k + t) * 128:(2 * k + t + 1) * 128],
                                    rhs=wov[:, half * 512:(half + 1) * 512], start=(oc == 0), stop=(oc == 7)),
                                    reads=[mT[oc].B, wo.B], writes=[bk.B], inc=(oc == 7 or (t == 1 and half == 1)))
                    for t in range(2):
                        tile_i = tb * 4 + 2 * k + t
                        for half in range(2):
                            bk = banks[t * 2 + half]
                            fw.dve(lambda h, half=half, bk=bk: h.bn_stats(out=st6[:, half, :], in_=bk[:]),
                                   reads=[bk.B], writes=[st6.B])
                        fw.dve(lambda h: h.bn_aggr(out=mv[:], in_=st6[:].rearrange("p a b -> p (a b)")),
                               reads=[st6.B], writes=[mv.B])
                        fw.dve(lambda h: h.scalar_tensor_tensor(out=e2[:], in0=mv[:, 0:1], scalar=mv[:, 0:1],
                                                                in1=mv[:, 1:2], op0=ALU.mult, op1=ALU.add),
                               reads=[mv.B], writes=[e2.B])
                        fw.act(lambda h: h.activation(out=e2[:], in_=e2[:], func=AF.Sqrt, bias=EPS, scale=1.0),
                               reads=[e2.B], writes=[e2.B])
                        fw.dve(lambda h: h.reciprocal(out=rstd[:], in_=e2[:]), reads=[e2.B], writes=[rstd.B])
                        for half in range(2):
                            bk = banks[t * 2 + half]
                            hs = slice(half * 512, (half + 1) * 512)
                            fw.dve(lambda h, bk=bk, hs=hs: h.scalar_tensor_tensor(
                                out=wf[3][:], in0=bk[:], scalar=rstd[:, 0:1], in1=gpost[:, hs],
                                op0=ALU.mult, op1=ALU.mult), reads=[bk.B, rstd.B, gpost.B], writes=[wf[3].B])
                            fw.dve(lambda h, hs=hs, tile_i=tile_i: h.tensor_tensor(
                                out=x_sb[:, tile_i, hs], in0=x_sb[:, tile_i, hs], in1=wf[3][:], op=ALU.add),
                                reads=[x_sb.b[tile_i], wf[3].B], writes=[x_sb.b[tile_i]])

                for k in range(2):
                    do_pair(k)

            for tb in range(LIM.get('tb', 4)):
                do_block(tb)

        for si in range(nseq):
            for tq in range(NT // 4):
                fw.dma("sp", x_sb[:, tq * 4:(tq + 1) * 4, :],
                       x_d[si, tq * 512:(tq + 1) * 512, :].rearrange("(t p) d -> p t d", p=128),
                       writes=[x_sb.b[tq * 4 + i] for i in range(4)])
            for l in range(nlayers):
                if l == 1 and "l2phases" in LIM:
                    phases = LIM["l2phases"]
                if not LIM.get('nosetup'):
                    layer_setup(l)
                phase0(si, l)
                if "hT" in debug and si == 0 and l == 0:
                    fw.dma("sp", dbg_d["hT"], hT[:], reads=hT.b)
                if "C" in phases:
                    phaseC(si, l)
                    if "yc" in debug and si == 0 and l == 0:
                        fw.dma("sp", dbg_d["yc"], yc[:], reads=yc.b)
                if "A" in phases:
                    phaseA(si, l)
                    if "ya" in debug and si == 0 and l == 0:
                        fw.dma("sp", dbg_d["ya"], ya[:], reads=ya.b)
                if "B" in phases:
                    phaseB(si, l)
                    if "yb" in debug and si == 0 and l == 0:
                        fw.dma("sp", dbg_d["yb"], yb[:], reads=yb.b)
                if "M" in phases:
                    phaseM(si, l)
            for tq in range(NT // 4):
                fw.dma("sp", out_d[si, tq * 512:(tq + 1) * 512, :].rearrange("(t p) d -> p t d", p=128),
                       x_sb[:, tq * 4:(tq + 1) * 4, :],
                       reads=[x_sb.b[tq * 4 + i] for i in range(4)])
        allb = x_sb.b + hT.b + ya.b + yb.b + yc.b
        fw.wait_all("sp", allb)
        fw.emit()
        print("instr counts:", {k: v.n for k, v in fw.eng.items()})
    return nc


def make_shared(inputs):
    f = lambda k: np.ascontiguousarray(np.asarray(inputs[k], dtype=np.float32))
    shared = dict(make_consts())
    shared["w_in"] = f("w_in")
    shared["pre_norm"] = np.ascontiguousarray(f("pre_norm").reshape(DEPTH, 8, 128).transpose(0, 2, 1))
    for n in ("w_proj_rwkv", "w_proj_ret", "w_proj_s5", "w_out", "post_norm"):
        shared[n] = f(n)
    shared["b_merge"] = np.ascontiguousarray(f("b_merge").reshape(DEPTH, 3, 8, 128).transpose(0, 3, 1, 2))
    shared["rwkv_mu_rkv"] = f("rwkv_mu_rkv")
    shared["rwkv_mu_wa"] = np.ascontiguousarray(f("rwkv_mu_wa").reshape(DEPTH, 1, 128))
    shared["rwkv_w2a2"] = np.ascontiguousarray(np.stack([f("rwkv_w2"), f("rwkv_a2")], axis=1))
    vec = np.zeros((DEPTH, 8, 512), np.float32)
    for j, n in enumerate(("rwkv_w0", "rwkv_a0", "rwkv_k_k", "rwkv_k_a", "rwkv_ln_w", "rwkv_ln_b")):
        vec[:, j] = f(n)
    vec[:, 6] = f("rwkv_r_k").reshape(DEPTH, 512)
    shared["rwkv_vec"] = np.ascontiguousarray(vec.reshape(DEPTH, 8, 4, 128).transpose(0, 3, 2, 1))
    dup = lambda a: np.concatenate([a, a], axis=1)
    a_re = dup(f("s5_A_re").transpose(0, 2, 1))
    a_im = dup(f("s5_A_im").transpose(0, 2, 1))
    ldt = np.broadcast_to(f("s5_log_dt")[:, None, :], (DEPTH, 128, 32))
    shared["s5_Aab"] = np.ascontiguousarray(np.stack([a_re, a_im, ldt], axis=1))
    b_re = dup(f("s5_B_re").transpose(0, 2, 1, 3).reshape(DEPTH, 64, 512))
    b_im = dup(f("s5_B_im").transpose(0, 2, 1, 3).reshape(DEPTH, 64, 512))
    shared["s5_Bst"] = np.ascontiguousarray(np.stack([b_re, b_im], axis=1))
    c_re = f("s5_C_re").transpose(0, 3, 1, 2).reshape(DEPTH, 64, 512)
    c_im = f("s5_C_im").transpose(0, 3, 1, 2).reshape(DEPTH, 64, 512)
    ca = np.concatenate([c_re, c_im], axis=1)
    cb = np.concatenate([c_im, c_re], axis=1)
    shared["s5_Cst"] = np.ascontiguousarray(np.stack([ca, cb], axis=1))
    dv = f("s5_D").reshape(DEPTH, 4, 128).transpose(0, 2, 1)
    gb = f("s5_glu_b").reshape(DEPTH, 4, 128).transpose(0, 2, 1)
    shared["s5_vec"] = np.ascontiguousarray(np.concatenate([dv, gb], axis=2))
    shared["s5_glu_w"] = f("s5_glu_w")
    return shared


def make_inputs(inputs, s0, n):
    m = make_shared(inputs)
    m["x"] = np.ascontiguousarray(np.asarray(inputs["x"], dtype=np.float32)[s0:s0 + n])
    return m


def kernel(**inputs):
    ncores = 8
    x = np.ascontiguousarray(np.asarray(inputs["x"], dtype=np.float32))
    shared = make_shared(inputs)
    nlaunch = N_LAUNCH
    per = NSEQ // nlaunch
    nc = build_program(nseq=per)
    out = np.zeros_like(x)
    for j in range(nlaunch):
        in_maps = []
        for c in range(ncores):
            m = dict(shared)
            s0 = c * NSEQ + j * per
            m["x"] = x[s0:s0 + per]
            in_maps.append(m)
        res = run_bass_kernel_spmd(nc, in_maps, core_ids=list(range(ncores)))
        for c in range(ncores):
            s0 = c * NSEQ + j * per
            out[s0:s0 + per] = np.asarray(res.results[c]["out"])
    return out.astype(np.float32)
```

```python
import contextlib
import numpy as np
import ml_dtypes
import concourse.bass as bass
import concourse.mybir as mybir
from concourse.bass_utils import run_bass_kernel_spmd

F32 = mybir.dt.float32
BF16 = mybir.dt.bfloat16
AF = mybir.ActivationFunctionType
ALU = mybir.AluOpType
AX = mybir.AxisListType

D = 1024
S = 2048
DEPTH = 2
NSEQ = 2
D_IN = 8320
EPS = 1e-6
NT = S // 128

O_AR, O_AK, O_AV, O_XW, O_XA, O_AG = 0, 512, 1024, 1536, 1600, 1664
O_BQ, O_BK, O_BV, O_BG = 2176, 2688, 3200, 3712
O_CU, O_CG = 4224, 4736
O_GA, O_GB, O_GC = 5248, 6272, 7296


class Buf:
    def __init__(self, name):
        self.name = name
        self.last_write = None
        self.reads = []


class Engine:
    EPOCH = 30000

    def __init__(self, fw, name):
        self.fw = fw
        self.name = name
        self.sems = []
        self.count = 0
        self.waited = {}
        self.ops = []
        self.n = 0
        self._new_sem()

    def _new_sem(self):
        s = self.fw.stack.enter_context(self.fw.nc.semaphore(f"s_{self.name}_{len(self.sems)}"))
        self.sems.append(s)
        self.count = 0

    def need(self, ev):
        sem, val = ev
        key = id(sem)
        if self.waited.get(key, 0) >= val:
            return None
        self.waited[key] = val
        return ev


class Fw:
    def __init__(self, nc, stack):
        self.nc = nc
        self.stack = stack
        self.eng = {n: Engine(self, n) for n in ("pe", "act", "dve", "pool", "sp")}
        self.dsem = {}
        for q, k in (("sp", 12), ("act", 6), ("pool", 6)):
            self.dsem[q] = [[stack.enter_context(nc.semaphore(f"d_{q}_{i}")), 0] for i in range(k)]
        self.dnext = {q: 0 for q in self.dsem}

    def _deps(self, e, reads, writes):
        evs = []
        for b in reads:
            if b.last_write is not None:
                evs.append(b.last_write)
        for b in writes:
            if b.last_write is not None:
                evs.append(b.last_write)
            evs.extend(b.reads)
        out = []
        for ev in evs:
            if e.name == "pe" and any(ev[0] is s_ for s_ in e.sems):
                continue
            ev2 = e.need(ev)
            if ev2 is not None:
                out.append(ev2)
        return out

    def op(self, engname, fn, reads=(), writes=(), inc=True):
        e = self.eng[engname]
        waits = self._deps(e, reads, writes)
        if e.count >= Engine.EPOCH and inc:
            e._new_sem()
        sem = e.sems[-1]
        if inc:
            e.count += 1
        ev = (sem, e.count if inc else e.count + 1)
        for b in reads:
            b.reads.append(ev)
        for b in writes:
            b.last_write = ev
            b.reads = []
        e.n += 1

        def run(h, waits=waits, fn=fn, sem=sem, inc=inc):
            for (s, v) in waits[1:]:
                h.wait_ge(s, v)
            ins = fn(h)
            if waits:
                ins._wait_ge(waits[0][0], waits[0][1])
            if inc:
                ins.then_inc(sem, 1)

        e.ops.append(run)
        return ev

    def dma(self, q, out, in_, reads=(), writes=()):
        e = self.eng[q]
        waits = self._deps(e, reads, writes)
        slots = self.dsem[q]
        i = self.dnext[q]
        self.dnext[q] = (i + 1) % len(slots)
        slot = slots[i]
        sem = slot[0]
        prev = slot[1]
        if prev > 0:
            w = e.need((sem, prev))
            if w is not None:
                waits.append(w)
        slot[1] = prev + 16
        ev = (sem, slot[1])
        for b in reads:
            b.reads.append(ev)
        for b in writes:
            b.last_write = ev
            b.reads = []

        def run(h, waits=waits, sem=sem, out=out, in_=in_):
            for (s, v) in waits:
                h.wait_ge(s, v)
            h.dma_start(out=out, in_=in_).then_inc(sem, 16)

        e.ops.append(run)
        return ev

    def wait_all(self, engname, bufs):
        e = self.eng[engname]
        waits = []
        for b in bufs:
            for ev in ([b.last_write] if b.last_write else []) + list(b.reads):
                w = e.need(ev)
                if w is not None:
                    waits.append(w)

        def run(h, waits=waits):
            for (s, v) in waits:
                h.wait_ge(s, v)

        e.ops.append(run)

    def pe(self, fn, reads=(), writes=(), inc=True):
        return self.op("pe", fn, reads, writes, inc)

    def act(self, fn, reads=(), writes=()):
        return self.op("act", fn, reads, writes)

    def dve(self, fn, reads=(), writes=()):
        return self.op("dve", fn, reads, writes)

    def pool(self, fn, reads=(), writes=()):
        return self.op("pool", fn, reads, writes)

    def emit(self):
        nc = self.nc
        with nc.Block() as block:
            @block.tensor
            def _(h):
                for f in self.eng["pe"].ops:
                    f(h)

            @block.scalar
            def _(h):
                for f in self.eng["act"].ops:
                    f(h)

            @block.vector
            def _(h):
                for f in self.eng["dve"].ops:
                    f(h)

            @block.gpsimd
            def _(h):
                for f in self.eng["pool"].ops:
                    f(h)

            @block.sync
            def _(h):
                for f in self.eng["sp"].ops:
                    f(h)


class T:
    def __init__(self, fw, shape, dtype, name, psum=False, nsub=1):
        nc = fw.nc
        if psum:
            self.t = fw.stack.enter_context(nc.psum_tensor("ps_" + name, shape, dtype))
        else:
            self.t = fw.stack.enter_context(nc.sbuf_tensor("sb_" + name, shape, dtype))
        self.b = [Buf(f"{name}.{i}") for i in range(nsub)]
        self.name = name

    def __getitem__(self, idx):
        return self.t[idx]

    @property
    def B(self):
        return self.b[0]


def make_consts():
    c = {}
    c["ident"] = np.eye(128, dtype=np.float32).astype(ml_dtypes.bfloat16)
    bd = np.zeros((128, 128), np.float32)
    bd[:64, :64] = 1.0
    bd[64:, 64:] = 1.0
    c["bd32"] = bd
    c["bdo64"] = (bd / 64.0).astype(ml_dtypes.bfloat16)
    half = 32
    inv = (np.float32(10000.0) ** (-np.arange(half, dtype=np.float32) / np.float32(half))).astype(np.float32)
    pos = np.arange(S, dtype=np.float32)
    ang = (pos[None, :] * inv[:, None]).astype(np.float32).astype(np.float64)
    cos32, sin32 = np.cos(ang), np.sin(ang)
    cosT = np.zeros((128, S), np.float32)
    sinS = np.zeros((128, S), np.float32)
    for p in range(128):
        d = p % 64
        i = d % 32
        cosT[p] = cos32[i]
        sinS[p] = -sin32[i] if d < 32 else sin32[i]
    c["rope_cos"] = cosT
    c["rope_sin"] = sinS
    lg = np.log(1.0 - 2.0 ** (-5.0 - np.arange(8, dtype=np.float64)))
    idx = np.arange(128, dtype=np.float64)
    dmT = np.zeros((4, 128, 256), np.float32)
    kwt = np.zeros((4, 128, 128), np.float32)
    qw = np.zeros((4, 128, 128), np.float32)
    gc = np.zeros((128, 4), np.float32)
    for hp in range(4):
        for hh in range(2):
            g = lg[hp * 2 + hh]
            diff = idx[None, :] - idx[:, None]
            m = np.where(diff >= 0, np.exp(g * np.maximum(diff, 0.0)), 0.0) / 8.0
            dmT[hp, :, hh * 128:(hh + 1) * 128] = m
            kwt[hp, :, hh * 64:(hh + 1) * 64] = (np.exp(g * (127.0 - idx)) / 8.0)[:, None]
            qw[hp, hh * 64:(hh + 1) * 64, :] = np.exp(g * (idx + 1.0))[None, :]
            gc[hh * 64:(hh + 1) * 64, hp] = np.exp(g * 128.0)
    c["ret_dmT"] = dmT
    c["ret_kwt"] = kwt
    c["ret_qw"] = qw
    c["ret_gc"] = gc
    sw = np.zeros((128, 128), np.float32)
    for k in range(128):
        sw[k, (k + 64) % 128] = 1.0
    c["swapb"] = sw.astype(ml_dtypes.bfloat16)
    sg = np.zeros((128, 2), np.float32)
    sg[:64, 0], sg[64:, 0] = -1.0, 1.0
    sg[:64, 1], sg[64:, 1] = 1.0, -1.0
    c["sgn"] = sg
    rm = np.zeros((128, 8), np.float32)
    for p in range(128):
        rm[p, p // 16] = 1.0
    c["rowmask"] = rm
    ii = np.arange(128)
    su = (ii[:, None] < ii[None, :]).astype(np.float32)
    iu = (ii[:, None] <= ii[None, :]).astype(np.float32)
    sl_ = (ii[None, :] < ii[:, None]).astype(np.float32)
    c["rw_masks"] = np.concatenate([su, iu, su, iu, sl_, sl_], axis=1).astype(ml_dtypes.bfloat16)
    return c


CONST_SPECS = {
    "ident": ([128, 128], BF16), "bd32": ([128, 128], F32), "bdo64": ([128, 128], BF16),
    "rope_cos": ([128, S], F32), "rope_sin": ([128, S], F32),
    "ret_dmT": ([4, 128, 256], F32), "ret_kwt": ([4, 128, 128], F32), "ret_qw": ([4, 128, 128], F32),
    "ret_gc": ([128, 4], F32),
    "rw_masks": ([128, 768], BF16),
    "swapb": ([128, 128], BF16), "sgn": ([128, 2], F32), "rowmask": ([128, 8], F32),
}


LIM = {}
WENG = "dve"
N_LAUNCH = 1


def build_program(nlayers=DEPTH, nseq=NSEQ, debug=None, phases="0CABM"):
    debug = debug or {}
    nc = bass.Bass("TRN2", target_bir_lowering=False)
    dr = {}

    def din(name, shape, dt=F32):
        dr[name] = nc.dram_tensor(name, list(shape), dt, kind="ExternalInput").ap()
        return dr[name]

    x_d = din("x", [nseq, S, D])
    pre_norm_d = din("pre_norm", [DEPTH, 128, 8])
    w_in_d = din("w_in", [DEPTH, D, D_IN])
    cd = {k: din(k, shp, dt) for k, (shp, dt) in CONST_SPECS.items()}
    wp_d = [din(n, [DEPTH, 512, D]) for n in ("w_proj_rwkv", "w_proj_ret", "w_proj_s5")]
    wout_d = din("w_out", [DEPTH, D, D])
    bmerge_d = din("b_merge", [DEPTH, 128, 3, 8])
    postn_d = din("post_norm", [DEPTH, D])
    s5A_d = din("s5_Aab", [DEPTH, 3, 128, 32])
    s5B_d = din("s5_Bst", [DEPTH, 2, 128, 512])
    s5C_d = din("s5_Cst", [DEPTH, 2, 128, 512])
    s5v_d = din("s5_vec", [DEPTH, 128, 8])
    gluw_d = din("s5_glu_w", [DEPTH, 512, 512])
    mu_rkv_d = din("rwkv_mu_rkv", [DEPTH, 3, 512])
    mu_wa_d = din("rwkv_mu_wa", [DEPTH, 1, 128])
    w2a2_d = din("rwkv_w2a2", [DEPTH, 2, 64, 512])
    rvec_d = din("rwkv_vec", [DEPTH, 128, 4, 8])
    out_d = nc.dram_tensor("out", [nseq, S, D], F32, kind="ExternalOutput").ap()
    wcache_d = nc.dram_tensor("wcache", [DEPTH, 56, 128, 1024], BF16).ap()
    dbg_d = {}
    for k, shp in debug.items():
        dbg_d[k] = nc.dram_tensor("dbg_" + k, list(shp[0]), shp[1], kind="ExternalOutput").ap()

    with contextlib.ExitStack() as stack:
        fw = Fw(nc, stack)
        x_sb = T(fw, [128, NT, D], F32, "x_sb", nsub=NT)
        hT = T(fw, [128, 8, S + 1], BF16, "hT", nsub=NT + 1)
        ya = T(fw, [128, 4, S], BF16, "ya", nsub=16)
        yb = T(fw, [128, 4, S], BF16, "yb", nsub=16)
        yc = T(fw, [128, 4, S], BF16, "yc", nsub=16)
        ident = T(fw, [128, 128], BF16, "ident")
        bd32 = T(fw, [128, 128], F32, "bd32")
        bdo64 = T(fw, [128, 128], BF16, "bdo64")
        gpre = T(fw, [128, DEPTH, 8], F32, "gpre")
        gexp = T(fw, [128, 8, 128], F32, "gexp")
        NF, NH = 8, 12
        wf = [T(fw, [128, 512], F32, f"wf{i}") for i in range(NF)]
        wh = [T(fw, [128, 512], BF16, f"wh{i}") for i in range(NH)]
        st6 = T(fw, [128, 2, 6], F32, "st6")
        mv = T(fw, [128, 2], F32, "mv")
        e2 = T(fw, [128, 1], F32, "e2")
        rstd = T(fw, [128, 1], F32, "rstd")
        R32t = T(fw, [128, 128], F32, "R32t")
        Rbt = T(fw, [128, 128], BF16, "Rbt")
        gct = T(fw, [128, 4], F32, "gct")
        bmt = T(fw, [128, DEPTH, 3, 8], F32, "bmt")
        swapb = T(fw, [128, 128], BF16, "swapb")
        chl = T(fw, [128, 32], BF16, "chl")
        cbk = T(fw, [128, 8], F32, "cbk")
        sgn = T(fw, [128, 2], F32, "sgn")
        rowmask = T(fw, [128, 8], F32, "rowmask")
        s5v = T(fw, [128, 8], F32, "s5v")
        carry = T(fw, [128, 8], F32, "carry")
        cst = T(fw, [128, 16], F32, "cst")
        onec = T(fw, [128, 1], F32, "onec")
        rvec = T(fw, [128, 4, 8], F32, "rvec")
        omka = T(fw, [128, 4], F32, "omka")
        lw2 = T(fw, [128, 128], BF16, "lw2")
        la2 = T(fw, [128, 128], BF16, "la2")
        St32, Stb = R32t, Rbt
        gpost = T(fw, [128, D], F32, "gpost")
        NSLOT = 7
        wslot = [T(fw, [128, 8, 128], BF16, f"wslot{i}") for i in range(NSLOT)]
        wst = [T(fw, [128, 8, 128], F32, f"wst{i}") for i in range(2)]
        wctr = [0, 0]
        tp_ps = [T(fw, [128, 8, 128], BF16, f"tp{i}", psum=True) for i in range(2)]
        pg = [T(fw, [128, 512], F32, f"pg{i}", psum=True) for i in range(6)]
        pctr = [0]

        def ps_next():
            t = pg[pctr[0] % 4]
            pctr[0] += 1
            return t

        yctr = [0]

        def ps_y():
            t = pg[4 + yctr[0] % 2]
            yctr[0] += 1
            return t

        fw.dma("sp", ident[:], cd["ident"], writes=[ident.B])
        fw.dma("sp", bd32[:], cd["bd32"], writes=[bd32.B])
        fw.dma("sp", bdo64[:], cd["bdo64"], writes=[bdo64.B])
        fw.dma("sp", gpre[:], pre_norm_d.rearrange("l p c -> p l c"), writes=[gpre.B])
        fw.dma("sp", bmt[:], bmerge_d.rearrange("l p b c -> p l b c"), writes=[bmt.B])
        fw.dma("sp", swapb[:], cd["swapb"], writes=[swapb.B])
        fw.dma("sp", sgn[:], cd["sgn"], writes=[sgn.B])
        fw.dma("sp", rowmask[:], cd["rowmask"], writes=[rowmask.B])
        fw.pool(lambda h: h.memset(hT[:, :, 0:1], 0.0), writes=[hT.b[NT]])
        fw.dve(lambda h: h.memset(onec[:], 1.0), writes=[onec.B])

        def hT_bufs(tok0, ntok, shift=0):
            a = tok0 - shift
            bl = []
            if a < 0:
                bl.append(hT.b[NT])
                a = 0
            for tt in range(a // 128, (tok0 - shift + ntok - 1) // 128 + 1):
                bl.append(hT.b[tt])
            return bl

        def layer_setup(l):
            for c in range(8):
                fw.dve(lambda h, c=c: h.tensor_copy(out=gexp[:, c, :], in_=gpre[:, l, c:c + 1].to_broadcast([128, 128])),
                       reads=[gpre.B], writes=[gexp.B])

        def wload(l, segs):
            slot = wslot[wctr[0] % NSLOT]
            wctr[0] += 1
            stg = wst[wctr[1] % 2]
            wctr[1] += 1
            o = 0
            for (c0, n) in segs:
                fw.dma("sp", stg[:, :, o:o + n],
                       w_in_d[l, :, c0:c0 + n].rearrange("(c p) n -> p c n", p=128),
                       writes=[stg.B])
                o += n
            assert o == 128
            fw.op(WENG, lambda h, slot=slot, stg=stg: h.tensor_tensor(out=slot[:], in0=stg[:], in1=gexp[:], op=ALU.mult),
                  reads=[stg.B, gexp.B], writes=[slot.B])
            return slot

        def proj(ps, slot, tok0, ntok=512, shift=0, start=True, stop=True):
            hb = hT_bufs(tok0, ntok, shift)
            for c in range(8):
                fw.pe(lambda h, c=c: h.matmul(out=ps[:, 0:ntok], lhsT=slot[:, c, :],
                                              rhs=hT[:, c, 1 + tok0 - shift:1 + tok0 - shift + ntok],
                                              start=(start and c == 0), stop=(stop and c == 7)),
                      reads=[slot.B] + hb, writes=[ps.B], inc=(c == 7))

        def silu_from_psum(dst, ps):
            fw.act(lambda h: h.activation(out=dst[:], in_=ps[:], func=AF.Sigmoid), reads=[ps.B], writes=[dst.B])
            fw.dve(lambda h: h.tensor_tensor(out=dst[:], in0=ps[:], in1=dst[:], op=ALU.mult),
                   reads=[ps.B, dst.B], writes=[dst.B])

        def phase0(si, l):
            for tt in range(NT):
                xb = x_sb.b[tt]
                xt = x_sb[:, tt, :]
                for j in range(2):
                    fw.dve(lambda h, j=j, xt=xt: h.bn_stats(out=st6[:, j, :], in_=xt[:, j * 512:(j + 1) * 512]),
                           reads=[xb], writes=[st6.B])
                fw.dve(lambda h: h.bn_aggr(out=mv[:], in_=st6[:].rearrange("p a b -> p (a b)")),
                       reads=[st6.B], writes=[mv.B])
                fw.dve(lambda h: h.scalar_tensor_tensor(out=e2[:], in0=mv[:, 0:1], scalar=mv[:, 0:1],
                                                        in1=mv[:, 1:2], op0=ALU.mult, op1=ALU.add),
                       reads=[mv.B], writes=[e2.B])
                fw.act(lambda h: h.activation(out=e2[:], in_=e2[:], func=AF.Sqrt, bias=EPS, scale=1.0),
                       reads=[e2.B], writes=[e2.B])
                fw.dve(lambda h: h.reciprocal(out=rstd[:], in_=e2[:]), reads=[e2.B], writes=[rstd.B])
                for hf in range(2):
                    fw.dve(lambda h, hf=hf, xt=xt: h.tensor_scalar(out=wh[hf][:], in0=xt[:, hf * 512:(hf + 1) * 512],
                                                                   scalar1=rstd[:, 0:1], scalar2=None, op0=ALU.mult),
                           reads=[xb, rstd.B], writes=[wh[hf].B])
                ps = tp_ps[tt % 2]
                for c in range(8):
                    fw.pe(lambda h, ps=ps, c=c: h.transpose(out=ps[:, c, :],
                                                            in_=wh[c // 4][:, (c % 4) * 128:(c % 4 + 1) * 128],
                                                            identity=ident[:]),
                          reads=[wh[c // 4].B, ident.B], writes=[ps.B], inc=(c == 7))
                fw.act(lambda h, ps=ps, tt=tt: h.activation(out=hT[:, :, 1 + tt * 128:1 + (tt + 1) * 128],
                                                            in_=ps[:], func=AF.Copy),
                       reads=[ps.B], writes=[hT.b[tt]])

        def headnorm_gate(y_ps, sg, dst, dstb, eps, affine=None):
            y32, ybf, ycen, sq, rs = wf[0], wh[0], wf[1], wh[1], wf[2]
            fw.act(lambda h: h.activation(out=y32[:], in_=y_ps[:], func=AF.Copy), reads=[y_ps.B], writes=[y32.B])
            fw.dve(lambda h: h.tensor_copy(out=ybf[:], in_=y32[:]), reads=[y32.B], writes=[ybf.B])
            mean_ps = ps_next()
            fw.pe(lambda h: h.matmul(out=mean_ps[:], lhsT=bdo64[:], rhs=ybf[:], start=True, stop=True),
                  reads=[bdo64.B, ybf.B], writes=[mean_ps.B])
            fw.dve(lambda h: h.tensor_tensor(out=ycen[:], in0=y32[:], in1=mean_ps[:], op=ALU.subtract),
                   reads=[y32.B, mean_ps.B], writes=[ycen.B])
            fw.act(lambda h: h.activation(out=sq[:], in_=ycen[:], func=AF.Square), reads=[ycen.B], writes=[sq.B])
            var_ps = ps_next()
            fw.pe(lambda h: h.matmul(out=var_ps[:], lhsT=bdo64[:], rhs=sq[:], start=True, stop=True),
                  reads=[bdo64.B, sq.B], writes=[var_ps.B])
            fw.act(lambda h: h.activation(out=rs[:], in_=var_ps[:], func=AF.Sqrt, bias=eps, scale=1.0),
                   reads=[var_ps.B], writes=[rs.B])
            fw.dve(lambda h: h.reciprocal(out=rs[:], in_=rs[:]), reads=[rs.B], writes=[rs.B])
            fw.dve(lambda h: h.tensor_tensor(out=ycen[:], in0=ycen[:], in1=rs[:], op=ALU.mult),
                    reads=[ycen.B, rs.B], writes=[ycen.B])
            if affine is not None:
                affine(ycen)
            fw.dve(lambda h: h.tensor_tensor(out=dst, in0=ycen[:], in1=sg[:], op=ALU.mult),
                    reads=[ycen.B, sg.B], writes=[dstb])

        def phaseB(si, l):
            dmT = wf[4]
            kwt = wf[5]
            qwt = wf[6]
            fw.dma("sp", gct[:, 0:4], cd["ret_gc"], writes=[gct.B])
            R32 = R32t
            t1, t2 = wf[0], wf[1]
            cosb, sinb = wf[2], wf[3]
            qr, kr, qc, vsb, ktok, vp0, vp1 = wh[2], wh[3], wh[4], wh[5], wh[6], wh[7], wh[8]
            qpad = [wh[9], wh[10]]
            Ssb = wh[11]
            Rb = Rbt
            sgate = wf[7]
            for hp in range(LIM.get('hp', 4)):
                cb = O_BQ + hp * 128
                kb = O_BK + hp * 128
                sw = lambda b: [(b + 32, 32), (b, 32), (b + 96, 32), (b + 64, 32)]
                w_q = wload(l, [(cb, 128)])
                w_qs = wload(l, sw(cb))
                w_k = wload(l, [(kb, 128)])
                w_ks = wload(l, sw(kb))
                w_v = wload(l, [(O_BV + hp * 128, 128)])
                w_g = wload(l, [(O_BG + hp * 128, 128)])
                fw.dma("sp", dmT[:, 0:256], cd["ret_dmT"][hp], writes=[dmT.B])
                fw.dma("sp", kwt[:, 0:128], cd["ret_kwt"][hp], writes=[kwt.B])
                fw.dma("sp", qwt[:, 0:128], cd["ret_qw"][hp], writes=[qwt.B])
                fw.pool(lambda h: h.memset(R32[:], 0.0), writes=[R32.B])
                fw.pool(lambda h: h.memset(Rb[:], 0.0), writes=[Rb.B])
                for qp in qpad:
                    fw.pool(lambda h, qp=qp: h.memset(qp[:], 0.0), writes=[qp.B])
                fw.pool(lambda h: h.memset(vp0[:], 0.0), writes=[vp0.B])
                fw.pool(lambda h: h.memset(vp1[:], 0.0), writes=[vp1.B])
                def do_block(tb, hp=hp, w_q=w_q, w_qs=w_qs, w_k=w_k, w_ks=w_ks, w_v=w_v, w_g=w_g):
                    tok0 = tb * 512
                    fw.dma("sp", cosb[:], cd["rope_cos"][:, tok0:tok0 + 512], writes=[cosb.B])
                    fw.dma("sp", sinb[:], cd["rope_sin"][:, tok0:tok0 + 512], writes=[sinb.B])
                    if LIM.get('stage', 99) < 1:
                        return
                    pq, pqs = ps_next(), ps_next()
                    proj(pq, w_q, tok0)
                    proj(pqs, w_qs, tok0)
                    fw.dve(lambda h: h.tensor_tensor(out=t1[:], in0=pq[:], in1=cosb[:], op=ALU.mult),
                           reads=[pq.B, cosb.B], writes=[t1.B])
                    fw.dve(lambda h: h.tensor_tensor(out=t2[:], in0=pqs[:], in1=sinb[:], op=ALU.mult),
                           reads=[pqs.B, sinb.B], writes=[t2.B])
                    fw.dve(lambda h: h.tensor_tensor(out=qr[:], in0=t1[:], in1=t2[:], op=ALU.add),
                            reads=[t1.B, t2.B], writes=[qr.B])
                    if LIM.get('stage', 99) < 2:
                        return
                    for half in range(2):
                        qp = qpad[half]
                        for hh in range(2):
                            src = qr[hh * 64:(hh + 1) * 64, half * 256:(half + 1) * 256].rearrange("p (c i) -> p c i", c=2)
                            dstv = qp[hh * 64:(hh + 1) * 64, :].rearrange("p (c h i) -> p c h i", c=2, h=2)[:, :, hh, :]
                            fw.act(lambda h, src=src, dstv=dstv: h.activation(out=dstv, in_=src, func=AF.Copy),
                                   reads=[qr.B], writes=[qp.B])
                    for c4 in range(4):
                        fw.dve(lambda h, c4=c4: h.tensor_tensor(out=qc[:, c4 * 128:(c4 + 1) * 128],
                                                                 in0=qr[:, c4 * 128:(c4 + 1) * 128],
                                                                 in1=qwt[:, 0:128], op=ALU.mult),
                                reads=[qr.B, qwt.B], writes=[qc.B])
                    if LIM.get('stage', 99) < 3:
                        return
                    pk, pks = ps_next(), ps_next()
                    proj(pk, w_k, tok0)
                    proj(pks, w_ks, tok0)
                    fw.dve(lambda h: h.tensor_tensor(out=t1[:], in0=pk[:], in1=cosb[:], op=ALU.mult),
                           reads=[pk.B, cosb.B], writes=[t1.B])
                    fw.dve(lambda h: h.tensor_tensor(out=t2[:], in0=pks[:], in1=sinb[:], op=ALU.mult),
                           reads=[pks.B, sinb.B], writes=[t2.B])
                    fw.dve(lambda h: h.tensor_tensor(out=kr[:], in0=t1[:], in1=t2[:], op=ALU.add),
                            reads=[t1.B, t2.B], writes=[kr.B])
                    if LIM.get('stage', 99) < 4:
                        return
                    pv = ps_next()
                    proj(pv, w_v, tok0)
                    fw.act(lambda h: h.activation(out=vsb[:], in_=pv[:], func=AF.Copy), reads=[pv.B], writes=[vsb.B])
                    pgate = ps_next()
                    proj(pgate, w_g, tok0)
                    silu_from_psum(sgate, pgate)
                    if LIM.get('stage', 99) < 5:
                        return
                    tp = tp_ps[0]
                    for c4 in range(4):
                        fw.pe(lambda h, c4=c4: h.transpose(out=tp[:, c4, :], in_=kr[:, c4 * 128:(c4 + 1) * 128],
                                                           identity=ident[:]),
                              reads=[kr.B, ident.B], writes=[tp.B], inc=False)
                    for c4 in range(4):
                        fw.pe(lambda h, c4=c4: h.transpose(out=tp[:, 4 + c4, :], in_=vsb[:, c4 * 128:(c4 + 1) * 128],
                                                           identity=ident[:]),
                              reads=[vsb.B, ident.B], writes=[tp.B], inc=(c4 == 3))
                    for c4 in range(4):
                        fw.dve(lambda h, c4=c4: h.tensor_tensor(out=ktok[:, c4 * 128:(c4 + 1) * 128], in0=tp[:, c4, :],
                                                                in1=kwt[:, 0:128], op=ALU.mult),
                               reads=[tp.B, kwt.B], writes=[ktok.B])
                    fw.act(lambda h: h.activation(
                        out=vp0[:].rearrange("p (c f) -> p c f", c=4)[:, :, 0:64], in_=tp[:, 4:8, 0:64], func=AF.Copy),
                        reads=[tp.B], writes=[vp0.B])
                    fw.act(lambda h: h.activation(
                        out=vp1[:].rearrange("p (c f) -> p c f", c=4)[:, :, 64:128], in_=tp[:, 4:8, 64:128], func=AF.Copy),
                        reads=[tp.B], writes=[vp1.B])
                    if LIM.get('stage', 99) < 6:
                        return
                    y_ps = ps_y()

                    def do_chunk(c4):
                        cs = slice(c4 * 128, (c4 + 1) * 128)
                        sc = ps_next()
                        qp = qpad[c4 // 2]
                        fw.pe(lambda h, cs=cs, qp=qp, c4=c4, sc=sc: h.matmul(
                            out=sc[:, 0:256], lhsT=kr[:, cs], rhs=qp[:, (c4 % 2) * 256:(c4 % 2) * 256 + 256],
                            start=True, stop=True), reads=[kr.B, qp.B], writes=[sc.B])
                        sv = Ssb[:, (c4 % 2) * 256:(c4 % 2) * 256 + 256]
                        fw.dve(lambda h, sc=sc, sv=sv: h.tensor_tensor(out=sv, in0=sc[:, 0:256], in1=dmT[:, 0:256],
                                                                       op=ALU.mult),
                               reads=[sc.B, dmT.B], writes=[Ssb.B])
                        fw.pe(lambda h, cs=cs, sv=sv: h.matmul(out=y_ps[:, cs], lhsT=vp0[:, cs], rhs=sv[:, 0:128],
                                                               start=True, stop=False),
                              reads=[vp0.B, Ssb.B], writes=[y_ps.B], inc=False)
                        fw.pe(lambda h, cs=cs, sv=sv: h.matmul(out=y_ps[:, cs], lhsT=vp1[:, cs], rhs=sv[:, 128:256],
                                                               start=False, stop=False),
                              reads=[vp1.B, Ssb.B], writes=[y_ps.B], inc=False)
                        fw.pe(lambda h, cs=cs: h.matmul(out=y_ps[:, cs], lhsT=Rb[:], rhs=qc[:, cs],
                                                        start=False, stop=True),
                              reads=[Rb.B, qc.B], writes=[y_ps.B])
                        kv = ps_next()
                        fw.pe(lambda h, cs=cs, kv=kv: h.matmul(out=kv[:, 0:128], lhsT=ktok[:, cs], rhs=vp0[:, cs],
                                                               start=True, stop=False),
                              reads=[ktok.B, vp0.B], writes=[kv.B], inc=False)
                        fw.pe(lambda h, cs=cs, kv=kv: h.matmul(out=kv[:, 0:128], lhsT=ktok[:, cs], rhs=vp1[:, cs],
                                                               start=False, stop=True),
                              reads=[ktok.B, vp1.B], writes=[kv.B])
                        fw.dve(lambda h, kv=kv, hp=hp: h.scalar_tensor_tensor(
                            out=R32[:], in0=R32[:], scalar=gct[:, hp:hp + 1], in1=kv[:, 0:128],
                            op0=ALU.mult, op1=ALU.add), reads=[R32.B, gct.B, kv.B], writes=[R32.B])
                        fw.dve(lambda h: h.tensor_tensor(out=Rb[:], in0=R32[:], in1=bd32[:],
                                                          op=ALU.mult),
                                reads=[R32.B, bd32.B], writes=[Rb.B])
                    for c4 in range(LIM.get('c4', 4)):
                        do_chunk(c4)
                    if LIM.get('stage', 99) < 7:
                        return
                    headnorm_gate(y_ps, sgate, yb[:, hp, tok0:tok0 + 512], yb.b[hp * 4 + tb], EPS)

                for tb in range(LIM.get('tb', 4)):
                    do_block(tb)

        def phaseC(si, l):
            PA, PB = wf[0], wf[1]
            sl = lambda t, i: t[:, i * 32:(i + 1) * 32]
            A_RE, A_IM, DT, MAG, ANG, CC, SS, T1, T2, T3, PM, RDEN, CRE, CIM, QQ, SLS = range(16)
            tabs = ya.b + yb.b
            cosT = ya[:].rearrange("p a s -> p (a s)").bitcast(F32).rearrange("p (g j) -> p g j", g=32)
            sinT = yb[:].rearrange("p a s -> p (a s)").bitcast(F32).rearrange("p (g j) -> p g j", g=32)
            ycf = yc[:].rearrange("p a s -> p (a s)").bitcast(F32)
            tmp1 = ycf[:, 0:2048].rearrange("p (g j) -> p g j", g=32)
            tmp2 = ycf[:, 2048:4096].rearrange("p (g j) -> p g j", g=32)

            def pa(fn_, eng="dve", extra=()):
                fw.op(eng, fn_, reads=[PA.B, PB.B] + list(extra), writes=[PA.B, PB.B])

            def tt(o, a, b, op):
                pa(lambda h: h.tensor_tensor(out=o, in0=a, in1=b, op=op))

            P = lambda i: sl(PA, i)
            for i in range(3):
                fw.dma("sp", P(i), s5A_d[l, i], writes=[PA.B])
            fw.dma("sp", s5v[:], s5v_d[l], writes=[s5v.B])
            pa(lambda h: h.activation(out=P(DT), in_=P(DT), func=AF.Exp), "act")
            tt(P(T1), P(DT), P(A_RE), ALU.mult)
            pa(lambda h: h.activation(out=P(MAG), in_=P(T1), func=AF.Exp), "act")
            tt(P(ANG), P(DT), P(A_IM), ALU.mult)
            pa(lambda h: h.activation(out=P(SS), in_=P(ANG), func=AF.Sin, scale=1.0 / 16.0), "act")
            pa(lambda h: h.activation(out=P(CC), in_=P(ANG), func=AF.Sin, scale=1.0 / 16.0, bias=float(np.pi / 2)), "act")

            def dbl(co, so, ci, si_):
                tt(P(T1), ci, ci, ALU.mult)
                tt(P(T2), si_, si_, ALU.mult)
                tt(P(T3), si_, ci, ALU.mult)
                tt(co, P(T1), P(T2), ALU.subtract)
                pa(lambda h: h.tensor_scalar(out=so, in0=P(T3), scalar1=2.0, scalar2=None, op0=ALU.mult))

            for _ in range(3):
                dbl(P(CC), P(SS), P(CC), P(SS))
            dbl(sl(PB, 0), sl(PB, 8), P(CC), P(SS))
            for k in range(1, 8):
                dbl(sl(PB, k), sl(PB, 8 + k), sl(PB, k - 1), sl(PB, 8 + k - 1))
            pa(lambda h: h.tensor_scalar(out=P(SLS), in0=sl(PB, 15), scalar1=sgn[:, 0:1], scalar2=None, op0=ALU.mult),
               extra=[sgn.B])
            tt(P(PM), P(MAG), sl(PB, 0), ALU.mult)
            pa(lambda h: h.tensor_scalar(out=P(PM), in0=P(PM), scalar1=-1.0, scalar2=None, op0=ALU.add))
            tt(P(QQ), P(MAG), sl(PB, 8), ALU.mult)
            tt(P(T1), P(A_RE), P(A_RE), ALU.mult)
            tt(P(T2), P(A_IM), P(A_IM), ALU.mult)
            tt(P(T1), P(T1), P(T2), ALU.add)
            pa(lambda h: h.reciprocal(out=P(RDEN), in_=P(T1)))
            tt(P(T1), P(PM), P(A_RE), ALU.mult)
            tt(P(T2), P(QQ), P(A_IM), ALU.mult)
            tt(P(T1), P(T1), P(T2), ALU.add)
            tt(P(CRE), P(T1), P(RDEN), ALU.mult)
            tt(P(T1), P(QQ), P(A_RE), ALU.mult)
            tt(P(T2), P(PM), P(A_IM), ALU.mult)
            tt(P(T1), P(T1), P(T2), ALU.subtract)
            tt(P(CIM), P(T1), P(RDEN), ALU.mult)

            fw.dve(lambda h: h.memset(cosT[:, :, 0:1], 1.0), writes=tabs)
            fw.dve(lambda h: h.memset(sinT[:, :, 0:1], 0.0), writes=tabs)
            for k in range(7):
                m = 1 << k
                cmb = sl(PB, k).unsqueeze(2).to_broadcast([128, 32, m])
                smb = sl(PB, 8 + k).unsqueeze(2).to_broadcast([128, 32, m])

                def lvl(m=m, cmb=cmb, smb=smb):
                    rw = dict(reads=tabs + yc.b + [PB.B], writes=tabs + yc.b)
                    fw.dve(lambda h: h.tensor_tensor(out=tmp1[:, :, 0:m], in0=cosT[:, :, 0:m], in1=cmb, op=ALU.mult), **rw)
                    fw.dve(lambda h: h.tensor_tensor(out=tmp2[:, :, 0:m], in0=sinT[:, :, 0:m], in1=smb, op=ALU.mult), **rw)
                    fw.dve(lambda h: h.tensor_tensor(out=cosT[:, :, m:2 * m], in0=tmp1[:, :, 0:m], in1=tmp2[:, :, 0:m],
                                                     op=ALU.subtract), **rw)
                    fw.dve(lambda h: h.tensor_tensor(out=tmp1[:, :, 0:m], in0=sinT[:, :, 0:m], in1=cmb, op=ALU.mult), **rw)
                    fw.dve(lambda h: h.tensor_tensor(out=tmp2[:, :, 0:m], in0=cosT[:, :, 0:m], in1=smb, op=ALU.mult), **rw)
                    fw.dve(lambda h: h.tensor_tensor(out=sinT[:, :, m:2 * m], in0=tmp1[:, :, 0:m], in1=tmp2[:, :, 0:m],
                                                     op=ALU.add), **rw)
                lvl()

            xh = [wf[2], wf[3]]
            stg = wf[4]
            stg2 = wf[5]
            tri = wf[6]
            BmT = wh[0:4]
            CmT = wh[4:8]
            u_bf, g12 = wh[8], wh[9]
            for t_ in CmT:
                fw.dve(lambda h, t_=t_: h.memset(t_[:], 0.0), writes=[t_.B])

            def xh_ap(g8):
                return xh[g8 // 4][:, (g8 % 4) * 128:(g8 % 4 + 1) * 128]

            def do_gc(gc):
                gs = slice(gc * 128, (gc + 1) * 128)
                fw.dma("sp", stg[:, 0:128], s5B_d[l, 0, :, gs], writes=[stg.B])
                fw.dma("sp", stg[:, 128:256], s5B_d[l, 1, :, gs], writes=[stg.B])
                v3 = lambda ap: ap.rearrange("p (g q) -> p g q", g=8)
                creb = P(CRE)[:, gc * 8:(gc + 1) * 8].unsqueeze(2).to_broadcast([128, 8, 16])
                cimb = P(CIM)[:, gc * 8:(gc + 1) * 8].unsqueeze(2).to_broadcast([128, 8, 16])
                rw = dict(reads=[stg.B, stg2.B, PA.B], writes=[stg2.B])
                bre, bim = v3(stg[:, 0:128]), v3(stg[:, 128:256])
                ta, tb_ = v3(stg2[:, 0:128]), v3(stg2[:, 128:256])
                bbre, bbim = v3(g12[:, 0:128]), v3(g12[:, 128:256])
                rwb = dict(reads=[stg.B, stg2.B, PA.B], writes=[g12.B])
                fw.dve(lambda h: h.tensor_tensor(out=ta, in0=bre, in1=creb, op=ALU.mult), **rw)
                fw.dve(lambda h: h.tensor_tensor(out=tb_, in0=bim, in1=cimb, op=ALU.mult), **rw)
                fw.dve(lambda h: h.tensor_tensor(out=bbre, in0=ta, in1=tb_, op=ALU.subtract), **rwb)
                fw.dve(lambda h: h.tensor_tensor(out=ta, in0=bim, in1=creb, op=ALU.mult), **rw)
                fw.dve(lambda h: h.tensor_tensor(out=tb_, in0=bre, in1=cimb, op=ALU.mult), **rw)
                fw.dve(lambda h: h.tensor_tensor(out=bbim, in0=ta, in1=tb_, op=ALU.add), **rwb)
                tpx = tp_ps[0]
                ptr, pti = tpx[:, 0, :], tpx[:, 1, :]
                fw.pe(lambda h: h.transpose(out=ptr, in_=g12[:, 0:128], identity=ident[:]),
                      reads=[g12.B, ident.B], writes=[tpx.B], inc=False)
                fw.pe(lambda h: h.transpose(out=pti, in_=g12[:, 128:256], identity=ident[:]),
                      reads=[g12.B, ident.B], writes=[tpx.B])
                fw.act(lambda h: h.activation(out=tri[:, 0:64], in_=ptr[:, 0:64], func=AF.Copy), reads=[tpx.B], writes=[tri.B])
                fw.act(lambda h: h.activation(out=tri[:, 64:128], in_=pti[:, 0:64], func=AF.Copy), reads=[tpx.B], writes=[tri.B])
                fw.act(lambda h: h.activation(out=tri[:, 128:192], in_=pti[:, 0:64], func=AF.Copy), reads=[tpx.B], writes=[tri.B])
                fw.act(lambda h: h.activation(out=tri[:, 192:256], in_=ptr[:, 0:64], func=AF.Copy, scale=-1.0),
                       reads=[tpx.B], writes=[tri.B])
                for g8 in range(8):
                    bt = BmT[g8 // 2]
                    o = (g8 % 2) * 256
                    fw.dve(lambda h, bt=bt, o=o, g8=g8: h.tensor_scalar(
                        out=bt[:, o:o + 256], in0=tri[:, 0:256], scalar1=rowmask[:, g8:g8 + 1], scalar2=None, op0=ALU.mult),
                        reads=[tri.B, rowmask.B], writes=[bt.B])
                fw.dma("sp", stg[:, 0:128], s5C_d[l, 0, :, gs], writes=[stg.B])
                fw.dma("sp", stg[:, 128:256], s5C_d[l, 1, :, gs], writes=[stg.B])
                fw.dve(lambda h: h.tensor_scalar(out=stg[:, 0:128], in0=stg[:, 0:128], scalar1=sgn[:, 1:2], scalar2=None,
                                                 op0=ALU.mult), reads=[stg.B, sgn.B], writes=[stg.B])
                fw.dve(lambda h: h.tensor_scalar(out=stg[:, 128:256], in0=stg[:, 128:256], scalar1=-1.0, scalar2=None,
                                                 op0=ALU.mult), reads=[stg.B], writes=[stg.B])
                for g8 in range(8):
                    ct = CmT[g8 // 2]
                    o = (g8 % 2) * 256
                    for ver in range(2):
                        fw.act(lambda h, ct=ct, o=o, g8=g8, ver=ver: h.activation(
                            out=ct[:, o + ver * 128 + g8 * 16:o + ver * 128 + (g8 + 1) * 16],
                            in_=stg[:, ver * 128 + g8 * 16:ver * 128 + (g8 + 1) * 16], func=AF.Copy),
                            reads=[stg.B], writes=[ct.B])
                w_u = wload(l, [(O_CU + gc * 128, 128)])

                def do_block(tb):
                    tok0 = tb * 512
                    pu = ps_next()
                    proj(pu, w_u, tok0)
                    u32 = wf[7]
                    fw.act(lambda h: h.activation(out=u_bf[:], in_=pu[:], func=AF.Copy), reads=[pu.B], writes=[u_bf.B])
                    fw.act(lambda h: h.activation(out=u32[:], in_=pu[:], func=AF.Copy), reads=[pu.B], writes=[u32.B])
                    y_ps = ps_y()

                    def do_sb(sb):
                        ts = slice(sb * 128, (sb + 1) * 128)
                        first = (tb == 0 and sb == 0)

                        def do_batch(bi):
                            g0 = gc * 8 + bi * 4
                            bun, bus = ps_next(), ps_next()
                            for q in range(4):
                                g8 = bi * 4 + q
                                bt = BmT[g8 // 2]
                                o = (g8 % 2) * 256
                                fw.pe(lambda h, q=q, bt=bt, o=o: h.matmul(out=bun[:, q * 128:(q + 1) * 128], lhsT=bt[:, o:o + 128],
                                                                          rhs=u_bf[:, ts], start=True, stop=True),
                                      reads=[bt.B, u_bf.B], writes=[bun.B], inc=(q == 3))
                            for q in range(4):
                                g8 = bi * 4 + q
                                bt = BmT[g8 // 2]
                                o = (g8 % 2) * 256
                                fw.pe(lambda h, q=q, bt=bt, o=o: h.matmul(out=bus[:, q * 128:(q + 1) * 128],
                                                                          lhsT=bt[:, o + 128:o + 256], rhs=u_bf[:, ts],
                                                                          start=True, stop=True),
                                      reads=[bt.B, u_bf.B], writes=[bus.B], inc=(q == 3))
                            w1, w2 = stg, stg2
                            cs4 = cosT[:, g0:g0 + 4, :]
                            sn4 = sinT[:, g0:g0 + 4, :]
                            v4 = lambda ap: ap.rearrange("p (g j) -> p g j", g=4)
                            fw.dve(lambda h: h.tensor_tensor(out=v4(w1[:]), in0=v4(bun[:]), in1=cs4, op=ALU.mult),
                                   reads=[bun.B] + tabs, writes=[w1.B])
                            fw.dve(lambda h: h.tensor_tensor(out=v4(w2[:]), in0=v4(bus[:]), in1=sn4, op=ALU.mult),
                                   reads=[bus.B] + tabs, writes=[w2.B])
                            fw.dve(lambda h: h.tensor_tensor(out=w1[:], in0=w1[:], in1=w2[:], op=ALU.add),
                                   reads=[w1.B, w2.B], writes=[w1.B])
                            xt_ = xh[bi]
                            for q in range(4):
                                g8 = bi * 4 + q
                                g = gc * 8 + g8
                                init = 0.0 if first else carry[:, g8:g8 + 1]
                                fw.dve(lambda h, q=q, g=g, init=init: h.tensor_tensor_scan(
                                    out=xt_[:, q * 128:(q + 1) * 128], data0=P(MAG)[:, g:g + 1].to_broadcast([128, 128]),
                                    data1=w1[:, q * 128:(q + 1) * 128], initial=init, op0=ALU.mult, op1=ALU.add),
                                    reads=[w1.B, PA.B, carry.B], writes=[xt_.B])
                            G1, G2 = g12, wh[10]
                            fw.dve(lambda h: h.tensor_tensor(out=v4(G1[:]), in0=v4(xt_[:]), in1=cs4, op=ALU.mult),
                                   reads=[xt_.B] + tabs, writes=[G1.B])
                            fw.dve(lambda h: h.tensor_tensor(out=v4(G2[:]), in0=v4(xt_[:]), in1=sn4, op=ALU.mult),
                                   reads=[xt_.B] + tabs, writes=[G2.B])
                            for q in range(4):
                                g8 = bi * 4 + q
                                ct = CmT[g8 // 2]
                                o = (g8 % 2) * 256
                                fw.pe(lambda h, q=q, ct=ct, o=o, g8=g8: h.matmul(
                                    out=y_ps[:, ts], lhsT=ct[:, o:o + 128], rhs=G1[:, q * 128:(q + 1) * 128],
                                    start=(g8 == 0), stop=False), reads=[ct.B, G1.B], writes=[y_ps.B], inc=(q == 3))
                            for q in range(4):
                                g8 = bi * 4 + q
                                ct = CmT[g8 // 2]
                                o = (g8 % 2) * 256
                                fw.pe(lambda h, q=q, ct=ct, o=o, g8=g8: h.matmul(
                                    out=y_ps[:, ts], lhsT=ct[:, o + 128:o + 256], rhs=G2[:, q * 128:(q + 1) * 128],
                                    start=False, stop=(g8 == 7)), reads=[ct.B, G2.B], writes=[y_ps.B], inc=(q == 3))

                        for bi in range(2):
                            do_batch(bi)
                        csw = ps_next()
                        for hx in range(2):
                            xl = xh[hx][:].rearrange("p (g j) -> p g j", g=4)[:, :, 127]
                            fw.dve(lambda h, hx=hx, xl=xl: h.tensor_copy(out=chl[:, hx * 4:(hx + 1) * 4], in_=xl),
                                   reads=[xh[hx].B], writes=[chl.B])
                        fw.dve(lambda h: h.tensor_copy(out=cbk[:], in_=chl[:, 0:8]), reads=[chl.B], writes=[cbk.B])
                        for hx in range(2):
                            xl = xh[hx][:].rearrange("p (g j) -> p g j", g=4)[:, :, 127]
                            fw.dve(lambda h, hx=hx, xl=xl: h.tensor_tensor(out=chl[:, 8 + hx * 4:8 + (hx + 1) * 4], in0=xl,
                                                                          in1=cbk[:, hx * 4:(hx + 1) * 4], op=ALU.subtract),
                                   reads=[xh[hx].B, cbk.B], writes=[chl.B])
                        fw.pe(lambda h: h.matmul(out=csw[:, 0:8], lhsT=swapb[:], rhs=chl[:, 0:8], start=True, stop=False),
                              reads=[swapb.B, chl.B], writes=[csw.B], inc=False)
                        fw.pe(lambda h: h.matmul(out=csw[:, 0:8], lhsT=swapb[:], rhs=chl[:, 8:16], start=False, stop=True),
                              reads=[swapb.B, chl.B], writes=[csw.B])
                        fw.dve(lambda h: h.tensor_tensor(out=cst[:, 0:8], in0=csw[:, 0:8], in1=P(SLS)[:, gc * 8:(gc + 1) * 8],
                                                         op=ALU.mult), reads=[csw.B, PA.B], writes=[cst.B])
                        for hx in range(2):
                            xl = xh[hx][:].rearrange("p (g j) -> p g j", g=4)[:, :, 127]
                            fw.dve(lambda h, hx=hx, xl=xl: h.tensor_tensor(
                                out=cst[:, 8 + hx * 4:8 + (hx + 1) * 4], in0=xl,
                                in1=sl(PB, 7)[:, gc * 8 + hx * 4:gc * 8 + (hx + 1) * 4], op=ALU.mult),
                                reads=[xh[hx].B, PB.B], writes=[cst.B])
                        fw.dve(lambda h: h.tensor_tensor(out=carry[:], in0=cst[:, 0:8], in1=cst[:, 8:16], op=ALU.add),
                               reads=[cst.B], writes=[carry.B])

                    for sb in range(4):
                        do_sb(sb)
                    y32, gt = wf[6], wf[7]
                    fw.dve(lambda h: h.scalar_tensor_tensor(out=y32[:], in0=u32[:], scalar=s5v[:, gc:gc + 1], in1=y_ps[:],
                                                            op0=ALU.mult, op1=ALU.add),
                           reads=[u32.B, s5v.B, y_ps.B], writes=[y32.B])
                    fw.act(lambda h: h.activation(out=gt[:], in_=y32[:], func=AF.Square), reads=[y32.B], writes=[gt.B])
                    fw.dve(lambda h: h.tensor_scalar(out=gt[:], in0=gt[:], scalar1=0.044715, scalar2=1.0, op0=ALU.mult,
                                                     op1=ALU.add), reads=[gt.B], writes=[gt.B])
                    fw.dve(lambda h: h.tensor_tensor(out=gt[:], in0=gt[:], in1=y32[:], op=ALU.mult),
                           reads=[gt.B, y32.B], writes=[gt.B])
                    fw.act(lambda h: h.activation(out=gt[:], in_=gt[:], func=AF.Tanh, scale=0.7978845608028654),
                           reads=[gt.B], writes=[gt.B])
                    fw.dve(lambda h: h.tensor_scalar(out=gt[:], in0=gt[:], scalar1=1.0, scalar2=0.5, op0=ALU.add,
                                                     op1=ALU.mult), reads=[gt.B], writes=[gt.B])
                    fw.dve(lambda h: h.tensor_tensor(out=yc[:, gc, tok0:tok0 + 512], in0=gt[:], in1=y32[:], op=ALU.mult),
                           reads=[gt.B, y32.B], writes=[yc.b[gc * 4 + tb]])

                for tb in range(LIM.get('tb', 4)):
                    do_block(tb)

            for gc in range(4):
                do_gc(gc)

            gw = wh[0:4]
            for c in range(4):
                fw.dma("sp", stg[:], gluw_d[l, c * 128:(c + 1) * 128, :], writes=[stg.B])
                fw.dve(lambda h, c=c: h.tensor_copy(out=gw[c][:], in_=stg[:]), reads=[stg.B], writes=[gw[c].B])
            wgs = [wload(l, [(O_CG + oc * 128, 128)]) for oc in range(4)]

            def glu_block(tb):
                tok0 = tb * 512
                sgl = [wf[0], wf[1], wf[2], wf[3]]
                for oc in range(4):
                    gp = pg[oc]
                    for c in range(4):
                        fw.pe(lambda h, oc=oc, c=c, gp=gp: h.matmul(out=gp[:], lhsT=gw[c][:, oc * 128:(oc + 1) * 128],
                                                                    rhs=yc[:, c, tok0:tok0 + 512], start=(c == 0), stop=(c == 3)),
                              reads=[gw[c].B, yc.b[c * 4 + tb]], writes=[gp.B], inc=(c == 3))
                    fw.act(lambda h, oc=oc, gp=gp: h.activation(out=sgl[oc][:], in_=gp[:], func=AF.Sigmoid,
                                                                bias=s5v[:, 4 + oc:5 + oc], scale=1.0),
                           reads=[gp.B, s5v.B], writes=[sgl[oc].B])
                for oc in range(4):
                    pgt = ps_y()
                    proj(pgt, wgs[oc], tok0)
                    sgt = wf[4]
                    silu_from_psum(sgt, pgt)
                    fw.dve(lambda h, oc=oc, sgt=sgt: h.tensor_tensor(out=sgt[:], in0=sgt[:], in1=sgl[oc][:], op=ALU.mult),
                           reads=[sgt.B, sgl[oc].B], writes=[sgt.B])
                    fw.dve(lambda h, oc=oc, sgt=sgt: h.tensor_tensor(out=yc[:, oc, tok0:tok0 + 512],
                                                                     in0=yc[:, oc, tok0:tok0 + 512], in1=sgt[:], op=ALU.mult),
                           reads=[sgt.B, yc.b[oc * 4 + tb]], writes=[yc.b[oc * 4 + tb]])

            for tb in range(LIM.get('tb', 4)):
                glu_block(tb)

        def wload_shift(l, c0, mu_src):
            s1 = wslot[wctr[0] % NSLOT]
            wctr[0] += 1
            s2 = wslot[wctr[0] % NSLOT]
            wctr[0] += 1
            stg = wst[wctr[1] % 2]
            wctr[1] += 1
            mub = wf[2]
            fw.dma("sp", mub[:, 0:128], mu_src.broadcast_to([128, 128]), writes=[mub.B])
            fw.dve(lambda h: h.tensor_scalar(out=mub[:, 128:256], in0=mub[:, 0:128], scalar1=-1.0, scalar2=1.0,
                                             op0=ALU.mult, op1=ALU.add), reads=[mub.B], writes=[mub.B])
            fw.dma("sp", stg[:], w_in_d[l, :, c0:c0 + 128].rearrange("(c p) n -> p c n", p=128), writes=[stg.B])
            fw.dve(lambda h: h.tensor_tensor(out=stg[:], in0=stg[:], in1=gexp[:], op=ALU.mult),
                   reads=[stg.B, gexp.B], writes=[stg.B])
            fw.dve(lambda h: h.tensor_tensor(out=s2[:], in0=stg[:], in1=mub[:, 0:128].unsqueeze(1).to_broadcast([128, 8, 128]),
                                             op=ALU.mult), reads=[stg.B, mub.B], writes=[s2.B])
            fw.dve(lambda h: h.tensor_tensor(out=s1[:], in0=stg[:], in1=mub[:, 128:256].unsqueeze(1).to_broadcast([128, 8, 128]),
                                             op=ALU.mult), reads=[stg.B, mub.B], writes=[s1.B])
            return s1, s2

        def proj_shift(ps, s12, tok0):
            proj(ps, s12[0], tok0, shift=0, start=True, stop=False)
            proj(ps, s12[1], tok0, shift=1, start=False, stop=True)

        def phaseA(si, l):
            CDEC = 0.6065306597126334
            ybf = yb[:].rearrange("p a s -> p (a s)")
            SB = [Buf(f"scrA{i}") for i in range(16)]
            SV = [ybf[:, i * 512:(i + 1) * 512] for i in range(16)]
            fw.dve(lambda h: h.memset(cst[:, 0:1], 0.0), reads=yb.b, writes=SB + [cst.B])
            QP = [SV[i] for i in range(4)]
            QPB = SB[0:4]
            MK1, MK1B = SV[4], SB[4]
            MK2, MK2B = SV[5][:, 0:256], SB[5]
            E1, E2, E3 = SV[6], SV[7], SV[8]
            XB = [SV[9], SV[10]]
            PP = SV[11]
            MISC = SV[12]
            kt_tok, bt_tok, vp0 = SV[13], SV[14], SV[15]
            vp1 = wh[9]
            v_bf, sqk, rk_bf, rt, at, kt, bt = wh[2], wh[3], wh[4], wh[5], wh[6], wh[7], wh[8]
            sgate, Pq, r32, wf6, wf7, wf0, wf1, wf2 = wf[3], wf[4], wf[5], wf[6], wf[7], wf[0], wf[1], wf[2]
            fw.dma("sp", MK1, cd["rw_masks"][:, 0:512], writes=[MK1B])
            fw.dma("sp", MK2, cd["rw_masks"][:, 512:768], writes=[MK2B])
            fw.dma("sp", rvec[:], rvec_d[l], writes=[rvec.B])
            fw.dve(lambda h: h.tensor_scalar(out=omka[:], in0=rvec[:, :, 3], scalar1=-1.0, scalar2=1.0, op0=ALU.mult,
                                             op1=ALU.add), reads=[rvec.B], writes=[omka.B])
            for i in range(4):
                fw.dve(lambda h, i=i: h.memset(QP[i], 0.0), writes=[QPB[i]])
            fw.dve(lambda h: h.memset(vp0, 0.0), writes=[SB[15]])
            fw.dve(lambda h: h.memset(vp1[:], 0.0), writes=[vp1.B])
            fw.dve(lambda h: h.memset(MISC, 0.0), writes=[SB[12]])
            fw.dve(lambda h: h.memset(lw2[:], 0.0), writes=[lw2.B])
            fw.dve(lambda h: h.memset(la2[:], 0.0), writes=[la2.B])
            w_wa = wload_shift(l, O_XW, mu_wa_d[l])
            for tb in range(4):
                pwa = ps_next()
                proj_shift(pwa, w_wa, tb * 512)
                dst = ya[:, 3, tb * 512:(tb + 1) * 512]
                fw.act(lambda h, pwa=pwa, dst=dst: h.activation(out=dst[0:64, :], in_=pwa[0:64, :], func=AF.Tanh),
                       reads=[pwa.B], writes=[ya.b[12 + tb]])
                fw.act(lambda h, pwa=pwa, dst=dst: h.activation(out=dst[64:128, :], in_=pwa[64:128, :], func=AF.Copy),
                       reads=[pwa.B], writes=[ya.b[12 + tb]])

            def do_hp(hp):
                V = lambda j: rvec[:, hp, j:j + 1]
                W0, A0, KK, KA, LNW, LNB, RK = (V(j) for j in range(7))
                w_r = wload_shift(l, O_AR + hp * 128, mu_rkv_d[l, 0:1, hp * 128:(hp + 1) * 128])
                w_k = wload_shift(l, O_AK + hp * 128, mu_rkv_d[l, 1:2, hp * 128:(hp + 1) * 128])
                w_v = wload_shift(l, O_AV + hp * 128, mu_rkv_d[l, 2:3, hp * 128:(hp + 1) * 128])
                w_g = wload(l, [(O_AG + hp * 128, 128)])
                stg = wst[wctr[1] % 2]
                wctr[1] += 1
                stv = stg[:].rearrange("p c n -> p (c n)")
                fw.dma("sp", stv[0:64, 0:128], w2a2_d[l, 0, :, hp * 128:(hp + 1) * 128], writes=[stg.B])
                fw.dma("sp", stv[64:128, 0:128], w2a2_d[l, 1, :, hp * 128:(hp + 1) * 128], writes=[stg.B])
                fw.dve(lambda h: h.tensor_copy(out=lw2[0:64, :], in_=stv[0:64, 0:128]), reads=[stg.B], writes=[lw2.B])
                fw.dve(lambda h: h.tensor_copy(out=la2[64:128, :], in_=stv[64:128, 0:128]), reads=[stg.B], writes=[la2.B])
                fw.dve(lambda h: h.memset(St32[:], 0.0), writes=[St32.B])
                fw.dve(lambda h: h.memset(Stb[:], 0.0), writes=[Stb.B])

                def do_block(tb):
                    tok0 = tb * 512
                    twa = ya[:, 3, tok0:tok0 + 512]
                    twab = ya.b[12 + tb]
                    pr, pk = ps_next(), ps_next()
                    proj_shift(pr, w_r, tok0)
                    proj_shift(pk, w_k, tok0)
                    fw.act(lambda h: h.activation(out=r32[:], in_=pr[:], func=AF.Copy), reads=[pr.B], writes=[r32.B])
                    pw_, pa_ = ps_next(), ps_next()
                    fw.pe(lambda h: h.matmul(out=pw_[:], lhsT=lw2[:], rhs=twa, start=True, stop=True),
                          reads=[lw2.B, twab], writes=[pw_.B])
                    fw.pe(lambda h: h.matmul(out=pa_[:], lhsT=la2[:], rhs=twa, start=True, stop=True),
                          reads=[la2.B, twab], writes=[pa_.B])
                    sg, aa = wf0, wf6
                    fw.act(lambda h: h.activation(out=sg[:], in_=pw_[:], func=AF.Sigmoid, bias=W0, scale=1.0),
                           reads=[pw_.B, rvec.B], writes=[sg.B])
                    fw.act(lambda h: h.activation(out=aa[:], in_=pa_[:], func=AF.Sigmoid, bias=A0, scale=1.0),
                           reads=[pa_.B, rvec.B], writes=[aa.B])
                    kkn = wf7
                    fw.dve(lambda h: h.tensor_scalar(out=kkn[:], in0=pk[:], scalar1=KK, scalar2=None, op0=ALU.mult),
                           reads=[pk.B, rvec.B], writes=[kkn.B])
                    fw.act(lambda h: h.activation(out=sqk[:], in_=kkn[:], func=AF.Square), reads=[kkn.B], writes=[sqk.B])
                    ss = ps_next()
                    fw.pe(lambda h: h.matmul(out=ss[:], lhsT=bdo64[:], rhs=sqk[:], start=True, stop=True),
                          reads=[bdo64.B, sqk.B], writes=[ss.B])
                    rn = wf1
                    fw.act(lambda h: h.activation(out=rn[:], in_=ss[:], func=AF.Sqrt, bias=1e-12, scale=64.0),
                           reads=[ss.B], writes=[rn.B])
                    fw.dve(lambda h: h.reciprocal(out=rn[:], in_=rn[:]), reads=[rn.B], writes=[rn.B])
                    fw.dve(lambda h: h.tensor_tensor(out=kkn[:], in0=kkn[:], in1=rn[:], op=ALU.mult),
                           reads=[kkn.B, rn.B], writes=[kkn.B])
                    k2 = wf2
                    fw.dve(lambda h: h.tensor_scalar(out=wf1[:], in0=aa[:], scalar1=KA, scalar2=omka[:, hp:hp + 1],
                                                     op0=ALU.mult, op1=ALU.add), reads=[aa.B, rvec.B, omka.B], writes=[wf1.B])
                    fw.dve(lambda h: h.tensor_tensor(out=k2[:], in0=pk[:], in1=wf1[:], op=ALU.mult),
                           reads=[pk.B, wf1.B], writes=[k2.B])
                    fw.dve(lambda h: h.scalar_tensor_tensor(out=rk_bf[:], in0=r32[:], scalar=RK, in1=k2[:], op0=ALU.mult,
                                                            op1=ALU.mult), reads=[r32.B, rvec.B, k2.B], writes=[rk_bf.B])
                    bb_ = wf1
                    fw.dve(lambda h: h.tensor_tensor(out=bb_[:], in0=kkn[:], in1=aa[:], op=ALU.mult),
                           reads=[kkn.B, aa.B], writes=[bb_.B])
                    pv = ps_next()
                    proj_shift(pv, w_v, tok0)
                    fw.act(lambda h: h.activation(out=v_bf[:], in_=pv[:], func=AF.Copy), reads=[pv.B], writes=[v_bf.B])
                    pgt = ps_next()
                    proj(pgt, w_g, tok0)
                    silu_from_psum(sgate, pgt)
                    cs = wf6
                    for c4 in range(4):
                        fw.dve(lambda h, c4=c4: h.tensor_tensor_scan(
                            out=cs[:, c4 * 128:(c4 + 1) * 128], data0=onec[:, 0:1].to_broadcast([128, 128]),
                            data1=sg[:, c4 * 128:(c4 + 1) * 128], initial=0.0, op0=ALU.mult, op1=ALU.add),
                            reads=[sg.B, onec.B], writes=[cs.B])
                    fw.dve(lambda h: h.tensor_tensor(out=sg[:], in0=cs[:], in1=sg[:], op=ALU.subtract),
                           reads=[cs.B, sg.B], writes=[sg.B])
                    fw.act(lambda h: h.activation(out=Pq[:], in_=cs[:], func=AF.Exp, scale=-CDEC), reads=[cs.B], writes=[Pq.B])
                    fw.act(lambda h: h.activation(out=sg[:], in_=sg[:], func=AF.Exp, scale=-CDEC), reads=[sg.B], writes=[sg.B])
                    fw.act(lambda h: h.activation(out=cs[:], in_=cs[:], func=AF.Exp, scale=CDEC), reads=[cs.B], writes=[cs.B])
                    PqA, Pk = sg, cs
                    fw.dve(lambda h: h.tensor_tensor(out=rt[:], in0=r32[:], in1=Pq[:], op=ALU.mult),
                           reads=[r32.B, Pq.B], writes=[rt.B])
                    fw.dve(lambda h: h.scalar_tensor_tensor(out=at[:], in0=kkn[:], scalar=-1.0, in1=PqA[:], op0=ALU.mult,
                                                            op1=ALU.mult), reads=[kkn.B, PqA.B], writes=[at.B])
                    fw.dve(lambda h: h.tensor_tensor(out=kt[:], in0=k2[:], in1=Pk[:], op=ALU.mult),
                           reads=[k2.B, Pk.B], writes=[kt.B])
                    fw.dve(lambda h: h.tensor_tensor(out=bt[:], in0=bb_[:], in1=Pk[:], op=ALU.mult),
                           reads=[bb_.B, Pk.B], writes=[bt.B])
                    for c4 in range(4):
                        for hh in range(2):
                            rows = slice(hh * 64, (hh + 1) * 64)
                            fw.act(lambda h, c4=c4, hh=hh, rows=rows: h.activation(
                                out=QP[c4][rows, (2 * hh) * 128:(2 * hh + 1) * 128], in_=at[rows, c4 * 128:(c4 + 1) * 128],
                                func=AF.Copy), reads=[at.B], writes=[QPB[c4]])
                            fw.act(lambda h, c4=c4, hh=hh, rows=rows: h.activation(
                                out=QP[c4][rows, (2 * hh + 1) * 128:(2 * hh + 2) * 128], in_=rt[rows, c4 * 128:(c4 + 1) * 128],
                                func=AF.Copy), reads=[rt.B], writes=[QPB[c4]])
                    tpa, tpb = tp_ps[0], tp_ps[1]
                    for c4 in range(4):
                        fw.pe(lambda h, c4=c4: h.transpose(out=tpa[:, c4, :], in_=kt[:, c4 * 128:(c4 + 1) * 128],
                                                           identity=ident[:]), reads=[kt.B, ident.B], writes=[tpa.B], inc=False)
                    for c4 in range(4):
                        fw.pe(lambda h, c4=c4: h.transpose(out=tpa[:, 4 + c4, :], in_=bt[:, c4 * 128:(c4 + 1) * 128],
                                                           identity=ident[:]), reads=[bt.B, ident.B], writes=[tpa.B],
                              inc=(c4 == 3))
                    for c4 in range(4):
                        fw.pe(lambda h, c4=c4: h.transpose(out=tpb[:, c4, :], in_=v_bf[:, c4 * 128:(c4 + 1) * 128],
                                                           identity=ident[:]), reads=[v_bf.B, ident.B], writes=[tpb.B],
                              inc=(c4 == 3))
                    v4 = lambda ap: ap.rearrange("p (c f) -> p c f", c=4)
                    fw.act(lambda h: h.activation(out=v4(kt_tok), in_=tpa[:, 0:4, :], func=AF.Copy),
                           reads=[tpa.B], writes=[SB[13]])
                    fw.dve(lambda h: h.tensor_copy(out=v4(bt_tok), in_=tpa[:, 4:8, :]), reads=[tpa.B], writes=[SB[14]])
                    fw.act(lambda h: h.activation(out=v4(vp0)[:, :, 0:64], in_=tpb[:, 0:4, 0:64], func=AF.Copy),
                           reads=[tpb.B], writes=[SB[15]])
                    fw.dve(lambda h: h.tensor_copy(out=v4(vp1[:])[:, :, 64:128], in_=tpb[:, 0:4, 64:128]),
                           reads=[tpb.B], writes=[vp1.B])
                    y_ps = ps_y()

                    ESET = [(SV[6], SB[6], SV[7], SB[7], SV[11], SB[11]),
                            (wh[10][:], wh[10].B, wh[11][:], wh[11].B, wh[0][:], wh[0].B)]
                    vps = [vp0, vp1[:]]
                    vpb = [SB[15], vp1.B]

                    def inv_stages(c4):
                        cs_ = slice(c4 * 128, (c4 + 1) * 128)
                        E1, E1B, E2, E2B, PP, PPB = ESET[c4 % 2]
                        st = []

                        def aprod():
                            pa1, pa2, pa3 = ps_next(), ps_next(), ps_next()
                            fw.pe(lambda h: h.matmul(out=pa1[:], lhsT=kt[:, cs_], rhs=QP[c4], start=True, stop=True),
                                  reads=[kt.B, QPB[c4]], writes=[pa1.B])
                            fw.pe(lambda h: h.matmul(out=pa2[:], lhsT=bt[:, cs_], rhs=QP[c4], start=True, stop=True),
                                  reads=[bt.B, QPB[c4]], writes=[pa2.B])
                            for hh in range(2):
                                fw.pe(lambda h, hh=hh: h.matmul(out=pa3[:, hh * 128:(hh + 1) * 128],
                                                                lhsT=QP[c4][:, (2 * hh) * 128:(2 * hh + 1) * 128],
                                                                rhs=bt[:, cs_], start=True, stop=True),
                                      reads=[bt.B, QPB[c4]], writes=[pa3.B], inc=(hh == 1))
                            fw.dve(lambda h: h.tensor_tensor(out=E1, in0=pa1[:], in1=MK1, op=ALU.mult),
                                   reads=[pa1.B, MK1B], writes=[E1B])
                            fw.dve(lambda h: h.tensor_tensor(out=E2, in0=pa2[:], in1=MK1, op=ALU.mult),
                                   reads=[pa2.B, MK1B], writes=[E2B])
                            fw.dve(lambda h: h.tensor_tensor(out=E3[:, 0:256], in0=pa3[:, 0:256], in1=MK2, op=ALU.mult),
                                   reads=[pa3.B, MK2B], writes=[SB[8]])
                            e2v = E2.rearrange("p (a b) -> p a b", a=2)[:, :, 0:128]
                            fw.dve(lambda h: h.tensor_tensor(out=PP[:, 0:256].rearrange("p (a b) -> p a b", a=2), in0=e2v,
                                                             in1=ident[:].unsqueeze(1).to_broadcast([128, 2, 128]),
                                                             op=ALU.add), reads=[E2B, ident.B], writes=[PPB])
                        st.append(aprod)

                        def Xj(j, hh):
                            if j == 0:
                                return E3[:, hh * 128:(hh + 1) * 128], SB[8]
                            return XB[j % 2][:, (2 * hh) * 128:(2 * hh + 1) * 128], SB[9 + j % 2]

                        def Bj(j, hh):
                            if j == 0:
                                return E2[:, (2 * hh) * 128:(2 * hh + 1) * 128], E2B
                            return XB[j % 2][:, (2 * hh + 1) * 128:(2 * hh + 2) * 128], SB[9 + j % 2]

                        def Pj(j, hh):
                            o = (j % 2) * 256 + hh * 128
                            return PP[:, o:o + 128]

                        def step(j):
                            last = j == 5
                            pxb = ps_next()
                            for hh in range(2):
                                xa_, xb_ = Xj(j, hh)
                                ba_, bb2 = Bj(j, hh)
                                fw.pe(lambda h, hh=hh, xa_=xa_, ba_=ba_: h.matmul(
                                    out=pxb[:, (2 * hh) * 128:(2 * hh + 1) * 128], lhsT=ba_, rhs=xa_, start=True, stop=True),
                                    reads=[xb_, bb2], writes=[pxb.B], inc=(last and hh == 1))
                                if not last:
                                    fw.pe(lambda h, hh=hh, xa_=xa_, ba_=ba_: h.matmul(
                                        out=pxb[:, (2 * hh + 1) * 128:(2 * hh + 2) * 128], lhsT=xa_, rhs=ba_, start=True,
                                        stop=True), reads=[xb_, bb2], writes=[pxb.B], inc=(hh == 1))
                            nxt = XB[(j + 1) % 2]
                            if last:
                                fw.act(lambda h: h.activation(
                                    out=nxt.rearrange("p (a b) -> p a b", a=2)[:, :, 0:128],
                                    in_=pxb[:].rearrange("p (a b) -> p a b", a=2)[:, :, 0:128], func=AF.Copy),
                                    reads=[pxb.B], writes=[SB[9 + (j + 1) % 2]])
                            else:
                                fw.act(lambda h: h.activation(out=nxt, in_=pxb[:], func=AF.Copy),
                                       reads=[pxb.B], writes=[SB[9 + (j + 1) % 2]])
                            pp = ps_next()
                            for hh in range(2):
                                xn_, xnb_ = Xj(j + 1, hh)
                                fw.pe(lambda h, hh=hh: h.matmul(out=pp[:, hh * 128:(hh + 1) * 128], lhsT=ident[:],
                                                                rhs=Pj(j, hh), start=True, stop=False),
                                      reads=[ident.B, PPB], writes=[pp.B], inc=False)
                                fw.pe(lambda h, hh=hh, xn_=xn_: h.matmul(
                                    out=pp[:, hh * 128:(hh + 1) * 128], lhsT=xn_, rhs=Pj(j, hh), start=False, stop=True),
                                    reads=[xnb_, PPB], writes=[pp.B], inc=(hh == 1))
                            o = ((j + 1) % 2) * 256
                            fw.dve(lambda h: h.tensor_copy(out=PP[:, o:o + 256], in_=pp[:, 0:256]),
                                   reads=[pp.B], writes=[PPB])

                        for j in range(6):
                            st.append(lambda j=j: step(j))
                        return st

                    def state_stages(c4):
                        cs_ = slice(c4 * 128, (c4 + 1) * 128)
                        E1, E1B, E2, E2B, PP, PPB = ESET[c4 % 2]
                        r0b = MISC[:, 0:128]
                        upad = [MISC[:, 128:256], MISC[:, 256:384]]

                        def s_rhs0():
                            r0 = ps_next()
                            fw.pe(lambda h: h.matmul(out=r0[:, 0:128], lhsT=at[:, cs_], rhs=Stb[:], start=True, stop=False),
                                  reads=[at.B, Stb.B], writes=[r0.B], inc=False)
                            for hh in range(2):
                                fw.pe(lambda h, hh=hh: h.matmul(out=r0[:, hh * 64:(hh + 1) * 64],
                                                                lhsT=E1[:, (2 * hh) * 128:(2 * hh + 1) * 128],
                                                                rhs=vps[hh][:, c4 * 128 + hh * 64:c4 * 128 + (hh + 1) * 64],
                                                                start=False, stop=(hh == 1)),
                                      reads=[E1B, vpb[hh]], writes=[r0.B], inc=(hh == 1))
                            fw.act(lambda h: h.activation(out=r0b, in_=r0[:, 0:128], func=AF.Copy),
                                   reads=[r0.B], writes=[SB[12]])

                        def s_u():
                            up = ps_next()
                            for hh in range(2):
                                fw.pe(lambda h, hh=hh: h.matmul(out=up[:, hh * 64:(hh + 1) * 64], lhsT=PP[:, hh * 128:(hh + 1) * 128],
                                                                rhs=r0b[:, hh * 64:(hh + 1) * 64], start=True, stop=True),
                                      reads=[PPB, SB[12]], writes=[up.B], inc=(hh == 1))
                            fw.act(lambda h: h.activation(out=upad[0][:, 0:64], in_=up[:, 0:64], func=AF.Copy),
                                   reads=[up.B], writes=[SB[12]])
                            fw.dve(lambda h: h.tensor_copy(out=upad[1][:, 64:128], in_=up[:, 64:128]),
                                   reads=[up.B], writes=[SB[12]])

                        def s_y():
                            fw.pe(lambda h: h.matmul(out=y_ps[:, cs_], lhsT=Stb[:], rhs=rt[:, cs_], start=True, stop=False),
                                  reads=[Stb.B, rt.B], writes=[y_ps.B], inc=False)
                            for hh in range(2):
                                fw.pe(lambda h, hh=hh: h.matmul(out=y_ps[:, cs_], lhsT=upad[hh],
                                                                rhs=E2[:, (2 * hh + 1) * 128:(2 * hh + 2) * 128],
                                                                start=False, stop=False),
                                      reads=[SB[12], E2B], writes=[y_ps.B], inc=False)
                                fw.pe(lambda h, hh=hh: h.matmul(out=y_ps[:, cs_], lhsT=vps[hh][:, c4 * 128:(c4 + 1) * 128],
                                                                rhs=E1[:, (2 * hh + 1) * 128:(2 * hh + 2) * 128],
                                                                start=False, stop=(hh == 1)),
                                      reads=[vpb[hh], E1B], writes=[y_ps.B], inc=(hh == 1))

                        def s_state():
                            su = ps_next()
                            for hh in range(2):
                                fw.pe(lambda h, hh=hh: h.matmul(out=su[:, 0:128], lhsT=bt_tok[:, cs_], rhs=upad[hh],
                                                                start=(hh == 0), stop=False),
                                      reads=[SB[14], SB[12]], writes=[su.B], inc=False)
                            for hh in range(2):
                                fw.pe(lambda h, hh=hh: h.matmul(out=su[:, 0:128], lhsT=kt_tok[:, cs_],
                                                                rhs=vps[hh][:, c4 * 128:(c4 + 1) * 128],
                                                                start=False, stop=(hh == 1)),
                                      reads=[SB[13], vpb[hh]], writes=[su.B], inc=(hh == 1))
                            fw.dve(lambda h: h.tensor_tensor(out=St32[:], in0=su[:, 0:128], in1=St32[:], op=ALU.add),
                                   reads=[su.B, St32.B], writes=[St32.B])
                            pend = Pq[:, c4 * 128 + 127:c4 * 128 + 128]
                            fw.dve(lambda h: h.tensor_scalar(out=St32[:], in0=St32[:], scalar1=pend, scalar2=None,
                                                             op0=ALU.mult), reads=[St32.B, Pq.B], writes=[St32.B])
                            fw.dve(lambda h: h.tensor_tensor(out=Stb[:], in0=St32[:], in1=bd32[:], op=ALU.mult),
                                   reads=[St32.B, bd32.B], writes=[Stb.B])

                        return [s_rhs0, s_u, s_y, s_state]

                    nch = LIM.get('c4', 4)
                    prev = []
                    for c4 in range(nch + 1):
                        cur = inv_stages(c4) if c4 < nch else []
                        for i in range(max(len(cur), len(prev))):
                            if i < len(cur):
                                cur[i]()
                            if i < len(prev):
                                prev[i]()
                        prev = state_stages(c4) if c4 < nch else []
                    bs = ps_next()
                    fw.pe(lambda h: h.matmul(out=bs[:], lhsT=bdo64[:], rhs=rk_bf[:], start=True, stop=True),
                          reads=[bdo64.B, rk_bf.B], writes=[bs.B])
                    bon = wf7
                    fw.dve(lambda h: h.scalar_tensor_tensor(out=bon[:], in0=bs[:], scalar=64.0, in1=v_bf[:], op0=ALU.mult,
                                                            op1=ALU.mult), reads=[bs.B, v_bf.B], writes=[bon.B])

                    def affine(ycen):
                        fw.dve(lambda h: h.tensor_scalar(out=ycen[:], in0=ycen[:], scalar1=LNW, scalar2=LNB, op0=ALU.mult,
                                                         op1=ALU.add), reads=[ycen.B, rvec.B], writes=[ycen.B])
                        fw.dve(lambda h: h.tensor_tensor(out=ycen[:], in0=ycen[:], in1=bon[:], op=ALU.add),
                               reads=[ycen.B, bon.B], writes=[ycen.B])

                    headnorm_gate(y_ps, sgate, ya[:, hp, tok0:tok0 + 512], ya.b[hp * 4 + tb], 64e-5, affine=affine)

                for tb in range(LIM.get('tb', 4)):
                    do_block(tb)

            for hp in range(LIM.get('hp', 4)):
                do_hp(hp)
            fw.dve(lambda h: h.memset(cst[:, 0:1], 0.0), reads=SB, writes=yb.b + [cst.B])

        def wload_plain(src_ap, nchunk):
            slot = wslot[wctr[0] % NSLOT]
            wctr[0] += 1
            stg = wst[wctr[1] % 2]
            wctr[1] += 1
            fw.dma("sp", stg[:, 0:nchunk, :], src_ap, writes=[stg.B])
            fw.act(lambda h: h.activation(out=slot[:, 0:nchunk, :], in_=stg[:, 0:nchunk, :], func=AF.Copy),
                   reads=[stg.B], writes=[slot.B])
            return slot

        wc_bufs = {}

        def cached(l, idx, nchunk, loader):
            n = nchunk * 128
            key = (l, idx)
            if key not in wc_bufs:
                slot = loader()
                wc_bufs[key] = Buf(f"wc{l}_{idx}")
                fw.dma("sp", wcache_d[l, idx, :, 0:n], slot[:, 0:nchunk, :].rearrange("p c n -> p (c n)"),
                       reads=[slot.B], writes=[wc_bufs[key]])
                return slot
            slot = wslot[wctr[0] % NSLOT]
            wctr[0] += 1
            fw.dma("sp", slot[:, 0:nchunk, :].rearrange("p c n -> p (c n)"), wcache_d[l, idx, :, 0:n],
                   reads=[wc_bufs[key]], writes=[slot.B])
            return slot

        def phaseM(si, l):
            fw.dma("sp", gpost[:], postn_d[l:l + 1, :].broadcast_to([128, D]), writes=[gpost.B])
            ybr = [ya, yb, yc]
            gofs = [O_GA, O_GB, O_GC]
            mT = wh[0:8]
            sig, tt_, macc = wf[0], wf[1], wf[2]

            def do_block(tb):
                tok0 = tb * 512

                def do_oc(oc):
                    for br in range(3):
                        if br not in LIM.get("branches", (0, 1, 2)):
                            continue
                        first = br == min(LIM.get("branches", (0, 1, 2)))
                        last = br == max(LIM.get("branches", (0, 1, 2)))
                        wg = cached(l, br * 8 + oc, 8, lambda br=br: wload(l, [(gofs[br] + oc * 128, 128)]))
                        gl = ps_next()
                        proj(gl, wg, tok0)
                        fw.act(lambda h, gl=gl, br=br: h.activation(out=sig[:], in_=gl[:], func=AF.Sigmoid,
                                                                    bias=bmt[:, l, br, oc:oc + 1], scale=1.0),
                               reads=[gl.B, bmt.B], writes=[sig.B])
                        wp = cached(l, 24 + br * 8 + oc, 4, lambda br=br: wload_plain(
                            wp_d[br][l, :, oc * 128:(oc + 1) * 128].rearrange("(c p) n -> p c n", p=128), 4))
                        pb = ps_next()
                        for c in range(4):
                            fw.pe(lambda h, c=c, pb=pb, wp=wp, br=br: h.matmul(
                                out=pb[:], lhsT=wp[:, c, :], rhs=ybr[br][:, c, tok0:tok0 + 512],
                                start=(c == 0), stop=(c == 3)),
                                reads=[wp.B, ybr[br].b[c * 4 + tb]], writes=[pb.B], inc=(c == 3))
                        if first and last:
                            fw.dve(lambda h, pb=pb: h.tensor_tensor(out=mT[oc][:], in0=pb[:], in1=sig[:], op=ALU.mult),
                                   reads=[pb.B, sig.B], writes=[mT[oc].B])
                        elif first:
                            fw.dve(lambda h, pb=pb: h.tensor_tensor(out=macc[:], in0=pb[:], in1=sig[:], op=ALU.mult),
                                   reads=[pb.B, sig.B], writes=[macc.B])
                        else:
                            fw.dve(lambda h, pb=pb: h.tensor_tensor(out=tt_[:], in0=pb[:], in1=sig[:], op=ALU.mult),
                                   reads=[pb.B, sig.B], writes=[tt_.B])
                            dst = mT[oc] if last else macc
                            fw.dve(lambda h, dst=dst: h.tensor_tensor(out=dst[:], in0=macc[:], in1=tt_[:], op=ALU.add),
                                   reads=[macc.B, tt_.B], writes=[dst.B])

                for oc in range(8):
                    do_oc(oc)

                def do_pair(k):
                    banks = [pg[0], pg[1], pg[2], pg[3]]
                    for oc in range(8):
                        wo = cached(l, 48 + oc, 8, lambda oc=oc: wload_plain(
                            wout_d[l, oc * 128:(oc + 1) * 128, :].rearrange("p (c n) -> p c n", c=8), 8))
                        wov = wo[:].rearrange("p c n -> p (c n)")
                        for t in range(2):
                            for half in range(2):
                                bk = banks[t * 2 + half]
                                fw.pe(lambda h, oc=oc, t=t, half=half, bk=bk, wov=wov: h.matmul(
                                    out=bk[:], lhsT=mT[oc][:, (2 * k + t) * 128:(2 * k + t + 1) * 128],
                                    rhs=wov[:, half * 512:(half + 1) * 512], start=(oc == 0), stop=(oc == 7)),
                                    reads=[mT[oc].B, wo.B], writes=[bk.B], inc=(oc == 7 or (t == 1 and half == 1)))
                    for t in range(2):
                        tile_i = tb * 4 + 2 * k + t
                        for half in range(2):
                            bk = banks[t * 2 + half]
                            fw.dve(lambda h, half=half, bk=bk: h.bn_stats(out=st6[:, half, :], in_=bk[:]),
                                   reads=[bk.B], writes=[st6.B])
                        fw.dve(lambda h: h.bn_aggr(out=mv[:], in_=st6[:].rearrange("p a b -> p (a b)")),
                               reads=[st6.B], writes=[mv.B])
                        fw.dve(lambda h: h.scalar_tensor_tensor(out=e2[:], in0=mv[:, 0:1], scalar=mv[:, 0:1],
                                                                in1=mv[:, 1:2], op0=ALU.mult, op1=ALU.add),
                               reads=[mv.B], writes=[e2.B])
                        fw.act(lambda h: h.activation(out=e2[:], in_=e2[:], func=AF.Sqrt, bias=EPS, scale=1.0),
                               reads=[e2.B], writes=[e2.B])
                        fw.dve(lambda h: h.reciprocal(out=rstd[:], in_=e2[:]), reads=[e2.B], writes=[rstd.B])
                        for half in range(2):
                            bk = banks[t * 2 + half]
                            hs = slice(half * 512, (half + 1) * 512)
                            fw.dve(lambda h, bk=bk, hs=hs: h.scalar_tensor_tensor(
                                out=wf[3][:], in0=bk[:], scalar=rstd[:, 0:1], in1=gpost[:, hs],
                                op0=ALU.mult, op1=ALU.mult), reads=[bk.B, rstd.B, gpost.B], writes=[wf[3].B])
                            fw.dve(lambda h, hs=hs, tile_i=tile_i: h.tensor_tensor(
                                out=x_sb[:, tile_i, hs], in0=x_sb[:, tile_i, hs], in1=wf[3][:], op=ALU.add),
                                reads=[x_sb.b[tile_i], wf[3].B], writes=[x_sb.b[tile_i]])

                for k in range(2):
                    do_pair(k)

            for tb in range(LIM.get('tb', 4)):
                do_block(tb)

        for si in range(nseq):
            for tq in range(NT // 4):
                fw.dma("sp", x_sb[:, tq * 4:(tq + 1) * 4, :],
                       x_d[si, tq * 512:(tq + 1) * 512, :].rearrange("(t p) d -> p t d", p=128),
                       writes=[x_sb.b[tq * 4 + i] for i in range(4)])
            for l in range(nlayers):
                if l == 1 and "l2phases" in LIM:
                    phases = LIM["l2phases"]
                if not LIM.get('nosetup'):
                    layer_setup(l)
                phase0(si, l)
                if "hT" in debug and si == 0 and l == 0:
                    fw.dma("sp", dbg_d["hT"], hT[:], reads=hT.b)
                if "C" in phases:
                    phaseC(si, l)
                    if "yc" in debug and si == 0 and l == 0:
                        fw.dma("sp", dbg_d["yc"], yc[:], reads=yc.b)
                if "A" in phases:
                    phaseA(si, l)
                    if "ya" in debug and si == 0 and l == 0:
                        fw.dma("sp", dbg_d["ya"], ya[:], reads=ya.b)
                if "B" in phases:
                    phaseB(si, l)
                    if "yb" in debug and si == 0 and l == 0:
                        fw.dma("sp", dbg_d["yb"], yb[:], reads=yb.b)
                if "M" in phases:
                    phaseM(si, l)
            for tq in range(NT // 4):
                fw.dma("sp", out_d[si, tq * 512:(tq + 1) * 512, :].rearrange("(t p) d -> p t d", p=128),
                       x_sb[:, tq * 4:(tq + 1) * 4, :],
                       reads=[x_sb.b[tq * 4 + i] for i in range(4)])
        allb = x_sb.b + hT.b + ya.b + yb.b + yc.b
        fw.wait_all("sp", allb)
        fw.emit()
        print("instr counts:", {k: v.n for k, v in fw.eng.items()})
    return nc


def make_shared(inputs):
    f = lambda k: np.ascontiguousarray(np.asarray(inputs[k], dtype=np.float32))
    shared = dict(make_consts())
    shared["w_in"] = f("w_in")
    shared["pre_norm"] = np.ascontiguousarray(f("pre_norm").reshape(DEPTH, 8, 128).transpose(0, 2, 1))
    for n in ("w_proj_rwkv", "w_proj_ret", "w_proj_s5", "w_out", "post_norm"):
        shared[n] = f(n)
    shared["b_merge"] = np.ascontiguousarray(f("b_merge").reshape(DEPTH, 3, 8, 128).transpose(0, 3, 1, 2))
    shared["rwkv_mu_rkv"] = f("rwkv_mu_rkv")
    shared["rwkv_mu_wa"] = np.ascontiguousarray(f("rwkv_mu_wa").reshape(DEPTH, 1, 128))
    shared["rwkv_w2a2"] = np.ascontiguousarray(np.stack([f("rwkv_w2"), f("rwkv_a2")], axis=1))
    vec = np.zeros((DEPTH, 8, 512), np.float32)
    for j, n in enumerate(("rwkv_w0", "rwkv_a0", "rwkv_k_k", "rwkv_k_a", "rwkv_ln_w", "rwkv_ln_b")):
        vec[:, j] = f(n)
    vec[:, 6] = f("rwkv_r_k").reshape(DEPTH, 512)
    shared["rwkv_vec"] = np.ascontiguousarray(vec.reshape(DEPTH, 8, 4, 128).transpose(0, 3, 2, 1))
    dup = lambda a: np.concatenate([a, a], axis=1)
    a_re = dup(f("s5_A_re").transpose(0, 2, 1))
    a_im = dup(f("s5_A_im").transpose(0, 2, 1))
    ldt = np.broadcast_to(f("s5_log_dt")[:, None, :], (DEPTH, 128, 32))
    shared["s5_Aab"] = np.ascontiguousarray(np.stack([a_re, a_im, ldt], axis=1))
    b_re = dup(f("s5_B_re").transpose(0, 2, 1, 3).reshape(DEPTH, 64, 512))
    b_im = dup(f("s5_B_im").transpose(0, 2, 1, 3).reshape(DEPTH, 64, 512))
    shared["s5_Bst"] = np.ascontiguousarray(np.stack([b_re, b_im], axis=1))
    c_re = f("s5_C_re").transpose(0, 3, 1, 2).reshape(DEPTH, 64, 512)
    c_im = f("s5_C_im").transpose(0, 3, 1, 2).reshape(DEPTH, 64, 512)
    ca = np.concatenate([c_re, c_im], axis=1)
    cb = np.concatenate([c_im, c_re], axis=1)
    shared["s5_Cst"] = np.ascontiguousarray(np.stack([ca, cb], axis=1))
    dv = f("s5_D").reshape(DEPTH, 4, 128).transpose(0, 2, 1)
    gb = f("s5_glu_b").reshape(DEPTH, 4, 128).transpose(0, 2, 1)
    shared["s5_vec"] = np.ascontiguousarray(np.concatenate([dv, gb], axis=2))
    shared["s5_glu_w"] = f("s5_glu_w")
    return shared


def make_inputs(inputs, s0, n):
    m = make_shared(inputs)
    m["x"] = np.ascontiguousarray(np.asarray(inputs["x"], dtype=np.float32)[s0:s0 + n])
    return m


def kernel(**inputs):
    ncores = 8
    x = np.ascontiguousarray(np.asarray(inputs["x"], dtype=np.float32))
    shared = make_shared(inputs)
    nlaunch = N_LAUNCH
    per = NSEQ // nlaunch
    nc = build_program(nseq=per)
    out = np.zeros_like(x)
    for j in range(nlaunch):
        in_maps = []
        for c in range(ncores):
            m = dict(shared)
            s0 = c * NSEQ + j * per
            m["x"] = x[s0:s0 + per]
            in_maps.append(m)
        res = run_bass_kernel_spmd(nc, in_maps, core_ids=list(range(ncores)))
        for c in range(ncores):
            s0 = c * NSEQ + j * per
            out[s0:s0 + per] = np.asarray(res.results[c]["out"])
    return out.astype(np.float32)
```

```python
import contextlib
import numpy as np
import ml_dtypes
import concourse.bass as bass
import concourse.mybir as mybir
from concourse.bass_utils import run_bass_kernel_spmd

F32 = mybir.dt.float32
BF16 = mybir.dt.bfloat16
AF = mybir.ActivationFunctionType
ALU = mybir.AluOpType
AX = mybir.AxisListType

D = 1024
S = 2048
DEPTH = 2
NSEQ = 2
D_IN = 8320
EPS = 1e-6
NT = S // 128

O_AR, O_AK, O_AV, O_XW, O_XA, O_AG = 0, 512, 1024, 1536, 1600, 1664
O_BQ, O_BK, O_BV, O_BG = 2176, 2688, 3200, 3712
O_CU, O_CG = 4224, 4736
O_GA, O_GB, O_GC = 5248, 6272, 7296


class Buf:
    def __init__(self, name):
        self.name = name
        self.last_write = None
        self.reads = []


class Engine:
    EPOCH = 30000

    def __init__(self, fw, name):
        self.fw = fw
        self.name = name
        self.sems = []
        self.count = 0
        self.waited = {}
        self.ops = []
        self.n = 0
        self._new_sem()

    def _new_sem(self):
        s = self.fw.stack.enter_context(self.fw.nc.semaphore(f"s_{self.name}_{len(self.sems)}"))
        self.sems.append(s)
        self.count = 0

    def need(self, ev):
        sem, val = ev
        key = id(sem)
        if self.waited.get(key, 0) >= val:
            return None
        self.waited[key] = val
        return ev


class Fw:
    def __init__(self, nc, stack):
        self.nc = nc
        self.stack = stack
        self.eng = {n: Engine(self, n) for n in ("pe", "act", "dve", "pool", "sp")}
        self.dsem = {}
        for q, k in (("sp", 12), ("act", 6), ("pool", 6)):
            self.dsem[q] = [[stack.enter_context(nc.semaphore(f"d_{q}_{i}")), 0] for i in range(k)]
        self.dnext = {q: 0 for q in self.dsem}

    def _deps(self, e, reads, writes):
        evs = []
        for b in reads:
            if b.last_write is not None:
                evs.append(b.last_write)
        for b in writes:
            if b.last_write is not None:
                evs.append(b.last_write)
            evs.extend(b.reads)
        out = []
        for ev in evs:
            if e.name == "pe" and any(ev[0] is s_ for s_ in e.sems):
                continue
            ev2 = e.need(ev)
            if ev2 is not None:
                out.append(ev2)
        return out

    def op(self, engname, fn, reads=(), writes=(), inc=True):
        e = self.eng[engname]
        waits = self._deps(e, reads, writes)
        if e.count >= Engine.EPOCH and inc:
            e._new_sem()
        sem = e.sems[-1]
        if inc:
            e.count += 1
        ev = (sem, e.count if inc else e.count + 1)
        for b in reads:
            b.reads.append(ev)
        for b in writes:
            b.last_write = ev
            b.reads = []
        e.n += 1

        def run(h, waits=waits, fn=fn, sem=sem, inc=inc):
            for (s, v) in waits[1:]:
                h.wait_ge(s, v)
            ins = fn(h)
            if waits:
                ins._wait_ge(waits[0][0], waits[0][1])
            if inc:
                ins.then_inc(sem, 1)

        e.ops.append(run)
        return ev

    def dma(self, q, out, in_, reads=(), writes=()):
        e = self.eng[q]
        waits = self._deps(e, reads, writes)
        slots = self.dsem[q]
        i = self.dnext[q]
        self.dnext[q] = (i + 1) % len(slots)
        slot = slots[i]
        sem = slot[0]
        prev = slot[1]
        if prev > 0:
            w = e.need((sem, prev))
            if w is not None:
                waits.append(w)
        slot[1] = prev + 16
        ev = (sem, slot[1])
        for b in reads:
            b.reads.append(ev)
        for b in writes:
            b.last_write = ev
            b.reads = []

        def run(h, waits=waits, sem=sem, out=out, in_=in_):
            for (s, v) in waits:
                h.wait_ge(s, v)
            h.dma_start(out=out, in_=in_).then_inc(sem, 16)

        e.ops.append(run)
        return ev

    def wait_all(self, engname, bufs):
        e = self.eng[engname]
        waits = []
        for b in bufs:
            for ev in ([b.last_write] if b.last_write else []) + list(b.reads):
                w = e.need(ev)
                if w is not None:
                    waits.append(w)

        def run(h, waits=waits):
            for (s, v) in waits:
                h.wait_ge(s, v)

        e.ops.append(run)

    def pe(self, fn, reads=(), writes=(), inc=True):
        return self.op("pe", fn, reads, writes, inc)

    def act(self, fn, reads=(), writes=()):
        return self.op("act", fn, reads, writes)

    def dve(self, fn, reads=(), writes=()):
        return self.op("dve", fn, reads, writes)

    def pool(self, fn, reads=(), writes=()):
        return self.op("pool", fn, reads, writes)

    def emit(self):
        nc = self.nc
        with nc.Block() as block:
            @block.tensor
            def _(h):
                for f in self.eng["pe"].ops:
                    f(h)

            @block.scalar
            def _(h):
                for f in self.eng["act"].ops:
                    f(h)

            @block.vector
            def _(h):
                for f in self.eng["dve"].ops:
                    f(h)

            @block.gpsimd
            def _(h):
                for f in self.eng["pool"].ops:
                    f(h)

            @block.sync
            def _(h):
                for f in self.eng["sp"].ops:
                    f(h)


class T:
    def __init__(self, fw, shape, dtype, name, psum=False, nsub=1):
        nc = fw.nc
        if psum:
            self.t = fw.stack.enter_context(nc.psum_tensor("ps_" + name, shape, dtype))
        else:
            self.t = fw.stack.enter_context(nc.sbuf_tensor("sb_" + name, shape, dtype))
        self.b = [Buf(f"{name}.{i}") for i in range(nsub)]
        self.name = name

    def __getitem__(self, idx):
        return self.t[idx]

    @property
    def B(self):
        return self.b[0]


def make_consts():
    c = {}
    c["ident"] = np.eye(128, dtype=np.float32).astype(ml_dtypes.bfloat16)
    bd = np.zeros((128, 128), np.float32)
    bd[:64, :64] = 1.0
    bd[64:, 64:] = 1.0
    c["bd32"] = bd
    c["bdo64"] = (bd / 64.0).astype(ml_dtypes.bfloat16)
    half = 32
    inv = (np.float32(10000.0) ** (-np.arange(half, dtype=np.float32) / np.float32(half))).astype(np.float32)
    pos = np.arange(S, dtype=np.float32)
    ang = (pos[None, :] * inv[:, None]).astype(np.float32).astype(np.float64)
    cos32, sin32 = np.cos(ang), np.sin(ang)
    cosT = np.zeros((128, S), np.float32)
    sinS = np.zeros((128, S), np.float32)
    for p in range(128):
        d = p % 64
        i = d % 32
        cosT[p] = cos32[i]
        sinS[p] = -sin32[i] if d < 32 else sin32[i]
    c["rope_cos"] = cosT
    c["rope_sin"] = sinS
    lg = np.log(1.0 - 2.0 ** (-5.0 - np.arange(8, dtype=np.float64)))
    idx = np.arange(128, dtype=np.float64)
    dmT = np.zeros((4, 128, 256), np.float32)
    kwt = np.zeros((4, 128, 128), np.float32)
    qw = np.zeros((4, 128, 128), np.float32)
    gc = np.zeros((128, 4), np.float32)
    for hp in range(4):
        for hh in range(2):
            g = lg[hp * 2 + hh]
            diff = idx[None, :] - idx[:, None]
            m = np.where(diff >= 0, np.exp(g * np.maximum(diff, 0.0)), 0.0) / 8.0
            dmT[hp, :, hh * 128:(hh + 1) * 128] = m
            kwt[hp, :, hh * 64:(hh + 1) * 64] = (np.exp(g * (127.0 - idx)) / 8.0)[:, None]
            qw[hp, hh * 64:(hh + 1) * 64, :] = np.exp(g * (idx + 1.0))[None, :]
            gc[hh * 64:(hh + 1) * 64, hp] = np.exp(g * 128.0)
    c["ret_dmT"] = dmT
    c["ret_kwt"] = kwt
    c["ret_qw"] = qw
    c["ret_gc"] = gc
    sw = np.zeros((128, 128), np.float32)
    for k in range(128):
        sw[k, (k + 64) % 128] = 1.0
    c["swapb"] = sw.astype(ml_dtypes.bfloat16)
    sg = np.zeros((128, 2), np.float32)
    sg[:64, 0], sg[64:, 0] = -1.0, 1.0
    sg[:64, 1], sg[64:, 1] = 1.0, -1.0
    c["sgn"] = sg
    rm = np.zeros((128, 8), np.float32)
    for p in range(128):
        rm[p, p // 16] = 1.0
    c["rowmask"] = rm
    ii = np.arange(128)
    su = (ii[:, None] < ii[None, :]).astype(np.float32)
    iu = (ii[:, None] <= ii[None, :]).astype(np.float32)
    sl_ = (ii[None, :] < ii[:, None]).astype(np.float32)
    c["rw_masks"] = np.concatenate([su, iu, su, iu, sl_, sl_], axis=1).astype(ml_dtypes.bfloat16)
    return c


CONST_SPECS = {
    "ident": ([128, 128], BF16), "bd32": ([128, 128], F32), "bdo64": ([128, 128], BF16),
    "rope_cos": ([128, S], F32), "rope_sin": ([128, S], F32),
    "ret_dmT": ([4, 128, 256], F32), "ret_kwt": ([4, 128, 128], F32), "ret_qw": ([4, 128, 128], F32),
    "ret_gc": ([128, 4], F32),
    "rw_masks": ([128, 768], BF16),
    "swapb": ([128, 128], BF16), "sgn": ([128, 2], F32), "rowmask": ([128, 8], F32),
}


LIM = {}
WENG = "dve"
N_LAUNCH = 1


def build_program(nlayers=DEPTH, nseq=NSEQ, debug=None, phases="0CABM"):
    debug = debug or {}
    nc = bass.Bass("TRN2", target_bir_lowering=False)
    dr = {}

    def din(name, shape, dt=F32):
        dr[name] = nc.dram_tensor(name, list(shape), dt, kind="ExternalInput").ap()
        return dr[name]

    x_d = din("x", [nseq, S, D])
    pre_norm_d = din("pre_norm", [DEPTH, 128, 8])
    w_in_d = din("w_in", [DEPTH, D, D_IN])
    cd = {k: din(k, shp, dt) for k, (shp, dt) in CONST_SPECS.items()}
    wp_d = [din(n, [DEPTH, 512, D]) for n in ("w_proj_rwkv", "w_proj_ret", "w_proj_s5")]
    wout_d = din("w_out", [DEPTH, D, D])
    bmerge_d = din("b_merge", [DEPTH, 128, 3, 8])
    postn_d = din("post_norm", [DEPTH, D])
    s5A_d = din("s5_Aab", [DEPTH, 3, 128, 32])
    s5B_d = din("s5_Bst", [DEPTH, 2, 128, 512])
    s5C_d = din("s5_Cst", [DEPTH, 2, 128, 512])
    s5v_d = din("s5_vec", [DEPTH, 128, 8])
    gluw_d = din("s5_glu_w", [DEPTH, 512, 512])
    mu_rkv_d = din("rwkv_mu_rkv", [DEPTH, 3, 512])
    mu_wa_d = din("rwkv_mu_wa", [DEPTH, 1, 128])
    w2a2_d = din("rwkv_w2a2", [DEPTH, 2, 64, 512])
    rvec_d = din("rwkv_vec", [DEPTH, 128, 4, 8])
    out_d = nc.dram_tensor("out", [nseq, S, D], F32, kind="ExternalOutput").ap()
    wcache_d = nc.dram_tensor("wcache", [DEPTH, 56, 128, 1024], BF16).ap()
    dbg_d = {}
    for k, shp in debug.items():
        dbg_d[k] = nc.dram_tensor("dbg_" + k, list(shp[0]), shp[1], kind="ExternalOutput").ap()

    with contextlib.ExitStack() as stack:
        fw = Fw(nc, stack)
        x_sb = T(fw, [128, NT, D], F32, "x_sb", nsub=NT)
        hT = T(fw, [128, 8, S + 1], BF16, "hT", nsub=NT + 1)
        ya = T(fw, [128, 4, S], BF16, "ya", nsub=16)
        yb = T(fw, [128, 4, S], BF16, "yb", nsub=16)
        yc = T(fw, [128, 4, S], BF16, "yc", nsub=16)
        ident = T(fw, [128, 128], BF16, "ident")
        bd32 = T(fw, [128, 128], F32, "bd32")
        bdo64 = T(fw, [128, 128], BF16, "bdo64")
        gpre = T(fw, [128, DEPTH, 8], F32, "gpre")
        gexp = T(fw, [128, 8, 128], F32, "gexp")
        NF, NH = 8, 12
        wf = [T(fw, [128, 512], F32, f"wf{i}") for i in range(NF)]
        wh = [T(fw, [128, 512], BF16, f"wh{i}") for i in range(NH)]
        st6 = T(fw, [128, 2, 6], F32, "st6")
        mv = T(fw, [128, 2], F32, "mv")
        e2 = T(fw, [128, 1], F32, "e2")
        rstd = T(fw, [128, 1], F32, "rstd")
        R32t = T(fw, [128, 128], F32, "R32t")
        Rbt = T(fw, [128, 128], BF16, "Rbt")
        gct = T(fw, [128, 4], F32, "gct")
        bmt = T(fw, [128, DEPTH, 3, 8], F32, "bmt")
        swapb = T(fw, [128, 128], BF16, "swapb")
        chl = T(fw, [128, 32], BF16, "chl")
        cbk = T(fw, [128, 8], F32, "cbk")
        sgn = T(fw, [128, 2], F32, "sgn")
        rowmask = T(fw, [128, 8], F32, "rowmask")
        s5v = T(fw, [128, 8], F32, "s5v")
        carry = T(fw, [128, 8], F32, "carry")
        cst = T(fw, [128, 16], F32, "cst")
        onec = T(fw, [128, 1], F32, "onec")
        rvec = T(fw, [128, 4, 8], F32, "rvec")
        omka = T(fw, [128, 4], F32, "omka")
        lw2 = T(fw, [128, 128], BF16, "lw2")
        la2 = T(fw, [128, 128], BF16, "la2")
        St32, Stb = R32t, Rbt
        gpost = T(fw, [128, D], F32, "gpost")
        NSLOT = 7
        wslot = [T(fw, [128, 8, 128], BF16, f"wslot{i}") for i in range(NSLOT)]
        wst = [T(fw, [128, 8, 128], F32, f"wst{i}", nsub=4) for i in range(2)]
        wctr = [0, 0]
        tp_ps = [T(fw, [128, 8, 128], BF16, f"tp{i}", psum=True) for i in range(2)]
        pg = [T(fw, [128, 512], F32, f"pg{i}", psum=True) for i in range(6)]
        pctr = [0]

        def ps_next():
            t = pg[pctr[0] % 4]
            pctr[0] += 1
            return t

        yctr = [0]

        def ps_y():
            t = pg[4 + yctr[0] % 2]
            yctr[0] += 1
            return t

        fw.dma("sp", ident[:], cd["ident"], writes=[ident.B])
        fw.dma("sp", bd32[:], cd["bd32"], writes=[bd32.B])
        fw.dma("sp", bdo64[:], cd["bdo64"], writes=[bdo64.B])
        fw.dma("sp", gpre[:], pre_norm_d.rearrange("l p c -> p l c"), writes=[gpre.B])
        fw.dma("sp", bmt[:], bmerge_d.rearrange("l p b c -> p l b c"), writes=[bmt.B])
        fw.dma("sp", swapb[:], cd["swapb"], writes=[swapb.B])
        fw.dma("sp", sgn[:], cd["sgn"], writes=[sgn.B])
        fw.dma("sp", rowmask[:], cd["rowmask"], writes=[rowmask.B])
        fw.pool(lambda h: h.memset(hT[:, :, 0:1], 0.0), writes=[hT.b[NT]])
        fw.dve(lambda h: h.memset(onec[:], 1.0), writes=[onec.B])

        def hT_bufs(tok0, ntok, shift=0):
            a = tok0 - shift
            bl = []
            if a < 0:
                bl.append(hT.b[NT])
                a = 0
            for tt in range(a // 128, (tok0 - shift + ntok - 1) // 128 + 1):
                bl.append(hT.b[tt])
            return bl

        def layer_setup(l):
            for c in range(8):
                fw.dve(lambda h, c=c: h.tensor_copy(out=gexp[:, c, :], in_=gpre[:, l, c:c + 1].to_broadcast([128, 128])),
                       reads=[gpre.B], writes=[gexp.B])

        def wload(l, segs):
            slot = wslot[wctr[0] % NSLOT]
            wctr[0] += 1
            stg = wst[wctr[1] % 2]
            wctr[1] += 1
            o = 0
            for i, (c0, n) in enumerate(segs):
                fw.dma("sp", stg[:, :, o:o + n],
                       w_in_d[l, :, c0:c0 + n].rearrange("(c p) n -> p c n", p=128),
                       writes=[stg.b[i]])
                o += n
            assert o == 128
            fw.op(WENG, lambda h, slot=slot, stg=stg: h.tensor_tensor(out=slot[:], in0=stg[:], in1=gexp[:], op=ALU.mult),
                  reads=stg.b + [gexp.B], writes=[slot.B])
            return slot

        def proj(ps, slot, tok0, ntok=512, shift=0, start=True, stop=True):
            hb = hT_bufs(tok0, ntok, shift)
            for c in range(8):
                fw.pe(lambda h, c=c: h.matmul(out=ps[:, 0:ntok], lhsT=slot[:, c, :],
                                              rhs=hT[:, c, 1 + tok0 - shift:1 + tok0 - shift + ntok],
                                              start=(start and c == 0), stop=(stop and c == 7)),
                      reads=[slot.B] + hb, writes=[ps.B], inc=(c == 7))

        def silu_from_psum(dst, ps):
            fw.act(lambda h: h.activation(out=dst[:], in_=ps[:], func=AF.Sigmoid), reads=[ps.B], writes=[dst.B])
            fw.dve(lambda h: h.tensor_tensor(out=dst[:], in0=ps[:], in1=dst[:], op=ALU.mult),
                   reads=[ps.B, dst.B], writes=[dst.B])

        def phase0(si, l):
            for tt in range(NT):
                xb = x_sb.b[tt]
                xt = x_sb[:, tt, :]
                for j in range(2):
                    fw.dve(lambda h, j=j, xt=xt: h.bn_stats(out=st6[:, j, :], in_=xt[:, j * 512:(j + 1) * 512]),
                           reads=[xb], writes=[st6.B])
                fw.dve(lambda h: h.bn_aggr(out=mv[:], in_=st6[:].rearrange("p a b -> p (a b)")),
                       reads=[st6.B], writes=[mv.B])
                fw.dve(lambda h: h.scalar_tensor_tensor(out=e2[:], in0=mv[:, 0:1], scalar=mv[:, 0:1],
                                                        in1=mv[:, 1:2], op0=ALU.mult, op1=ALU.add),
                       reads=[mv.B], writes=[e2.B])
                fw.act(lambda h: h.activation(out=e2[:], in_=e2[:], func=AF.Sqrt, bias=EPS, scale=1.0),
                       reads=[e2.B], writes=[e2.B])
                fw.dve(lambda h: h.reciprocal(out=rstd[:], in_=e2[:]), reads=[e2.B], writes=[rstd.B])
                for hf in range(2):
                    fw.dve(lambda h, hf=hf, xt=xt: h.tensor_scalar(out=wh[hf][:], in0=xt[:, hf * 512:(hf + 1) * 512],
                                                                   scalar1=rstd[:, 0:1], scalar2=None, op0=ALU.mult),
                           reads=[xb, rstd.B], writes=[wh[hf].B])
                ps = tp_ps[tt % 2]
                for c in range(8):
                    fw.pe(lambda h, ps=ps, c=c: h.transpose(out=ps[:, c, :],
                                                            in_=wh[c // 4][:, (c % 4) * 128:(c % 4 + 1) * 128],
                                                            identity=ident[:]),
                          reads=[wh[c // 4].B, ident.B], writes=[ps.B], inc=(c == 7))
                fw.act(lambda h, ps=ps, tt=tt: h.activation(out=hT[:, :, 1 + tt * 128:1 + (tt + 1) * 128],
                                                            in_=ps[:], func=AF.Copy),
                       reads=[ps.B], writes=[hT.b[tt]])

        def headnorm_gate(y_ps, sg, dst, dstb, eps, affine=None):
            y32, ybf, ycen, sq, rs = wf[0], wh[0], wf[1], wh[1], wf[2]
            fw.act(lambda h: h.activation(out=y32[:], in_=y_ps[:], func=AF.Copy), reads=[y_ps.B], writes=[y32.B])
            fw.dve(lambda h: h.tensor_copy(out=ybf[:], in_=y32[:]), reads=[y32.B], writes=[ybf.B])
            mean_ps = ps_next()
            fw.pe(lambda h: h.matmul(out=mean_ps[:], lhsT=bdo64[:], rhs=ybf[:], start=True, stop=True),
                  reads=[bdo64.B, ybf.B], writes=[mean_ps.B])
            fw.dve(lambda h: h.tensor_tensor(out=ycen[:], in0=y32[:], in1=mean_ps[:], op=ALU.subtract),
                   reads=[y32.B, mean_ps.B], writes=[ycen.B])
            fw.act(lambda h: h.activation(out=sq[:], in_=ycen[:], func=AF.Square), reads=[ycen.B], writes=[sq.B])
            var_ps = ps_next()
            fw.pe(lambda h: h.matmul(out=var_ps[:], lhsT=bdo64[:], rhs=sq[:], start=True, stop=True),
                  reads=[bdo64.B, sq.B], writes=[var_ps.B])
            fw.act(lambda h: h.activation(out=rs[:], in_=var_ps[:], func=AF.Sqrt, bias=eps, scale=1.0),
                   reads=[var_ps.B], writes=[rs.B])
            fw.dve(lambda h: h.reciprocal(out=rs[:], in_=rs[:]), reads=[rs.B], writes=[rs.B])
            fw.dve(lambda h: h.tensor_tensor(out=ycen[:], in0=ycen[:], in1=rs[:], op=ALU.mult),
                    reads=[ycen.B, rs.B], writes=[ycen.B])
            if affine is not None:
                affine(ycen)
            fw.dve(lambda h: h.tensor_tensor(out=dst, in0=ycen[:], in1=sg[:], op=ALU.mult),
                    reads=[ycen.B, sg.B], writes=[dstb])

        def phaseB(si, l):
            dmT = wf[4]
            kwt = wf[5]
            qwt = wf[6]
            fw.dma("sp", gct[:, 0:4], cd["ret_gc"], writes=[gct.B])
            R32 = R32t
            t1, t2 = wf[0], wf[1]
            cosb, sinb = wf[2], wf[3]
            qr, kr, qc, vsb, ktok, vp0, vp1 = wh[2], wh[3], wh[4], wh[5], wh[6], wh[7], wh[8]
            qpad = [wh[9], wh[10]]
            Ssb = wh[11]
            Rb = Rbt
            sgate = wf[7]
            for hp in range(LIM.get('hp', 4)):
                cb = O_BQ + hp * 128
                kb = O_BK + hp * 128
                sw = lambda b: [(b + 32, 32), (b, 32), (b + 96, 32), (b + 64, 32)]
                w_q = wload(l, [(cb, 128)])
                w_qs = wload(l, sw(cb))
                w_k = wload(l, [(kb, 128)])
                w_ks = wload(l, sw(kb))
                w_v = wload(l, [(O_BV + hp * 128, 128)])
                w_g = wload(l, [(O_BG + hp * 128, 128)])
                fw.dma("sp", dmT[:, 0:256], cd["ret_dmT"][hp], writes=[dmT.B])
                fw.dma("sp", kwt[:, 0:128], cd["ret_kwt"][hp], writes=[kwt.B])
                fw.dma("sp", qwt[:, 0:128], cd["ret_qw"][hp], writes=[qwt.B])
                fw.pool(lambda h: h.memset(R32[:], 0.0), writes=[R32.B])
                fw.pool(lambda h: h.memset(Rb[:], 0.0), writes=[Rb.B])
                for qp in qpad:
                    fw.pool(lambda h, qp=qp: h.memset(qp[:], 0.0), writes=[qp.B])
                fw.pool(lambda h: h.memset(vp0[:], 0.0), writes=[vp0.B])
                fw.pool(lambda h: h.memset(vp1[:], 0.0), writes=[vp1.B])
                def do_block(tb, hp=hp, w_q=w_q, w_qs=w_qs, w_k=w_k, w_ks=w_ks, w_v=w_v, w_g=w_g):
                    tok0 = tb * 512
                    fw.dma("sp", cosb[:], cd["rope_cos"][:, tok0:tok0 + 512], writes=[cosb.B])
                    fw.dma("sp", sinb[:], cd["rope_sin"][:, tok0:tok0 + 512], writes=[sinb.B])
                    if LIM.get('stage', 99) < 1:
                        return
                    pq, pqs = ps_next(), ps_next()
                    proj(pq, w_q, tok0)
                    proj(pqs, w_qs, tok0)
                    fw.dve(lambda h: h.tensor_tensor(out=t1[:], in0=pq[:], in1=cosb[:], op=ALU.mult),
                           reads=[pq.B, cosb.B], writes=[t1.B])
                    fw.dve(lambda h: h.tensor_tensor(out=t2[:], in0=pqs[:], in1=sinb[:], op=ALU.mult),
                           reads=[pqs.B, sinb.B], writes=[t2.B])
                    fw.dve(lambda h: h.tensor_tensor(out=qr[:], in0=t1[:], in1=t2[:], op=ALU.add),
                            reads=[t1.B, t2.B], writes=[qr.B])
                    if LIM.get('stage', 99) < 2:
                        return
                    for half in range(2):
                        qp = qpad[half]
                        for hh in range(2):
                            src = qr[hh * 64:(hh + 1) * 64, half * 256:(half + 1) * 256].rearrange("p (c i) -> p c i", c=2)
                            dstv = qp[hh * 64:(hh + 1) * 64, :].rearrange("p (c h i) -> p c h i", c=2, h=2)[:, :, hh, :]
                            fw.act(lambda h, src=src, dstv=dstv: h.activation(out=dstv, in_=src, func=AF.Copy),
                                   reads=[qr.B], writes=[qp.B])
                    for c4 in range(4):
                        fw.dve(lambda h, c4=c4: h.tensor_tensor(out=qc[:, c4 * 128:(c4 + 1) * 128],
                                                                 in0=qr[:, c4 * 128:(c4 + 1) * 128],
                                                                 in1=qwt[:, 0:128], op=ALU.mult),
                                reads=[qr.B, qwt.B], writes=[qc.B])
                    if LIM.get('stage', 99) < 3:
                        return
                    pk, pks = ps_next(), ps_next()
                    proj(pk, w_k, tok0)
                    proj(pks, w_ks, tok0)
                    fw.dve(lambda h: h.tensor_tensor(out=t1[:], in0=pk[:], in1=cosb[:], op=ALU.mult),
                           reads=[pk.B, cosb.B], writes=[t1.B])
                    fw.dve(lambda h: h.tensor_tensor(out=t2[:], in0=pks[:], in1=sinb[:], op=ALU.mult),
                           reads=[pks.B, sinb.B], writes=[t2.B])
                    fw.dve(lambda h: h.tensor_tensor(out=kr[:], in0=t1[:], in1=t2[:], op=ALU.add),
                            reads=[t1.B, t2.B], writes=[kr.B])
                    if LIM.get('stage', 99) < 4:
                        return
                    pv = ps_next()
                    proj(pv, w_v, tok0)
                    fw.act(lambda h: h.activation(out=vsb[:], in_=pv[:], func=AF.Copy), reads=[pv.B], writes=[vsb.B])
                    pgate = ps_next()
                    proj(pgate, w_g, tok0)
                    silu_from_psum(sgate, pgate)
                    if LIM.get('stage', 99) < 5:
                        return
                    tp = tp_ps[0]
                    for c4 in range(4):
                        fw.pe(lambda h, c4=c4: h.transpose(out=tp[:, c4, :], in_=kr[:, c4 * 128:(c4 + 1) * 128],
                                                           identity=ident[:]),
                              reads=[kr.B, ident.B], writes=[tp.B], inc=False)
                    for c4 in range(4):
                        fw.pe(lambda h, c4=c4: h.transpose(out=tp[:, 4 + c4, :], in_=vsb[:, c4 * 128:(c4 + 1) * 128],
                                                           identity=ident[:]),
                              reads=[vsb.B, ident.B], writes=[tp.B], inc=(c4 == 3))
                    for c4 in range(4):
                        fw.dve(lambda h, c4=c4: h.tensor_tensor(out=ktok[:, c4 * 128:(c4 + 1) * 128], in0=tp[:, c4, :],
                                                                in1=kwt[:, 0:128], op=ALU.mult),
                               reads=[tp.B, kwt.B], writes=[ktok.B])
                    fw.act(lambda h: h.activation(
                        out=vp0[:].rearrange("p (c f) -> p c f", c=4)[:, :, 0:64], in_=tp[:, 4:8, 0:64], func=AF.Copy),
                        reads=[tp.B], writes=[vp0.B])
                    fw.act(lambda h: h.activation(
                        out=vp1[:].rearrange("p (c f) -> p c f", c=4)[:, :, 64:128], in_=tp[:, 4:8, 64:128], func=AF.Copy),
                        reads=[tp.B], writes=[vp1.B])
                    if LIM.get('stage', 99) < 6:
                        return
                    y_ps = ps_y()

                    def do_chunk(c4):
                        cs = slice(c4 * 128, (c4 + 1) * 128)
                        sc = ps_next()
                        qp = qpad[c4 // 2]
                        fw.pe(lambda h, cs=cs, qp=qp, c4=c4, sc=sc: h.matmul(
                            out=sc[:, 0:256], lhsT=kr[:, cs], rhs=qp[:, (c4 % 2) * 256:(c4 % 2) * 256 + 256],
                            start=True, stop=True), reads=[kr.B, qp.B], writes=[sc.B])
                        sv = Ssb[:, (c4 % 2) * 256:(c4 % 2) * 256 + 256]
                        fw.dve(lambda h, sc=sc, sv=sv: h.tensor_tensor(out=sv, in0=sc[:, 0:256], in1=dmT[:, 0:256],
                                                                       op=ALU.mult),
                               reads=[sc.B, dmT.B], writes=[Ssb.B])
                        fw.pe(lambda h, cs=cs, sv=sv: h.matmul(out=y_ps[:, cs], lhsT=vp0[:, cs], rhs=sv[:, 0:128],
                                                               start=True, stop=False),
                              reads=[vp0.B, Ssb.B], writes=[y_ps.B], inc=False)
                        fw.pe(lambda h, cs=cs, sv=sv: h.matmul(out=y_ps[:, cs], lhsT=vp1[:, cs], rhs=sv[:, 128:256],
                                                               start=False, stop=False),
                              reads=[vp1.B, Ssb.B], writes=[y_ps.B], inc=False)
                        fw.pe(lambda h, cs=cs: h.matmul(out=y_ps[:, cs], lhsT=Rb[:], rhs=qc[:, cs],
                                                        start=False, stop=True),
                              reads=[Rb.B, qc.B], writes=[y_ps.B])
                        kv = ps_next()
                        fw.pe(lambda h, cs=cs, kv=kv: h.matmul(out=kv[:, 0:128], lhsT=ktok[:, cs], rhs=vp0[:, cs],
                                                               start=True, stop=False),
                              reads=[ktok.B, vp0.B], writes=[kv.B], inc=False)
                        fw.pe(lambda h, cs=cs, kv=kv: h.matmul(out=kv[:, 0:128], lhsT=ktok[:, cs], rhs=vp1[:, cs],
                                                               start=False, stop=True),
                              reads=[ktok.B, vp1.B], writes=[kv.B])
                        fw.dve(lambda h, kv=kv, hp=hp: h.scalar_tensor_tensor(
                            out=R32[:], in0=R32[:], scalar=gct[:, hp:hp + 1], in1=kv[:, 0:128],
                            op0=ALU.mult, op1=ALU.add), reads=[R32.B, gct.B, kv.B], writes=[R32.B])
                        fw.dve(lambda h: h.tensor_tensor(out=Rb[:], in0=R32[:], in1=bd32[:],
                                                          op=ALU.mult),
                                reads=[R32.B, bd32.B], writes=[Rb.B])
                    for c4 in range(LIM.get('c4', 4)):
                        do_chunk(c4)
                    if LIM.get('stage', 99) < 7:
                        return
                    headnorm_gate(y_ps, sgate, yb[:, hp, tok0:tok0 + 512], yb.b[hp * 4 + tb], EPS)

                for tb in range(LIM.get('tb', 4)):
                    do_block(tb)

        def phaseC(si, l):
            PA, PB = wf[0], wf[1]
            sl = lambda t, i: t[:, i * 32:(i + 1) * 32]
            A_RE, A_IM, DT, MAG, ANG, CC, SS, T1, T2, T3, PM, RDEN, CRE, CIM, QQ, SLS = range(16)
            tabs = ya.b + yb.b
            cosT = ya[:].rearrange("p a s -> p (a s)").bitcast(F32).rearrange("p (g j) -> p g j", g=32)
            sinT = yb[:].rearrange("p a s -> p (a s)").bitcast(F32).rearrange("p (g j) -> p g j", g=32)
            ycf = yc[:].rearrange("p a s -> p (a s)").bitcast(F32)
            tmp1 = ycf[:, 0:2048].rearrange("p (g j) -> p g j", g=32)
            tmp2 = ycf[:, 2048:4096].rearrange("p (g j) -> p g j", g=32)

            def pa(fn_, eng="dve", extra=()):
                fw.op(eng, fn_, reads=[PA.B, PB.B] + list(extra), writes=[PA.B, PB.B])

            def tt(o, a, b, op):
                pa(lambda h: h.tensor_tensor(out=o, in0=a, in1=b, op=op))

            P = lambda i: sl(PA, i)
            for i in range(3):
                fw.dma("sp", P(i), s5A_d[l, i], writes=[PA.B])
            fw.dma("sp", s5v[:], s5v_d[l], writes=[s5v.B])
            pa(lambda h: h.activation(out=P(DT), in_=P(DT), func=AF.Exp), "act")
            tt(P(T1), P(DT), P(A_RE), ALU.mult)
            pa(lambda h: h.activation(out=P(MAG), in_=P(T1), func=AF.Exp), "act")
            tt(P(ANG), P(DT), P(A_IM), ALU.mult)
            pa(lambda h: h.activation(out=P(SS), in_=P(ANG), func=AF.Sin, scale=1.0 / 16.0), "act")
            pa(lambda h: h.activation(out=P(CC), in_=P(ANG), func=AF.Sin, scale=1.0 / 16.0, bias=float(np.pi / 2)), "act")

            def dbl(co, so, ci, si_):
                tt(P(T1), ci, ci, ALU.mult)
                tt(P(T2), si_, si_, ALU.mult)
                tt(P(T3), si_, ci, ALU.mult)
                tt(co, P(T1), P(T2), ALU.subtract)
                pa(lambda h: h.tensor_scalar(out=so, in0=P(T3), scalar1=2.0, scalar2=None, op0=ALU.mult))

            for _ in range(3):
                dbl(P(CC), P(SS), P(CC), P(SS))
            dbl(sl(PB, 0), sl(PB, 8), P(CC), P(SS))
            for k in range(1, 8):
                dbl(sl(PB, k), sl(PB, 8 + k), sl(PB, k - 1), sl(PB, 8 + k - 1))
            pa(lambda h: h.tensor_scalar(out=P(SLS), in0=sl(PB, 15), scalar1=sgn[:, 0:1], scalar2=None, op0=ALU.mult),
               extra=[sgn.B])
            tt(P(PM), P(MAG), sl(PB, 0), ALU.mult)
            pa(lambda h: h.tensor_scalar(out=P(PM), in0=P(PM), scalar1=-1.0, scalar2=None, op0=ALU.add))
            tt(P(QQ), P(MAG), sl(PB, 8), ALU.mult)
            tt(P(T1), P(A_RE), P(A_RE), ALU.mult)
            tt(P(T2), P(A_IM), P(A_IM), ALU.mult)
            tt(P(T1), P(T1), P(T2), ALU.add)
            pa(lambda h: h.reciprocal(out=P(RDEN), in_=P(T1)))
            tt(P(T1), P(PM), P(A_RE), ALU.mult)
            tt(P(T2), P(QQ), P(A_IM), ALU.mult)
            tt(P(T1), P(T1), P(T2), ALU.add)
            tt(P(CRE), P(T1), P(RDEN), ALU.mult)
            tt(P(T1), P(QQ), P(A_RE), ALU.mult)
            tt(P(T2), P(PM), P(A_IM), ALU.mult)
            tt(P(T1), P(T1), P(T2), ALU.subtract)
            tt(P(CIM), P(T1), P(RDEN), ALU.mult)

            fw.dve(lambda h: h.memset(cosT[:, :, 0:1], 1.0), writes=tabs)
            fw.dve(lambda h: h.memset(sinT[:, :, 0:1], 0.0), writes=tabs)
            for k in range(7):
                m = 1 << k
                cmb = sl(PB, k).unsqueeze(2).to_broadcast([128, 32, m])
                smb = sl(PB, 8 + k).unsqueeze(2).to_broadcast([128, 32, m])

                def lvl(m=m, cmb=cmb, smb=smb):
                    rw = dict(reads=tabs + yc.b + [PB.B], writes=tabs + yc.b)
                    fw.dve(lambda h: h.tensor_tensor(out=tmp1[:, :, 0:m], in0=cosT[:, :, 0:m], in1=cmb, op=ALU.mult), **rw)
                    fw.dve(lambda h: h.tensor_tensor(out=tmp2[:, :, 0:m], in0=sinT[:, :, 0:m], in1=smb, op=ALU.mult), **rw)
                    fw.dve(lambda h: h.tensor_tensor(out=cosT[:, :, m:2 * m], in0=tmp1[:, :, 0:m], in1=tmp2[:, :, 0:m],
                                                     op=ALU.subtract), **rw)
                    fw.dve(lambda h: h.tensor_tensor(out=tmp1[:, :, 0:m], in0=sinT[:, :, 0:m], in1=cmb, op=ALU.mult), **rw)
                    fw.dve(lambda h: h.tensor_tensor(out=tmp2[:, :, 0:m], in0=cosT[:, :, 0:m], in1=smb, op=ALU.mult), **rw)
                    fw.dve(lambda h: h.tensor_tensor(out=sinT[:, :, m:2 * m], in0=tmp1[:, :, 0:m], in1=tmp2[:, :, 0:m],
                                                     op=ALU.add), **rw)
                lvl()

            xh = [wf[2], wf[3]]
            stg = wf[4]
            stg2 = wf[5]
            tri = wf[6]
            BmT = wh[0:4]
            CmT = wh[4:8]
            u_bf, g12 = wh[8], wh[9]
            for t_ in CmT:
                fw.dve(lambda h, t_=t_: h.memset(t_[:], 0.0), writes=[t_.B])

            def xh_ap(g8):
                return xh[g8 // 4][:, (g8 % 4) * 128:(g8 % 4 + 1) * 128]

            def do_gc(gc):
                gs = slice(gc * 128, (gc + 1) * 128)
                fw.dma("sp", stg[:, 0:128], s5B_d[l, 0, :, gs], writes=[stg.B])
                fw.dma("sp", stg[:, 128:256], s5B_d[l, 1, :, gs], writes=[stg.B])
                v3 = lambda ap: ap.rearrange("p (g q) -> p g q", g=8)
                creb = P(CRE)[:, gc * 8:(gc + 1) * 8].unsqueeze(2).to_broadcast([128, 8, 16])
                cimb = P(CIM)[:, gc * 8:(gc + 1) * 8].unsqueeze(2).to_broadcast([128, 8, 16])
                rw = dict(reads=[stg.B, stg2.B, PA.B], writes=[stg2.B])
                bre, bim = v3(stg[:, 0:128]), v3(stg[:, 128:256])
                ta, tb_ = v3(stg2[:, 0:128]), v3(stg2[:, 128:256])
                bbre, bbim = v3(g12[:, 0:128]), v3(g12[:, 128:256])
                rwb = dict(reads=[stg.B, stg2.B, PA.B], writes=[g12.B])
                fw.dve(lambda h: h.tensor_tensor(out=ta, in0=bre, in1=creb, op=ALU.mult), **rw)
                fw.dve(lambda h: h.tensor_tensor(out=tb_, in0=bim, in1=cimb, op=ALU.mult), **rw)
                fw.dve(lambda h: h.tensor_tensor(out=bbre, in0=ta, in1=tb_, op=ALU.subtract), **rwb)
                fw.dve(lambda h: h.tensor_tensor(out=ta, in0=bim, in1=creb, op=ALU.mult), **rw)
                fw.dve(lambda h: h.tensor_tensor(out=tb_, in0=bre, in1=cimb, op=ALU.mult), **rw)
                fw.dve(lambda h: h.tensor_tensor(out=bbim, in0=ta, in1=tb_, op=ALU.add), **rwb)
                tpx = tp_ps[0]
                ptr, pti = tpx[:, 0, :], tpx[:, 1, :]
                fw.pe(lambda h: h.transpose(out=ptr, in_=g12[:, 0:128], identity=ident[:]),
                      reads=[g12.B, ident.B], writes=[tpx.B], inc=False)
                fw.pe(lambda h: h.transpose(out=pti, in_=g12[:, 128:256], identity=ident[:]),
                      reads=[g12.B, ident.B], writes=[tpx.B])
                fw.act(lambda h: h.activation(out=tri[:, 0:64], in_=ptr[:, 0:64], func=AF.Copy), reads=[tpx.B], writes=[tri.B])
                fw.act(lambda h: h.activation(out=tri[:, 64:128], in_=pti[:, 0:64], func=AF.Copy), reads=[tpx.B], writes=[tri.B])
                fw.act(lambda h: h.activation(out=tri[:, 128:192], in_=pti[:, 0:64], func=AF.Copy), reads=[tpx.B], writes=[tri.B])
                fw.act(lambda h: h.activation(out=tri[:, 192:256], in_=ptr[:, 0:64], func=AF.Copy, scale=-1.0),
                       reads=[tpx.B], writes=[tri.B])
                for g8 in range(8):
                    bt = BmT[g8 // 2]
                    o = (g8 % 2) * 256
                    fw.dve(lambda h, bt=bt, o=o, g8=g8: h.tensor_scalar(
                        out=bt[:, o:o + 256], in0=tri[:, 0:256], scalar1=rowmask[:, g8:g8 + 1], scalar2=None, op0=ALU.mult),
                        reads=[tri.B, rowmask.B], writes=[bt.B])
                fw.dma("sp", stg[:, 0:128], s5C_d[l, 0, :, gs], writes=[stg.B])
                fw.dma("sp", stg[:, 128:256], s5C_d[l, 1, :, gs], writes=[stg.B])
                fw.dve(lambda h: h.tensor_scalar(out=stg[:, 0:128], in0=stg[:, 0:128], scalar1=sgn[:, 1:2], scalar2=None,
                                                 op0=ALU.mult), reads=[stg.B, sgn.B], writes=[stg.B])
                fw.dve(lambda h: h.tensor_scalar(out=stg[:, 128:256], in0=stg[:, 128:256], scalar1=-1.0, scalar2=None,
                                                 op0=ALU.mult), reads=[stg.B], writes=[stg.B])
                for g8 in range(8):
                    ct = CmT[g8 // 2]
                    o = (g8 % 2) * 256
                    for ver in range(2):
                        fw.act(lambda h, ct=ct, o=o, g8=g8, ver=ver: h.activation(
                            out=ct[:, o + ver * 128 + g8 * 16:o + ver * 128 + (g8 + 1) * 16],
                            in_=stg[:, ver * 128 + g8 * 16:ver * 128 + (g8 + 1) * 16], func=AF.Copy),
                            reads=[stg.B], writes=[ct.B])
                w_u = wload(l, [(O_CU + gc * 128, 128)])

                def do_block(tb):
                    tok0 = tb * 512
                    pu = ps_next()
                    proj(pu, w_u, tok0)
                    u32 = wf[7]
                    fw.act(lambda h: h.activation(out=u_bf[:], in_=pu[:], func=AF.Copy), reads=[pu.B], writes=[u_bf.B])
                    fw.act(lambda h: h.activation(out=u32[:], in_=pu[:], func=AF.Copy), reads=[pu.B], writes=[u32.B])
                    y_ps = ps_y()

                    def do_sb(sb):
                        ts = slice(sb * 128, (sb + 1) * 128)
                        first = (tb == 0 and sb == 0)

                        def do_batch(bi):
                            g0 = gc * 8 + bi * 4
                            bun, bus = ps_next(), ps_next()
                            for q in range(4):
                                g8 = bi * 4 + q
                                bt = BmT[g8 // 2]
                                o = (g8 % 2) * 256
                                fw.pe(lambda h, q=q, bt=bt, o=o: h.matmul(out=bun[:, q * 128:(q + 1) * 128], lhsT=bt[:, o:o + 128],
                                                                          rhs=u_bf[:, ts], start=True, stop=True),
                                      reads=[bt.B, u_bf.B], writes=[bun.B], inc=(q == 3))
                            for q in range(4):
                                g8 = bi * 4 + q
                                bt = BmT[g8 // 2]
                                o = (g8 % 2) * 256
                                fw.pe(lambda h, q=q, bt=bt, o=o: h.matmul(out=bus[:, q * 128:(q + 1) * 128],
                                                                          lhsT=bt[:, o + 128:o + 256], rhs=u_bf[:, ts],
                                                                          start=True, stop=True),
                                      reads=[bt.B, u_bf.B], writes=[bus.B], inc=(q == 3))
                            w1, w2 = stg, stg2
                            cs4 = cosT[:, g0:g0 + 4, :]
                            sn4 = sinT[:, g0:g0 + 4, :]
                            v4 = lambda ap: ap.rearrange("p (g j) -> p g j", g=4)
                            fw.dve(lambda h: h.tensor_tensor(out=v4(w1[:]), in0=v4(bun[:]), in1=cs4, op=ALU.mult),
                                   reads=[bun.B] + tabs, writes=[w1.B])
                            fw.dve(lambda h: h.tensor_tensor(out=v4(w2[:]), in0=v4(bus[:]), in1=sn4, op=ALU.mult),
                                   reads=[bus.B] + tabs, writes=[w2.B])
                            fw.dve(lambda h: h.tensor_tensor(out=w1[:], in0=w1[:], in1=w2[:], op=ALU.add),
                                   reads=[w1.B, w2.B], writes=[w1.B])
                            xt_ = xh[bi]
                            for q in range(4):
                                g8 = bi * 4 + q
                                g = gc * 8 + g8
                                init = 0.0 if first else carry[:, g8:g8 + 1]
                                fw.dve(lambda h, q=q, g=g, init=init: h.tensor_tensor_scan(
                                    out=xt_[:, q * 128:(q + 1) * 128], data0=P(MAG)[:, g:g + 1].to_broadcast([128, 128]),
                                    data1=w1[:, q * 128:(q + 1) * 128], initial=init, op0=ALU.mult, op1=ALU.add),
                                    reads=[w1.B, PA.B, carry.B], writes=[xt_.B])
                            G1, G2 = g12, wh[10]
                            fw.dve(lambda h: h.tensor_tensor(out=v4(G1[:]), in0=v4(xt_[:]), in1=cs4, op=ALU.mult),
                                   reads=[xt_.B] + tabs, writes=[G1.B])
                            fw.dve(lambda h: h.tensor_tensor(out=v4(G2[:]), in0=v4(xt_[:]), in1=sn4, op=ALU.mult),
                                   reads=[xt_.B] + tabs, writes=[G2.B])
                            for q in range(4):
                                g8 = bi * 4 + q
                                ct = CmT[g8 // 2]
                                o = (g8 % 2) * 256
                                fw.pe(lambda h, q=q, ct=ct, o=o, g8=g8: h.matmul(
                                    out=y_ps[:, ts], lhsT=ct[:, o:o + 128], rhs=G1[:, q * 128:(q + 1) * 128],
                                    start=(g8 == 0), stop=False), reads=[ct.B, G1.B], writes=[y_ps.B], inc=(q == 3))
                            for q in range(4):
                                g8 = bi * 4 + q
                                ct = CmT[g8 // 2]
                                o = (g8 % 2) * 256
                                fw.pe(lambda h, q=q, ct=ct, o=o, g8=g8: h.matmul(
                                    out=y_ps[:, ts], lhsT=ct[:, o + 128:o + 256], rhs=G2[:, q * 128:(q + 1) * 128],
                                    start=False, stop=(g8 == 7)), reads=[ct.B, G2.B], writes=[y_ps.B], inc=(q == 3))

                        for bi in range(2):
                            do_batch(bi)
                        csw = ps_next()
                        for hx in range(2):
                            xl = xh[hx][:].rearrange("p (g j) -> p g j", g=4)[:, :, 127]
                            fw.dve(lambda h, hx=hx, xl=xl: h.tensor_copy(out=chl[:, hx * 4:(hx + 1) * 4], in_=xl),
                                   reads=[xh[hx].B], writes=[chl.B])
                        fw.dve(lambda h: h.tensor_copy(out=cbk[:], in_=chl[:, 0:8]), reads=[chl.B], writes=[cbk.B])
                        for hx in range(2):
                            xl = xh[hx][:].rearrange("p (g j) -> p g j", g=4)[:, :, 127]
                            fw.dve(lambda h, hx=hx, xl=xl: h.tensor_tensor(out=chl[:, 8 + hx * 4:8 + (hx + 1) * 4], in0=xl,
                                                                          in1=cbk[:, hx * 4:(hx + 1) * 4], op=ALU.subtract),
                                   reads=[xh[hx].B, cbk.B], writes=[chl.B])
                        fw.pe(lambda h: h.matmul(out=csw[:, 0:8], lhsT=swapb[:], rhs=chl[:, 0:8], start=True, stop=False),
                              reads=[swapb.B, chl.B], writes=[csw.B], inc=False)
                        fw.pe(lambda h: h.matmul(out=csw[:, 0:8], lhsT=swapb[:], rhs=chl[:, 8:16], start=False, stop=True),
                              reads=[swapb.B, chl.B], writes=[csw.B])
                        fw.dve(lambda h: h.tensor_tensor(out=cst[:, 0:8], in0=csw[:, 0:8], in1=P(SLS)[:, gc * 8:(gc + 1) * 8],
                                                         op=ALU.mult), reads=[csw.B, PA.B], writes=[cst.B])
                        for hx in range(2):
                            xl = xh[hx][:].rearrange("p (g j) -> p g j", g=4)[:, :, 127]
                            fw.dve(lambda h, hx=hx, xl=xl: h.tensor_tensor(
                                out=cst[:, 8 + hx * 4:8 + (hx + 1) * 4], in0=xl,
                                in1=sl(PB, 7)[:, gc * 8 + hx * 4:gc * 8 + (hx + 1) * 4], op=ALU.mult),
                                reads=[xh[hx].B, PB.B], writes=[cst.B])
                        fw.dve(lambda h: h.tensor_tensor(out=carry[:], in0=cst[:, 0:8], in1=cst[:, 8:16], op=ALU.add),
                               reads=[cst.B], writes=[carry.B])

                    for sb in range(4):
                        do_sb(sb)
                    y32, gt = wf[6], wf[7]
                    fw.dve(lambda h: h.scalar_tensor_tensor(out=y32[:], in0=u32[:], scalar=s5v[:, gc:gc + 1], in1=y_ps[:],
                                                            op0=ALU.mult, op1=ALU.add),
                           reads=[u32.B, s5v.B, y_ps.B], writes=[y32.B])
                    fw.act(lambda h: h.activation(out=gt[:], in_=y32[:], func=AF.Square), reads=[y32.B], writes=[gt.B])
                    fw.dve(lambda h: h.tensor_scalar(out=gt[:], in0=gt[:], scalar1=0.044715, scalar2=1.0, op0=ALU.mult,
                                                     op1=ALU.add), reads=[gt.B], writes=[gt.B])
                    fw.dve(lambda h: h.tensor_tensor(out=gt[:], in0=gt[:], in1=y32[:], op=ALU.mult),
                           reads=[gt.B, y32.B], writes=[gt.B])
                    fw.act(lambda h: h.activation(out=gt[:], in_=gt[:], func=AF.Tanh, scale=0.7978845608028654),
                           reads=[gt.B], writes=[gt.B])
                    fw.dve(lambda h: h.tensor_scalar(out=gt[:], in0=gt[:], scalar1=1.0, scalar2=0.5, op0=ALU.add,
                                                     op1=ALU.mult), reads=[gt.B], writes=[gt.B])
                    fw.dve(lambda h: h.tensor_tensor(out=yc[:, gc, tok0:tok0 + 512], in0=gt[:], in1=y32[:], op=ALU.mult),
                           reads=[gt.B, y32.B], writes=[yc.b[gc * 4 + tb]])

                for tb in range(LIM.get('tb', 4)):
                    do_block(tb)

            for gc in range(4):
                do_gc(gc)

            gw = wh[0:4]
            for c in range(4):
                fw.dma("sp", stg[:], gluw_d[l, c * 128:(c + 1) * 128, :], writes=[stg.B])
                fw.dve(lambda h, c=c: h.tensor_copy(out=gw[c][:], in_=stg[:]), reads=[stg.B], writes=[gw[c].B])
            wgs = [wload(l, [(O_CG + oc * 128, 128)]) for oc in range(4)]

            def glu_block(tb):
                tok0 = tb * 512
                sgl = [wf[0], wf[1], wf[2], wf[3]]
                for oc in range(4):
                    gp = pg[oc]
                    for c in range(4):
                        fw.pe(lambda h, oc=oc, c=c, gp=gp: h.matmul(out=gp[:], lhsT=gw[c][:, oc * 128:(oc + 1) * 128],
                                                                    rhs=yc[:, c, tok0:tok0 + 512], start=(c == 0), stop=(c == 3)),
                              reads=[gw[c].B, yc.b[c * 4 + tb]], writes=[gp.B], inc=(c == 3))
                    fw.act(lambda h, oc=oc, gp=gp: h.activation(out=sgl[oc][:], in_=gp[:], func=AF.Sigmoid,
                                                                bias=s5v[:, 4 + oc:5 + oc], scale=1.0),
                           reads=[gp.B, s5v.B], writes=[sgl[oc].B])
                for oc in range(4):
                    pgt = ps_y()
                    proj(pgt, wgs[oc], tok0)
                    sgt = wf[4]
                    silu_from_psum(sgt, pgt)
                    fw.dve(lambda h, oc=oc, sgt=sgt: h.tensor_tensor(out=sgt[:], in0=sgt[:], in1=sgl[oc][:], op=ALU.mult),
                           reads=[sgt.B, sgl[oc].B], writes=[sgt.B])
                    fw.dve(lambda h, oc=oc, sgt=sgt: h.tensor_tensor(out=yc[:, oc, tok0:tok0 + 512],
                                                                     in0=yc[:, oc, tok0:tok0 + 512], in1=sgt[:], op=ALU.mult),
                           reads=[sgt.B, yc.b[oc * 4 + tb]], writes=[yc.b[oc * 4 + tb]])

            for tb in range(LIM.get('tb', 4)):
                glu_block(tb)

        def wload_shift(l, c0, mu_src):
            s1 = wslot[wctr[0] % NSLOT]
            wctr[0] += 1
            s2 = wslot[wctr[0] % NSLOT]
            wctr[0] += 1
            stg = wst[wctr[1] % 2]
            wctr[1] += 1
            mub = wf[2]
            fw.dma("sp", mub[:, 0:128], mu_src.broadcast_to([128, 128]), writes=[mub.B])
            fw.dve(lambda h: h.tensor_scalar(out=mub[:, 128:256], in0=mub[:, 0:128], scalar1=-1.0, scalar2=1.0,
                                             op0=ALU.mult, op1=ALU.add), reads=[mub.B], writes=[mub.B])
            fw.dma("sp", stg[:], w_in_d[l, :, c0:c0 + 128].rearrange("(c p) n -> p c n", p=128), writes=stg.b)
            fw.dve(lambda h: h.tensor_tensor(out=stg[:], in0=stg[:], in1=gexp[:], op=ALU.mult),
                   reads=stg.b + [gexp.B], writes=stg.b)
            fw.dve(lambda h: h.tensor_tensor(out=s2[:], in0=stg[:], in1=mub[:, 0:128].unsqueeze(1).to_broadcast([128, 8, 128]),
                                             op=ALU.mult), reads=stg.b + [mub.B], writes=[s2.B])
            fw.dve(lambda h: h.tensor_tensor(out=s1[:], in0=stg[:], in1=mub[:, 128:256].unsqueeze(1).to_broadcast([128, 8, 128]),
                                             op=ALU.mult), reads=stg.b + [mub.B], writes=[s1.B])
            return s1, s2

        def proj_shift(ps, s12, tok0):
            proj(ps, s12[0], tok0, shift=0, start=True, stop=False)
            proj(ps, s12[1], tok0, shift=1, start=False, stop=True)

        def phaseA(si, l):
            CDEC = 0.6065306597126334
            ybf = yb[:].rearrange("p a s -> p (a s)")
            SB = [Buf(f"scrA{i}") for i in range(16)]
            SV = [ybf[:, i * 512:(i + 1) * 512] for i in range(16)]
            fw.dve(lambda h: h.memset(cst[:, 0:1], 0.0), reads=yb.b, writes=SB + [cst.B])
            QP = [SV[i] for i in range(4)]
            QPB = SB[0:4]
            MK1, MK1B = SV[4], SB[4]
            MK2, MK2B = SV[5][:, 0:256], SB[5]
            E1, E2, E3 = SV[6], SV[7], SV[8]
            XB = [SV[9], SV[10]]
            PP = SV[11]
            MISC = SV[12]
            kt_tok, bt_tok, vp0 = SV[13], SV[14], SV[15]
            vp1 = wh[9]
            v_bf, sqk, rk_bf, rt, at, kt, bt = wh[2], wh[3], wh[4], wh[5], wh[6], wh[7], wh[8]
            sgate, Pq, r32, wf6, wf7, wf0, wf1, wf2 = wf[3], wf[4], wf[5], wf[6], wf[7], wf[0], wf[1], wf[2]
            fw.dma("sp", MK1, cd["rw_masks"][:, 0:512], writes=[MK1B])
            fw.dma("sp", MK2, cd["rw_masks"][:, 512:768], writes=[MK2B])
            fw.dma("sp", rvec[:], rvec_d[l], writes=[rvec.B])
            fw.dve(lambda h: h.tensor_scalar(out=omka[:], in0=rvec[:, :, 3], scalar1=-1.0, scalar2=1.0, op0=ALU.mult,
                                             op1=ALU.add), reads=[rvec.B], writes=[omka.B])
            for i in range(4):
                fw.dve(lambda h, i=i: h.memset(QP[i], 0.0), writes=[QPB[i]])
            fw.dve(lambda h: h.memset(vp0, 0.0), writes=[SB[15]])
            fw.dve(lambda h: h.memset(vp1[:], 0.0), writes=[vp1.B])
            fw.dve(lambda h: h.memset(MISC, 0.0), writes=[SB[12]])
            fw.dve(lambda h: h.memset(lw2[:], 0.0), writes=[lw2.B])
            fw.dve(lambda h: h.memset(la2[:], 0.0), writes=[la2.B])
            w_wa = wload_shift(l, O_XW, mu_wa_d[l])
            for tb in range(4):
                pwa = ps_next()
                proj_shift(pwa, w_wa, tb * 512)
                dst = ya[:, 3, tb * 512:(tb + 1) * 512]
                fw.act(lambda h, pwa=pwa, dst=dst: h.activation(out=dst[0:64, :], in_=pwa[0:64, :], func=AF.Tanh),
                       reads=[pwa.B], writes=[ya.b[12 + tb]])
                fw.act(lambda h, pwa=pwa, dst=dst: h.activation(out=dst[64:128, :], in_=pwa[64:128, :], func=AF.Copy),
                       reads=[pwa.B], writes=[ya.b[12 + tb]])

            def do_hp(hp):
                V = lambda j: rvec[:, hp, j:j + 1]
                W0, A0, KK, KA, LNW, LNB, RK = (V(j) for j in range(7))
                w_r = wload_shift(l, O_AR + hp * 128, mu_rkv_d[l, 0:1, hp * 128:(hp + 1) * 128])
                w_k = wload_shift(l, O_AK + hp * 128, mu_rkv_d[l, 1:2, hp * 128:(hp + 1) * 128])
                w_v = wload_shift(l, O_AV + hp * 128, mu_rkv_d[l, 2:3, hp * 128:(hp + 1) * 128])
                w_g = wload(l, [(O_AG + hp * 128, 128)])
                stg = wst[wctr[1] % 2]
                wctr[1] += 1
                stv = stg[:].rearrange("p c n -> p (c n)")
                fw.dma("sp", stv[0:64, 0:128], w2a2_d[l, 0, :, hp * 128:(hp + 1) * 128], writes=stg.b)
                fw.dma("sp", stv[64:128, 0:128], w2a2_d[l, 1, :, hp * 128:(hp + 1) * 128], writes=stg.b)
                fw.dve(lambda h: h.tensor_copy(out=lw2[0:64, :], in_=stv[0:64, 0:128]), reads=stg.b, writes=[lw2.B])
                fw.dve(lambda h: h.tensor_copy(out=la2[64:128, :], in_=stv[64:128, 0:128]), reads=stg.b, writes=[la2.B])
                fw.dve(lambda h: h.memset(St32[:], 0.0), writes=[St32.B])
                fw.dve(lambda h: h.memset(Stb[:], 0.0), writes=[Stb.B])

                def do_block(tb):
                    tok0 = tb * 512
                    twa = ya[:, 3, tok0:tok0 + 512]
                    twab = ya.b[12 + tb]
                    pr, pk = ps_next(), ps_next()
                    proj_shift(pr, w_r, tok0)
                    proj_shift(pk, w_k, tok0)
                    fw.act(lambda h: h.activation(out=r32[:], in_=pr[:], func=AF.Copy), reads=[pr.B], writes=[r32.B])
                    pw_, pa_ = ps_next(), ps_next()
                    fw.pe(lambda h: h.matmul(out=pw_[:], lhsT=lw2[:], rhs=twa, start=True, stop=True),
                          reads=[lw2.B, twab], writes=[pw_.B])
                    fw.pe(lambda h: h.matmul(out=pa_[:], lhsT=la2[:], rhs=twa, start=True, stop=True),
                          reads=[la2.B, twab], writes=[pa_.B])
                    sg, aa = wf0, wf6
                    fw.act(lambda h: h.activation(out=sg[:], in_=pw_[:], func=AF.Sigmoid, bias=W0, scale=1.0),
                           reads=[pw_.B, rvec.B], writes=[sg.B])
                    fw.act(lambda h: h.activation(out=aa[:], in_=pa_[:], func=AF.Sigmoid, bias=A0, scale=1.0),
                           reads=[pa_.B, rvec.B], writes=[aa.B])
                    kkn = wf7
                    fw.dve(lambda h: h.tensor_scalar(out=kkn[:], in0=pk[:], scalar1=KK, scalar2=None, op0=ALU.mult),
                           reads=[pk.B, rvec.B], writes=[kkn.B])
                    fw.act(lambda h: h.activation(out=sqk[:], in_=kkn[:], func=AF.Square), reads=[kkn.B], writes=[sqk.B])
                    ss = ps_next()
                    fw.pe(lambda h: h.matmul(out=ss[:], lhsT=bdo64[:], rhs=sqk[:], start=True, stop=True),
                          reads=[bdo64.B, sqk.B], writes=[ss.B])
                    rn = wf1
                    fw.act(lambda h: h.activation(out=rn[:], in_=ss[:], func=AF.Sqrt, bias=1e-12, scale=64.0),
                           reads=[ss.B], writes=[rn.B])
                    fw.dve(lambda h: h.reciprocal(out=rn[:], in_=rn[:]), reads=[rn.B], writes=[rn.B])
                    fw.dve(lambda h: h.tensor_tensor(out=kkn[:], in0=kkn[:], in1=rn[:], op=ALU.mult),
                           reads=[kkn.B, rn.B], writes=[kkn.B])
                    k2 = wf2
                    fw.dve(lambda h: h.tensor_scalar(out=wf1[:], in0=aa[:], scalar1=KA, scalar2=omka[:, hp:hp + 1],
                                                     op0=ALU.mult, op1=ALU.add), reads=[aa.B, rvec.B, omka.B], writes=[wf1.B])
                    fw.dve(lambda h: h.tensor_tensor(out=k2[:], in0=pk[:], in1=wf1[:], op=ALU.mult),
                           reads=[pk.B, wf1.B], writes=[k2.B])
                    fw.dve(lambda h: h.scalar_tensor_tensor(out=rk_bf[:], in0=r32[:], scalar=RK, in1=k2[:], op0=ALU.mult,
                                                            op1=ALU.mult), reads=[r32.B, rvec.B, k2.B], writes=[rk_bf.B])
                    bb_ = wf1
                    fw.dve(lambda h: h.tensor_tensor(out=bb_[:], in0=kkn[:], in1=aa[:], op=ALU.mult),
                           reads=[kkn.B, aa.B], writes=[bb_.B])
                    pv = ps_next()
                    proj_shift(pv, w_v, tok0)
                    fw.act(lambda h: h.activation(out=v_bf[:], in_=pv[:], func=AF.Copy), reads=[pv.B], writes=[v_bf.B])
                    pgt = ps_next()
                    proj(pgt, w_g, tok0)
                    silu_from_psum(sgate, pgt)
                    cs = wf6
                    for c4 in range(4):
                        fw.dve(lambda h, c4=c4: h.tensor_tensor_scan(
                            out=cs[:, c4 * 128:(c4 + 1) * 128], data0=onec[:, 0:1].to_broadcast([128, 128]),
                            data1=sg[:, c4 * 128:(c4 + 1) * 128], initial=0.0, op0=ALU.mult, op1=ALU.add),
                            reads=[sg.B, onec.B], writes=[cs.B])
                    fw.dve(lambda h: h.tensor_tensor(out=sg[:], in0=cs[:], in1=sg[:], op=ALU.subtract),
                           reads=[cs.B, sg.B], writes=[sg.B])
                    fw.act(lambda h: h.activation(out=Pq[:], in_=cs[:], func=AF.Exp, scale=-CDEC), reads=[cs.B], writes=[Pq.B])
                    fw.act(lambda h: h.activation(out=sg[:], in_=sg[:], func=AF.Exp, scale=-CDEC), reads=[sg.B], writes=[sg.B])
                    fw.act(lambda h: h.activation(out=cs[:], in_=cs[:], func=AF.Exp, scale=CDEC), reads=[cs.B], writes=[cs.B])
                    PqA, Pk = sg, cs
                    fw.dve(lambda h: h.tensor_tensor(out=rt[:], in0=r32[:], in1=Pq[:], op=ALU.mult),
                           reads=[r32.B, Pq.B], writes=[rt.B])
                    fw.dve(lambda h: h.scalar_tensor_tensor(out=at[:], in0=kkn[:], scalar=-1.0, in1=PqA[:], op0=ALU.mult,
                                                            op1=ALU.mult), reads=[kkn.B, PqA.B], writes=[at.B])
                    fw.dve(lambda h: h.tensor_tensor(out=kt[:], in0=k2[:], in1=Pk[:], op=ALU.mult),
                           reads=[k2.B, Pk.B], writes=[kt.B])
                    fw.dve(lambda h: h.tensor_tensor(out=bt[:], in0=bb_[:], in1=Pk[:], op=ALU.mult),
                           reads=[bb_.B, Pk.B], writes=[bt.B])
                    for c4 in range(4):
                        for hh in range(2):
                            rows = slice(hh * 64, (hh + 1) * 64)
                            fw.act(lambda h, c4=c4, hh=hh, rows=rows: h.activation(
                                out=QP[c4][rows, (2 * hh) * 128:(2 * hh + 1) * 128], in_=at[rows, c4 * 128:(c4 + 1) * 128],
                                func=AF.Copy), reads=[at.B], writes=[QPB[c4]])
                            fw.act(lambda h, c4=c4, hh=hh, rows=rows: h.activation(
                                out=QP[c4][rows, (2 * hh + 1) * 128:(2 * hh + 2) * 128], in_=rt[rows, c4 * 128:(c4 + 1) * 128],
                                func=AF.Copy), reads=[rt.B], writes=[QPB[c4]])
                    tpa, tpb = tp_ps[0], tp_ps[1]
                    for c4 in range(4):
                        fw.pe(lambda h, c4=c4: h.transpose(out=tpa[:, c4, :], in_=kt[:, c4 * 128:(c4 + 1) * 128],
                                                           identity=ident[:]), reads=[kt.B, ident.B], writes=[tpa.B], inc=False)
                    for c4 in range(4):
                        fw.pe(lambda h, c4=c4: h.transpose(out=tpa[:, 4 + c4, :], in_=bt[:, c4 * 128:(c4 + 1) * 128],
                                                           identity=ident[:]), reads=[bt.B, ident.B], writes=[tpa.B],
                              inc=(c4 == 3))
                    for c4 in range(4):
                        fw.pe(lambda h, c4=c4: h.transpose(out=tpb[:, c4, :], in_=v_bf[:, c4 * 128:(c4 + 1) * 128],
                                                           identity=ident[:]), reads=[v_bf.B, ident.B], writes=[tpb.B],
                              inc=(c4 == 3))
                    v4 = lambda ap: ap.rearrange("p (c f) -> p c f", c=4)
                    fw.act(lambda h: h.activation(out=v4(kt_tok), in_=tpa[:, 0:4, :], func=AF.Copy),
                           reads=[tpa.B], writes=[SB[13]])
                    fw.dve(lambda h: h.tensor_copy(out=v4(bt_tok), in_=tpa[:, 4:8, :]), reads=[tpa.B], writes=[SB[14]])
                    fw.act(lambda h: h.activation(out=v4(vp0)[:, :, 0:64], in_=tpb[:, 0:4, 0:64], func=AF.Copy),
                           reads=[tpb.B], writes=[SB[15]])
                    fw.dve(lambda h: h.tensor_copy(out=v4(vp1[:])[:, :, 64:128], in_=tpb[:, 0:4, 64:128]),
                           reads=[tpb.B], writes=[vp1.B])
                    y_ps = ps_y()

                    ESET = [(SV[6], SB[6], SV[7], SB[7], SV[11], SB[11]),
                            (wh[10][:], wh[10].B, wh[11][:], wh[11].B, wh[0][:], wh[0].B)]
                    vps = [vp0, vp1[:]]
                    vpb = [SB[15], vp1.B]

                    def inv_stages(c4):
                        cs_ = slice(c4 * 128, (c4 + 1) * 128)
                        E1, E1B, E2, E2B, PP, PPB = ESET[c4 % 2]
                        st = []

                        def aprod():
                            pa1, pa2, pa3 = ps_next(), ps_next(), ps_next()
                            fw.pe(lambda h: h.matmul(out=pa1[:], lhsT=kt[:, cs_], rhs=QP[c4], start=True, stop=True),
                                  reads=[kt.B, QPB[c4]], writes=[pa1.B])
                            fw.pe(lambda h: h.matmul(out=pa2[:], lhsT=bt[:, cs_], rhs=QP[c4], start=True, stop=True),
                                  reads=[bt.B, QPB[c4]], writes=[pa2.B])
                            for hh in range(2):
                                fw.pe(lambda h, hh=hh: h.matmul(out=pa3[:, hh * 128:(hh + 1) * 128],
                                                                lhsT=QP[c4][:, (2 * hh) * 128:(2 * hh + 1) * 128],
                                                                rhs=bt[:, cs_], start=True, stop=True),
                                      reads=[bt.B, QPB[c4]], writes=[pa3.B], inc=(hh == 1))
                            fw.dve(lambda h: h.tensor_tensor(out=E1, in0=pa1[:], in1=MK1, op=ALU.mult),
                                   reads=[pa1.B, MK1B], writes=[E1B])
                            fw.dve(lambda h: h.tensor_tensor(out=E2, in0=pa2[:], in1=MK1, op=ALU.mult),
                                   reads=[pa2.B, MK1B], writes=[E2B])
                            fw.dve(lambda h: h.tensor_tensor(out=E3[:, 0:256], in0=pa3[:, 0:256], in1=MK2, op=ALU.mult),
                                   reads=[pa3.B, MK2B], writes=[SB[8]])
                            e2v = E2.rearrange("p (a b) -> p a b", a=2)[:, :, 0:128]
                            fw.dve(lambda h: h.tensor_tensor(out=PP[:, 0:256].rearrange("p (a b) -> p a b", a=2), in0=e2v,
                                                             in1=ident[:].unsqueeze(1).to_broadcast([128, 2, 128]),
                                                             op=ALU.add), reads=[E2B, ident.B], writes=[PPB])
                        st.append(aprod)

                        def Xj(j, hh):
                            if j == 0:
                                return E3[:, hh * 128:(hh + 1) * 128], SB[8]
                            return XB[j % 2][:, (2 * hh) * 128:(2 * hh + 1) * 128], SB[9 + j % 2]

                        def Bj(j, hh):
                            if j == 0:
                                return E2[:, (2 * hh) * 128:(2 * hh + 1) * 128], E2B
                            return XB[j % 2][:, (2 * hh + 1) * 128:(2 * hh + 2) * 128], SB[9 + j % 2]

                        def Pj(j, hh):
                            o = (j % 2) * 256 + hh * 128
                            return PP[:, o:o + 128]

                        def step(j):
                            last = j == 5
                            pxb = ps_next()
                            for hh in range(2):
                                xa_, xb_ = Xj(j, hh)
                                ba_, bb2 = Bj(j, hh)
                                fw.pe(lambda h, hh=hh, xa_=xa_, ba_=ba_: h.matmul(
                                    out=pxb[:, (2 * hh) * 128:(2 * hh + 1) * 128], lhsT=ba_, rhs=xa_, start=True, stop=True),
                                    reads=[xb_, bb2], writes=[pxb.B], inc=(last and hh == 1))
                                if not last:
                                    fw.pe(lambda h, hh=hh, xa_=xa_, ba_=ba_: h.matmul(
                                        out=pxb[:, (2 * hh + 1) * 128:(2 * hh + 2) * 128], lhsT=xa_, rhs=ba_, start=True,
                                        stop=True), reads=[xb_, bb2], writes=[pxb.B], inc=(hh == 1))
                            nxt = XB[(j + 1) % 2]
                            if last:
                                fw.act(lambda h: h.activation(
                                    out=nxt.rearrange("p (a b) -> p a b", a=2)[:, :, 0:128],
                                    in_=pxb[:].rearrange("p (a b) -> p a b", a=2)[:, :, 0:128], func=AF.Copy),
                                    reads=[pxb.B], writes=[SB[9 + (j + 1) % 2]])
                            else:
                                fw.act(lambda h: h.activation(out=nxt, in_=pxb[:], func=AF.Copy),
                                       reads=[pxb.B], writes=[SB[9 + (j + 1) % 2]])
                            pp = ps_next()
                            for hh in range(2):
                                xn_, xnb_ = Xj(j + 1, hh)
                                fw.pe(lambda h, hh=hh: h.matmul(out=pp[:, hh * 128:(hh + 1) * 128], lhsT=ident[:],
                                                                rhs=Pj(j, hh), start=True, stop=False),
                                      reads=[ident.B, PPB], writes=[pp.B], inc=False)
                                fw.pe(lambda h, hh=hh, xn_=xn_: h.matmul(
                                    out=pp[:, hh * 128:(hh + 1) * 128], lhsT=xn_, rhs=Pj(j, hh), start=False, stop=True),
                                    reads=[xnb_, PPB], writes=[pp.B], inc=(hh == 1))
                            o = ((j + 1) % 2) * 256
                            fw.dve(lambda h: h.tensor_copy(out=PP[:, o:o + 256], in_=pp[:, 0:256]),
                                   reads=[pp.B], writes=[PPB])

                        for j in range(6):
                            st.append(lambda j=j: step(j))
                        return st

                    def state_stages(c4):
                        cs_ = slice(c4 * 128, (c4 + 1) * 128)
                        E1, E1B, E2, E2B, PP, PPB = ESET[c4 % 2]
                        r0b = MISC[:, 0:128]
                        upad = [MISC[:, 128:256], MISC[:, 256:384]]

                        def s_rhs0():
                            r0 = ps_next()
                            fw.pe(lambda h: h.matmul(out=r0[:, 0:128], lhsT=at[:, cs_], rhs=Stb[:], start=True, stop=False),
                                  reads=[at.B, Stb.B], writes=[r0.B], inc=False)
                            for hh in range(2):
                                fw.pe(lambda h, hh=hh: h.matmul(out=r0[:, hh * 64:(hh + 1) * 64],
                                                                lhsT=E1[:, (2 * hh) * 128:(2 * hh + 1) * 128],
                                                                rhs=vps[hh][:, c4 * 128 + hh * 64:c4 * 128 + (hh + 1) * 64],
                                                                start=False, stop=(hh == 1)),
                                      reads=[E1B, vpb[hh]], writes=[r0.B], inc=(hh == 1))
                            fw.act(lambda h: h.activation(out=r0b, in_=r0[:, 0:128], func=AF.Copy),
                                   reads=[r0.B], writes=[SB[12]])

                        def s_u():
                            up = ps_next()
                            for hh in range(2):
                                fw.pe(lambda h, hh=hh: h.matmul(out=up[:, hh * 64:(hh + 1) * 64], lhsT=PP[:, hh * 128:(hh + 1) * 128],
                                                                rhs=r0b[:, hh * 64:(hh + 1) * 64], start=True, stop=True),
                                      reads=[PPB, SB[12]], writes=[up.B], inc=(hh == 1))
                            fw.act(lambda h: h.activation(out=upad[0][:, 0:64], in_=up[:, 0:64], func=AF.Copy),
                                   reads=[up.B], writes=[SB[12]])
                            fw.dve(lambda h: h.tensor_copy(out=upad[1][:, 64:128], in_=up[:, 64:128]),
                                   reads=[up.B], writes=[SB[12]])

                        def s_y():
                            fw.pe(lambda h: h.matmul(out=y_ps[:, cs_], lhsT=Stb[:], rhs=rt[:, cs_], start=True, stop=False),
                                  reads=[Stb.B, rt.B], writes=[y_ps.B], inc=False)
                            for hh in range(2):
                                fw.pe(lambda h, hh=hh: h.matmul(out=y_ps[:, cs_], lhsT=upad[hh],
                                                                rhs=E2[:, (2 * hh + 1) * 128:(2 * hh + 2) * 128],
                                                                start=False, stop=False),
                                      reads=[SB[12], E2B], writes=[y_ps.B], inc=False)
                                fw.pe(lambda h, hh=hh: h.matmul(out=y_ps[:, cs_], lhsT=vps[hh][:, c4 * 128:(c4 + 1) * 128],
                                                                rhs=E1[:, (2 * hh + 1) * 128:(2 * hh + 2) * 128],
                                                                start=False, stop=(hh == 1)),
                                      reads=[vpb[hh], E1B], writes=[y_ps.B], inc=(hh == 1))

                        def s_state():
                            su = ps_next()
                            for hh in range(2):
                                fw.pe(lambda h, hh=hh: h.matmul(out=su[:, 0:128], lhsT=bt_tok[:, cs_], rhs=upad[hh],
                                                                start=(hh == 0), stop=False),
                                      reads=[SB[14], SB[12]], writes=[su.B], inc=False)
                            for hh in range(2):
                                fw.pe(lambda h, hh=hh: h.matmul(out=su[:, 0:128], lhsT=kt_tok[:, cs_],
                                                                rhs=vps[hh][:, c4 * 128:(c4 + 1) * 128],
                                                                start=False, stop=(hh == 1)),
                                      reads=[SB[13], vpb[hh]], writes=[su.B], inc=(hh == 1))
                            fw.dve(lambda h: h.tensor_tensor(out=St32[:], in0=su[:, 0:128], in1=St32[:], op=ALU.add),
                                   reads=[su.B, St32.B], writes=[St32.B])
                            pend = Pq[:, c4 * 128 + 127:c4 * 128 + 128]
                            fw.dve(lambda h: h.tensor_scalar(out=St32[:], in0=St32[:], scalar1=pend, scalar2=None,
                                                             op0=ALU.mult), reads=[St32.B, Pq.B], writes=[St32.B])
                            fw.dve(lambda h: h.tensor_tensor(out=Stb[:], in0=St32[:], in1=bd32[:], op=ALU.mult),
                                   reads=[St32.B, bd32.B], writes=[Stb.B])

                        return [s_rhs0, s_u, s_y, s_state]

                    nch = LIM.get('c4', 4)
                    prev = []
                    for c4 in range(nch + 1):
                        cur = inv_stages(c4) if c4 < nch else []
                        for i in range(max(len(cur), len(prev))):
                            if i < len(cur):
                                cur[i]()
                            if i < len(prev):
                                prev[i]()
                        prev = state_stages(c4) if c4 < nch else []
                    bs = ps_next()
                    fw.pe(lambda h: h.matmul(out=bs[:], lhsT=bdo64[:], rhs=rk_bf[:], start=True, stop=True),
                          reads=[bdo64.B, rk_bf.B], writes=[bs.B])
                    bon = wf7
                    fw.dve(lambda h: h.scalar_tensor_tensor(out=bon[:], in0=bs[:], scalar=64.0, in1=v_bf[:], op0=ALU.mult,
                                                            op1=ALU.mult), reads=[bs.B, v_bf.B], writes=[bon.B])

                    def affine(ycen):
                        fw.dve(lambda h: h.tensor_scalar(out=ycen[:], in0=ycen[:], scalar1=LNW, scalar2=LNB, op0=ALU.mult,
                                                         op1=ALU.add), reads=[ycen.B, rvec.B], writes=[ycen.B])
                        fw.dve(lambda h: h.tensor_tensor(out=ycen[:], in0=ycen[:], in1=bon[:], op=ALU.add),
                               reads=[ycen.B, bon.B], writes=[ycen.B])

                    headnorm_gate(y_ps, sgate, ya[:, hp, tok0:tok0 + 512], ya.b[hp * 4 + tb], 64e-5, affine=affine)

                for tb in range(LIM.get('tb', 4)):
                    do_block(tb)

            for hp in range(LIM.get('hp', 4)):
                do_hp(hp)
            fw.dve(lambda h: h.memset(cst[:, 0:1], 0.0), reads=SB, writes=yb.b + [cst.B])

        def wload_plain(src_ap, nchunk):
            slot = wslot[wctr[0] % NSLOT]
            wctr[0] += 1
            stg = wst[wctr[1] % 2]
            wctr[1] += 1
            fw.dma("sp", stg[:, 0:nchunk, :], src_ap, writes=stg.b)
            fw.act(lambda h: h.activation(out=slot[:, 0:nchunk, :], in_=stg[:, 0:nchunk, :], func=AF.Copy),
                   reads=stg.b, writes=[slot.B])
            return slot

        wc_bufs = {}

        def cached(l, idx, nchunk, loader):
            n = nchunk * 128
            key = (l, idx)
            if key not in wc_bufs:
                slot = loader()
                wc_bufs[key] = Buf(f"wc{l}_{idx}")
                fw.dma("sp", wcache_d[l, idx, :, 0:n], slot[:, 0:nchunk, :].rearrange("p c n -> p (c n)"),
                       reads=[slot.B], writes=[wc_bufs[key]])
                return slot
            slot = wslot[wctr[0] % NSLOT]
            wctr[0] += 1
            fw.dma("sp", slot[:, 0:nchunk, :].rearrange("p c n -> p (c n)"), wcache_d[l, idx, :, 0:n],
                   reads=[wc_bufs[key]], writes=[slot.B])
            return slot

        def phaseM(si, l):
            fw.dma("sp", gpost[:], postn_d[l:l + 1, :].broadcast_to([128, D]), writes=[gpost.B])
            ybr = [ya, yb, yc]
            gofs = [O_GA, O_GB, O_GC]
            mT = wh[0:8]
            sig, tt_, macc = wf[0], wf[1], wf[2]

            def do_block(tb):
                tok0 = tb * 512

                def do_oc(oc):
                    for br in range(3):
                        if br not in LIM.get("branches", (0, 1, 2)):
                            continue
                        first = br == min(LIM.get("branches", (0, 1, 2)))
                        last = br == max(LIM.get("branches", (0, 1, 2)))
                        wg = cached(l, br * 8 + oc, 8, lambda br=br: wload(l, [(gofs[br] + oc * 128, 128)]))
                        gl = ps_next()
                        proj(gl, wg, tok0)
                        fw.act(lambda h, gl=gl, br=br: h.activation(out=sig[:], in_=gl[:], func=AF.Sigmoid,
                                                                    bias=bmt[:, l, br, oc:oc + 1], scale=1.0),
                               reads=[gl.B, bmt.B], writes=[sig.B])
                        wp = cached(l, 24 + br * 8 + oc, 4, lambda br=br: wload_plain(
                            wp_d[br][l, :, oc * 128:(oc + 1) * 128].rearrange("(c p) n -> p c n", p=128), 4))
                        pb = ps_next()
                        for c in range(4):
                            fw.pe(lambda h, c=c, pb=pb, wp=wp, br=br: h.matmul(
                                out=pb[:], lhsT=wp[:, c, :], rhs=ybr[br][:, c, tok0:tok0 + 512],
                                start=(c == 0), stop=(c == 3)),
                                reads=[wp.B, ybr[br].b[c * 4 + tb]], writes=[pb.B], inc=(c == 3))
                        if first and last:
                            fw.dve(lambda h, pb=pb: h.tensor_tensor(out=mT[oc][:], in0=pb[:], in1=sig[:], op=ALU.mult),
                                   reads=[pb.B, sig.B], writes=[mT[oc].B])
                        elif first:
                            fw.dve(lambda h, pb=pb: h.tensor_tensor(out=macc[:], in0=pb[:], in1=sig[:], op=ALU.mult),
                                   reads=[pb.B, sig.B], writes=[macc.B])
                        else:
                            fw.dve(lambda h, pb=pb: h.tensor_tensor(out=tt_[:], in0=pb[:], in1=sig[:], op=ALU.mult),
                                   reads=[pb.B, sig.B], writes=[tt_.B])
                            dst = mT[oc] if last else macc
                            fw.dve(lambda h, dst=dst: h.tensor_tensor(out=dst[:], in0=macc[:], in1=tt_[:], op=ALU.add),
                                   reads=[macc.B, tt_.B], writes=[dst.B])

                for oc in range(8):
                    do_oc(oc)

                def do_pair(k):
                    banks = [pg[0], pg[1], pg[2], pg[3]]
                    for oc in range(8):
                        wo = cached(l, 48 + oc, 8, lambda oc=oc: wload_plain(
                            wout_d[l, oc * 128:(oc + 1) * 128, :].rearrange("p (c n) -> p c n", c=8), 8))
                        wov = wo[:].rearrange("p c n -> p (c n)")
                        for t in range(2):
                            for half in range(2):
                                bk = banks[t * 2 + half]
                                fw.pe(lambda h, oc=oc, t=t, half=half, bk=bk, wov=wov: h.matmul(
                                    out=bk[:], lhsT=mT[oc][:, (2 * k + t) * 128:(2 * k + t + 1) * 128],
                                    rhs=wov[:, half * 512:(half + 1) * 512], start=(oc == 0), stop=(oc == 7)),
                                    reads=[mT[oc].B, wo.B], writes=[bk.B], inc=(oc == 7 or (t == 1 and half == 1)))
                    for t in range(2):
                        tile_i = tb * 4 + 2 * k + t
                        for half in range(2):
                            bk = banks[t * 2 + half]
                            fw.dve(lambda h, half=half, bk=bk: h.bn_stats(out=st6[:, half, :], in_=bk[:]),
                                   reads=[bk.B], writes=[st6.B])
                        fw.dve(lambda h: h.bn_aggr(out=mv[:], in_=st6[:].rearrange("p a b -> p (a b)")),
                               reads=[st6.B], writes=[mv.B])
                        fw.dve(lambda h: h.scalar_tensor_tensor(out=e2[:], in0=mv[:, 0:1], scalar=mv[:, 0:1],
                                                                in1=mv[:, 1:2], op0=ALU.mult, op1=ALU.add),
                               reads=[mv.B], writes=[e2.B])
                        fw.act(lambda h: h.activation(out=e2[:], in_=e2[:], func=AF.Sqrt, bias=EPS, scale=1.0),
                               reads=[e2.B], writes=[e2.B])
                        fw.dve(lambda h: h.reciprocal(out=rstd[:], in_=e2[:]), reads=[e2.B], writes=[rstd.B])
                        for half in range(2):
                            bk = banks[t * 2 + half]
                            hs = slice(half * 512, (half + 1) * 512)
                            fw.dve(lambda h, bk=bk, hs=hs: h.scalar_tensor_tensor(
                                out=wf[3][:], in0=bk[:], scalar=rstd[:, 0:1], in1=gpost[:, hs],
                                op0=ALU.mult, op1=ALU.mult), reads=[bk.B, rstd.B, gpost.B], writes=[wf[3].B])
                            fw.dve(lambda h, hs=hs, tile_i=tile_i: h.tensor_tensor(
                                out=x_sb[:, tile_i, hs], in0=x_sb[:, tile_i, hs], in1=wf[3][:], op=ALU.add),
                                reads=[x_sb.b[tile_i], wf[3].B], writes=[x_sb.b[tile_i]])

                for k in range(2):
                    do_pair(k)

            for tb in range(LIM.get('tb', 4)):
                do_block(tb)

        for si in range(nseq):
            for tq in range(NT // 4):
                fw.dma("sp", x_sb[:, tq * 4:(tq + 1) * 4, :],
                       x_d[si, tq * 512:(tq + 1) * 512, :].rearrange("(t p) d -> p t d", p=128),
                       writes=[x_sb.b[tq * 4 + i] for i in range(4)])
            for l in range(nlayers):
                if l == 1 and "l2phases" in LIM:
                    phases = LIM["l2phases"]
                if not LIM.get('nosetup'):
                    layer_setup(l)
                phase0(si, l)
                if "hT" in debug and si == 0 and l == 0:
                    fw.dma("sp", dbg_d["hT"], hT[:], reads=hT.b)
                if "C" in phases:
                    phaseC(si, l)
                    if "yc" in debug and si == 0 and l == 0:
                        fw.dma("sp", dbg_d["yc"], yc[:], reads=yc.b)
                if "A" in phases:
                    phaseA(si, l)
                    if "ya" in debug and si == 0 and l == 0:
                        fw.dma("sp", dbg_d["ya"], ya[:], reads=ya.b)
                if "B" in phases:
                    phaseB(si, l)
                    if "yb" in debug and si == 0 and l == 0:
                        fw.dma("sp", dbg_d["yb"], yb[:], reads=yb.b)
                if "M" in phases:
                    phaseM(si, l)
            for tq in range(NT // 4):
                fw.dma("sp", out_d[si, tq * 512:(tq + 1) * 512, :].rearrange("(t p) d -> p t d", p=128),
                       x_sb[:, tq * 4:(tq + 1) * 4, :],
                       reads=[x_sb.b[tq * 4 + i] for i in range(4)])
        allb = x_sb.b + hT.b + ya.b + yb.b + yc.b
        fw.wait_all("sp", allb)
        fw.emit()
        print("instr counts:", {k: v.n for k, v in fw.eng.items()})
    return nc


def make_shared(inputs):
    f = lambda k: np.ascontiguousarray(np.asarray(inputs[k], dtype=np.float32))
    shared = dict(make_consts())
    shared["w_in"] = f("w_in")
    shared["pre_norm"] = np.ascontiguousarray(f("pre_norm").reshape(DEPTH, 8, 128).transpose(0, 2, 1))
    for n in ("w_proj_rwkv", "w_proj_ret", "w_proj_s5", "w_out", "post_norm"):
        shared[n] = f(n)
    shared["b_merge"] = np.ascontiguousarray(f("b_merge").reshape(DEPTH, 3, 8, 128).transpose(0, 3, 1, 2))
    shared["rwkv_mu_rkv"] = f("rwkv_mu_rkv")
    shared["rwkv_mu_wa"] = np.ascontiguousarray(f("rwkv_mu_wa").reshape(DEPTH, 1, 128))
    shared["rwkv_w2a2"] = np.ascontiguousarray(np.stack([f("rwkv_w2"), f("rwkv_a2")], axis=1))
    vec = np.zeros((DEPTH, 8, 512), np.float32)
    for j, n in enumerate(("rwkv_w0", "rwkv_a0", "rwkv_k_k", "rwkv_k_a", "rwkv_ln_w", "rwkv_ln_b")):
        vec[:, j] = f(n)
    vec[:, 6] = f("rwkv_r_k").reshape(DEPTH, 512)
    shared["rwkv_vec"] = np.ascontiguousarray(vec.reshape(DEPTH, 8, 4, 128).transpose(0, 3, 2, 1))
    dup = lambda a: np.concatenate([a, a], axis=1)
    a_re = dup(f("s5_A_re").transpose(0, 2, 1))
    a_im = dup(f("s5_A_im").transpose(0, 2, 1))
    ldt = np.broadcast_to(f("s5_log_dt")[:, None, :], (DEPTH, 128, 32))
    shared["s5_Aab"] = np.ascontiguousarray(np.stack([a_re, a_im, ldt], axis=1))
    b_re = dup(f("s5_B_re").transpose(0, 2, 1, 3).reshape(DEPTH, 64, 512))
    b_im = dup(f("s5_B_im").transpose(0, 2, 1, 3).reshape(DEPTH, 64, 512))
    shared["s5_Bst"] = np.ascontiguousarray(np.stack([b_re, b_im], axis=1))
    c_re = f("s5_C_re").transpose(0, 3, 1, 2).reshape(DEPTH, 64, 512)
    c_im = f("s5_C_im").transpose(0, 3, 1, 2).reshape(DEPTH, 64, 512)
    ca = np.concatenate([c_re, c_im], axis=1)
    cb = np.concatenate([c_im, c_re], axis=1)
    shared["s5_Cst"] = np.ascontiguousarray(np.stack([ca, cb], axis=1))
    dv = f("s5_D").reshape(DEPTH, 4, 128).transpose(0, 2, 1)
    gb = f("s5_glu_b").reshape(DEPTH, 4, 128).transpose(0, 2, 1)
    shared["s5_vec"] = np.ascontiguousarray(np.concatenate([dv, gb], axis=2))
    shared["s5_glu_w"] = f("s5_glu_w")
    return shared


def make_inputs(inputs, s0, n):
    m = make_shared(inputs)
    m["x"] = np.ascontiguousarray(np.asarray(inputs["x"], dtype=np.float32)[s0:s0 + n])
    return m


def kernel(**inputs):
    ncores = 8
    x = np.ascontiguousarray(np.asarray(inputs["x"], dtype=np.float32))
    shared = make_shared(inputs)
    nlaunch = N_LAUNCH
    per = NSEQ // nlaunch
    nc = build_program(nseq=per)
    out = np.zeros_like(x)
    for j in range(nlaunch):
        in_maps = []
        for c in range(ncores):
            m = dict(shared)
            s0 = c * NSEQ + j * per
            m["x"] = x[s0:s0 + per]
            in_maps.append(m)
        res = run_bass_kernel_spmd(nc, in_maps, core_ids=list(range(ncores)))
        for c in range(ncores):
            s0 = c * NSEQ + j * per
            out[s0:s0 + per] = np.asarray(res.results[c]["out"])
    return out.astype(np.float32)
```
